# Optimizing a Trainium2 kernel written in Bass

```python
import math
import numpy as np
import jax
import jax.numpy as jnp
from jax import lax

D_MODEL = 1024
BATCH = 8
SEQ = 8192
DEPTH = 4

CTX_LEN = 256
GRID_W = 64
HEAD_DIM = 64
N_BRANCH = 4
BRANCH_W = D_MODEL // 4
RET_HEADS = BRANCH_W // HEAD_DIM
RET_CHUNK = 128
NA_HEADS = BRANCH_W // HEAD_DIM
NA_KH = 8
NA_KW = 16
NA_QCB = 16
NA_KRW = NA_QCB + NA_KW
S5_GROUP_CH = 16
S5_GROUPS = BRANCH_W // S5_GROUP_CH
S5_STATE = 64
GQA_Q_HEADS = BRANCH_W // HEAD_DIM
GQA_KV_HEADS = GQA_Q_HEADS // 2
GQA_KV_W = GQA_KV_HEADS * HEAD_DIM
WIN = 128
ATT_BLOCK = 128
MOE_GROUPS = 4
EXPERTS_PER_GROUP = 8
N_EXPERTS = MOE_GROUPS * EXPERTS_PER_GROUP
TOP_K = 2
D_FF_EXPERT = D_MODEL // 2
MOE_BLOCK = 128
ROPE_BASE = 10000.0
EPS = 1e-6
NEG = -1e30
IN_SIZES = (BRANCH_W, BRANCH_W, BRANCH_W, BRANCH_W, BRANCH_W, BRANCH_W, BRANCH_W, BRANCH_W, BRANCH_W, GQA_KV_W, GQA_KV_W, N_BRANCH * D_MODEL)
MIX_COLS = 9 * BRANCH_W + 2 * GQA_KV_W
IN_COLS = MIX_COLS + N_BRANCH * D_MODEL

kernel_name = 'hybrid_flow_backbone'


def _rms(t, gain=None):
    tf = t.astype(jnp.float32)
    y = tf * lax.rsqrt(jnp.mean(tf * tf, axis=-1, keepdims=True) + EPS)
    if gain is not None:
        y = y * gain.astype(jnp.float32)
    return y.astype(t.dtype)


def _modulate(t, shift, scale):
    return t * (1.0 + scale) + shift


def _heads(t, n):
    return t.reshape(t.shape[0], t.shape[1], n, -1)


def _flip(t):
    return t[:, ::-1]


def _split(t, n_pieces):
    pts, acc = [], 0
    for s in IN_SIZES[:n_pieces - 1]:
        acc += s
        pts.append(acc)
    return jnp.split(t, pts, axis=-1)


def rope_tables(length):
    pos = jnp.arange(length, dtype=jnp.int32)
    n_freq = HEAD_DIM // 4
    inv = ROPE_BASE ** (-jnp.arange(n_freq, dtype=jnp.float32) / n_freq)
    ang_r = (pos // GRID_W).astype(jnp.float32)[:, None] * inv
    ang_c = (pos % GRID_W).astype(jnp.float32)[:, None] * inv
    return (jnp.cos(ang_r), jnp.sin(ang_r), jnp.cos(ang_c), jnp.sin(ang_c))


def _rope_1d(t, cos, sin):
    t1, t2 = jnp.split(t, 2, axis=-1)
    cos = cos[None, :, None, :].astype(t.dtype)
    sin = sin[None, :, None, :].astype(t.dtype)
    return jnp.concatenate([t1 * cos - t2 * sin, t1 * sin + t2 * cos], axis=-1)


def axial_rope(t, tabs):
    cr, sr, cc, sc = tabs
    half = t.shape[-1] // 2
    return jnp.concatenate([_rope_1d(t[..., :half], cr, sr), _rope_1d(t[..., half:], cc, sc)], axis=-1)


def retention_chunkwise(q, k, v, log_g, s0):
    b, length, h, dk = q.shape
    dv = v.shape[-1]
    nc = length // RET_CHUNK
    qc = q.astype(jnp.float32).reshape(b, nc, RET_CHUNK, h, dk)
    kc = k.astype(jnp.float32).reshape(b, nc, RET_CHUNK, h, dk)
    vc = v.astype(jnp.float32).reshape(b, nc, RET_CHUNK, h, dv)
    idx = jnp.arange(RET_CHUNK, dtype=jnp.float32)
    diff = idx[:, None] - idx[None, :]
    intra = jnp.where(diff >= 0, jnp.exp(jnp.maximum(diff, 0.0)[None] * log_g[:, None, None]), 0.0)
    scores = jnp.einsum('bnihd,bnjhd->bnhij', qc, kc) * intra
    o_intra = jnp.einsum('bnhij,bnjhe->bnihe', scores, vc)
    k_dec = jnp.exp((RET_CHUNK - 1 - idx)[None, :] * log_g[:, None])
    kv = jnp.einsum('bnjhd,hj,bnjhe->nbhde', kc, k_dec, vc)
    c_dec = jnp.exp(RET_CHUNK * log_g)[None, :, None, None]

    def step(s, kv_n):
        return c_dec * s + kv_n, s

    s_last, s_prev = lax.scan(step, s0, kv)
    q_dec = jnp.exp((idx + 1.0)[None, :] * log_g[:, None])
    o_cross = jnp.einsum('bnihd,hi,nbhde->bnihe', qc, q_dec, s_prev)
    return (o_intra + o_cross).reshape(b, length, h, dv), s_last


def _ret_out(o, g):
    b, length = o.shape[0], o.shape[1]
    return _rms(o).reshape(b, length, BRANCH_W).astype(g.dtype) * jax.nn.silu(g)


def retention_mixer(q, k, v, g, qc, kc, vc, gc, log_decay, tabs, with_ctx):
    log_g = -jnp.exp(log_decay.astype(jnp.float32))
    scale = HEAD_DIM ** -0.5
    q = axial_rope(_heads(q, RET_HEADS), tabs) * scale
    k = axial_rope(_heads(k, RET_HEADS), tabs)
    v = _heads(v, RET_HEADS)
    qc = _heads(qc, RET_HEADS) * scale
    kc = _heads(kc, RET_HEADS)
    vc = _heads(vc, RET_HEADS)
    s0 = jnp.zeros((q.shape[0], RET_HEADS, HEAD_DIM, HEAD_DIM), jnp.float32)
    oc_f, sc_f = retention_chunkwise(qc, kc, vc, log_g[0], s0)
    o_f, _ = retention_chunkwise(q, k, v, log_g[0], sc_f)
    oc_b, sc_b = retention_chunkwise(_flip(qc), _flip(kc), _flip(vc), log_g[1], s0)
    o_b, _ = retention_chunkwise(_flip(q), _flip(k), _flip(v), log_g[1], sc_b)
    out = _ret_out(o_f + _flip(o_b), g)
    out_c = _ret_out(oc_f + _flip(oc_b), gc) if with_ctx else None
    return out, out_c


def neighborhood_mixer(q, k, v, qc, kc, vc, qk_gain, rpb, with_ctx):
    b, length, _ = q.shape
    rows = length // GRID_W
    kh = min(NA_KH, rows)
    scale = HEAD_DIM ** -0.5
    q = _rms(_heads(q, NA_HEADS), qk_gain[0]) * scale
    k = _rms(_heads(k, NA_HEADS), qk_gain[1])
    v = _heads(v, NA_HEADS)
    qc = _rms(_heads(qc, NA_HEADS), qk_gain[0]) * scale
    kc = _rms(_heads(kc, NA_HEADS), qk_gain[1])
    vc = _heads(vc, NA_HEADS)
    qg = q.reshape(b, rows, GRID_W, NA_HEADS, HEAD_DIM)
    kg = k.reshape(b, rows, GRID_W, NA_HEADS, HEAD_DIM)
    vg = v.reshape(b, rows, GRID_W, NA_HEADS, HEAD_DIM)
    n_cb = GRID_W // NA_QCB
    cb = np.arange(n_cb)
    key_cols = np.clip(cb * NA_QCB - NA_KW // 2, 0, GRID_W - NA_KRW)[:, None] + np.arange(NA_KRW)
    q_cols = cb[:, None] * NA_QCB + np.arange(NA_QCB)
    win_start = np.clip(q_cols - NA_KW // 2, 0, GRID_W - NA_KW)
    kcol = key_cols[:, None, :]
    col_mask = (kcol >= win_start[:, :, None]) & (kcol < win_start[:, :, None] + NA_KW)
    col_idx = np.clip(kcol - q_cols[:, :, None] + NA_KW - 1, 0, 2 * NA_KW - 2)
    col_bias = rpb.astype(jnp.float32)[:, :, col_idx]
    mask = jnp.asarray(col_mask)[:, :, None, :]
    n_nb = kh * NA_KRW

    def row_fn(r):
        r0 = jnp.clip(r - kh // 2, 0, rows - kh)
        q_r = lax.dynamic_index_in_dim(qg, r, axis=1, keepdims=False).reshape(b, n_cb, NA_QCB, NA_HEADS, HEAD_DIM)
        k_r = lax.dynamic_slice_in_dim(kg, r0, kh, axis=1)[:, :, key_cols]
        v_r = lax.dynamic_slice_in_dim(vg, r0, kh, axis=1)[:, :, key_cols]
        bias = jnp.take(col_bias, r0 - r + jnp.arange(kh) + NA_KH - 1, axis=1).transpose(0, 2, 3, 1, 4)
        s_nb = jnp.einsum('bjqhd,brjkhd->bhjqrk', q_r, k_r).astype(jnp.float32) + bias
        s_nb = jnp.where(mask, s_nb, NEG).reshape(b, NA_HEADS, n_cb, NA_QCB, n_nb)
        s_cx = jnp.einsum('bjqhd,bchd->bhjqc', q_r, kc).astype(jnp.float32)
        prob = jax.nn.softmax(jnp.concatenate([s_nb, s_cx], axis=-1), axis=-1).astype(v.dtype)
        p_nb = prob[..., :n_nb].reshape(b, NA_HEADS, n_cb, NA_QCB, kh, NA_KRW)
        o = jnp.einsum('bhjqrk,brjkhd->bjqhd', p_nb, v_r) + jnp.einsum('bhjqc,bchd->bjqhd', prob[..., n_nb:], vc)
        return o.reshape(b, GRID_W, BRANCH_W)

    out = lax.map(row_fn, jnp.arange(rows))
    out = jnp.moveaxis(out, 0, 1).reshape(b, length, BRANCH_W)
    out_c = None
    if with_ctx:
        prob = jax.nn.softmax(jnp.einsum('bqhd,bkhd->bhqk', qc, kc).astype(jnp.float32), axis=-1).astype(vc.dtype)
        out_c = jnp.einsum('bhqk,bkhd->bqhd', prob, vc).reshape(qc.shape[0], qc.shape[1], BRANCH_W)
    return out, out_c


def s5_discretise(lam_re, lam_im, log_step, b_re, b_im):
    dt = jnp.exp(log_step)[:, None]
    mag = jnp.exp(lam_re * dt)
    lb_re = mag * jnp.cos(lam_im * dt)
    lb_im = mag * jnp.sin(lam_im * dt)
    den = lam_re * lam_re + lam_im * lam_im
    nr = lb_re - 1.0
    coef_re = (nr * lam_re + lb_im * lam_im) / den
    coef_im = (lb_im * lam_re - nr * lam_im) / den
    bb_re = coef_re[..., None] * b_re - coef_im[..., None] * b_im
    bb_im = coef_re[..., None] * b_im + coef_im[..., None] * b_re
    return lb_re, lb_im, bb_re, bb_im


def _cplx_combine(e1, e2):
    a1r, a1i, b1r, b1i = e1
    a2r, a2i, b2r, b2i = e2
    return (a1r * a2r - a1i * a2i, a1r * a2i + a1i * a2r,
            a2r * b1r - a2i * b1i + b2r, a2r * b1i + a2i * b1r + b2i)


def s5_scan(u, lb_re, lb_im, bb_re, bb_im, x0_re, x0_im):
    length = u.shape[1]
    bu_re = jnp.einsum('blgc,gpc->blgp', u, bb_re)
    bu_im = jnp.einsum('blgc,gpc->blgp', u, bb_im)
    bu_re = bu_re.at[:, 0].add(lb_re * x0_re - lb_im * x0_im)
    bu_im = bu_im.at[:, 0].add(lb_re * x0_im + lb_im * x0_re)
    a_re = jnp.broadcast_to(lb_re[None, None], (1, length) + lb_re.shape)
    a_im = jnp.broadcast_to(lb_im[None, None], (1, length) + lb_im.shape)
    _, _, x_re, x_im = lax.associative_scan(_cplx_combine, (a_re, a_im, bu_re, bu_im), axis=1)
    return x_re, x_im


def _s5_out(y, u, d_skip, glu_w, glu_b):
    b, length = u.shape[0], u.shape[1]
    z = y.reshape(b, length, BRANCH_W) + d_skip.astype(jnp.float32) * u.astype(jnp.float32)
    z = jax.nn.gelu(z).astype(u.dtype)
    return z * jax.nn.sigmoid(z @ glu_w + glu_b)


def s5_mixer(u, uc, lam_re, lam_im, log_step, b_re, b_im, c_re, c_im, d_skip, glu_w, glu_b, with_ctx):
    f32 = jnp.float32
    ug = u.astype(f32).reshape(u.shape[0], u.shape[1], S5_GROUPS, S5_GROUP_CH)
    ucg = uc.astype(f32).reshape(uc.shape[0], uc.shape[1], S5_GROUPS, S5_GROUP_CH)
    cr, ci = c_re.astype(f32), c_im.astype(f32)
    zero = jnp.zeros((u.shape[0], S5_GROUPS, S5_STATE), f32)
    ys, ycs = [], []
    for direction in range(2):
        disc = s5_discretise(lam_re[direction].astype(f32), lam_im[direction].astype(f32),
                             log_step[direction].astype(f32), b_re.astype(f32), b_im.astype(f32))
        orient = _flip if direction == 1 else (lambda t: t)
        xc_re, xc_im = s5_scan(orient(ucg), *disc, zero, zero)
        x_re, x_im = s5_scan(orient(ug), *disc, xc_re[:, -1], xc_im[:, -1])
        ys.append(orient(jnp.einsum('blgp,gcp->blgc', x_re, cr) - jnp.einsum('blgp,gcp->blgc', x_im, ci)))
        if with_ctx:
            ycs.append(orient(jnp.einsum('blgp,gcp->blgc', xc_re, cr) - jnp.einsum('blgp,gcp->blgc', xc_im, ci)))
    out = _s5_out(ys[0] + ys[1], u, d_skip, glu_w, glu_b)
    out_c = _s5_out(ycs[0] + ycs[1], uc, d_skip, glu_w, glu_b) if with_ctx else None
    return out, out_c


def _softmax_with_sink(s, sink_g):
    sink_col = jnp.broadcast_to(sink_g[None, :, :, None, None], s.shape[:-1] + (1,))
    return jax.nn.softmax(jnp.concatenate([s, sink_col], axis=-1), axis=-1)[..., :-1]


def window_gqa_mixer(q, k, v, qc, kc, vc, qk_gain, sink, tabs, with_ctx):
    b, length, _ = q.shape
    grp = GQA_Q_HEADS // GQA_KV_HEADS
    scale = HEAD_DIM ** -0.5
    q = axial_rope(_rms(_heads(q, GQA_Q_HEADS), qk_gain[0]), tabs) * scale
    k = axial_rope(_rms(_heads(k, GQA_KV_HEADS), qk_gain[1]), tabs)
    v = _heads(v, GQA_KV_HEADS)
    qc = _rms(_heads(qc, GQA_Q_HEADS), qk_gain[0]) * scale
    kc = _rms(_heads(kc, GQA_KV_HEADS), qk_gain[1])
    vc = _heads(vc, GQA_KV_HEADS)
    sink_g = sink.astype(jnp.float32).reshape(GQA_KV_HEADS, grp)
    qg = q.reshape(b, length, GQA_KV_HEADS, grp, HEAD_DIM)
    pad = ((0, 0), (WIN, WIN), (0, 0), (0, 0))
    k_pad, v_pad = jnp.pad(k, pad), jnp.pad(v, pad)
    kb_len = ATT_BLOCK + 2 * WIN
    rel = (jnp.arange(kb_len) - WIN)[None, :] - jnp.arange(ATT_BLOCK)[:, None]
    in_band = jnp.abs(rel) <= WIN

    def block_fn(n):
        s0 = n * ATT_BLOCK
        qb = lax.dynamic_slice_in_dim(qg, s0, ATT_BLOCK, axis=1)
        kb = lax.dynamic_slice_in_dim(k_pad, s0, kb_len, axis=1)
        vb = lax.dynamic_slice_in_dim(v_pad, s0, kb_len, axis=1)
        kpos = s0 - WIN + jnp.arange(kb_len)
        valid = in_band & ((kpos >= 0) & (kpos < length))[None, :]
        s_w = jnp.where(valid, jnp.einsum('bqkgd,bskd->bkgqs', qb, kb).astype(jnp.float32), NEG)
        s_c = jnp.einsum('bqkgd,bckd->bkgqc', qb, kc).astype(jnp.float32)
        prob = _softmax_with_sink(jnp.concatenate([s_w, s_c], axis=-1), sink_g).astype(v.dtype)
        o = (jnp.einsum('bkgqs,bskd->bqkgd', prob[..., :kb_len], vb)
             + jnp.einsum('bkgqc,bckd->bqkgd', prob[..., kb_len:], vc))
        return o.reshape(b, ATT_BLOCK, BRANCH_W)

    out = lax.map(block_fn, jnp.arange(length // ATT_BLOCK))
    out = jnp.moveaxis(out, 0, 1).reshape(b, length, BRANCH_W)
    out_c = None
    if with_ctx:
        qcg = qc.reshape(qc.shape[0], qc.shape[1], GQA_KV_HEADS, grp, HEAD_DIM)
        s = jnp.einsum('bqkgd,bckd->bkgqc', qcg, kc).astype(jnp.float32)
        prob = _softmax_with_sink(s, sink_g).astype(vc.dtype)
        out_c = jnp.einsum('bkgqc,bckd->bqkgd', prob, vc).reshape(qc.shape[0], qc.shape[1], BRANCH_W)
    return out, out_c


def _merge(ys, gates, w_branch, w_out):
    gates = gates.reshape(gates.shape[:-1] + (N_BRANCH, D_MODEL))
    merged = None
    for i in range(N_BRANCH):
        term = jax.nn.sigmoid(gates[..., i, :]) * (ys[i] @ w_branch[i])
        merged = term if merged is None else merged + term
    return merged @ w_out


def mixer_sublayer(a, ac, tabs, with_ctx, w_in, w_branch, w_out, ret_log_decay, na_qk_gain, na_rpb,
                   s5_lambda_re, s5_lambda_im, s5_log_step, s5_b_re, s5_b_im, s5_c_re, s5_c_im, s5_d,
                   s5_glu_w, s5_glu_b, gqa_qk_gain, gqa_sink):
    p = _split(a @ w_in, len(IN_SIZES))
    if with_ctx:
        pc = _split(ac @ w_in, len(IN_SIZES))
    else:
        pc = _split(ac @ w_in[:, :MIX_COLS], len(IN_SIZES) - 1)
    ya, yca = retention_mixer(p[0], p[1], p[2], p[3], pc[0], pc[1], pc[2], pc[3], ret_log_decay, tabs, with_ctx)
    yb, ycb = neighborhood_mixer(p[4], p[5], p[6], pc[4], pc[5], pc[6], na_qk_gain, na_rpb, with_ctx)
    yc, ycc = s5_mixer(p[7], pc[7], s5_lambda_re, s5_lambda_im, s5_log_step, s5_b_re, s5_b_im,
                       s5_c_re, s5_c_im, s5_d, s5_glu_w, s5_glu_b, with_ctx)
    yd, ycd = window_gqa_mixer(p[8], p[9], p[10], pc[8], pc[9], pc[10], gqa_qk_gain, gqa_sink, tabs, with_ctx)
    out = _merge((ya, yb, yc, yd), p[11], w_branch, w_out)
    out_c = _merge((yca, ycb, ycc, ycd), pc[11], w_branch, w_out) if with_ctx else None
    return out, out_c


def hier_moe(xf, rw1, rb1, rw2, rb2, w1, w3, w2):
    n, d = xf.shape
    f32 = jnp.float32
    lg1 = (xf @ rw1).astype(f32) + rb1.astype(f32)
    grp = jnp.argmax(lg1, axis=-1)
    p_grp = jnp.take_along_axis(jax.nn.softmax(lg1, axis=-1), grp[:, None], axis=-1)
    lg2 = ((xf @ rw2).astype(f32) + rb2.astype(f32)).reshape(n, MOE_GROUPS, EXPERTS_PER_GROUP)
    lg2 = jnp.take_along_axis(lg2, grp[:, None, None], axis=1)[:, 0]
    top_v, top_i = lax.top_k(lg2, TOP_K)
    gate = p_grp * jax.nn.softmax(top_v, axis=-1)
    expert = grp[:, None].astype(jnp.int32) * EXPERTS_PER_GROUP + top_i.astype(jnp.int32)
    n_assign = n * TOP_K
    e_flat = expert.reshape(-1)
    tok_flat = jnp.repeat(jnp.arange(n, dtype=jnp.int32), TOP_K)
    g_flat = gate.reshape(-1)
    order = jnp.argsort(e_flat)
    e_s, t_s, g_s = e_flat[order], tok_flat[order], g_flat[order]
    counts = jnp.bincount(e_flat, length=N_EXPERTS)
    starts = jnp.cumsum(counts) - counts
    padded = (counts + MOE_BLOCK - 1) // MOE_BLOCK * MOE_BLOCK
    pad_ends = jnp.cumsum(padded)
    pad_starts = pad_ends - padded
    dest = pad_starts[e_s] + jnp.arange(n_assign, dtype=jnp.int32) - starts[e_s]
    n_blocks = -(-(n_assign + N_EXPERTS * (MOE_BLOCK - 1)) // MOE_BLOCK)
    cap = n_blocks * MOE_BLOCK
    buf_tok = jnp.full((cap,), n, jnp.int32).at[dest].set(t_s)
    buf_gate = jnp.zeros((cap,), f32).at[dest].set(g_s)
    blk_expert = jnp.minimum(jnp.searchsorted(pad_ends, jnp.arange(n_blocks, dtype=jnp.int32) * MOE_BLOCK, side='right'),
                             N_EXPERTS - 1)
    x_pad = jnp.concatenate([xf, jnp.zeros((1, d), xf.dtype)], axis=0)
    xb = x_pad[buf_tok].reshape(n_blocks, MOE_BLOCK, d)

    def expert_block(args):
        xblk, e = args
        hid = jax.nn.silu(xblk @ w1[e]) * (xblk @ w3[e])
        return hid @ w2[e]

    yb = lax.map(expert_block, (xb, blk_expert)).reshape(cap, d)
    y = jax.ops.segment_sum(yb * buf_gate[:, None].astype(yb.dtype), buf_tok, num_segments=n + 1)
    return y[:n]


def setup_inputs(seed: int = 0) -> dict:
    key = jax.random.key(seed)
    ks = jax.random.split(key, 40)
    f32 = jnp.float32

    def nrm(k, shape, scale):
        return jax.random.normal(k, shape, f32) * scale

    ret_base = jnp.asarray(np.log(-np.log(1.0 - 2.0 ** (-5.0 - np.arange(RET_HEADS)))), f32)
    return {
        'x': nrm(ks[0], (BATCH, SEQ, D_MODEL), 1.0),
        'c': nrm(ks[1], (BATCH, D_MODEL), 1.0),
        'ctx': nrm(ks[2], (BATCH, CTX_LEN, D_MODEL), 1.0),
        'c_ctx': nrm(ks[3], (D_MODEL,), 1.0),
        'mod_w': nrm(ks[4], (DEPTH, D_MODEL, 6 * D_MODEL), 0.5 * D_MODEL ** -0.5),
        'mod_b': nrm(ks[5], (DEPTH, 6 * D_MODEL), 0.02),
        'norm1_g': 1.0 + nrm(ks[6], (DEPTH, D_MODEL), 0.02),
        'norm2_g': 1.0 + nrm(ks[7], (DEPTH, D_MODEL), 0.02),
        'w_in': nrm(ks[8], (DEPTH, D_MODEL, IN_COLS), D_MODEL ** -0.5),
        'w_branch': nrm(ks[9], (DEPTH, N_BRANCH, BRANCH_W, D_MODEL), BRANCH_W ** -0.5),
        'w_out': nrm(ks[10], (DEPTH, D_MODEL, D_MODEL), D_MODEL ** -0.5),
        'ret_log_decay': ret_base[None, None, :] + nrm(ks[11], (DEPTH, 2, RET_HEADS), 0.01),
        'na_qk_gain': 1.0 + nrm(ks[12], (DEPTH, 2, HEAD_DIM), 0.02),
        'na_rpb': nrm(ks[13], (DEPTH, NA_HEADS, 2 * NA_KH - 1, 2 * NA_KW - 1), 0.1),
        's5_lambda_re': -0.5 + nrm(ks[14], (DEPTH, 2, S5_GROUPS, S5_STATE), 0.01),
        's5_lambda_im': jnp.pi * jnp.arange(S5_STATE, dtype=f32) + nrm(ks[15], (DEPTH, 2, S5_GROUPS, S5_STATE), 0.01),
        's5_log_step': jax.random.uniform(ks[16], (DEPTH, 2, S5_GROUPS), f32, math.log(1e-3), math.log(1e-1)),
        's5_b_re': nrm(ks[17], (DEPTH, S5_GROUPS, S5_STATE, S5_GROUP_CH), (2.0 * S5_GROUP_CH) ** -0.5),
        's5_b_im': nrm(ks[18], (DEPTH, S5_GROUPS, S5_STATE, S5_GROUP_CH), (2.0 * S5_GROUP_CH) ** -0.5),
        's5_c_re': nrm(ks[19], (DEPTH, S5_GROUPS, S5_GROUP_CH, S5_STATE), S5_STATE ** -0.5),
        's5_c_im': nrm(ks[20], (DEPTH, S5_GROUPS, S5_GROUP_CH, S5_STATE), S5_STATE ** -0.5),
        's5_d': nrm(ks[21], (DEPTH, BRANCH_W), 0.5),
        's5_glu_w': nrm(ks[22], (DEPTH, BRANCH_W, BRANCH_W), BRANCH_W ** -0.5),
        's5_glu_b': nrm(ks[23], (DEPTH, BRANCH_W), 0.02),
        'gqa_qk_gain': 1.0 + nrm(ks[24], (DEPTH, 2, HEAD_DIM), 0.02),
        'gqa_sink': nrm(ks[25], (DEPTH, GQA_Q_HEADS), 0.5),
        'router_w1': nrm(ks[26], (DEPTH, D_MODEL, MOE_GROUPS), D_MODEL ** -0.5),
        'router_b1': nrm(ks[27], (DEPTH, MOE_GROUPS), 0.01),
        'router_w2': nrm(ks[28], (DEPTH, D_MODEL, N_EXPERTS), D_MODEL ** -0.5),
        'router_b2': nrm(ks[29], (DEPTH, N_EXPERTS), 0.01),
        'exp_w1': nrm(ks[30], (DEPTH, N_EXPERTS, D_MODEL, D_FF_EXPERT), D_MODEL ** -0.5),
        'exp_w3': nrm(ks[31], (DEPTH, N_EXPERTS, D_MODEL, D_FF_EXPERT), D_MODEL ** -0.5),
        'exp_w2': nrm(ks[32], (DEPTH, N_EXPERTS, D_FF_EXPERT, D_MODEL), D_FF_EXPERT ** -0.5),
    }


def reference(x, c, ctx, c_ctx, mod_w, mod_b, norm1_g, norm2_g, w_in, w_branch, w_out, ret_log_decay,
              na_qk_gain, na_rpb, s5_lambda_re, s5_lambda_im, s5_log_step, s5_b_re, s5_b_im, s5_c_re,
              s5_c_im, s5_d, s5_glu_w, s5_glu_b, gqa_qk_gain, gqa_sink, router_w1, router_b1, router_w2,
              router_b2, exp_w1, exp_w3, exp_w2):
    tabs = rope_tables(x.shape[1])
    h, hc = x, ctx
    sc, scc = jax.nn.silu(c), jax.nn.silu(c_ctx)
    for i in range(DEPTH):
        with_ctx = i < DEPTH - 1
        mod = jnp.split((sc @ mod_w[i] + mod_b[i])[:, None, :], 6, axis=-1)
        modc = jnp.split(scc @ mod_w[i] + mod_b[i], 6)
        a = _modulate(_rms(h, norm1_g[i]), mod[0], mod[1])
        ac = _modulate(_rms(hc, norm1_g[i]), modc[0], modc[1])
        y, yc = mixer_sublayer(a, ac, tabs, with_ctx, w_in[i], w_branch[i], w_out[i], ret_log_decay[i],
                               na_qk_gain[i], na_rpb[i], s5_lambda_re[i], s5_lambda_im[i], s5_log_step[i],
                               s5_b_re[i], s5_b_im[i], s5_c_re[i], s5_c_im[i], s5_d[i], s5_glu_w[i],
                               s5_glu_b[i], gqa_qk_gain[i], gqa_sink[i])
        h = h + mod[2] * y
        if with_ctx:
            hc = hc + modc[2] * yc
        f = _modulate(_rms(h, norm2_g[i]), mod[3], mod[4])
        if with_ctx:
            fc = _modulate(_rms(hc, norm2_g[i]), modc[3], modc[4])
            n_ctx = fc.shape[0] * fc.shape[1]
            tokens = jnp.concatenate([fc.reshape(n_ctx, D_MODEL), f.reshape(-1, D_MODEL)], axis=0)
            out = hier_moe(tokens, router_w1[i], router_b1[i], router_w2[i], router_b2[i],
                           exp_w1[i], exp_w3[i], exp_w2[i])
            hc = hc + modc[5] * out[:n_ctx].reshape(hc.shape)
            h = h + mod[5] * out[n_ctx:].reshape(h.shape)
        else:
            out = hier_moe(f.reshape(-1, D_MODEL), router_w1[i], router_b1[i], router_w2[i], router_b2[i],
                           exp_w1[i], exp_w3[i], exp_w2[i])
            h = h + mod[5] * out.reshape(h.shape)
    return h
```

```python
import contextlib
import math
import numpy as np
import ml_dtypes
import concourse.bass as bass
import concourse.mybir as mybir
from concourse.bass_utils import run_bass_kernel_spmd

F32 = mybir.dt.float32
BF16 = mybir.dt.bfloat16
I32 = mybir.dt.int32
AF = mybir.ActivationFunctionType
ALU = mybir.AluOpType
AX = mybir.AxisListType

D = 1024
L = 8192
NCTX = 256
T = L + NCTX
NT = T // 128
NXT = L // 128
DEPTH = 4
MIXC = 2560
EPS = 1e-6
NEGM = -240000.0


class Res:
    __slots__ = ("name", "w", "rd")

    def __init__(self, name=""):
        self.name = name
        self.w = None
        self.rd = {}


class Prog:
    NDS = 12

    def __init__(self, nc, same_sync=True, dma_queues=("sp", "pool", "act")):
        self.nc = nc
        self.E = {"pe": nc.tensor, "dve": nc.vector, "act": nc.scalar, "pool": nc.gpsimd, "sp": nc.sync}
        self.same_sync = same_sync
        self.semh = {}
        self.cnt = {}
        for k in self.E:
            self.semh[("c", k)] = nc.alloc_semaphore(f"sc_{k}")
            self.cnt[k] = 0
        self.seen = {k: {} for k in self.E}
        self.duse = {}
        self.dnext = {}
        for q in dma_queues:
            self.duse[q] = [0] * self.NDS
            self.dnext[q] = 0
            for i in range(self.NDS):
                self.semh[("d", q, i)] = nc.alloc_semaphore(f"sd_{q}_{i}")
        self.ninst = 0

    def _wait(self, eng, key, val):
        if val <= 0 or self.seen[eng].get(key, 0) >= val:
            return
        self.E[eng].wait_ge(self.semh[key], val)
        self.seen[eng][key] = val

    def _deps(self, eng, r, w):
        deps = {}
        for res in r:
            if res.w is not None:
                k, v = res.w
                if deps.get(k, 0) < v:
                    deps[k] = v
        for res in w:
            if res.w is not None:
                k, v = res.w
                if deps.get(k, 0) < v:
                    deps[k] = v
            for k, v in res.rd.items():
                if deps.get(k, 0) < v:
                    deps[k] = v
        for k, v in deps.items():
            if k == ("c", eng) and not self.same_sync:
                continue
            self._wait(eng, k, v)

    def _mark(self, tok, r, w):
        k, v = tok
        for res in r:
            if res.rd.get(k, 0) < v:
                res.rd[k] = v
        for res in w:
            res.w = tok
            res.rd = {}

    def op(self, eng, fn, r=(), w=()):
        self._deps(eng, r, w)
        ins = fn(self.E[eng])
        self.cnt[eng] += 1
        ins.then_inc(self.semh[("c", eng)], 1)
        self._mark((("c", eng), self.cnt[eng]), r, w)
        self.ninst += 1
        return ins

    def dma(self, q, out, in_, r=(), w=(), **kw):
        self._deps(q, r, w)
        i = self.dnext[q]
        self.dnext[q] = (i + 1) % self.NDS
        key = ("d", q, i)
        self._wait(q, key, 16 * self.duse[q][i])
        ins = self.E[q].dma_start(out=out, in_=in_, **kw)
        self.duse[q][i] += 1
        ins.then_inc(self.semh[key], 16)
        self._mark((key, 16 * self.duse[q][i]), r, w)
        self.ninst += 1
        return ins

    def barrier(self, engines=None):
        engines = engines or list(self.E)
        for eng in engines:
            for k in self.E:
                if k != eng:
                    self._wait(eng, ("c", k), self.cnt[k])
            for q in self.duse:
                for i in range(self.NDS):
                    self._wait(eng, ("d", q, i), 16 * self.duse[q][i])


class Ctx:
    pass


_TCNT = [0]


def _tile(es, nc, name, shape, dt, psum=False):
    _TCNT[0] += 1
    name = f"{name}_{_TCNT[0]}"
    if not psum:
        t = es.enter_context(nc.sbuf_tensor(name, shape, dt))
        return t, Res(name)
    esz = 2 if dt == BF16 else 4
    n = int(np.prod(shape[1:]))
    per_bank = 2048 // esz
    nb = (n + per_bank - 1) // per_bank
    t = es.enter_context(nc.psum_tensor(name, [128, nb * per_bank], dt))
    ap = t[0:shape[0], 0:n]
    if len(shape) == 3:
        ap = ap.rearrange("p (a b) -> p a b", b=shape[2])
    elif len(shape) == 4:
        ap = ap.rearrange("p (a b c) -> p a b c", b=shape[2], c=shape[3])
    return ap, Res(name)


NA_NCLS = 21
S5_TC = 256
MAGIC = 12582912.0


def na_class_list():
    lst = [(10, 10 + dc) for dc in (-2, -1, 0, 1, 2)]
    for j in (0, 1):
        lst += [(j, c) for c in range(4)]
    for j in (62, 63):
        lst += [(j, c) for c in range(60, 64)]
    return lst


def na_chunks(j):
    if 2 <= j <= 61:
        return [(j + dc, dc + 2) for dc in (-2, -1, 0, 1, 2)]
    base = {0: 5, 1: 9, 62: 13, 63: 17}[j]
    c0 = 0 if j < 2 else 60
    return [(c0 + i, base + i) for i in range(4)]


def make_consts():
    c = {}
    pos = np.arange(L)
    inv = (10000.0 ** (-np.arange(16, dtype=np.float32) / 16)).astype(np.float32)
    ang_r = (pos // 64).astype(np.float32)[:, None] * inv
    ang_c = (pos % 64).astype(np.float32)[:, None] * inv
    cr, sr, cc, sc = np.cos(ang_r), np.sin(ang_r), np.cos(ang_c), np.sin(ang_c)
    cosf = np.concatenate([cr, cr, cc, cc], axis=1)
    sinf = np.concatenate([-sr, sr, -sc, sc], axis=1)
    cosf = np.concatenate([cosf, np.ones((NCTX, 64))], axis=0)
    sinf = np.concatenate([sinf, np.zeros((NCTX, 64))], axis=0)
    c["ropecs"] = np.concatenate([cosf, sinf], axis=1).astype(np.float32)
    c["ident"] = np.eye(128, dtype=np.float32)
    kl = np.arange(128)[:, None]
    ql = np.arange(128)[None, :]
    lo = np.where(kl >= ql, 0.0, NEGM)
    hi = np.where(kl <= ql, 0.0, NEGM)
    c["na_jx"] = np.zeros((128, 128), np.float32)
    for q in range(128):
        c["na_jx"][(q // 64) * 64 + 63 - q % 64, q] = 1.0
    rm = np.zeros((NA_NCLS, 128, 128), np.float32)
    for cls, (j, cch) in enumerate(na_class_list()):
        for qp in range(128):
            rq = 2 * j + qp // 64
            cq = 63 - qp % 64
            r0 = min(max(rq - 4, 0), 120)
            ws = min(max(cq - 8, 0), 48)
            for key in range(128):
                rk = 2 * cch + key // 64
                ck = key % 64
                ok = (r0 <= rk < r0 + 8) and (ws <= ck < ws + 16)
                rm[cls, qp, key] = 0.0 if ok else NEGM
    c["na_rm"] = rm
    si = np.arange(128, dtype=np.float32)[:, None]
    ti = np.arange(128, dtype=np.float32)[None, :]
    c["ret_dpos"] = np.maximum(ti - si, 0.0).astype(np.float32)
    c["ret_dneg"] = np.maximum(si - ti, 0.0).astype(np.float32)
    c["ret_diag"] = ((si == ti) * math.log(2.0) + math.log(0.125)).astype(np.float32)
    c["ret_tp1"] = np.broadcast_to(ti + 1.0, (128, 128)).astype(np.float32).copy()
    c["ret_tr"] = np.broadcast_to(128.0 - ti, (128, 128)).astype(np.float32).copy()
    c["ret_pcol"] = np.concatenate([127.0 - si, si], axis=1).astype(np.float32)
    c["s5_iota1"] = np.broadcast_to(np.arange(1, S5_TC + 1, dtype=np.float32)[None, :], (128, S5_TC)).copy()
    bm = np.zeros((128, 128), np.float32)
    for r in range(128):
        a = (r // 16) % 2
        bm[r, a * 64:(a + 1) * 64] = 1.0
    c["s5_bdmask"] = bm
    c["gqa_mask"] = np.stack([np.tile(lo, (1, 2)), np.tile(hi, (1, 2))], axis=1).astype(np.float32)
    return c


def stage_prep(K, l):
    nc, P, Dm = K.nc, K.P, K.dram
    with contextlib.ExitStack() as es:
        cc, rcc = _tile(es, nc, "pp_cc", [128, 8, 2], F32)
        sc, rsc = _tile(es, nc, "pp_sc", [128, 8, 2], F32)
        mw, rmw = _tile(es, nc, "pp_mw", [128, 8, 512], F32)
        mw2, rmw2 = _tile(es, nc, "pp_mw2", [128, 8, 512], F32)
        mws = [(mw, rmw), (mw2, rmw2)]
        mv, rmv = _tile(es, nc, "pp_mv", [2, 6144], F32)
        mb, rmb = _tile(es, nc, "pp_mb", [2, 6144], F32)
        g12, rg12 = _tile(es, nc, "pp_g", [2, 2048], F32)
        ps, rps = _tile(es, nc, "pp_ps", [2, 512], F32, psum=True)
        ps2, rps2 = _tile(es, nc, "pp_ps2", [2, 512], F32, psum=True)
        pss = [(ps, rps), (ps2, rps2)]
        with nc.allow_non_contiguous_dma(reason="tiny"):
            P.dma("sp", cc[:, :, 0], Dm["c"].rearrange("o (k p) -> p (o k)", p=128), w=[rcc])
            P.dma("sp", cc[:, :, 1], Dm["c_ctx"].rearrange("(k p) -> p k", p=128), w=[rcc])
        P.dma("sp", mb[:], Dm["mod_b"][l].partition_broadcast(2), w=[rmb])
        P.dma("sp", g12[:, 0:1024], Dm["norm1_g"][l].partition_broadcast(2), w=[rg12])
        P.dma("sp", g12[:, 1024:2048], Dm["norm2_g"][l].partition_broadcast(2), w=[rg12])
        P.op("act", lambda e: e.activation(out=sc[:], in_=cc[:], func=AF.Silu), r=[rcc], w=[rsc])
        mwv = Dm["mod_w"][l].rearrange("(k p) n -> p k n", p=128)
        for n in range(12):
            w_, rw_ = mws[n % 2]
            p_, rp_ = pss[n % 2]
            P.dma("sp", w_[:], mwv[:, :, n * 512:(n + 1) * 512], w=[rw_])
            for k in range(8):
                P.op("pe", lambda e: e.matmul(p_[:], lhsT=sc[:, k, :], rhs=w_[:, k, :], start=(k == 0), stop=(k == 7)),
                     r=[rsc, rw_], w=[rp_])
            P.op("dve", lambda e: e.tensor_tensor(out=mv[:, n * 512:(n + 1) * 512], in0=p_[:], in1=mb[:, n * 512:(n + 1) * 512], op=ALU.add),
                 r=[rp_, rmb], w=[rmv])
        for slot, goff in ((1, 0), (4, 1024)):
            P.op("dve", lambda e: e.scalar_tensor_tensor(out=mv[:, slot * 1024:(slot + 1) * 1024], in0=mv[:, slot * 1024:(slot + 1) * 1024],
                                                         scalar=1.0, in1=g12[:, goff:goff + 1024], op0=ALU.add, op1=ALU.mult),
                 r=[rmv, rg12], w=[rmv])
        order = [1, 0, 2, 4, 3, 5]
        mvd = Dm["modv"].rearrange("(a r) d -> a r d", a=2)
        for j, slot in enumerate(order):
            P.dma("sp", mvd[:, j, :], mv[:, slot * 1024:(slot + 1) * 1024], r=[rmv], w=[K.R["modv"]])
    P.barrier()


def stage_A(K, l, tiles=None):
    nc, P, Dm, R = K.nc, K.P, K.dram, K.R
    tiles = list(range(NT)) if tiles is None else tiles
    with contextlib.ExitStack() as es:
        win, rwin = _tile(es, nc, "A_win", [128, 8, MIXC], BF16)
        ident, rident = _tile(es, nc, "A_ident", [128, 128], BF16)
        identf, ridentf = _tile(es, nc, "A_identf", [128, 128], F32)
        bc, rbc = _tile(es, nc, "A_bc", [128, 4, 1024], F32)
        gains, rgains = _tile(es, nc, "A_gains", [128, 4, 64], F32)
        epsc, repsc = _tile(es, nc, "A_eps", [128, 1], F32)
        hts = [_tile(es, nc, f"A_h{i}", [128, 1024], F32) for i in range(2)]
        rps_ = [_tile(es, nc, f"A_rope{i}", [128, 128], F32) for i in range(2)]
        junk, rjunk = _tile(es, nc, "A_junk", [128, 1024], BF16)
        ssq, rssq = _tile(es, nc, "A_ssq", [128, 1], F32)
        rstd, rrstd = _tile(es, nc, "A_rstd", [128, 1], F32)
        t1, rt1 = _tile(es, nc, "A_t1", [128, 1024], F32)
        abf, rabf = _tile(es, nc, "A_abf", [128, 1024], BF16)
        aT, raT = _tile(es, nc, "A_aT", [128, 8, 128], BF16)
        pT, rpT = _tile(es, nc, "A_pT", [128, 8, 128], BF16, psum=True)
        pm = [_tile(es, nc, f"A_pm{i}", [128, 512], F32, psum=True) for i in range(5)]
        pT2, rpT2 = _tile(es, nc, "A_pT2", [128, 4, 128], BF16, psum=True)
        sA, rsA = _tile(es, nc, "A_sA", [128, 512], F32)
        sB, rsB = _tile(es, nc, "A_sB", [128, 512], F32)
        o_rqk, ro_rqk = _tile(es, nc, "A_orqk", [128, 512], BF16)
        o_rv, ro_rv = _tile(es, nc, "A_orv", [128, 256], BF16)
        o_rg, ro_rg = _tile(es, nc, "A_org", [128, 256], F32)
        o_nqk, ro_nqk = _tile(es, nc, "A_onqk", [128, 512], BF16)
        o_nv, ro_nv = _tile(es, nc, "A_onv", [128, 256], BF16)
        o_su, ro_su = _tile(es, nc, "A_osu", [128, 256], F32)
        o_sub, ro_sub = _tile(es, nc, "A_osub", [128, 256], BF16)
        o_gqk, ro_gqk = _tile(es, nc, "A_ogqk", [128, 384], BF16)
        o_gv, ro_gv = _tile(es, nc, "A_ogv", [128, 128], BF16)
        oT, roT = _tile(es, nc, "A_oT", [128, 4, 128], BF16)
        ss8, rss8 = _tile(es, nc, "A_ss8", [128, 8], F32)
        rs8, rrs8 = _tile(es, nc, "A_rs8", [128, 8], F32)

        P.dma("pool", win[:], Dm["w_in"][l].rearrange("(k p) n -> p k n", p=128)[:, :, 0:MIXC], w=[rwin])
        P.dma("sp", identf[:], Dm["ident"], w=[ridentf])
        P.op("dve", lambda e: e.tensor_copy(ident[:], identf[:]), r=[ridentf], w=[rident])
        for j, row in enumerate((0, 1, 6, 7)):
            P.dma("sp", bc[:, j, :], Dm["modv"][row].partition_broadcast(128), r=[R["modv"]], w=[rbc])
        P.dma("sp", gains[:, 0:2, :], Dm["na_qk_gain"][l].partition_broadcast(128), w=[rgains])
        P.dma("sp", gains[:, 2:4, :], Dm["gqa_qk_gain"][l].partition_broadcast(128), w=[rgains])
        P.op("dve", lambda e: e.memset(epsc[:], EPS), w=[repsc])

        hsrc = K.hsrc(l)

        def load(t, par):
            ht, rht = hts[par]
            rp, rrp = rps_[par]
            P.dma("sp", ht[:], hsrc(t), r=[R["H"]], w=[rht])
            P.dma("sp", rp[:], Dm["ropecs"][t * 128:(t + 1) * 128, :], w=[rrp])

        def rmsn(src, nh, gidx, dst, rdst_list, rsrc_list):
            P.op("act", lambda e: e.activation(out=sB[:, 0:nh * 64], in_=src, func=AF.Square), r=rsrc_list, w=[rsB])
            P.op("dve", lambda e: e.tensor_reduce(out=ss8[:, 0:nh], in_=sB[:, 0:nh * 64].rearrange("p (h d) -> p h d", d=64), axis=AX.X, op=ALU.add),
                 r=[rsB], w=[rss8])
            P.op("act", lambda e: e.activation(out=rs8[:, 0:nh], in_=ss8[:, 0:nh], func=AF.Sqrt, scale=1.0 / 64, bias=epsc[:, 0:1]),
                 r=[rss8, repsc], w=[rrs8])
            P.op("dve", lambda e: e.reciprocal(out=rs8[:, 0:nh], in_=rs8[:, 0:nh]), r=[rrs8], w=[rrs8])
            P.op("dve", lambda e: e.tensor_tensor(out=dst.rearrange("p (h d) -> p h d", d=64), in0=src.rearrange("p (h d) -> p h d", d=64),
                                                  in1=rs8[:, 0:nh].unsqueeze(2).to_broadcast([128, nh, 64]), op=ALU.mult),
                 r=rsrc_list + [rrs8], w=rdst_list)
            P.op("dve", lambda e: e.tensor_tensor(out=dst.rearrange("p (h d) -> p h d", d=64), in0=dst.rearrange("p (h d) -> p h d", d=64),
                                                  in1=gains[:, gidx, :].unsqueeze(1).to_broadcast([128, nh, 64]), op=ALU.mult),
                 r=rdst_list + [rgains], w=rdst_list)

        def rope(src, nh, rp, rrp, dst_bf, rsrc_list, rdst_list):
            v5 = lambda ap: ap.rearrange("p (h a b c) -> p h a b c", a=2, b=2, c=16)
            cosb = rp[:, 0:64].rearrange("p (a b c) -> p a b c", a=2, b=2).unsqueeze(1).to_broadcast([128, nh, 2, 2, 16])
            sinb = rp[:, 64:128].rearrange("p (a b c) -> p a b c", a=2, b=2).unsqueeze(1).to_broadcast([128, nh, 2, 2, 16])
            P.op("dve", lambda e: e.tensor_tensor(out=v5(sA[:, 0:nh * 64]), in0=v5(src), in1=cosb, op=ALU.mult), r=rsrc_list + [rrp], w=[rsA])
            P.op("dve", lambda e: e.tensor_tensor(out=v5(sB[:, 0:nh * 64]), in0=v5(src)[:, :, :, ::-1, :], in1=sinb, op=ALU.mult),
                 r=rsrc_list + [rrp], w=[rsB])
            P.op("dve", lambda e: e.tensor_tensor(out=dst_bf, in0=sA[:, 0:nh * 64], in1=sB[:, 0:nh * 64], op=ALU.add), r=[rsA, rsB], w=rdst_list)

        def transp_out(src_bf, rsrc, nchunk, dram_rows, t):
            for k in range(nchunk):
                P.op("pe", lambda e: e.transpose(out=pT2[:, k, :], in_=src_bf[:, k * 128:(k + 1) * 128], identity=ident[:]),
                     r=[rsrc, rident], w=[rpT2])
            P.op("act", lambda e: e.copy(out=oT[:, 0:nchunk, :], in_=pT2[:, 0:nchunk, :]), r=[rpT2], w=[roT])
            P.dma("pool", dram_rows.rearrange("(k p) n -> p k n", p=128)[:, :, t * 128:(t + 1) * 128], oT[:, 0:nchunk, :], r=[roT], w=[R["mix"]])

        load(tiles[0], 0)
        for idx, t in enumerate(tiles):
            if idx + 1 < len(tiles):
                load(tiles[idx + 1], (idx + 1) % 2)
            ht, rht = hts[idx % 2]
            rp, rrp = rps_[idx % 2]
            isx = t < NXT
            g1 = bc[:, 0 if isx else 2, :]
            sh1 = bc[:, 1 if isx else 3, :]
            ts = slice(t * 128, (t + 1) * 128)
            P.op("act", lambda e: e.activation(out=junk[:], in_=ht[:], func=AF.Square, accum_out=ssq[:]), r=[rht], w=[rjunk, rssq])
            P.op("act", lambda e: e.activation(out=rstd[:], in_=ssq[:], func=AF.Sqrt, scale=1.0 / D, bias=epsc[:, 0:1]), r=[rssq, repsc], w=[rrstd])
            P.op("dve", lambda e: e.reciprocal(out=rstd[:], in_=rstd[:]), r=[rrstd], w=[rrstd])
            P.op("dve", lambda e: e.scalar_tensor_tensor(out=t1[:], in0=ht[:], scalar=rstd[:, 0:1], in1=g1, op0=ALU.mult, op1=ALU.mult),
                 r=[rht, rrstd, rbc], w=[rt1])
            P.op("dve", lambda e: e.tensor_tensor(out=abf[:], in0=t1[:], in1=sh1, op=ALU.add), r=[rt1, rbc], w=[rabf])
            for k in range(8):
                P.op("pe", lambda e: e.transpose(out=pT[:, k, :], in_=abf[:, k * 128:(k + 1) * 128], identity=ident[:]), r=[rabf, rident], w=[rpT])
            P.op("act", lambda e: e.copy(out=aT[:], in_=pT[:]), r=[rpT], w=[raT])
            P.dma("pool", Dm["aT"].rearrange("(k p) n -> p k n", p=128)[:, :, ts], aT[:], r=[raT], w=[R["aT"]])
            for n in range(5):
                pmn, rpmn = pm[n]
                for k in range(8):
                    P.op("pe", lambda e: e.matmul(pmn[:], lhsT=aT[:, k, :], rhs=win[:, k, n * 512:(n + 1) * 512], start=(k == 0), stop=(k == 7)),
                         r=[raT, rwin], w=[rpmn])
            rope(pm[0][0][:], 8, rp, rrp, o_rqk[:], [pm[0][1]], [ro_rqk])
            P.dma("pool", Dm["rk"][ts, :], o_rqk[:, 256:512], r=[ro_rqk], w=[R["mix"]])
            transp_out(o_rqk, ro_rqk, 4, Dm["rqkT"], t)
            P.op("act", lambda e: e.copy(out=o_rv[:], in_=pm[1][0][:, 0:256]), r=[pm[1][1]], w=[ro_rv])
            P.op("act", lambda e: e.activation(out=o_rg[:], in_=pm[1][0][:, 256:512], func=AF.Silu), r=[pm[1][1]], w=[ro_rg])
            P.dma("pool", Dm["rv"][ts, :], o_rv[:], r=[ro_rv], w=[R["mix"]])
            P.dma("pool", Dm["rg"][ts, :], o_rg[:], r=[ro_rg], w=[R["mix"]])
            rmsn(pm[2][0][:, 0:256], 4, 0, t1[:, 0:256], [rt1], [pm[2][1]])
            rmsn(pm[2][0][:, 256:512], 4, 1, t1[:, 256:512], [rt1], [pm[2][1]])
            P.op("act", lambda e: e.copy(out=o_nqk[:], in_=t1[:, 0:512]), r=[rt1], w=[ro_nqk])
            transp_out(o_nqk, ro_nqk, 4, Dm["nqkT"], t)
            P.op("act", lambda e: e.copy(out=o_nv[:], in_=pm[3][0][:, 0:256]), r=[pm[3][1]], w=[ro_nv])
            P.dma("pool", Dm["nv"][ts, :], o_nv[:], r=[ro_nv], w=[R["mix"]])
            P.op("act", lambda e: e.copy(out=o_su[:], in_=pm[3][0][:, 256:512]), r=[pm[3][1]], w=[ro_su])
            P.op("dve", lambda e: e.tensor_copy(out=o_sub[:], in_=pm[3][0][:, 256:512]), r=[pm[3][1]], w=[ro_sub])
            P.dma("pool", Dm["su"][ts, :], o_su[:], r=[ro_su], w=[R["mix"]])
            transp_out(o_sub, ro_sub, 2, Dm["suT"], t)
            rmsn(pm[4][0][:, 0:256], 4, 2, t1[:, 512:768], [rt1], [pm[4][1]])
            rmsn(pm[4][0][:, 256:384], 2, 3, t1[:, 768:896], [rt1], [pm[4][1]])
            rope(t1[:, 512:896], 6, rp, rrp, o_gqk[:], [rt1], [ro_gqk])
            transp_out(o_gqk, ro_gqk, 3, Dm["gqkT"], t)
            P.op("act", lambda e: e.copy(out=o_gv[:], in_=pm[4][0][:, 384:512]), r=[pm[4][1]], w=[ro_gv])
            P.dma("pool", Dm["gv"][ts, :], o_gv[:], r=[ro_gv], w=[R["mix"]])
    P.barrier()


INPUT_NAMES = ["x", "c", "ctx", "c_ctx", "mod_w", "mod_b", "norm1_g", "norm2_g", "w_in", "w_branch", "w_out", "ret_log_decay",
               "na_qk_gain", "na_rpb", "s5_lambda_re", "s5_lambda_im", "s5_log_step", "s5_b_re", "s5_b_im", "s5_c_re",
               "s5_c_im", "s5_d", "s5_glu_w", "s5_glu_b", "gqa_qk_gain", "gqa_sink", "router_w1", "router_b1", "router_w2",
               "router_b2", "exp_w1", "exp_w3", "exp_w2"]

SCRATCH = {
    "modv": ([12, 1024], F32),
    "H": ([T, D], F32),
    "aT": ([D, T], BF16),
    "rqkT": ([512, T], BF16), "rk": ([T, 256], BF16), "rv": ([T, 256], BF16), "rg": ([T, 256], F32),
    "nqkT": ([512, T], BF16), "nv": ([T, 256], BF16),
    "su": ([T, 256], F32), "suT": ([256, T], BF16),
    "gqkT": ([384, T], BF16), "gv": ([T, 128], BF16),
}


def build(in_shapes, consts, plan, expose=()):
    nc = bass.Bass("TRN2", target_bir_lowering=False)
    K = Ctx()
    K.nc = nc
    K.dram = {}
    for name, (shape, dt) in in_shapes.items():
        K.dram[name] = nc.dram_tensor(name, list(shape), dt, kind="ExternalInput").ap()
    for name, arr in consts.items():
        K.dram[name] = nc.dram_tensor(name, list(arr.shape), F32, kind="ExternalInput").ap()
    for name, (shape, dt) in SCRATCH.items():
        if name in K.dram:
            continue
        kind = "ExternalOutput" if name in expose else "Internal"
        K.dram[name] = nc.dram_tensor(name, list(shape), dt, kind=kind).ap()
    K.dram["out"] = nc.dram_tensor("out", [L, D], F32, kind="ExternalOutput").ap()
    K.R = {k: Res(k) for k in ["modv", "H", "aT", "mix", "y", "out", "s5y"]}
    K.P = Prog(nc)

    def hsrc(l):
        def f(t):
            if l == 0:
                if t < NXT:
                    return K.dram["x"][t * 128:(t + 1) * 128, :]
                return K.dram["ctx"][(t - NXT) * 128:(t - NXT + 1) * 128, :]
            return K.dram["H"][t * 128:(t + 1) * 128, :]
        return f
    K.hsrc = hsrc
    plan(K)
    K.P.barrier(["sp"])
    return nc, K


def run_attention(K, es, pfx, units, rd_res, epilogue, maxc):
    nc, P = K.nc, K.P
    wmax = max(u["nq"] for u in units) * 128
    spb = 512 // wmax
    nbank = (maxc + spb - 1) // spb
    S = [[_tile(es, nc, f"{pfx}_S{a}_{b}", [128, spb, wmax], F32, psum=True) for b in range(nbank)] for a in range(2)]
    O = _tile(es, nc, f"{pfx}_O", [128, 4, 65], F32, psum=True)
    pT = [[_tile(es, nc, f"{pfx}_pT{a}_{b}", [128, wmax], BF16) for b in range(maxc)] for a in range(2)]

    def emit_S(ui):
        u = units[ui]
        a = ui % 2
        w = u["nq"] * 128
        for ci, (kT, bias, _v) in enumerate(u["chunks"]):
            st, rst = S[a][ci // spb]
            sp = st[:, ci % spb, 0:w]
            P.op("pe", lambda e: e.matmul(sp, lhsT=kT, rhs=u["q"], start=True, stop=(bias is None)), r=rd_res, w=[rst])
            if bias is not None:
                P.op("pe", lambda e: e.matmul(sp, lhsT=bias[0], rhs=bias[1], start=False, stop=True), r=rd_res, w=[rst])
        for ci in range(len(u["chunks"])):
            st, rst = S[a][ci // spb]
            sp = st[:, ci % spb, 0:w]
            pt, rpt = pT[a][ci]
            P.op("act", lambda e: e.activation(out=pt[:, 0:w], in_=sp, func=AF.Exp, scale=0.125), r=[rst], w=[rpt])

    def emit_PV(ui):
        u = units[ui]
        a = ui % 2
        ot, rot = O
        nch = len(u["chunks"])
        for g, h in enumerate(u["heads"]):
            for ci, (_k, _b, v) in enumerate(u["chunks"]):
                pt, rpt = pT[a][ci]
                P.op("pe", lambda e: e.matmul(ot[:, h, :], lhsT=pt[:, g * 128:(g + 1) * 128], rhs=v, start=(ci == 0), stop=(ci == nch - 1)),
                     r=[rpt] + rd_res, w=[rot])
        if u["final"]:
            epilogue(u["j"], ot, rot)

    emit_S(0)
    for ui in range(len(units)):
        if ui + 1 < len(units):
            emit_S(ui + 1)
        emit_PV(ui)


def attn_epilogue_factory(K, es, pfx, ident, rident, yrow0, extra_den=None):
    nc, P, Dm, R = K.nc, K.P, K.dram, K.R
    den, rden = _tile(es, nc, pfx + "_den", [128, 4], F32)
    ybf, rybf = _tile(es, nc, pfx + "_ybf", [128, 256], BF16)
    pT2, rpT2 = _tile(es, nc, pfx + "_pT2", [128, 2, 128], BF16, psum=True)
    oT, roT = _tile(es, nc, pfx + "_oT", [128, 2, 128], BF16)

    def epi(j, ot, rot):
        if extra_den is not None:
            P.op("dve", lambda e: e.tensor_tensor(out=den[:], in0=ot[:, :, 64], in1=extra_den[0][:], op=ALU.add), r=[rot, extra_den[1]], w=[rden])
            P.op("dve", lambda e: e.reciprocal(out=den[:], in_=den[:]), r=[rden], w=[rden])
        else:
            P.op("dve", lambda e: e.reciprocal(out=den[:], in_=ot[:, :, 64]), r=[rot], w=[rden])
        P.op("dve", lambda e: e.tensor_tensor(out=ybf[:].rearrange("p (h d) -> p h d", d=64), in0=ot[:, :, 0:64],
                                              in1=den[:].unsqueeze(2).to_broadcast([128, 4, 64]), op=ALU.mult), r=[rot, rden], w=[rybf])
        for k in range(2):
            P.op("pe", lambda e: e.transpose(out=pT2[:, k, :], in_=ybf[:, k * 128:(k + 1) * 128], identity=ident[:]), r=[rybf, rident], w=[rpT2])
        P.op("act", lambda e: e.copy(out=oT[:], in_=pT2[:]), r=[rpT2], w=[roT])
        P.dma("pool", Dm["yT"][yrow0:yrow0 + 256, :].rearrange("(k p) n -> p k n", p=128)[:, :, j * 128:(j + 1) * 128], oT[:], r=[roT], w=[R["y"]])
    return epi


def stage_gqa(K, l, with_ctx, qtiles=None):
    nc, P, Dm, R = K.nc, K.P, K.dram, K.R
    with contextlib.ExitStack() as es:
        qT, rqT = _tile(es, nc, "G_qT", [64, 4, T], BF16)
        kT, rkT = _tile(es, nc, "G_kT", [64, 2, T], BF16)
        V, rV = _tile(es, nc, "G_V", [128, NT, 2, 65], BF16)
        mk, rmk = _tile(es, nc, "G_mask", [128, 2, 256], BF16)
        ident, rident = _tile(es, nc, "G_ident", [128, 128], BF16)
        esk, resk = _tile(es, nc, "G_esink", [128, 4], F32)
        for h in range(4):
            P.dma("sp", qT[:, h, :], Dm["gqkT"][h * 64:(h + 1) * 64, :], r=[R["mix"]], w=[rqT])
        for kv in range(2):
            P.dma("sp", kT[:, kv, :], Dm["gqkT"][256 + kv * 64:256 + (kv + 1) * 64, :], r=[R["mix"]], w=[rkT])
            P.dma("sp", V[:, :, kv, 0:64], Dm["gv"][:, kv * 64:(kv + 1) * 64].rearrange("(c p) d -> p c d", p=128), r=[R["mix"]], w=[rV])
        P.op("pool", lambda e: e.memset(V[:, :, :, 64:65], 1.0), w=[rV])
        P.dma("pool", mk[:], Dm["gqa_mask"], w=[rmk])
        P.dma("pool", ident[:], Dm["ident"], w=[rident])
        P.dma("sp", esk[:], Dm["gqa_sink"][l].partition_broadcast(128), w=[resk])
        P.op("act", lambda e: e.activation(out=esk[:], in_=esk[:], func=AF.Exp), r=[resk], w=[resk])
        rd = [rqT, rkT, rV, rmk, rident]
        epi = attn_epilogue_factory(K, es, "G", ident, rident, 768, extra_den=(esk, resk))
        qtiles = qtiles if qtiles is not None else list(range(NXT)) + ([64, 65] if with_ctx else [])
        units = []
        for j in qtiles:
            if j < NXT:
                ch = [(c, tag) for c, tag in ((j - 1, 0), (j, None), (j + 1, 1)) if 0 <= c < NXT] + [(64, None), (65, None)]
            else:
                ch = [(64, None), (65, None)]
            for kv in range(2):
                chunks = []
                for c, tag in ch:
                    bias = None if tag is None else (ident[:], mk[:, tag, :])
                    chunks.append((kT[:, kv, c * 128:(c + 1) * 128], bias, V[:, c, kv, :]))
                units.append(dict(j=j, q=qT[:, 2 * kv:2 * kv + 2, j * 128:(j + 1) * 128], nq=2, heads=[2 * kv, 2 * kv + 1], chunks=chunks, final=(kv == 1)))
        run_attention(K, es, "G", units, rd, epi, 5)
    P.barrier()


SCRATCH.update({"yT": ([1024, T], BF16)})


def stage_na(K, l, with_ctx, qtiles=None):
    nc, P, Dm, R = K.nc, K.P, K.dram, K.R
    with contextlib.ExitStack() as es:
        qT, rqT = _tile(es, nc, "N_qT", [128, 2, T], BF16)
        kT, rkT = _tile(es, nc, "N_kT", [128, 2, T], BF16)
        V, rV = _tile(es, nc, "N_V", [128, NT, 4, 65], BF16)
        Bp, rBp = _tile(es, nc, "N_Bp", [128, NA_NCLS, 4, 128], BF16)
        jx, rjx = _tile(es, nc, "N_jx", [128, 128], BF16)
        ident, rident = _tile(es, nc, "N_ident", [128, 128], BF16)
        zt, rzt = _tile(es, nc, "N_zero", [64, 128], F32)
        rp, rrp = _tile(es, nc, "N_rpb", [15, 4, 31], F32)
        stg = [_tile(es, nc, f"N_stg{i}", [128, 2, 64], F32) for i in range(2)]
        rms_ = [_tile(es, nc, f"N_rm{i}", [128, 128], F32) for i in range(2)]
        rpad = Res("rpbpad")
        for c2 in range(2):
            P.dma("sp", qT[:, c2, :], Dm["nqkT"][c2 * 128:(c2 + 1) * 128, :], r=[R["mix"]], w=[rqT])
            P.dma("sp", kT[:, c2, :], Dm["nqkT"][256 + c2 * 128:256 + (c2 + 1) * 128, :], r=[R["mix"]], w=[rkT])
        for h in range(4):
            P.dma("sp", V[:, :, h, 0:64], Dm["nv"][:, h * 64:(h + 1) * 64].rearrange("(c p) d -> p c d", p=128), r=[R["mix"]], w=[rV])
        P.op("pool", lambda e: e.memset(V[:, :, :, 64:65], 1.0), w=[rV])
        P.dma("pool", jx[:], Dm["na_jx"], w=[rjx])
        P.dma("pool", ident[:], Dm["ident"], w=[rident])
        P.op("dve", lambda e: e.memset(zt[:], 0.0), w=[rzt])
        P.dma("sp", Dm["rpbpad"].rearrange("h r j -> (h r) j"), zt[:], r=[rzt], w=[rpad])
        P.dma("sp", rp[:], Dm["na_rpb"][l].rearrange("h r j -> r h j"), w=[rrp])
        for h in range(4):
            P.dma("sp", Dm["rpbpad"][h, 0:15, 48:79], rp[:, h, :], r=[rrp], w=[rpad])
        padt = Dm["rpbpad"].tensor
        cl = na_class_list()
        for cls, (j, cch) in enumerate(cl):
            rmt, rrmt = rms_[cls % 2]
            P.dma("sp", rmt[:], Dm["na_rm"][cls], w=[rrmt])
            for h in range(4):
                st, rst = stg[(cls * 4 + h) % 2]
                for rq in range(2):
                    dr0 = 2 * (cch - j) + 0 - rq + 7
                    src = bass.AP(tensor=padt, offset=(h * 16 + dr0) * 128, ap=[[1, 64], [128, 2], [1, 64]])
                    P.dma("sp" if rq == 0 else "act", st[rq * 64:(rq + 1) * 64, :, :], src, r=[rpad], w=[rst])
                P.op("dve", lambda e: e.scalar_tensor_tensor(out=Bp[:, cls, h, :], in0=st[:].rearrange("p a b -> p (a b)"), scalar=8.0, in1=rmt[:],
                                                             op0=ALU.mult, op1=ALU.add), r=[rst, rrmt], w=[rBp])
        rd = [rqT, rkT, rV, rBp, rjx, rident]
        epi = attn_epilogue_factory(K, es, "N", ident, rident, 256)
        qtiles = qtiles if qtiles is not None else list(range(NXT)) + ([64, 65] if with_ctx else [])
        units = []
        for j in qtiles:
            ch = (na_chunks(j) if j < NXT else []) + [(64, None), (65, None)]
            for h in range(4):
                pb, c2 = (h % 2) * 64, h // 2
                chunks = []
                for c, cls in ch:
                    bias = None if cls is None else (Bp[:, cls, h, :], jx[:])
                    chunks.append((kT[pb:pb + 64, c2, c * 128:(c + 1) * 128], bias, V[:, c, h, :]))
                units.append(dict(j=j, q=qT[pb:pb + 64, c2, j * 128:(j + 1) * 128], nq=1, heads=[h], chunks=chunks, final=(h == 3)))
        run_attention(K, es, "N", units, rd, epi, 7)
    P.barrier()


SCRATCH.update({"rpbpad": ([4, 16, 128], F32)})


def stage_ret(K, l, with_ctx, out_chunks=None):
    nc, P, Dm, R = K.nc, K.P, K.dram, K.R
    LN8 = math.log(0.125)
    with contextlib.ExitStack() as es:
        qT, rqT = _tile(es, nc, "R_qT", [128, 2, T], BF16)
        kT, rkT = _tile(es, nc, "R_kT", [128, 2, T], BF16)
        Kt, rKt = _tile(es, nc, "R_Kt", [128, NT, 256], BF16)
        Vt, rVt = _tile(es, nc, "R_Vt", [128, NT, 256], BF16)
        SF, rSF = _tile(es, nc, "R_SF", [128, NT, 2, 64], BF16)
        ident, rident = _tile(es, nc, "R_ident", [128, 128], BF16)
        lg, rlg = _tile(es, nc, "R_lg", [128, 8], F32)
        cst, rcst = _tile(es, nc, "R_cst", [128, 5, 128], F32)
        pcol, rpcol = _tile(es, nc, "R_pcol", [128, 2], F32)
        lnc, rlnc = _tile(es, nc, "R_lnc", [128, 2], F32)
        tmp, rtmp = _tile(es, nc, "R_tmp", [128, 128], F32)
        DT, rDT = _tile(es, nc, "R_DT", [128, 4, 128], F32)
        QF, rQF = _tile(es, nc, "R_QF", [128, 2, 128], F32)
        QB, rQB = _tile(es, nc, "R_QB", [128, 2, 128], F32)
        KD, rKD = _tile(es, nc, "R_KD", [128, 8], F32)
        CF, rCF = _tile(es, nc, "R_CF", [128, 2, 64], F32)
        CB, rCB = _tile(es, nc, "R_CB", [128, 2, 64], F32)
        SM, rSM = _tile(es, nc, "R_SM", [128, 2, 64], F32)
        SBc = [_tile(es, nc, f"R_SBc{i}", [128, 2, 64], BF16) for i in range(2)]
        kw, rkw = _tile(es, nc, "R_kw", [128, 256], BF16)
        PT = [_tile(es, nc, f"R_PT{i}", [128, 4, 128], BF16) for i in range(2)]
        qf, rqf = _tile(es, nc, "R_qf", [128, 2, 128], BF16)
        qb, rqb = _tile(es, nc, "R_qb", [128, 2, 128], BF16)
        gt = [_tile(es, nc, f"R_g{i}", [128, 256], F32) for i in range(2)]
        sq, rsq = _tile(es, nc, "R_sq", [128, 256], F32)
        ss4, rss4 = _tile(es, nc, "R_ss4", [128, 4], F32)
        yf, ryf = _tile(es, nc, "R_yf", [128, 256], F32)
        ybf, rybf = _tile(es, nc, "R_ybf", [128, 256], BF16)
        oT, roT = _tile(es, nc, "R_oT", [128, 2, 128], BF16)
        Sp = [_tile(es, nc, f"R_Sp{i}", [128, 4, 128], F32, psum=True) for i in range(2)]
        Op = [_tile(es, nc, f"R_Op{i}", [128, 4, 64], F32, psum=True) for i in range(2)]
        KVp, rKVp = _tile(es, nc, "R_KVp", [128, 2, 128], F32, psum=True)
        pT2, rpT2 = _tile(es, nc, "R_pT2", [128, 2, 128], BF16, psum=True)

        for c2 in range(2):
            P.dma("sp", qT[:, c2, :], Dm["rqkT"][c2 * 128:(c2 + 1) * 128, :], r=[R["mix"]], w=[rqT])
            P.dma("sp", kT[:, c2, :], Dm["rqkT"][256 + c2 * 128:256 + (c2 + 1) * 128, :], r=[R["mix"]], w=[rkT])
        P.dma("act", Kt[:], Dm["rk"].rearrange("(c p) d -> p c d", p=128), r=[R["mix"]], w=[rKt])
        P.dma("act", Vt[:], Dm["rv"].rearrange("(c p) d -> p c d", p=128), r=[R["mix"]], w=[rVt])
        P.dma("pool", ident[:], Dm["ident"], w=[rident])
        for i, nm in enumerate(["ret_dpos", "ret_dneg", "ret_diag", "ret_tp1", "ret_tr"]):
            P.dma("sp", cst[:, i, :], Dm[nm], w=[rcst])
        P.dma("sp", pcol[:], Dm["ret_pcol"], w=[rpcol])
        P.dma("sp", lg[:], Dm["ret_log_decay"][l].rearrange("a h -> (a h)").partition_broadcast(128), w=[rlg])
        P.op("dve", lambda e: e.memset(lnc[:, 0:1], LN8), w=[rlnc])
        P.op("dve", lambda e: e.memset(lnc[:, 1:2], EPS), w=[rlnc])
        P.op("act", lambda e: e.activation(out=lg[:], in_=lg[:], func=AF.Exp), r=[rlg], w=[rlg])
        P.op("dve", lambda e: e.tensor_scalar(out=lg[:], in0=lg[:], scalar1=-1.0, scalar2=None, op0=ALU.mult), r=[rlg], w=[rlg])
        for h in range(4):
            pb, c2 = (h % 2) * 64, h // 2
            P.op("dve", lambda e: e.tensor_scalar(out=tmp[:], in0=cst[:, 0, :], scalar1=lg[:, h:h + 1], scalar2=None, op0=ALU.mult), r=[rcst, rlg], w=[rtmp])
            P.op("dve", lambda e: e.scalar_tensor_tensor(out=tmp[:], in0=cst[:, 1, :], scalar=lg[:, 4 + h:5 + h], in1=tmp[:], op0=ALU.mult, op1=ALU.add),
                 r=[rcst, rlg, rtmp], w=[rtmp])
            P.op("dve", lambda e: e.tensor_tensor(out=tmp[:], in0=tmp[:], in1=cst[:, 2, :], op=ALU.add), r=[rtmp, rcst], w=[rtmp])
            P.op("act", lambda e: e.activation(out=DT[:, h, :], in_=tmp[:], func=AF.Exp), r=[rtmp], w=[rDT])
            P.op("act", lambda e: e.activation(out=QF[pb:pb + 64, c2, :], in_=cst[pb:pb + 64, 3, :], func=AF.Exp, scale=lg[pb:pb + 64, h:h + 1], bias=lnc[pb:pb + 64, 0:1]),
                 r=[rcst, rlg, rlnc], w=[rQF])
            P.op("act", lambda e: e.activation(out=QB[pb:pb + 64, c2, :], in_=cst[pb:pb + 64, 4, :], func=AF.Exp, scale=lg[pb:pb + 64, 4 + h:5 + h], bias=lnc[pb:pb + 64, 0:1]),
                 r=[rcst, rlg, rlnc], w=[rQB])
            P.op("act", lambda e: e.activation(out=KD[:, h:h + 1], in_=lg[:, h:h + 1], func=AF.Exp, scale=pcol[:, 0:1]), r=[rlg, rpcol], w=[rKD])
            P.op("act", lambda e: e.activation(out=KD[:, 4 + h:5 + h], in_=lg[:, 4 + h:5 + h], func=AF.Exp, scale=pcol[:, 1:2]), r=[rlg, rpcol], w=[rKD])
            P.op("act", lambda e: e.activation(out=CF[pb:pb + 64, c2, :], in_=lg[pb:pb + 64, h:h + 1].to_broadcast([64, 64]), func=AF.Exp, scale=128.0), r=[rlg], w=[rCF])
            P.op("act", lambda e: e.activation(out=CB[pb:pb + 64, c2, :], in_=lg[pb:pb + 64, 4 + h:5 + h].to_broadcast([64, 64]), func=AF.Exp, scale=128.0), r=[rlg], w=[rCB])

        def state_update(n, kdoff, Ctab, rCtab):
            P.op("dve", lambda e: e.tensor_tensor(out=kw[:].rearrange("p (h d) -> p h d", d=64), in0=Kt[:, n, :].rearrange("p (h d) -> p h d", d=64),
                                                  in1=KD[:, kdoff:kdoff + 4].unsqueeze(2).to_broadcast([128, 4, 64]), op=ALU.mult), r=[rKt, rKD], w=[rkw])
            for c2 in range(2):
                P.op("pe", lambda e: e.matmul(KVp[:, c2, :], lhsT=kw[:, c2 * 128:(c2 + 1) * 128], rhs=Vt[:, n, c2 * 128:(c2 + 1) * 128], start=True, stop=True),
                     r=[rkw, rVt], w=[rKVp])
            P.op("dve", lambda e: e.tensor_tensor(out=SM[:], in0=SM[:], in1=Ctab[:], op=ALU.mult), r=[rSM, rCtab], w=[rSM])
            for hp in (0, 64):
                P.op("dve", lambda e: e.tensor_tensor(out=SM[hp:hp + 64, :, :], in0=SM[hp:hp + 64, :, :], in1=KVp[hp:hp + 64, :, hp:hp + 64], op=ALU.add),
                     r=[rSM, rKVp], w=[rSM])

        fo = [64, 65] + list(range(NXT))
        P.op("dve", lambda e: e.memset(SM[:], 0.0), w=[rSM])
        for i, n in enumerate(fo):
            P.op("act", lambda e: e.copy(out=SF[:, n, :, :], in_=SM[:]), r=[rSM], w=[rSF])
            if i + 1 < len(fo):
                state_update(n, 0, CF, rCF)

        bo = [65, 64] + list(range(NXT - 1, -1, -1))
        outs = [n for n in bo if (n < NXT or with_ctx)]
        if out_chunks is not None:
            outs = [n for n in outs if n in out_chunks]
        P.op("dve", lambda e: e.memset(SM[:], 0.0), w=[rSM])
        oi = {n: i for i, n in enumerate(outs)}

        def emit_S(n):
            i = oi[n]
            sp, rsp = Sp[i % 2]
            cs = slice(n * 128, (n + 1) * 128)
            for h in range(4):
                pb, c2 = (h % 2) * 64, h // 2
                P.op("pe", lambda e: e.matmul(sp[:, h, :], lhsT=kT[pb:pb + 64, c2, cs], rhs=qT[pb:pb + 64, c2, cs], start=True, stop=True), r=[rkT, rqT], w=[rsp])
            pt, rpt = PT[i % 2]
            P.op("dve", lambda e: e.tensor_tensor(out=pt[:], in0=sp, in1=DT[:], op=ALU.mult), r=[rsp, rDT], w=[rpt])
            g_, rg_ = gt[i % 2]
            P.dma("sp", g_[:], Dm["rg"][cs, :], r=[R["mix"]], w=[rg_])

        def emit_O(n, sbc, rsbc):
            i = oi[n]
            cs = slice(n * 128, (n + 1) * 128)
            pt, rpt = PT[i % 2]
            op_, rop = Op[i % 2]
            g_, rg_ = gt[i % 2]
            P.op("dve", lambda e: e.tensor_tensor(out=qf[:], in0=qT[:, :, cs], in1=QF[:], op=ALU.mult), r=[rqT, rQF], w=[rqf])
            P.op("pool", lambda e: e.tensor_tensor(out=qb[:], in0=qT[:, :, cs], in1=QB[:], op=ALU.mult), r=[rqT, rQB], w=[rqb])
            for h in range(4):
                pb, c2 = (h % 2) * 64, h // 2
                P.op("pe", lambda e: e.matmul(op_[:, h, :], lhsT=pt[:, h, :], rhs=Vt[:, n, h * 64:(h + 1) * 64], start=True, stop=False), r=[rpt, rVt], w=[rop])
                P.op("pe", lambda e: e.matmul(op_[:, h, :], lhsT=qf[pb:pb + 64, c2, :], rhs=SF[pb:pb + 64, n, c2, :], start=False, stop=False), r=[rqf, rSF], w=[rop])
                P.op("pe", lambda e: e.matmul(op_[:, h, :], lhsT=qb[pb:pb + 64, c2, :], rhs=sbc[pb:pb + 64, c2, :], start=False, stop=True), r=[rqb, rsbc], w=[rop])
            P.op("act", lambda e: e.activation(out=sq[:].rearrange("p (h d) -> p h d", d=64), in_=op_, func=AF.Square), r=[rop], w=[rsq])
            P.op("dve", lambda e: e.tensor_reduce(out=ss4[:], in_=sq[:].rearrange("p (h d) -> p h d", d=64), axis=AX.X, op=ALU.add), r=[rsq], w=[rss4])
            P.op("act", lambda e: e.activation(out=ss4[:], in_=ss4[:], func=AF.Sqrt, scale=1.0 / 64, bias=lnc[:, 1:2]), r=[rss4, rlnc], w=[rss4])
            P.op("dve", lambda e: e.reciprocal(out=ss4[:], in_=ss4[:]), r=[rss4], w=[rss4])
            P.op("dve", lambda e: e.tensor_tensor(out=yf[:].rearrange("p (h d) -> p h d", d=64), in0=op_, in1=ss4[:].unsqueeze(2).to_broadcast([128, 4, 64]), op=ALU.mult),
                 r=[rop, rss4], w=[ryf])
            P.op("dve", lambda e: e.tensor_tensor(out=ybf[:], in0=yf[:], in1=g_[:], op=ALU.mult), r=[ryf, rg_], w=[rybf])
            for k in range(2):
                P.op("pe", lambda e: e.transpose(out=pT2[:, k, :], in_=ybf[:, k * 128:(k + 1) * 128], identity=ident[:]), r=[rybf, rident], w=[rpT2])
            P.op("act", lambda e: e.copy(out=oT[:], in_=pT2), r=[rpT2], w=[roT])
            P.dma("pool", Dm["yT"][0:256, :].rearrange("(k p) n -> p k n", p=128)[:, :, cs], oT[:], r=[roT], w=[R["y"]])

        if outs:
            emit_S(outs[0])
        for bi, n in enumerate(bo):
            sbc, rsbc = SBc[bi % 2]
            if n in oi:
                P.op("act", lambda e: e.copy(out=sbc[:], in_=SM[:]), r=[rSM], w=[rsbc])
                i = oi[n]
                if i + 1 < len(outs):
                    emit_S(outs[i + 1])
                emit_O(n, sbc, rsbc)
            if bi + 1 < len(bo):
                state_update(n, 4, CB, rCB)
    P.barrier()


def _sin_reduced(P, out, in_, shift, t1, rt1, t2, rt2, r_in, w_out):
    i2p = 1.0 / (2 * math.pi)
    P.op("dve", lambda e: e.tensor_scalar(out=t1, in0=in_, scalar1=i2p, scalar2=shift * i2p, op0=ALU.mult, op1=ALU.add), r=r_in, w=[rt1])
    P.op("dve", lambda e: e.tensor_scalar(out=t2, in0=t1, scalar1=MAGIC, scalar2=None, op0=ALU.add), r=[rt1], w=[rt2])
    P.op("dve", lambda e: e.tensor_scalar(out=t2, in0=t2, scalar1=MAGIC, scalar2=None, op0=ALU.subtract), r=[rt2], w=[rt2])
    P.op("dve", lambda e: e.tensor_tensor(out=t1, in0=t1, in1=t2, op=ALU.subtract), r=[rt1, rt2], w=[rt1])
    P.op("act", lambda e: e.activation(out=out, in_=t1, func=AF.Sin, scale=2 * math.pi), r=[rt1], w=w_out)


def stage_s5(K, l, with_ctx, epi_tiles=None):
    nc, P, Dm, R = K.nc, K.P, K.dram, K.R
    TC = S5_TC
    NCH = T // TC
    with contextlib.ExitStack() as es:
        uT, ruT = _tile(es, nc, "S_uT", [128, 2, T], BF16)
        ident, rident = _tile(es, nc, "S_ident", [128, 128], BF16)
        identf, ridentf = _tile(es, nc, "S_identf", [128, 128], F32)
        BbT, rBbT = _tile(es, nc, "S_BbT", [128, 2, 2, 8, 128], BF16)
        Cm, rCm = _tile(es, nc, "S_Cm", [128, 4, 2, 128], BF16)
        RD, rRD = _tile(es, nc, "S_RD", [128, 2, 8], F32)
        TH, rTH = _tile(es, nc, "S_TH", [128, 2, 8], F32)
        COS, rCOS = _tile(es, nc, "S_COS", [128, 8, TC], F32)
        SIN, rSIN = _tile(es, nc, "S_SIN", [128, 8, TC], F32)
        iota1, riota1 = _tile(es, nc, "S_iota1", [128, TC], F32)
        P.dma("sp", uT[:, 0, :], Dm["suT"][0:128, :], r=[R["mix"]], w=[ruT])
        P.dma("sp", uT[:, 1, :], Dm["suT"][128:256, :], r=[R["mix"]], w=[ruT])
        P.dma("sp", identf[:], Dm["ident"], w=[ridentf])
        P.dma("pool", ident[:], Dm["ident"], w=[rident])
        P.dma("sp", iota1[:], Dm["s5_iota1"], w=[riota1])

        with contextlib.ExitStack() as es2:
            LRt, rLR = _tile(es2, nc, "S_LR", [128, 8], F32)
            LIt, rLI = _tile(es2, nc, "S_LI", [128, 8], F32)
            DTt, rDTt = _tile(es2, nc, "S_DT", [128, 8], F32)
            w8 = [_tile(es2, nc, f"S_w8_{i}", [128, 8], F32) for i in range(8)]
            BRt, rBRt = _tile(es2, nc, "S_BR", [128, 8, 16], F32)
            BIt, rBIt = _tile(es2, nc, "S_BI", [128, 8, 16], F32)
            bb = [_tile(es2, nc, f"S_bb{i}", [128, 8, 16], F32) for i in range(4)]
            Zp, rZp = _tile(es2, nc, "S_Zp", [128, 8, 128], F32)
            Cn, rCn = _tile(es2, nc, "S_Cn", [128, 128], F32)
            bdm, rbdm = _tile(es2, nc, "S_bdm", [128, 128], F32)
            tp, rtp = _tile(es2, nc, "S_tp", [128, 128], F32, psum=True)
            P.dma("sp", bdm[:], Dm["s5_bdmask"], w=[rbdm])
            with nc.allow_non_contiguous_dma(reason="small parameter tables"):
                P.dma("sp", BRt[:], Dm["s5_b_re"][l].rearrange("(gp a) p c -> (a p) gp c", a=2), w=[rBRt])
                P.dma("sp", BIt[:], Dm["s5_b_im"][l].rearrange("(gp a) p c -> (a p) gp c", a=2), w=[rBIt])
            for dirn in range(2):
                with nc.allow_non_contiguous_dma(reason="small parameter tables"):
                    P.dma("sp", LRt[:], Dm["s5_lambda_re"][l, dirn].rearrange("(gp a) p -> (a p) gp", a=2), w=[rLR])
                    P.dma("sp", LIt[:], Dm["s5_lambda_im"][l, dirn].rearrange("(gp a) p -> (a p) gp", a=2), w=[rLI])
                    for a in range(2):
                        src = Dm["s5_log_step"][l, dirn].rearrange("(gp a) -> a gp", a=2)[a].partition_broadcast(64)
                        P.dma("sp", DTt[a * 64:(a + 1) * 64, :], src, w=[rDTt])
                (dt_, rdt), (mag, rmag), (sn, rsn), (cs_, rcs), (t1, rt1), (t2, rt2), (cr, rcr), (ci, rci) = w8
                P.op("act", lambda e: e.activation(out=dt_[:], in_=DTt[:], func=AF.Exp), r=[rDTt], w=[rdt])
                P.op("dve", lambda e: e.tensor_tensor(out=mag[:], in0=LRt[:], in1=dt_[:], op=ALU.mult), r=[rLR, rdt], w=[rmag])
                P.op("act", lambda e: e.activation(out=RD[:, dirn, :], in_=mag[:], func=AF.Exp), r=[rmag], w=[rRD])
                P.op("dve", lambda e: e.tensor_tensor(out=TH[:, dirn, :], in0=LIt[:], in1=dt_[:], op=ALU.mult), r=[rLI, rdt], w=[rTH])
                _sin_reduced(P, sn[:], TH[:, dirn, :], 0.0, t1[:], rt1, t2[:], rt2, [rTH], [rsn])
                _sin_reduced(P, cs_[:], TH[:, dirn, :], math.pi / 2, t1[:], rt1, t2[:], rt2, [rTH], [rcs])
                P.op("dve", lambda e: e.tensor_tensor(out=cs_[:], in0=cs_[:], in1=RD[:, dirn, :], op=ALU.mult), r=[rcs, rRD], w=[rcs])
                P.op("dve", lambda e: e.tensor_tensor(out=sn[:], in0=sn[:], in1=RD[:, dirn, :], op=ALU.mult), r=[rsn, rRD], w=[rsn])
                P.op("dve", lambda e: e.tensor_scalar(out=cs_[:], in0=cs_[:], scalar1=-1.0, scalar2=None, op0=ALU.add), r=[rcs], w=[rcs])
                P.op("dve", lambda e: e.tensor_tensor(out=t1[:], in0=LRt[:], in1=LRt[:], op=ALU.mult), r=[rLR], w=[rt1])
                P.op("dve", lambda e: e.tensor_tensor(out=t2[:], in0=LIt[:], in1=LIt[:], op=ALU.mult), r=[rLI], w=[rt2])
                P.op("dve", lambda e: e.tensor_tensor(out=t1[:], in0=t1[:], in1=t2[:], op=ALU.add), r=[rt1, rt2], w=[rt1])
                P.op("dve", lambda e: e.reciprocal(out=t1[:], in_=t1[:]), r=[rt1], w=[rt1])
                P.op("dve", lambda e: e.tensor_tensor(out=cr[:], in0=cs_[:], in1=LRt[:], op=ALU.mult), r=[rcs, rLR], w=[rcr])
                P.op("dve", lambda e: e.tensor_tensor(out=t2[:], in0=sn[:], in1=LIt[:], op=ALU.mult), r=[rsn, rLI], w=[rt2])
                P.op("dve", lambda e: e.tensor_tensor(out=cr[:], in0=cr[:], in1=t2[:], op=ALU.add), r=[rcr, rt2], w=[rcr])
                P.op("dve", lambda e: e.tensor_tensor(out=cr[:], in0=cr[:], in1=t1[:], op=ALU.mult), r=[rcr, rt1], w=[rcr])
                P.op("dve", lambda e: e.tensor_tensor(out=ci[:], in0=sn[:], in1=LRt[:], op=ALU.mult), r=[rsn, rLR], w=[rci])
                P.op("dve", lambda e: e.tensor_tensor(out=t2[:], in0=cs_[:], in1=LIt[:], op=ALU.mult), r=[rcs, rLI], w=[rt2])
                P.op("dve", lambda e: e.tensor_tensor(out=ci[:], in0=ci[:], in1=t2[:], op=ALU.subtract), r=[rci, rt2], w=[rci])
                P.op("dve", lambda e: e.tensor_tensor(out=ci[:], in0=ci[:], in1=t1[:], op=ALU.mult), r=[rci, rt1], w=[rci])
                crb = cr[:].unsqueeze(2).to_broadcast([128, 8, 16])
                cib = ci[:].unsqueeze(2).to_broadcast([128, 8, 16])
                (b0, rb0), (b1, rb1), (b2, rb2), (b3, rb3) = bb
                P.op("dve", lambda e: e.tensor_tensor(out=b0[:], in0=BRt[:], in1=crb, op=ALU.mult), r=[rBRt, rcr], w=[rb0])
                P.op("dve", lambda e: e.tensor_tensor(out=b1[:], in0=BIt[:], in1=cib, op=ALU.mult), r=[rBIt, rci], w=[rb1])
                P.op("dve", lambda e: e.tensor_tensor(out=b0[:], in0=b0[:], in1=b1[:], op=ALU.subtract), r=[rb0, rb1], w=[rb0])
                P.op("dve", lambda e: e.tensor_tensor(out=b2[:], in0=BIt[:], in1=crb, op=ALU.mult), r=[rBIt, rcr], w=[rb2])
                P.op("dve", lambda e: e.tensor_tensor(out=b3[:], in0=BRt[:], in1=cib, op=ALU.mult), r=[rBRt, rci], w=[rb3])
                P.op("dve", lambda e: e.tensor_tensor(out=b2[:], in0=b2[:], in1=b3[:], op=ALU.add), r=[rb2, rb3], w=[rb2])
                for ri_, (bsrc, rbsrc) in enumerate(((b0, rb0), (b2, rb2))):
                    P.op("dve", lambda e: e.memset(Zp[:], 0.0), w=[rZp])
                    for gp in range(8):
                        for a in range(2):
                            col0 = ((2 * gp + a) % 8) * 16
                            P.op("dve", lambda e: e.tensor_copy(out=Zp[a * 64:(a + 1) * 64, gp, col0:col0 + 16], in_=bsrc[a * 64:(a + 1) * 64, gp, :]), r=[rbsrc], w=[rZp])
                    for gp in range(8):
                        P.op("pe", lambda e: e.transpose(out=tp, in_=Zp[:, gp, :], identity=identf[:]), r=[rZp, ridentf], w=[rtp])
                        P.op("act", lambda e: e.copy(out=BbT[:, dirn, ri_, gp, :], in_=tp), r=[rtp], w=[rBbT])
            for half in range(2):
                for src_name, kinds in (("s5_c_re", ((0, 1.0), (1, -1.0))), ("s5_c_im", ((2, -1.0),))):
                    srcv = Dm[src_name][l].rearrange("g co p -> (g co) p")[half * 128:(half + 1) * 128, :]
                    P.dma("sp", Cn[:, 0:64], srcv, w=[rCn])
                    P.dma("sp", Cn[:, 64:128], srcv, w=[rCn])
                    P.op("dve", lambda e: e.tensor_tensor(out=Cn[:], in0=Cn[:], in1=bdm[:], op=ALU.mult), r=[rCn, rbdm], w=[rCn])
                    P.op("pe", lambda e: e.transpose(out=tp, in_=Cn[:], identity=identf[:]), r=[rCn, ridentf], w=[rtp])
                    for kidx, sgn in kinds:
                        P.op("act", lambda e: e.activation(out=Cm[:, kidx, half, :], in_=tp, func=AF.Copy, scale=sgn), r=[rtp], w=[rCm])
        P.barrier()

        with contextlib.ExitStack() as es3:
            Bp = [[_tile(es3, nc, f"S_Bp{i}{j}", [128, TC], F32, psum=True) for j in range(2)] for i in range(2)]
            Yp = [[_tile(es3, nc, f"S_Yp{i}{j}", [128, 256], F32, psum=True) for j in range(2)] for i in range(2)]
            tq = [[_tile(es3, nc, f"S_tq{i}{j}", [128, TC], F32) for j in range(4)] for i in range(2)]
            bp_ = [[_tile(es3, nc, f"S_bp{i}{j}", [128, TC], F32) for j in range(2)] for i in range(2)]
            Wt = [[_tile(es3, nc, f"S_W{i}{j}", [128, TC], F32) for j in range(2)] for i in range(2)]
            Pr = [[_tile(es3, nc, f"S_Pr{i}{j}", [128, TC], BF16) for j in range(4)] for i in range(2)]
            XR, rXR = _tile(es3, nc, "S_XR", [128, 8], F32)
            XI, rXI = _tile(es3, nc, "S_XI", [128, 8], F32)
            tc1, rtc1 = _tile(es3, nc, "S_tc1", [128, 1], F32)
            ysb = [_tile(es3, nc, f"S_ysb{i}", [128, 2, 256], F32) for i in range(2)]
            ang, rang = _tile(es3, nc, "S_ang", [128, 8, TC], F32)
            at2, rat2 = _tile(es3, nc, "S_at2", [128, 8, TC], F32)
            for dirn in range(2):
                for gp in range(8):
                    P.op("dve", lambda e: e.tensor_scalar(out=ang[:, gp, :], in0=iota1[:], scalar1=TH[:, dirn, gp:gp + 1], scalar2=None, op0=ALU.mult), r=[riota1, rTH], w=[rang])
                _sin_reduced(P, SIN[:].rearrange("p a b -> p (a b)"), ang[:].rearrange("p a b -> p (a b)"), 0.0,
                             COS[:].rearrange("p a b -> p (a b)"), rCOS, at2[:].rearrange("p a b -> p (a b)"), rat2, [rang], [rSIN])
                _sin_reduced(P, COS[:].rearrange("p a b -> p (a b)"), ang[:].rearrange("p a b -> p (a b)"), math.pi / 2,
                             ang[:].rearrange("p a b -> p (a b)"), rang, at2[:].rearrange("p a b -> p (a b)"), rat2, [rang], [rCOS])
                P.op("dve", lambda e: e.memset(XR[:], 0.0), w=[rXR])
                P.op("dve", lambda e: e.memset(XI[:], 0.0), w=[rXI])
                ydst = Dm["s5yf"] if dirn == 0 else Dm["s5yb"]
                for ck in range(NCH):
                    if dirn == 0:
                        c0 = L if ck == 0 else (ck - 1) * TC
                    else:
                        c0 = L if ck == 0 else L - ck * TC
                    yp = Yp[ck % 2]
                    for gp in range(8):
                        par = gp % 2
                        ct = gp // 4
                        half, gl = gp // 4, gp % 4
                        usl = uT[:, ct, c0:c0 + TC]
                        if dirn == 1:
                            usl = usl[:, ::-1]
                        (bre, rbre), (bim, rbim) = Bp[par]
                        P.op("pe", lambda e: e.matmul(bre, lhsT=BbT[:, dirn, 0, gp, :], rhs=usl, start=True, stop=True), r=[rBbT, ruT], w=[rbre])
                        P.op("pe", lambda e: e.matmul(bim, lhsT=BbT[:, dirn, 1, gp, :], rhs=usl, start=True, stop=True), r=[rBbT, ruT], w=[rbim])
                        (q1, rq1), (q2, rq2), (q3, rq3), (q4, rq4) = tq[par]
                        cosg, sing = COS[:, gp, :], SIN[:, gp, :]
                        P.op("dve", lambda e: e.tensor_tensor(out=q1[:], in0=bre, in1=cosg, op=ALU.mult), r=[rbre, rCOS], w=[rq1])
                        P.op("dve", lambda e: e.tensor_tensor(out=q2[:], in0=bim, in1=sing, op=ALU.mult), r=[rbim, rSIN], w=[rq2])
                        P.op("dve", lambda e: e.tensor_tensor(out=q3[:], in0=bim, in1=cosg, op=ALU.mult), r=[rbim, rCOS], w=[rq3])
                        P.op("dve", lambda e: e.tensor_tensor(out=q4[:], in0=bre, in1=sing, op=ALU.mult), r=[rbre, rSIN], w=[rq4])
                        (br2, rbr2), (bi2, rbi2) = bp_[par]
                        P.op("pool", lambda e: e.tensor_tensor(out=br2[:], in0=q1[:], in1=q2[:], op=ALU.add), r=[rq1, rq2], w=[rbr2])
                        P.op("pool", lambda e: e.tensor_tensor(out=bi2[:], in0=q3[:], in1=q4[:], op=ALU.subtract), r=[rq3, rq4], w=[rbi2])
                        (wr, rwr), (wi, rwi) = Wt[par]
                        rdb = RD[:, dirn, gp:gp + 1].to_broadcast([128, TC])
                        P.op("dve", lambda e: e.tensor_tensor_scan(out=wr[:], data0=rdb, data1=br2[:], initial=XR[:, gp:gp + 1], op0=ALU.mult, op1=ALU.add),
                             r=[rRD, rbr2, rXR], w=[rwr])
                        P.op("dve", lambda e: e.tensor_tensor_scan(out=wi[:], data0=rdb, data1=bi2[:], initial=XI[:, gp:gp + 1], op0=ALU.mult, op1=ALU.add),
                             r=[rRD, rbi2, rXI], w=[rwi])
                        cl, sl = COS[:, gp, TC - 1:TC], SIN[:, gp, TC - 1:TC]
                        P.op("dve", lambda e: e.tensor_tensor(out=tc1[:], in0=wi[:, TC - 1:TC], in1=sl, op=ALU.mult), r=[rwi, rSIN], w=[rtc1])
                        P.op("dve", lambda e: e.scalar_tensor_tensor(out=XR[:, gp:gp + 1], in0=wr[:, TC - 1:TC], scalar=cl, in1=tc1[:], op0=ALU.mult, op1=ALU.subtract),
                             r=[rwr, rCOS, rtc1], w=[rXR])
                        P.op("dve", lambda e: e.tensor_tensor(out=tc1[:], in0=wi[:, TC - 1:TC], in1=cl, op=ALU.mult), r=[rwi, rCOS], w=[rtc1])
                        P.op("dve", lambda e: e.scalar_tensor_tensor(out=XI[:, gp:gp + 1], in0=wr[:, TC - 1:TC], scalar=sl, in1=tc1[:], op0=ALU.mult, op1=ALU.add),
                             r=[rwr, rSIN, rtc1], w=[rXI])
                        (pcc, rpcc), (pis, rpis), (prs, rprs), (pic, rpic) = Pr[par]
                        ov = (lambda t_: t_[:, ::-1]) if dirn == 1 else (lambda t_: t_[:])
                        P.op("dve", lambda e: e.tensor_tensor(out=ov(pcc), in0=wr[:], in1=cosg, op=ALU.mult), r=[rwr, rCOS], w=[rpcc])
                        P.op("pool", lambda e: e.tensor_tensor(out=ov(pis), in0=wi[:], in1=sing, op=ALU.mult), r=[rwi, rSIN], w=[rpis])
                        P.op("dve", lambda e: e.tensor_tensor(out=ov(prs), in0=wr[:], in1=sing, op=ALU.mult), r=[rwr, rSIN], w=[rprs])
                        P.op("pool", lambda e: e.tensor_tensor(out=ov(pic), in0=wi[:], in1=cosg, op=ALU.mult), r=[rwi, rCOS], w=[rpic])
                        for sub in range(TC // 128):
                            ypt, rypt = yp[sub]
                            osl = ypt[:, gp * 32:(gp + 1) * 32]
                            cs_sl = slice(gl * 32, (gl + 1) * 32)
                            ssl = slice(sub * 128, (sub + 1) * 128)
                            P.op("pe", lambda e: e.matmul(osl, lhsT=pcc[:, ssl], rhs=Cm[:, 0, half, cs_sl], start=True, stop=False), r=[rpcc, rCm], w=[rypt])
                            P.op("pe", lambda e: e.matmul(osl, lhsT=pis[:, ssl], rhs=Cm[:, 1, half, cs_sl], start=False, stop=False), r=[rpis, rCm], w=[rypt])
                            P.op("pe", lambda e: e.matmul(osl, lhsT=prs[:, ssl], rhs=Cm[:, 2, half, cs_sl], start=False, stop=False), r=[rprs, rCm], w=[rypt])
                            P.op("pe", lambda e: e.matmul(osl, lhsT=pic[:, ssl], rhs=Cm[:, 2, half, cs_sl], start=False, stop=True), r=[rpic, rCm], w=[rypt])
                    ys, rys = ysb[ck % 2]
                    for sub in range(TC // 128):
                        ypt, rypt = yp[sub]
                        P.op("act", lambda e: e.copy(out=ys[:, sub, :], in_=ypt), r=[rypt], w=[rys])
                    P.dma("sp", ydst[c0:c0 + TC, :].rearrange("(s p) d -> p s d", p=128), ys[:], r=[rys], w=[R["s5y"]])
        P.barrier()

        with contextlib.ExitStack() as es4:
            dsk, rdsk = _tile(es4, nc, "S_dsk", [128, 256], F32)
            glb, rglb = _tile(es4, nc, "S_glb", [128, 256], F32)
            glw, rglw = _tile(es4, nc, "S_glw", [128, 2, 256], BF16)
            ut = [_tile(es4, nc, f"S_ut{i}", [128, 3, 256], F32) for i in range(2)]
            z, rz = _tile(es4, nc, "S_z", [128, 256], F32)
            zg, rzg = _tile(es4, nc, "S_zg", [128, 256], F32)
            zgb, rzgb = _tile(es4, nc, "S_zgb", [128, 256], BF16)
            zT, rzT = _tile(es4, nc, "S_zT", [128, 2, 128], BF16)
            sg, rsg = _tile(es4, nc, "S_sg", [128, 256], F32)
            ob, rob = _tile(es4, nc, "S_ob", [128, 256], BF16)
            oT, roT = _tile(es4, nc, "S_oT", [128, 2, 128], BF16)
            pT2, rpT2 = _tile(es4, nc, "S_pT2", [128, 2, 128], BF16, psum=True)
            pT3, rpT3 = _tile(es4, nc, "S_pT3", [128, 2, 128], BF16, psum=True)
            gp_, rgp_ = _tile(es4, nc, "S_gps", [128, 256], F32, psum=True)
            P.dma("sp", dsk[:], Dm["s5_d"][l].partition_broadcast(128), w=[rdsk])
            P.dma("sp", glb[:], Dm["s5_glu_b"][l].partition_broadcast(128), w=[rglb])
            P.dma("pool", glw[:], Dm["s5_glu_w"][l].rearrange("(k p) n -> p k n", p=128), w=[rglw])
            tiles = list(range(NXT)) + ([64, 65] if with_ctx else [])
            if epi_tiles is not None:
                tiles = [t for t in tiles if t in epi_tiles]

            def load(i):
                t = tiles[i]
                u_, ru_ = ut[i % 2]
                ts = slice(t * 128, (t + 1) * 128)
                P.dma("sp", u_[:, 0, :], Dm["su"][ts, :], r=[R["mix"]], w=[ru_])
                P.dma("sp", u_[:, 1, :], Dm["s5yf"][ts, :], r=[R["s5y"]], w=[ru_])
                P.dma("sp", u_[:, 2, :], Dm["s5yb"][ts, :], r=[R["s5y"]], w=[ru_])
            if tiles:
                load(0)
            for i, t in enumerate(tiles):
                if i + 1 < len(tiles):
                    load(i + 1)
                u_, ru_ = ut[i % 2]
                ts = slice(t * 128, (t + 1) * 128)
                P.op("dve", lambda e: e.tensor_tensor(out=z[:], in0=u_[:, 0, :], in1=dsk[:], op=ALU.mult), r=[ru_, rdsk], w=[rz])
                P.op("dve", lambda e: e.tensor_tensor(out=z[:], in0=z[:], in1=u_[:, 1, :], op=ALU.add), r=[rz, ru_], w=[rz])
                P.op("dve", lambda e: e.tensor_tensor(out=z[:], in0=z[:], in1=u_[:, 2, :], op=ALU.add), r=[rz, ru_], w=[rz])
                P.op("act", lambda e: e.activation(out=zg[:], in_=z[:], func=AF.Gelu), r=[rz], w=[rzg])
                P.op("dve", lambda e: e.tensor_copy(out=zgb[:], in_=zg[:]), r=[rzg], w=[rzgb])
                for k in range(2):
                    P.op("pe", lambda e: e.transpose(out=pT2[:, k, :], in_=zgb[:, k * 128:(k + 1) * 128], identity=ident[:]), r=[rzgb, rident], w=[rpT2])
                P.op("act", lambda e: e.copy(out=zT[:], in_=pT2), r=[rpT2], w=[rzT])
                for k in range(2):
                    P.op("pe", lambda e: e.matmul(gp_, lhsT=zT[:, k, :], rhs=glw[:, k, :], start=(k == 0), stop=(k == 1)), r=[rzT, rglw], w=[rgp_])
                P.op("dve", lambda e: e.tensor_tensor(out=sg[:], in0=gp_, in1=glb[:], op=ALU.add), r=[rgp_, rglb], w=[rsg])
                P.op("act", lambda e: e.activation(out=sg[:], in_=sg[:], func=AF.Sigmoid), r=[rsg], w=[rsg])
                P.op("dve", lambda e: e.tensor_tensor(out=ob[:], in0=sg[:], in1=zg[:], op=ALU.mult), r=[rsg, rzg], w=[rob])
                for k in range(2):
                    P.op("pe", lambda e: e.transpose(out=pT3[:, k, :], in_=ob[:, k * 128:(k + 1) * 128], identity=ident[:]), r=[rob, rident], w=[rpT3])
                P.op("act", lambda e: e.copy(out=oT[:], in_=pT3), r=[rpT3], w=[roT])
                P.dma("pool", Dm["yT"][512:768, :].rearrange("(k p) n -> p k n", p=128)[:, :, ts], oT[:], r=[roT], w=[R["y"]])
    P.barrier()


SCRATCH.update({"s5yf": ([T, 256], F32), "s5yb": ([T, 256], F32)})


def stage_merge(K, l, with_ctx, tiles=None):
    nc, P, Dm, R = K.nc, K.P, K.dram, K.R
    with contextlib.ExitStack() as es:
        wg, rwg = _tile(es, nc, "M_wg", [128, 8, 4096], BF16)
        wb, rwb = _tile(es, nc, "M_wb", [128, 8, 1024], BF16)
        wo, rwo = _tile(es, nc, "M_wo", [128, 8, 1024], BF16)
        m2, rm2 = _tile(es, nc, "M_m2", [128, 2, 1024], F32)
        ident, rident = _tile(es, nc, "M_ident", [128, 128], BF16)
        yTt = [_tile(es, nc, f"M_yT{i}", [128, 8, 128], BF16) for i in range(2)]
        aTt = [_tile(es, nc, f"M_aT{i}", [128, 8, 128], BF16) for i in range(2)]
        ht = [_tile(es, nc, f"M_h{i}", [128, 1024], F32) for i in range(2)]
        sig = [_tile(es, nc, f"M_sig{i}", [128, 512], F32) for i in range(2)]
        term, rterm = _tile(es, nc, "M_term", [128, 512], F32)
        mg, rmg = _tile(es, nc, "M_mg", [128, 1024], F32)
        mb, rmb = _tile(es, nc, "M_mb", [128, 1024], BF16)
        mT, rmT = _tile(es, nc, "M_mT", [128, 8, 128], BF16)
        hn, rhn = _tile(es, nc, "M_hn", [128, 1024], F32)
        Gp = [_tile(es, nc, f"M_Gp{i}", [128, 512], F32, psum=True) for i in range(2)]
        Zp = [_tile(es, nc, f"M_Zp{i}", [128, 512], F32, psum=True) for i in range(2)]
        pT, rpT = _tile(es, nc, "M_pT", [128, 8, 128], BF16, psum=True)
        Op = [_tile(es, nc, f"M_Op{i}", [128, 512], F32, psum=True) for i in range(2)]
        w_in_v = Dm["w_in"][l].rearrange("(k p) n -> p k n", p=128)
        for i in range(4):
            P.dma("pool", wg[:, :, i * 1024:(i + 1) * 1024], w_in_v[:, :, MIXC + i * 1024:MIXC + (i + 1) * 1024], w=[rwg])
        P.dma("pool", wb[:], Dm["w_branch"][l].rearrange("i (k p) n -> p (i k) n", p=128), w=[rwb])
        P.dma("pool", wo[:], Dm["w_out"][l].rearrange("(k p) n -> p k n", p=128), w=[rwo])
        P.dma("pool", ident[:], Dm["ident"], w=[rident])
        P.dma("sp", m2[:, 0, :], Dm["modv"][2].partition_broadcast(128), r=[R["modv"]], w=[rm2])
        P.dma("sp", m2[:, 1, :], Dm["modv"][8].partition_broadcast(128), r=[R["modv"]], w=[rm2])
        tiles = tiles if tiles is not None else list(range(NXT)) + ([64, 65] if with_ctx else [])
        hsrc = K.hsrc(l)

        def load(i):
            t = tiles[i]
            ts = slice(t * 128, (t + 1) * 128)
            P.dma("sp", yTt[i % 2][0][:], Dm["yT"].rearrange("(k p) n -> p k n", p=128)[:, :, ts], r=[R["y"]], w=[yTt[i % 2][1]])
            P.dma("sp", aTt[i % 2][0][:], Dm["aT"].rearrange("(k p) n -> p k n", p=128)[:, :, ts], r=[R["aT"]], w=[aTt[i % 2][1]])
            P.dma("sp", ht[i % 2][0][:], hsrc(t), r=[R["H"]], w=[ht[i % 2][1]])
        load(0)
        for i, t in enumerate(tiles):
            if i + 1 < len(tiles):
                load(i + 1)
            yt, ryt = yTt[i % 2]
            at, rat = aTt[i % 2]
            h_, rh_ = ht[i % 2]
            ts = slice(t * 128, (t + 1) * 128)
            cnt = 0
            for nh in range(2):
                for br in range(4):
                    gp, rgp = Gp[cnt % 2]
                    zp, rzp = Zp[cnt % 2]
                    sg, rsg = sig[cnt % 2]
                    cnt += 1
                    c0 = br * 1024 + nh * 512
                    for k in range(8):
                        P.op("pe", lambda e: e.matmul(gp, lhsT=at[:, k, :], rhs=wg[:, k, c0:c0 + 512], start=(k == 0), stop=(k == 7)), r=[rat, rwg], w=[rgp])
                    for k2 in range(2):
                        P.op("pe", lambda e: e.matmul(zp, lhsT=yt[:, 2 * br + k2, :], rhs=wb[:, 2 * br + k2, nh * 512:(nh + 1) * 512], start=(k2 == 0), stop=(k2 == 1)),
                             r=[ryt, rwb], w=[rzp])
                    P.op("act", lambda e: e.activation(out=sg[:], in_=gp, func=AF.Sigmoid), r=[rgp], w=[rsg])
                    dst = mg[:, nh * 512:(nh + 1) * 512]
                    if br == 0:
                        P.op("dve", lambda e: e.tensor_tensor(out=dst, in0=zp, in1=sg[:], op=ALU.mult), r=[rzp, rsg], w=[rmg])
                    else:
                        P.op("dve", lambda e: e.tensor_tensor(out=term[:], in0=zp, in1=sg[:], op=ALU.mult), r=[rzp, rsg], w=[rterm])
                        P.op("dve", lambda e: e.tensor_tensor(out=dst, in0=dst, in1=term[:], op=ALU.add), r=[rmg, rterm], w=[rmg])
            P.op("act", lambda e: e.copy(out=mb[:], in_=mg[:]), r=[rmg], w=[rmb])
            for k in range(8):
                P.op("pe", lambda e: e.transpose(out=pT[:, k, :], in_=mb[:, k * 128:(k + 1) * 128], identity=ident[:]), r=[rmb, rident], w=[rpT])
            P.op("act", lambda e: e.copy(out=mT[:], in_=pT), r=[rpT], w=[rmT])
            for nh in range(2):
                op_, rop = Op[nh]
                for k in range(8):
                    P.op("pe", lambda e: e.matmul(op_, lhsT=mT[:, k, :], rhs=wo[:, k, nh * 512:(nh + 1) * 512], start=(k == 0), stop=(k == 7)), r=[rmT, rwo], w=[rop])
                sl = slice(nh * 512, (nh + 1) * 512)
                P.op("dve", lambda e: e.tensor_tensor(out=hn[:, sl], in0=op_, in1=m2[:, 0 if t < NXT else 1, sl], op=ALU.mult), r=[rop, rm2], w=[rhn])
            P.op("dve", lambda e: e.tensor_tensor(out=hn[:], in0=hn[:], in1=h_[:], op=ALU.add), r=[rhn, rh_], w=[rhn])
            P.dma("pool", Dm["H"][ts, :], hn[:], r=[rhn], w=[R["H"]])
    P.barrier()


SGT = 12
BIG = 1.0e30


def stage_moe(K, l, with_ctx, last, tiles=None, experts=None):
    nc, P, Dm, R = K.nc, K.P, K.dram, K.R
    tiles = tiles if tiles is not None else list(range(NXT)) + ([64, 65] if with_ctx else [])
    experts = list(range(32)) if experts is None else experts
    with contextlib.ExitStack() as es:
        bc, rbc = _tile(es, nc, "E_bc", [128, 6, 1024], F32)
        identf, ridentf = _tile(es, nc, "E_identf", [128, 128], F32)
        rw, rrw = _tile(es, nc, "E_rw", [128, 8, 36], F32)
        rb, rrb = _tile(es, nc, "E_rb", [128, 36], F32)
        epsc, repsc = _tile(es, nc, "E_eps", [128, 1], F32)
        FT, rFT = _tile(es, nc, "E_FT", [128, 8, SGT * 128], BF16)
        Gall, rGall = _tile(es, nc, "E_G", [128, SGT, 32], F32)
        yacc, ryacc = _tile(es, nc, "E_yacc", [128, SGT, 1024], F32)
        for j, row in enumerate((3, 4, 5, 9, 10, 11)):
            P.dma("sp", bc[:, j, :], Dm["modv"][row].partition_broadcast(128), r=[R["modv"]], w=[rbc])
        P.dma("sp", identf[:], Dm["ident"], w=[ridentf])
        with nc.allow_non_contiguous_dma(reason="tiny router weights"):
            P.dma("sp", rw[:, :, 0:4], Dm["router_w1"][l].rearrange("(k p) n -> p k n", p=128), w=[rrw])
            P.dma("sp", rw[:, :, 4:36], Dm["router_w2"][l].rearrange("(k p) n -> p k n", p=128), w=[rrw])
        P.dma("sp", rb[:, 0:4], Dm["router_b1"][l].partition_broadcast(128), w=[rrb])
        P.dma("sp", rb[:, 4:36], Dm["router_b2"][l].partition_broadcast(128), w=[rrb])
        P.op("dve", lambda e: e.memset(epsc[:], EPS), w=[repsc])
        P.barrier()
        sgs = [tiles[i:i + SGT] for i in range(0, len(tiles), SGT)]
        for sg in sgs:
            with contextlib.ExitStack() as e1:
                ht = [_tile(e1, nc, f"E1_h{i}", [128, 1024], F32) for i in range(2)]
                junk, rjunk = _tile(e1, nc, "E1_junk", [128, 1024], BF16)
                ssq, rssq = _tile(e1, nc, "E1_ssq", [128, 1], F32)
                rstd, rrstd = _tile(e1, nc, "E1_rstd", [128, 1], F32)
                f_, rf_ = _tile(e1, nc, "E1_f", [128, 1024], F32)
                lgt, rlgt = _tile(e1, nc, "E1_lg", [128, 36], F32)
                sm = [_tile(e1, nc, f"E1_s{i}", [128, 8], F32) for i in range(4)]
                oh = [_tile(e1, nc, f"E1_oh{i}", [128, 32], F32) for i in range(3)]
                pTf = [_tile(e1, nc, f"E1_pT{i}", [128, 4, 128], F32, psum=True) for i in range(2)]
                lp, rlp = _tile(e1, nc, "E1_lp", [128, 36], F32, psum=True)

                def load(i):
                    t = sg[i]
                    P.dma("sp", ht[i % 2][0][:], Dm["H"][t * 128:(t + 1) * 128, :], r=[R["H"]], w=[ht[i % 2][1]])
                load(0)
                for i, t in enumerate(sg):
                    if i + 1 < len(sg):
                        load(i + 1)
                    h_, rh_ = ht[i % 2]
                    o = 0 if t < NXT else 3
                    P.op("act", lambda e: e.activation(out=junk[:], in_=h_[:], func=AF.Square, accum_out=ssq[:]), r=[rh_], w=[rjunk, rssq])
                    P.op("act", lambda e: e.activation(out=rstd[:], in_=ssq[:], func=AF.Sqrt, scale=1.0 / D, bias=epsc[:, 0:1]), r=[rssq, repsc], w=[rrstd])
                    P.op("dve", lambda e: e.reciprocal(out=rstd[:], in_=rstd[:]), r=[rrstd], w=[rrstd])
                    P.op("dve", lambda e: e.scalar_tensor_tensor(out=f_[:], in0=h_[:], scalar=rstd[:, 0:1], in1=bc[:, o, :], op0=ALU.mult, op1=ALU.mult),
                         r=[rh_, rrstd, rbc], w=[rf_])
                    P.op("dve", lambda e: e.tensor_tensor(out=f_[:], in0=f_[:], in1=bc[:, o + 1, :], op=ALU.add), r=[rf_, rbc], w=[rf_])
                    for hf in range(2):
                        pt, rpt = pTf[hf]
                        for k in range(4):
                            kk = hf * 4 + k
                            P.op("pe", lambda e: e.transpose(out=pt[:, k, :], in_=f_[:, kk * 128:(kk + 1) * 128], identity=identf[:]), r=[rf_, ridentf], w=[rpt])
                    fTf, rfTf = f_, rf_
                    for hf in range(2):
                        pt, rpt = pTf[hf]
                        P.op("act", lambda e: e.copy(out=fTf[:, hf * 512:(hf + 1) * 512].rearrange("p (k n) -> p k n", n=128), in_=pt), r=[rpt], w=[rfTf])
                    P.op("dve", lambda e: e.tensor_copy(out=FT[:, :, i * 128:(i + 1) * 128], in_=fTf[:].rearrange("p (k n) -> p k n", n=128)), r=[rfTf], w=[rFT])
                    for k in range(8):
                        P.op("pe", lambda e: e.matmul(lp, lhsT=fTf[:, k * 128:(k + 1) * 128], rhs=rw[:, k, :], start=(k == 0), stop=(k == 7)), r=[rfTf, rrw], w=[rlp])
                    P.op("dve", lambda e: e.tensor_tensor(out=lgt[:], in0=lp, in1=rb[:], op=ALU.add), r=[rlp, rrb], w=[rlgt])
                    (s0, rs0), (s1, rs1), (s2, rs2), (s3, rs3) = sm
                    (oh1, roh1), (oh2, roh2), (l2, rl2) = oh
                    P.op("dve", lambda e: e.tensor_reduce(out=s0[:, 0:1], in_=lgt[:, 0:4], axis=AX.X, op=ALU.max), r=[rlgt], w=[rs0])
                    P.op("dve", lambda e: e.tensor_scalar(out=s0[:, 1:2], in0=s0[:, 0:1], scalar1=-1.0, scalar2=None, op0=ALU.mult), r=[rs0], w=[rs0])
                    P.op("act", lambda e: e.activation(out=s1[:, 0:4], in_=lgt[:, 0:4], func=AF.Exp, bias=s0[:, 1:2], accum_out=s0[:, 2:3]), r=[rlgt, rs0], w=[rs1, rs0])
                    P.op("dve", lambda e: e.reciprocal(out=s0[:, 3:4], in_=s0[:, 2:3]), r=[rs0], w=[rs0])
                    P.op("dve", lambda e: e.tensor_scalar(out=s2[:, 0:4], in0=lgt[:, 0:4], scalar1=s0[:, 0:1], scalar2=None, op0=ALU.is_equal), r=[rlgt, rs0], w=[rs2])
                    P.op("dve", lambda e: e.tensor_scalar(out=s2[:, 0:4], in0=s2[:, 0:4], scalar1=BIG, scalar2=-BIG, op0=ALU.mult, op1=ALU.add), r=[rs2], w=[rs2])
                    P.op("dve", lambda e: e.tensor_tensor(out=l2[:].rearrange("p (g e) -> p g e", e=8), in0=lgt[:, 4:36].rearrange("p (g e) -> p g e", e=8),
                                                          in1=s2[:, 0:4].unsqueeze(2).to_broadcast([128, 4, 8]), op=ALU.add), r=[rlgt, rs2], w=[rl2])
                    P.op("dve", lambda e: e.tensor_reduce(out=s3[:, 0:1], in_=l2[:], axis=AX.X, op=ALU.max), r=[rl2], w=[rs3])
                    P.op("dve", lambda e: e.tensor_scalar(out=oh1[:], in0=l2[:], scalar1=s3[:, 0:1], scalar2=None, op0=ALU.is_equal), r=[rl2, rs3], w=[roh1])
                    P.op("dve", lambda e: e.scalar_tensor_tensor(out=l2[:], in0=oh1[:], scalar=-BIG, in1=l2[:], op0=ALU.mult, op1=ALU.add), r=[roh1, rl2], w=[rl2])
                    P.op("dve", lambda e: e.tensor_reduce(out=s3[:, 1:2], in_=l2[:], axis=AX.X, op=ALU.max), r=[rl2], w=[rs3])
                    P.op("dve", lambda e: e.tensor_scalar(out=oh2[:], in0=l2[:], scalar1=s3[:, 1:2], scalar2=None, op0=ALU.is_equal), r=[rl2, rs3], w=[roh2])
                    P.op("dve", lambda e: e.tensor_tensor(out=s3[:, 2:3], in0=s3[:, 1:2], in1=s3[:, 0:1], op=ALU.subtract), r=[rs3], w=[rs3])
                    P.op("act", lambda e: e.activation(out=s3[:, 3:4], in_=s3[:, 2:3], func=AF.Exp), r=[rs3], w=[rs3])
                    P.op("dve", lambda e: e.tensor_scalar(out=s3[:, 4:5], in0=s3[:, 3:4], scalar1=1.0, scalar2=None, op0=ALU.add), r=[rs3], w=[rs3])
                    P.op("dve", lambda e: e.reciprocal(out=s3[:, 4:5], in_=s3[:, 4:5]), r=[rs3], w=[rs3])
                    P.op("dve", lambda e: e.tensor_tensor(out=s3[:, 5:6], in0=s3[:, 4:5], in1=s0[:, 3:4], op=ALU.mult), r=[rs3, rs0], w=[rs3])
                    P.op("dve", lambda e: e.tensor_tensor(out=s3[:, 6:7], in0=s3[:, 5:6], in1=s3[:, 3:4], op=ALU.mult), r=[rs3], w=[rs3])
                    P.op("dve", lambda e: e.tensor_scalar(out=Gall[:, i, :], in0=oh1[:], scalar1=s3[:, 5:6], scalar2=None, op0=ALU.mult), r=[roh1, rs3], w=[rGall])
                    P.op("dve", lambda e: e.scalar_tensor_tensor(out=Gall[:, i, :], in0=oh2[:], scalar=s3[:, 6:7], in1=Gall[:, i, :], op0=ALU.mult, op1=ALU.add),
                         r=[roh2, rs3, rGall], w=[rGall])
            P.barrier()
            with contextlib.ExitStack() as e2:
                w1 = [_tile(e2, nc, f"E2_w1_{i}", [128, 8, 512], BF16) for i in range(2)]
                w3 = [_tile(e2, nc, f"E2_w3_{i}", [128, 8, 512], BF16) for i in range(2)]
                w2 = [_tile(e2, nc, f"E2_w2_{i}", [128, 4, 1024], BF16) for i in range(2)]
                sl = [_tile(e2, nc, f"E2_sl{i}", [128, 512], F32) for i in range(2)]
                hid = [_tile(e2, nc, f"E2_hid{i}", [128, 4, 512], BF16) for i in range(2)]
                H1 = [_tile(e2, nc, f"E2_H1{i}", [128, 512], F32, psum=True) for i in range(2)]
                H3 = [_tile(e2, nc, f"E2_H3{i}", [128, 512], F32, psum=True) for i in range(2)]
                Yp = [[_tile(e2, nc, f"E2_Y{i}{j}", [128, 512], F32, psum=True) for j in range(2)] for i in range(2)]
                groups = [list(range(g0, min(g0 + 4, len(sg)))) for g0 in range(0, len(sg), 4)]

                def loadw(ei):
                    e_ = experts[ei]
                    P.dma("pool", w1[ei % 2][0][:], Dm["exp_w1"][l, e_].rearrange("(k p) n -> p k n", p=128), w=[w1[ei % 2][1]])
                    P.dma("pool", w3[ei % 2][0][:], Dm["exp_w3"][l, e_].rearrange("(k p) n -> p k n", p=128), w=[w3[ei % 2][1]])
                    P.dma("pool", w2[ei % 2][0][:], Dm["exp_w2"][l, e_].rearrange("(k p) n -> p k n", p=128), w=[w2[ei % 2][1]])
                loadw(0)
                cnt = 0
                ycnt = 0
                gi_ = 0
                for ei, e_ in enumerate(experts):
                    if ei + 1 < len(experts):
                        loadw(ei + 1)
                    (w1t, rw1), (w3t, rw3), (w2t, rw2) = w1[ei % 2], w3[ei % 2], w2[ei % 2]
                    for grp in groups:
                        ntok = len(grp) * 128
                        tsl = slice(grp[0] * 128, grp[0] * 128 + ntok)
                        hd, rhd = hid[gi_ % 2]
                        gi_ += 1
                        for fc in range(4):
                            h1, rh1 = H1[cnt % 2]
                            h3, rh3 = H3[cnt % 2]
                            s_, rs_ = sl[cnt % 2]
                            cnt += 1
                            for k in range(8):
                                P.op("pe", lambda e: e.matmul(h1[:, 0:ntok], lhsT=w1t[:, k, fc * 128:(fc + 1) * 128], rhs=FT[:, k, tsl], start=(k == 0), stop=(k == 7)),
                                     r=[rw1, rFT], w=[rh1])
                            for k in range(8):
                                P.op("pe", lambda e: e.matmul(h3[:, 0:ntok], lhsT=w3t[:, k, fc * 128:(fc + 1) * 128], rhs=FT[:, k, tsl], start=(k == 0), stop=(k == 7)),
                                     r=[rw3, rFT], w=[rh3])
                            P.op("act", lambda e: e.activation(out=s_[:, 0:ntok], in_=h1[:, 0:ntok], func=AF.Silu), r=[rh1], w=[rs_])
                            P.op("dve", lambda e: e.tensor_tensor(out=hd[:, fc, 0:ntok], in0=h3[:, 0:ntok], in1=s_[:, 0:ntok], op=ALU.mult), r=[rh3, rs_], w=[rhd])
                        for ti, tt in enumerate(grp):
                            yp = Yp[ycnt % 2]
                            ycnt += 1
                            for nh in range(2):
                                ypt, rypt = yp[nh]
                                for fc in range(4):
                                    P.op("pe", lambda e: e.matmul(ypt, lhsT=hd[:, fc, ti * 128:(ti + 1) * 128], rhs=w2t[:, fc, nh * 512:(nh + 1) * 512], start=(fc == 0), stop=(fc == 3)),
                                         r=[rhd, rw2], w=[rypt])
                                dst = yacc[:, tt, nh * 512:(nh + 1) * 512]
                                if ei == 0:
                                    P.op("dve", lambda e: e.tensor_scalar(out=dst, in0=ypt, scalar1=Gall[:, tt, e_:e_ + 1], scalar2=None, op0=ALU.mult), r=[rypt, rGall], w=[ryacc])
                                else:
                                    P.op("dve", lambda e: e.scalar_tensor_tensor(out=dst, in0=ypt, scalar=Gall[:, tt, e_:e_ + 1], in1=dst, op0=ALU.mult, op1=ALU.add),
                                         r=[rypt, rGall, ryacc], w=[ryacc])
            P.barrier()
            with contextlib.ExitStack() as e3:
                ht = [_tile(e3, nc, f"E3_h{i}", [128, 1024], F32) for i in range(2)]
                hn = [_tile(e3, nc, f"E3_hn{i}", [128, 1024], F32) for i in range(2)]
                for i, t in enumerate(sg):
                    h_, rh_ = ht[i % 2]
                    n_, rn_ = hn[i % 2]
                    ts = slice(t * 128, (t + 1) * 128)
                    P.dma("sp", h_[:], Dm["H"][ts, :], r=[R["H"]], w=[rh_])
                    P.op("dve", lambda e: e.tensor_tensor(out=n_[:], in0=yacc[:, i, :], in1=bc[:, 2 if t < NXT else 5, :], op=ALU.mult), r=[ryacc, rbc], w=[rn_])
                    P.op("dve", lambda e: e.tensor_tensor(out=n_[:], in0=n_[:], in1=h_[:], op=ALU.add), r=[rn_, rh_], w=[rn_])
                    if last:
                        P.dma("pool", Dm["out"][ts, :], n_[:], r=[rn_], w=[R["out"]])
                    else:
                        P.dma("pool", Dm["H"][ts, :], n_[:], r=[rn_], w=[R["H"]])
            P.barrier()
    P.barrier()


def full_plan(nlayers=DEPTH):
    def plan(K):
        for l in range(nlayers):
            with_ctx = l < DEPTH - 1
            last = l == DEPTH - 1
            stage_prep(K, l)
            stage_A(K, l)
            stage_ret(K, l, with_ctx)
            stage_na(K, l, with_ctx)
            stage_s5(K, l, with_ctx)
            stage_gqa(K, l, with_ctx)
            stage_merge(K, l, with_ctx)
            stage_moe(K, l, with_ctx, last)
    return plan


_CACHE = {}


def kernel(**inputs):
    consts = make_consts()
    n = 8
    x = np.asarray(inputs["x"], np.float32)
    in_maps = []
    shared = {k: np.ascontiguousarray(np.asarray(inputs[k], np.float32)) for k in INPUT_NAMES if k not in ("x", "c", "ctx")}
    for b in range(n):
        m = dict(shared)
        m["x"] = np.ascontiguousarray(x[b])
        m["ctx"] = np.ascontiguousarray(np.asarray(inputs["ctx"], np.float32)[b])
        m["c"] = np.ascontiguousarray(np.asarray(inputs["c"], np.float32)[b:b + 1])
        m.update(consts)
        in_maps.append(m)
    shapes = {k: (v.shape, F32) for k, v in in_maps[0].items() if k not in consts}
    if "nc" not in _CACHE:
        _CACHE["nc"] = build(shapes, consts, full_plan())[0]
    res = run_bass_kernel_spmd(_CACHE["nc"], in_maps, core_ids=list(range(n)))
    return np.stack([np.asarray(r["out"], np.float32) for r in res.results], axis=0)
```

```python
import contextlib
import math
import numpy as np
import ml_dtypes
import concourse.bass as bass
import concourse.mybir as mybir
from concourse.bass_utils import run_bass_kernel_spmd

F32 = mybir.dt.float32
BF16 = mybir.dt.bfloat16
I32 = mybir.dt.int32
AF = mybir.ActivationFunctionType
ALU = mybir.AluOpType
AX = mybir.AxisListType

D = 1024
L = 8192
NCTX = 256
T = L + NCTX
NT = T // 128
NXT = L // 128
DEPTH = 4
MIXC = 2560
EPS = 1e-6
NEGM = -240000.0


class Res:
    __slots__ = ("name", "w", "rd")

    def __init__(self, name=""):
        self.name = name
        self.w = None
        self.rd = {}


class Prog:
    NDS = 12

    def __init__(self, nc, same_sync=True, dma_queues=("sp", "pool", "act")):
        self.nc = nc
        self.E = {"pe": nc.tensor, "dve": nc.vector, "act": nc.scalar, "pool": nc.gpsimd, "sp": nc.sync}
        self.same_sync = same_sync
        self.semh = {}
        self.cnt = {}
        for k in self.E:
            self.semh[("c", k)] = nc.alloc_semaphore(f"sc_{k}")
            self.cnt[k] = 0
        self.seen = {k: {} for k in self.E}
        self.duse = {}
        self.dnext = {}
        for q in dma_queues:
            self.duse[q] = [0] * self.NDS
            self.dnext[q] = 0
            for i in range(self.NDS):
                self.semh[("d", q, i)] = nc.alloc_semaphore(f"sd_{q}_{i}")
        self.ninst = 0

    def _wait(self, eng, key, val):
        if val <= 0 or self.seen[eng].get(key, 0) >= val:
            return
        self.E[eng].wait_ge(self.semh[key], val)
        self.seen[eng][key] = val

    def _deps(self, eng, r, w):
        deps = {}
        for res in r:
            if res.w is not None:
                k, v = res.w
                if deps.get(k, 0) < v:
                    deps[k] = v
        for res in w:
            if res.w is not None:
                k, v = res.w
                if deps.get(k, 0) < v:
                    deps[k] = v
            for k, v in res.rd.items():
                if deps.get(k, 0) < v:
                    deps[k] = v
        for k, v in deps.items():
            if k == ("c", eng) and not self.same_sync:
                continue
            self._wait(eng, k, v)

    def _mark(self, tok, r, w):
        k, v = tok
        for res in r:
            if res.rd.get(k, 0) < v:
                res.rd[k] = v
        for res in w:
            res.w = tok
            res.rd = {}

    def op(self, eng, fn, r=(), w=()):
        self._deps(eng, r, w)
        ins = fn(self.E[eng])
        self.cnt[eng] += 1
        ins.then_inc(self.semh[("c", eng)], 1)
        self._mark((("c", eng), self.cnt[eng]), r, w)
        self.ninst += 1
        return ins

    def dma(self, q, out, in_, r=(), w=(), **kw):
        self._deps(q, r, w)
        i = self.dnext[q]
        self.dnext[q] = (i + 1) % self.NDS
        key = ("d", q, i)
        self._wait(q, key, 16 * self.duse[q][i])
        ins = self.E[q].dma_start(out=out, in_=in_, **kw)
        self.duse[q][i] += 1
        ins.then_inc(self.semh[key], 16)
        self._mark((key, 16 * self.duse[q][i]), r, w)
        self.ninst += 1
        return ins

    def barrier(self, engines=None):
        engines = engines or list(self.E)
        for eng in engines:
            for k in self.E:
                if k != eng:
                    self._wait(eng, ("c", k), self.cnt[k])
            for q in self.duse:
                for i in range(self.NDS):
                    self._wait(eng, ("d", q, i), 16 * self.duse[q][i])


class Ctx:
    pass


_TCNT = [0]


def _tile(es, nc, name, shape, dt, psum=False):
    _TCNT[0] += 1
    name = f"{name}_{_TCNT[0]}"
    if not psum:
        t = es.enter_context(nc.sbuf_tensor(name, shape, dt))
        return t, Res(name)
    esz = 2 if dt == BF16 else 4
    n = int(np.prod(shape[1:]))
    per_bank = 2048 // esz
    nb = (n + per_bank - 1) // per_bank
    t = es.enter_context(nc.psum_tensor(name, [128, nb * per_bank], dt))
    ap = t[0:shape[0], 0:n]
    if len(shape) == 3:
        ap = ap.rearrange("p (a b) -> p a b", b=shape[2])
    elif len(shape) == 4:
        ap = ap.rearrange("p (a b c) -> p a b c", b=shape[2], c=shape[3])
    return ap, Res(name)


NA_NCLS = 21
MOE_B = 256
MOE_NBMAX = 98
S5_TC = 256
MAGIC = 12582912.0


def na_class_list():
    lst = [(10, 10 + dc) for dc in (-2, -1, 0, 1, 2)]
    for j in (0, 1):
        lst += [(j, c) for c in range(4)]
    for j in (62, 63):
        lst += [(j, c) for c in range(60, 64)]
    return lst


def na_chunks(j):
    if 2 <= j <= 61:
        return [(j + dc, dc + 2) for dc in (-2, -1, 0, 1, 2)]
    base = {0: 5, 1: 9, 62: 13, 63: 17}[j]
    c0 = 0 if j < 2 else 60
    return [(c0 + i, base + i) for i in range(4)]


def make_consts():
    c = {}
    pos = np.arange(L)
    inv = (10000.0 ** (-np.arange(16, dtype=np.float32) / 16)).astype(np.float32)
    ang_r = (pos // 64).astype(np.float32)[:, None] * inv
    ang_c = (pos % 64).astype(np.float32)[:, None] * inv
    cr, sr, cc, sc = np.cos(ang_r), np.sin(ang_r), np.cos(ang_c), np.sin(ang_c)
    cosf = np.concatenate([cr, cr, cc, cc], axis=1)
    sinf = np.concatenate([-sr, sr, -sc, sc], axis=1)
    cosf = np.concatenate([cosf, np.ones((NCTX, 64))], axis=0)
    sinf = np.concatenate([sinf, np.zeros((NCTX, 64))], axis=0)
    c["ropecs"] = np.concatenate([cosf, sinf], axis=1).astype(np.float32)
    c["ident"] = np.eye(128, dtype=np.float32)
    kl = np.arange(128)[:, None]
    ql = np.arange(128)[None, :]
    lo = np.where(kl >= ql, 0.0, NEGM)
    hi = np.where(kl <= ql, 0.0, NEGM)
    c["na_jx"] = np.zeros((128, 128), np.float32)
    for q in range(128):
        c["na_jx"][(q // 64) * 64 + 63 - q % 64, q] = 1.0
    rm = np.zeros((NA_NCLS, 128, 128), np.float32)
    for cls, (j, cch) in enumerate(na_class_list()):
        for qp in range(128):
            rq = 2 * j + qp // 64
            cq = 63 - qp % 64
            r0 = min(max(rq - 4, 0), 120)
            ws = min(max(cq - 8, 0), 48)
            for key in range(128):
                rk = 2 * cch + key // 64
                ck = key % 64
                ok = (r0 <= rk < r0 + 8) and (ws <= ck < ws + 16)
                rm[cls, qp, key] = 0.0 if ok else NEGM
    c["na_rm"] = rm
    si = np.arange(128, dtype=np.float32)[:, None]
    ti = np.arange(128, dtype=np.float32)[None, :]
    c["ret_dpos"] = np.maximum(ti - si, 0.0).astype(np.float32)
    c["ret_dneg"] = np.maximum(si - ti, 0.0).astype(np.float32)
    c["ret_diag"] = ((si == ti) * math.log(2.0) + math.log(0.125)).astype(np.float32)
    c["ret_tp1"] = np.broadcast_to(ti + 1.0, (128, 128)).astype(np.float32).copy()
    c["ret_tr"] = np.broadcast_to(128.0 - ti, (128, 128)).astype(np.float32).copy()
    c["ret_pcol"] = np.concatenate([127.0 - si, si], axis=1).astype(np.float32)
    c["s5_iota1"] = np.broadcast_to(np.arange(1, S5_TC + 1, dtype=np.float32)[None, :], (128, S5_TC)).copy()
    bm = np.zeros((128, 128), np.float32)
    for r in range(128):
        a = (r // 16) % 2
        bm[r, a * 64:(a + 1) * 64] = 1.0
    c["s5_bdmask"] = bm
    c["moe_bb"] = np.broadcast_to((np.arange(MOE_NBMAX, dtype=np.float32) * MOE_B)[None, :, None], (128, MOE_NBMAX, 32)).reshape(128, MOE_NBMAX * 32).copy()
    c["moe_pcol"] = np.arange(128, dtype=np.float32)[:, None].copy()
    c["moe_ustrict"] = (np.arange(128)[:, None] < np.arange(128)[None, :]).astype(np.float32)
    c["moe_ones"] = np.ones((128, 128), np.float32)
    c["gqa_mask"] = np.stack([np.tile(lo, (1, 2)), np.tile(hi, (1, 2))], axis=1).astype(np.float32)
    return c


def stage_prep(K, l):
    nc, P, Dm = K.nc, K.P, K.dram
    with contextlib.ExitStack() as es:
        cc, rcc = _tile(es, nc, "pp_cc", [128, 8, 2], F32)
        sc, rsc = _tile(es, nc, "pp_sc", [128, 8, 2], F32)
        mw, rmw = _tile(es, nc, "pp_mw", [128, 8, 512], F32)
        mw2, rmw2 = _tile(es, nc, "pp_mw2", [128, 8, 512], F32)
        mws = [(mw, rmw), (mw2, rmw2)]
        mv, rmv = _tile(es, nc, "pp_mv", [2, 6144], F32)
        mb, rmb = _tile(es, nc, "pp_mb", [2, 6144], F32)
        g12, rg12 = _tile(es, nc, "pp_g", [2, 2048], F32)
        ps, rps = _tile(es, nc, "pp_ps", [2, 512], F32, psum=True)
        ps2, rps2 = _tile(es, nc, "pp_ps2", [2, 512], F32, psum=True)
        pss = [(ps, rps), (ps2, rps2)]
        with nc.allow_non_contiguous_dma(reason="tiny"):
            P.dma("sp", cc[:, :, 0], Dm["c"].rearrange("o (k p) -> p (o k)", p=128), w=[rcc])
            P.dma("sp", cc[:, :, 1], Dm["c_ctx"].rearrange("(k p) -> p k", p=128), w=[rcc])
        P.dma("sp", mb[:], Dm["mod_b"][l].partition_broadcast(2), w=[rmb])
        P.dma("sp", g12[:, 0:1024], Dm["norm1_g"][l].partition_broadcast(2), w=[rg12])
        P.dma("sp", g12[:, 1024:2048], Dm["norm2_g"][l].partition_broadcast(2), w=[rg12])
        P.op("act", lambda e: e.activation(out=sc[:], in_=cc[:], func=AF.Silu), r=[rcc], w=[rsc])
        mwv = Dm["mod_w"][l].rearrange("(k p) n -> p k n", p=128)
        for n in range(12):
            w_, rw_ = mws[n % 2]
            p_, rp_ = pss[n % 2]
            P.dma("sp", w_[:], mwv[:, :, n * 512:(n + 1) * 512], w=[rw_])
            for k in range(8):
                P.op("pe", lambda e: e.matmul(p_[:], lhsT=sc[:, k, :], rhs=w_[:, k, :], start=(k == 0), stop=(k == 7)),
                     r=[rsc, rw_], w=[rp_])
            P.op("dve", lambda e: e.tensor_tensor(out=mv[:, n * 512:(n + 1) * 512], in0=p_[:], in1=mb[:, n * 512:(n + 1) * 512], op=ALU.add),
                 r=[rp_, rmb], w=[rmv])
        for slot, goff in ((1, 0), (4, 1024)):
            P.op("dve", lambda e: e.scalar_tensor_tensor(out=mv[:, slot * 1024:(slot + 1) * 1024], in0=mv[:, slot * 1024:(slot + 1) * 1024],
                                                         scalar=1.0, in1=g12[:, goff:goff + 1024], op0=ALU.add, op1=ALU.mult),
                 r=[rmv, rg12], w=[rmv])
        order = [1, 0, 2, 4, 3, 5]
        mvd = Dm["modv"].rearrange("(a r) d -> a r d", a=2)
        for j, slot in enumerate(order):
            P.dma("sp", mvd[:, j, :], mv[:, slot * 1024:(slot + 1) * 1024], r=[rmv], w=[K.R["modv"]])
    P.barrier()


def stage_A(K, l, tiles=None):
    nc, P, Dm, R = K.nc, K.P, K.dram, K.R
    tiles = list(range(NT)) if tiles is None else tiles
    with contextlib.ExitStack() as es:
        win, rwin = _tile(es, nc, "A_win", [128, 8, MIXC], BF16)
        ident, rident = _tile(es, nc, "A_ident", [128, 128], BF16)
        identf, ridentf = _tile(es, nc, "A_identf", [128, 128], F32)
        bc, rbc = _tile(es, nc, "A_bc", [128, 4, 1024], F32)
        gains, rgains = _tile(es, nc, "A_gains", [128, 4, 64], F32)
        epsc, repsc = _tile(es, nc, "A_eps", [128, 1], F32)
        hts = [_tile(es, nc, f"A_h{i}", [128, 1024], F32) for i in range(2)]
        rps_ = [_tile(es, nc, f"A_rope{i}", [128, 128], F32) for i in range(2)]
        junk, rjunk = _tile(es, nc, "A_junk", [128, 1024], BF16)
        ssq, rssq = _tile(es, nc, "A_ssq", [128, 1], F32)
        rstd, rrstd = _tile(es, nc, "A_rstd", [128, 1], F32)
        t1, rt1 = _tile(es, nc, "A_t1", [128, 1024], F32)
        abf, rabf = _tile(es, nc, "A_abf", [128, 1024], BF16)
        aT, raT = _tile(es, nc, "A_aT", [128, 8, 128], BF16)
        pT, rpT = _tile(es, nc, "A_pT", [128, 8, 128], BF16, psum=True)
        pm = [_tile(es, nc, f"A_pm{i}", [128, 512], F32, psum=True) for i in range(5)]
        pT2, rpT2 = _tile(es, nc, "A_pT2", [128, 4, 128], BF16, psum=True)
        sA, rsA = _tile(es, nc, "A_sA", [128, 512], F32)
        sB, rsB = _tile(es, nc, "A_sB", [128, 512], F32)
        o_rqk, ro_rqk = _tile(es, nc, "A_orqk", [128, 512], BF16)
        o_rv, ro_rv = _tile(es, nc, "A_orv", [128, 256], BF16)
        o_rg, ro_rg = _tile(es, nc, "A_org", [128, 256], F32)
        o_nqk, ro_nqk = _tile(es, nc, "A_onqk", [128, 512], BF16)
        o_nv, ro_nv = _tile(es, nc, "A_onv", [128, 256], BF16)
        o_su, ro_su = _tile(es, nc, "A_osu", [128, 256], F32)
        o_sub, ro_sub = _tile(es, nc, "A_osub", [128, 256], BF16)
        o_gqk, ro_gqk = _tile(es, nc, "A_ogqk", [128, 384], BF16)
        o_gv, ro_gv = _tile(es, nc, "A_ogv", [128, 128], BF16)
        oT, roT = _tile(es, nc, "A_oT", [128, 4, 128], BF16)
        ss8, rss8 = _tile(es, nc, "A_ss8", [128, 8], F32)
        rs8, rrs8 = _tile(es, nc, "A_rs8", [128, 8], F32)

        P.dma("pool", win[:], Dm["w_in"][l].rearrange("(k p) n -> p k n", p=128)[:, :, 0:MIXC], w=[rwin])
        P.dma("sp", identf[:], Dm["ident"], w=[ridentf])
        P.op("dve", lambda e: e.tensor_copy(ident[:], identf[:]), r=[ridentf], w=[rident])
        for j, row in enumerate((0, 1, 6, 7)):
            P.dma("sp", bc[:, j, :], Dm["modv"][row].partition_broadcast(128), r=[R["modv"]], w=[rbc])
        P.dma("sp", gains[:, 0:2, :], Dm["na_qk_gain"][l].partition_broadcast(128), w=[rgains])
        P.dma("sp", gains[:, 2:4, :], Dm["gqa_qk_gain"][l].partition_broadcast(128), w=[rgains])
        P.op("dve", lambda e: e.memset(epsc[:], EPS), w=[repsc])

        hsrc = K.hsrc(l)

        def load(t, par):
            ht, rht = hts[par]
            rp, rrp = rps_[par]
            P.dma("sp", ht[:], hsrc(t), r=[R["H"]], w=[rht])
            P.dma("sp", rp[:], Dm["ropecs"][t * 128:(t + 1) * 128, :], w=[rrp])

        def rmsn(src, nh, gidx, dst, rdst_list, rsrc_list):
            P.op("act", lambda e: e.activation(out=sB[:, 0:nh * 64], in_=src, func=AF.Square), r=rsrc_list, w=[rsB])
            P.op("dve", lambda e: e.tensor_reduce(out=ss8[:, 0:nh], in_=sB[:, 0:nh * 64].rearrange("p (h d) -> p h d", d=64), axis=AX.X, op=ALU.add),
                 r=[rsB], w=[rss8])
            P.op("act", lambda e: e.activation(out=rs8[:, 0:nh], in_=ss8[:, 0:nh], func=AF.Sqrt, scale=1.0 / 64, bias=epsc[:, 0:1]),
                 r=[rss8, repsc], w=[rrs8])
            P.op("dve", lambda e: e.reciprocal(out=rs8[:, 0:nh], in_=rs8[:, 0:nh]), r=[rrs8], w=[rrs8])
            P.op("dve", lambda e: e.tensor_tensor(out=dst.rearrange("p (h d) -> p h d", d=64), in0=src.rearrange("p (h d) -> p h d", d=64),
                                                  in1=rs8[:, 0:nh].unsqueeze(2).to_broadcast([128, nh, 64]), op=ALU.mult),
                 r=rsrc_list + [rrs8], w=rdst_list)
            P.op("dve", lambda e: e.tensor_tensor(out=dst.rearrange("p (h d) -> p h d", d=64), in0=dst.rearrange("p (h d) -> p h d", d=64),
                                                  in1=gains[:, gidx, :].unsqueeze(1).to_broadcast([128, nh, 64]), op=ALU.mult),
                 r=rdst_list + [rgains], w=rdst_list)

        def rope(src, nh, rp, rrp, dst_bf, rsrc_list, rdst_list):
            v5 = lambda ap: ap.rearrange("p (h a b c) -> p h a b c", a=2, b=2, c=16)
            cosb = rp[:, 0:64].rearrange("p (a b c) -> p a b c", a=2, b=2).unsqueeze(1).to_broadcast([128, nh, 2, 2, 16])
            sinb = rp[:, 64:128].rearrange("p (a b c) -> p a b c", a=2, b=2).unsqueeze(1).to_broadcast([128, nh, 2, 2, 16])
            P.op("dve", lambda e: e.tensor_tensor(out=v5(sA[:, 0:nh * 64]), in0=v5(src), in1=cosb, op=ALU.mult), r=rsrc_list + [rrp], w=[rsA])
            P.op("dve", lambda e: e.tensor_tensor(out=v5(sB[:, 0:nh * 64]), in0=v5(src)[:, :, :, ::-1, :], in1=sinb, op=ALU.mult),
                 r=rsrc_list + [rrp], w=[rsB])
            P.op("dve", lambda e: e.tensor_tensor(out=dst_bf, in0=sA[:, 0:nh * 64], in1=sB[:, 0:nh * 64], op=ALU.add), r=[rsA, rsB], w=rdst_list)

        def transp_out(src_bf, rsrc, nchunk, dram_rows, t):
            for k in range(nchunk):
                P.op("pe", lambda e: e.transpose(out=pT2[:, k, :], in_=src_bf[:, k * 128:(k + 1) * 128], identity=ident[:]),
                     r=[rsrc, rident], w=[rpT2])
            P.op("act", lambda e: e.copy(out=oT[:, 0:nchunk, :], in_=pT2[:, 0:nchunk, :]), r=[rpT2], w=[roT])
            P.dma("pool", dram_rows.rearrange("(k p) n -> p k n", p=128)[:, :, t * 128:(t + 1) * 128], oT[:, 0:nchunk, :], r=[roT], w=[R["mix"]])

        load(tiles[0], 0)
        for idx, t in enumerate(tiles):
            if idx + 1 < len(tiles):
                load(tiles[idx + 1], (idx + 1) % 2)
            ht, rht = hts[idx % 2]
            rp, rrp = rps_[idx % 2]
            isx = t < NXT
            g1 = bc[:, 0 if isx else 2, :]
            sh1 = bc[:, 1 if isx else 3, :]
            ts = slice(t * 128, (t + 1) * 128)
            P.op("act", lambda e: e.activation(out=junk[:], in_=ht[:], func=AF.Square, accum_out=ssq[:]), r=[rht], w=[rjunk, rssq])
            P.op("act", lambda e: e.activation(out=rstd[:], in_=ssq[:], func=AF.Sqrt, scale=1.0 / D, bias=epsc[:, 0:1]), r=[rssq, repsc], w=[rrstd])
            P.op("dve", lambda e: e.reciprocal(out=rstd[:], in_=rstd[:]), r=[rrstd], w=[rrstd])
            P.op("dve", lambda e: e.scalar_tensor_tensor(out=t1[:], in0=ht[:], scalar=rstd[:, 0:1], in1=g1, op0=ALU.mult, op1=ALU.mult),
                 r=[rht, rrstd, rbc], w=[rt1])
            P.op("dve", lambda e: e.tensor_tensor(out=abf[:], in0=t1[:], in1=sh1, op=ALU.add), r=[rt1, rbc], w=[rabf])
            for k in range(8):
                P.op("pe", lambda e: e.transpose(out=pT[:, k, :], in_=abf[:, k * 128:(k + 1) * 128], identity=ident[:]), r=[rabf, rident], w=[rpT])
            P.op("act", lambda e: e.copy(out=aT[:], in_=pT[:]), r=[rpT], w=[raT])
            P.dma("pool", Dm["aT"].rearrange("(k p) n -> p k n", p=128)[:, :, ts], aT[:], r=[raT], w=[R["aT"]])
            for n in range(5):
                pmn, rpmn = pm[n]
                for k in range(8):
                    P.op("pe", lambda e: e.matmul(pmn[:], lhsT=aT[:, k, :], rhs=win[:, k, n * 512:(n + 1) * 512], start=(k == 0), stop=(k == 7)),
                         r=[raT, rwin], w=[rpmn])
            rope(pm[0][0][:], 8, rp, rrp, o_rqk[:], [pm[0][1]], [ro_rqk])
            P.dma("pool", Dm["rk"][ts, :], o_rqk[:, 256:512], r=[ro_rqk], w=[R["mix"]])
            transp_out(o_rqk, ro_rqk, 4, Dm["rqkT"], t)
            P.op("act", lambda e: e.copy(out=o_rv[:], in_=pm[1][0][:, 0:256]), r=[pm[1][1]], w=[ro_rv])
            P.op("act", lambda e: e.activation(out=o_rg[:], in_=pm[1][0][:, 256:512], func=AF.Silu), r=[pm[1][1]], w=[ro_rg])
            P.dma("pool", Dm["rv"][ts, :], o_rv[:], r=[ro_rv], w=[R["mix"]])
            P.dma("pool", Dm["rg"][ts, :], o_rg[:], r=[ro_rg], w=[R["mix"]])
            rmsn(pm[2][0][:, 0:256], 4, 0, t1[:, 0:256], [rt1], [pm[2][1]])
            rmsn(pm[2][0][:, 256:512], 4, 1, t1[:, 256:512], [rt1], [pm[2][1]])
            P.op("act", lambda e: e.copy(out=o_nqk[:], in_=t1[:, 0:512]), r=[rt1], w=[ro_nqk])
            transp_out(o_nqk, ro_nqk, 4, Dm["nqkT"], t)
            P.op("act", lambda e: e.copy(out=o_nv[:], in_=pm[3][0][:, 0:256]), r=[pm[3][1]], w=[ro_nv])
            P.dma("pool", Dm["nv"][ts, :], o_nv[:], r=[ro_nv], w=[R["mix"]])
            P.op("act", lambda e: e.copy(out=o_su[:], in_=pm[3][0][:, 256:512]), r=[pm[3][1]], w=[ro_su])
            P.op("dve", lambda e: e.tensor_copy(out=o_sub[:], in_=pm[3][0][:, 256:512]), r=[pm[3][1]], w=[ro_sub])
            P.dma("pool", Dm["su"][ts, :], o_su[:], r=[ro_su], w=[R["mix"]])
            transp_out(o_sub, ro_sub, 2, Dm["suT"], t)
            rmsn(pm[4][0][:, 0:256], 4, 2, t1[:, 512:768], [rt1], [pm[4][1]])
            rmsn(pm[4][0][:, 256:384], 2, 3, t1[:, 768:896], [rt1], [pm[4][1]])
            rope(t1[:, 512:896], 6, rp, rrp, o_gqk[:], [rt1], [ro_gqk])
            transp_out(o_gqk, ro_gqk, 3, Dm["gqkT"], t)
            P.op("act", lambda e: e.copy(out=o_gv[:], in_=pm[4][0][:, 384:512]), r=[pm[4][1]], w=[ro_gv])
            P.dma("pool", Dm["gv"][ts, :], o_gv[:], r=[ro_gv], w=[R["mix"]])
    P.barrier()


INPUT_NAMES = ["x", "c", "ctx", "c_ctx", "mod_w", "mod_b", "norm1_g", "norm2_g", "w_in", "w_branch", "w_out", "ret_log_decay",
               "na_qk_gain", "na_rpb", "s5_lambda_re", "s5_lambda_im", "s5_log_step", "s5_b_re", "s5_b_im", "s5_c_re",
               "s5_c_im", "s5_d", "s5_glu_w", "s5_glu_b", "gqa_qk_gain", "gqa_sink", "router_w1", "router_b1", "router_w2",
               "router_b2", "exp_w1", "exp_w3", "exp_w2"]

SCRATCH = {
    "modv": ([12, 1024], F32),
    "H": ([T, D], F32),
    "aT": ([D, T], BF16),
    "rqkT": ([512, T], BF16), "rk": ([T, 256], BF16), "rv": ([T, 256], BF16), "rg": ([T, 256], F32),
    "nqkT": ([512, T], BF16), "nv": ([T, 256], BF16),
    "su": ([T, 256], F32), "suT": ([256, T], BF16),
    "gqkT": ([384, T], BF16), "gv": ([T, 128], BF16),
}


def build(in_shapes, consts, plan, expose=()):
    nc = bass.Bass("TRN2", target_bir_lowering=False)
    K = Ctx()
    K.nc = nc
    K.dram = {}
    for name, (shape, dt) in in_shapes.items():
        K.dram[name] = nc.dram_tensor(name, list(shape), dt, kind="ExternalInput").ap()
    for name, arr in consts.items():
        K.dram[name] = nc.dram_tensor(name, list(arr.shape), F32, kind="ExternalInput").ap()
    for name, (shape, dt) in SCRATCH.items():
        if name in K.dram:
            continue
        kind = "ExternalOutput" if name in expose else "Internal"
        K.dram[name] = nc.dram_tensor(name, list(shape), dt, kind=kind).ap()
    K.dram["out"] = nc.dram_tensor("out", [L, D], F32, kind="ExternalOutput").ap()
    K.R = {k: Res(k) for k in ["modv", "H", "aT", "mix", "y", "out", "s5y"]}
    K.P = Prog(nc)

    def hsrc(l):
        def f(t):
            if l == 0:
                if t < NXT:
                    return K.dram["x"][t * 128:(t + 1) * 128, :]
                return K.dram["ctx"][(t - NXT) * 128:(t - NXT + 1) * 128, :]
            return K.dram["H"][t * 128:(t + 1) * 128, :]
        return f
    K.hsrc = hsrc
    plan(K)
    K.P.barrier(["sp"])
    return nc, K


def run_attention(K, es, pfx, units, rd_res, epilogue, maxc):
    nc, P = K.nc, K.P
    wmax = max(u["nq"] for u in units) * 128
    spb = 512 // wmax
    nbank = (maxc + spb - 1) // spb
    S = [[_tile(es, nc, f"{pfx}_S{a}_{b}", [128, spb, wmax], F32, psum=True) for b in range(nbank)] for a in range(2)]
    O = _tile(es, nc, f"{pfx}_O", [128, 4, 65], F32, psum=True)
    pT = [[_tile(es, nc, f"{pfx}_pT{a}_{b}", [128, wmax], BF16) for b in range(maxc)] for a in range(2)]

    def emit_S(ui):
        u = units[ui]
        a = ui % 2
        w = u["nq"] * 128
        for ci, (kT, bias, _v) in enumerate(u["chunks"]):
            st, rst = S[a][ci // spb]
            sp = st[:, ci % spb, 0:w]
            P.op("pe", lambda e: e.matmul(sp, lhsT=kT, rhs=u["q"], start=True, stop=(bias is None)), r=rd_res, w=[rst])
            if bias is not None:
                P.op("pe", lambda e: e.matmul(sp, lhsT=bias[0], rhs=bias[1], start=False, stop=True), r=rd_res, w=[rst])
        for ci in range(len(u["chunks"])):
            st, rst = S[a][ci // spb]
            sp = st[:, ci % spb, 0:w]
            pt, rpt = pT[a][ci]
            P.op("act", lambda e: e.activation(out=pt[:, 0:w], in_=sp, func=AF.Exp, scale=0.125), r=[rst], w=[rpt])

    def emit_PV(ui):
        u = units[ui]
        a = ui % 2
        ot, rot = O
        nch = len(u["chunks"])
        for g, h in enumerate(u["heads"]):
            for ci, (_k, _b, v) in enumerate(u["chunks"]):
                pt, rpt = pT[a][ci]
                P.op("pe", lambda e: e.matmul(ot[:, h, :], lhsT=pt[:, g * 128:(g + 1) * 128], rhs=v, start=(ci == 0), stop=(ci == nch - 1)),
                     r=[rpt] + rd_res, w=[rot])
        if u["final"]:
            epilogue(u["j"], ot, rot)

    emit_S(0)
    for ui in range(len(units)):
        if ui + 1 < len(units):
            emit_S(ui + 1)
        emit_PV(ui)


def attn_epilogue_factory(K, es, pfx, ident, rident, yrow0, extra_den=None):
    nc, P, Dm, R = K.nc, K.P, K.dram, K.R
    den, rden = _tile(es, nc, pfx + "_den", [128, 4], F32)
    ybf, rybf = _tile(es, nc, pfx + "_ybf", [128, 256], BF16)
    pT2, rpT2 = _tile(es, nc, pfx + "_pT2", [128, 2, 128], BF16, psum=True)
    oT, roT = _tile(es, nc, pfx + "_oT", [128, 2, 128], BF16)

    def epi(j, ot, rot):
        if extra_den is not None:
            P.op("dve", lambda e: e.tensor_tensor(out=den[:], in0=ot[:, :, 64], in1=extra_den[0][:], op=ALU.add), r=[rot, extra_den[1]], w=[rden])
            P.op("dve", lambda e: e.reciprocal(out=den[:], in_=den[:]), r=[rden], w=[rden])
        else:
            P.op("dve", lambda e: e.reciprocal(out=den[:], in_=ot[:, :, 64]), r=[rot], w=[rden])
        P.op("dve", lambda e: e.tensor_tensor(out=ybf[:].rearrange("p (h d) -> p h d", d=64), in0=ot[:, :, 0:64],
                                              in1=den[:].unsqueeze(2).to_broadcast([128, 4, 64]), op=ALU.mult), r=[rot, rden], w=[rybf])
        for k in range(2):
            P.op("pe", lambda e: e.transpose(out=pT2[:, k, :], in_=ybf[:, k * 128:(k + 1) * 128], identity=ident[:]), r=[rybf, rident], w=[rpT2])
        P.op("act", lambda e: e.copy(out=oT[:], in_=pT2[:]), r=[rpT2], w=[roT])
        P.dma("pool", Dm["yT"][yrow0:yrow0 + 256, :].rearrange("(k p) n -> p k n", p=128)[:, :, j * 128:(j + 1) * 128], oT[:], r=[roT], w=[R["y"]])
    return epi


def stage_gqa(K, l, with_ctx, qtiles=None):
    nc, P, Dm, R = K.nc, K.P, K.dram, K.R
    with contextlib.ExitStack() as es:
        qT, rqT = _tile(es, nc, "G_qT", [64, 4, T], BF16)
        kT, rkT = _tile(es, nc, "G_kT", [64, 2, T], BF16)
        V, rV = _tile(es, nc, "G_V", [128, NT, 2, 65], BF16)
        mk, rmk = _tile(es, nc, "G_mask", [128, 2, 256], BF16)
        ident, rident = _tile(es, nc, "G_ident", [128, 128], BF16)
        esk, resk = _tile(es, nc, "G_esink", [128, 4], F32)
        for h in range(4):
            P.dma("sp", qT[:, h, :], Dm["gqkT"][h * 64:(h + 1) * 64, :], r=[R["mix"]], w=[rqT])
        for kv in range(2):
            P.dma("sp", kT[:, kv, :], Dm["gqkT"][256 + kv * 64:256 + (kv + 1) * 64, :], r=[R["mix"]], w=[rkT])
            P.dma("sp", V[:, :, kv, 0:64], Dm["gv"][:, kv * 64:(kv + 1) * 64].rearrange("(c p) d -> p c d", p=128), r=[R["mix"]], w=[rV])
        P.op("pool", lambda e: e.memset(V[:, :, :, 64:65], 1.0), w=[rV])
        P.dma("pool", mk[:], Dm["gqa_mask"], w=[rmk])
        P.dma("pool", ident[:], Dm["ident"], w=[rident])
        P.dma("sp", esk[:], Dm["gqa_sink"][l].partition_broadcast(128), w=[resk])
        P.op("act", lambda e: e.activation(out=esk[:], in_=esk[:], func=AF.Exp), r=[resk], w=[resk])
        rd = [rqT, rkT, rV, rmk, rident]
        epi = attn_epilogue_factory(K, es, "G", ident, rident, 768, extra_den=(esk, resk))
        qtiles = qtiles if qtiles is not None else list(range(NXT)) + ([64, 65] if with_ctx else [])
        units = []
        for j in qtiles:
            if j < NXT:
                ch = [(c, tag) for c, tag in ((j - 1, 0), (j, None), (j + 1, 1)) if 0 <= c < NXT] + [(64, None), (65, None)]
            else:
                ch = [(64, None), (65, None)]
            for kv in range(2):
                chunks = []
                for c, tag in ch:
                    bias = None if tag is None else (ident[:], mk[:, tag, :])
                    chunks.append((kT[:, kv, c * 128:(c + 1) * 128], bias, V[:, c, kv, :]))
                units.append(dict(j=j, q=qT[:, 2 * kv:2 * kv + 2, j * 128:(j + 1) * 128], nq=2, heads=[2 * kv, 2 * kv + 1], chunks=chunks, final=(kv == 1)))
        run_attention(K, es, "G", units, rd, epi, 5)
    P.barrier()


SCRATCH.update({"yT": ([1024, T], BF16)})


def stage_na(K, l, with_ctx, qtiles=None):
    nc, P, Dm, R = K.nc, K.P, K.dram, K.R
    with contextlib.ExitStack() as es:
        qT, rqT = _tile(es, nc, "N_qT", [128, 2, T], BF16)
        kT, rkT = _tile(es, nc, "N_kT", [128, 2, T], BF16)
        V, rV = _tile(es, nc, "N_V", [128, NT, 4, 65], BF16)
        Bp, rBp = _tile(es, nc, "N_Bp", [128, NA_NCLS, 4, 128], BF16)
        jx, rjx = _tile(es, nc, "N_jx", [128, 128], BF16)
        ident, rident = _tile(es, nc, "N_ident", [128, 128], BF16)
        zt, rzt = _tile(es, nc, "N_zero", [64, 128], F32)
        rp, rrp = _tile(es, nc, "N_rpb", [15, 4, 31], F32)
        stg = [_tile(es, nc, f"N_stg{i}", [128, 2, 64], F32) for i in range(2)]
        rms_ = [_tile(es, nc, f"N_rm{i}", [128, 128], F32) for i in range(2)]
        rpad = Res("rpbpad")
        for c2 in range(2):
            P.dma("sp", qT[:, c2, :], Dm["nqkT"][c2 * 128:(c2 + 1) * 128, :], r=[R["mix"]], w=[rqT])
            P.dma("sp", kT[:, c2, :], Dm["nqkT"][256 + c2 * 128:256 + (c2 + 1) * 128, :], r=[R["mix"]], w=[rkT])
        for h in range(4):
            P.dma("sp", V[:, :, h, 0:64], Dm["nv"][:, h * 64:(h + 1) * 64].rearrange("(c p) d -> p c d", p=128), r=[R["mix"]], w=[rV])
        P.op("pool", lambda e: e.memset(V[:, :, :, 64:65], 1.0), w=[rV])
        P.dma("pool", jx[:], Dm["na_jx"], w=[rjx])
        P.dma("pool", ident[:], Dm["ident"], w=[rident])
        P.op("dve", lambda e: e.memset(zt[:], 0.0), w=[rzt])
        P.dma("sp", Dm["rpbpad"].rearrange("h r j -> (h r) j"), zt[:], r=[rzt], w=[rpad])
        P.dma("sp", rp[:], Dm["na_rpb"][l].rearrange("h r j -> r h j"), w=[rrp])
        for h in range(4):
            P.dma("sp", Dm["rpbpad"][h, 0:15, 48:79], rp[:, h, :], r=[rrp], w=[rpad])
        padt = Dm["rpbpad"].tensor
        cl = na_class_list()
        for cls, (j, cch) in enumerate(cl):
            rmt, rrmt = rms_[cls % 2]
            P.dma("sp", rmt[:], Dm["na_rm"][cls], w=[rrmt])
            for h in range(4):
                st, rst = stg[(cls * 4 + h) % 2]
                for rq in range(2):
                    dr0 = 2 * (cch - j) + 0 - rq + 7
                    src = bass.AP(tensor=padt, offset=(h * 16 + dr0) * 128, ap=[[1, 64], [128, 2], [1, 64]])
                    P.dma("sp" if rq == 0 else "act", st[rq * 64:(rq + 1) * 64, :, :], src, r=[rpad], w=[rst])
                P.op("dve", lambda e: e.scalar_tensor_tensor(out=Bp[:, cls, h, :], in0=st[:].rearrange("p a b -> p (a b)"), scalar=8.0, in1=rmt[:],
                                                             op0=ALU.mult, op1=ALU.add), r=[rst, rrmt], w=[rBp])
        rd = [rqT, rkT, rV, rBp, rjx, rident]
        epi = attn_epilogue_factory(K, es, "N", ident, rident, 256)
        qtiles = qtiles if qtiles is not None else list(range(NXT)) + ([64, 65] if with_ctx else [])
        units = []
        for j in qtiles:
            ch = (na_chunks(j) if j < NXT else []) + [(64, None), (65, None)]
            for h in range(4):
                pb, c2 = (h % 2) * 64, h // 2
                chunks = []
                for c, cls in ch:
                    bias = None if cls is None else (Bp[:, cls, h, :], jx[:])
                    chunks.append((kT[pb:pb + 64, c2, c * 128:(c + 1) * 128], bias, V[:, c, h, :]))
                units.append(dict(j=j, q=qT[pb:pb + 64, c2, j * 128:(j + 1) * 128], nq=1, heads=[h], chunks=chunks, final=(h == 3)))
        run_attention(K, es, "N", units, rd, epi, 7)
    P.barrier()


SCRATCH.update({"rpbpad": ([4, 16, 128], F32)})


def stage_ret(K, l, with_ctx, out_chunks=None):
    nc, P, Dm, R = K.nc, K.P, K.dram, K.R
    LN8 = math.log(0.125)
    with contextlib.ExitStack() as es:
        qT, rqT = _tile(es, nc, "R_qT", [128, 2, T], BF16)
        kT, rkT = _tile(es, nc, "R_kT", [128, 2, T], BF16)
        Kt, rKt = _tile(es, nc, "R_Kt", [128, NT, 256], BF16)
        Vt, rVt = _tile(es, nc, "R_Vt", [128, NT, 256], BF16)
        SF, rSF = _tile(es, nc, "R_SF", [128, NT, 2, 64], BF16)
        ident, rident = _tile(es, nc, "R_ident", [128, 128], BF16)
        lg, rlg = _tile(es, nc, "R_lg", [128, 8], F32)
        cst, rcst = _tile(es, nc, "R_cst", [128, 5, 128], F32)
        pcol, rpcol = _tile(es, nc, "R_pcol", [128, 2], F32)
        lnc, rlnc = _tile(es, nc, "R_lnc", [128, 2], F32)
        tmp, rtmp = _tile(es, nc, "R_tmp", [128, 128], F32)
        DT, rDT = _tile(es, nc, "R_DT", [128, 4, 128], F32)
        QF, rQF = _tile(es, nc, "R_QF", [128, 2, 128], F32)
        QB, rQB = _tile(es, nc, "R_QB", [128, 2, 128], F32)
        KD, rKD = _tile(es, nc, "R_KD", [128, 8], F32)
        CF, rCF = _tile(es, nc, "R_CF", [128, 2, 64], F32)
        CB, rCB = _tile(es, nc, "R_CB", [128, 2, 64], F32)
        SM, rSM = _tile(es, nc, "R_SM", [128, 2, 64], F32)
        SBc = [_tile(es, nc, f"R_SBc{i}", [128, 2, 64], BF16) for i in range(2)]
        kw, rkw = _tile(es, nc, "R_kw", [128, 256], BF16)
        PT = [_tile(es, nc, f"R_PT{i}", [128, 4, 128], BF16) for i in range(2)]
        qf, rqf = _tile(es, nc, "R_qf", [128, 2, 128], BF16)
        qb, rqb = _tile(es, nc, "R_qb", [128, 2, 128], BF16)
        gt = [_tile(es, nc, f"R_g{i}", [128, 256], F32) for i in range(2)]
        sq, rsq = _tile(es, nc, "R_sq", [128, 256], F32)
        ss4, rss4 = _tile(es, nc, "R_ss4", [128, 4], F32)
        yf, ryf = _tile(es, nc, "R_yf", [128, 256], F32)
        ybf, rybf = _tile(es, nc, "R_ybf", [128, 256], BF16)
        oT, roT = _tile(es, nc, "R_oT", [128, 2, 128], BF16)
        Sp = [_tile(es, nc, f"R_Sp{i}", [128, 4, 128], F32, psum=True) for i in range(2)]
        Op = [_tile(es, nc, f"R_Op{i}", [128, 4, 64], F32, psum=True) for i in range(2)]
        KVp, rKVp = _tile(es, nc, "R_KVp", [128, 2, 128], F32, psum=True)
        pT2, rpT2 = _tile(es, nc, "R_pT2", [128, 2, 128], BF16, psum=True)

        for c2 in range(2):
            P.dma("sp", qT[:, c2, :], Dm["rqkT"][c2 * 128:(c2 + 1) * 128, :], r=[R["mix"]], w=[rqT])
            P.dma("sp", kT[:, c2, :], Dm["rqkT"][256 + c2 * 128:256 + (c2 + 1) * 128, :], r=[R["mix"]], w=[rkT])
        P.dma("act", Kt[:], Dm["rk"].rearrange("(c p) d -> p c d", p=128), r=[R["mix"]], w=[rKt])
        P.dma("act", Vt[:], Dm["rv"].rearrange("(c p) d -> p c d", p=128), r=[R["mix"]], w=[rVt])
        P.dma("pool", ident[:], Dm["ident"], w=[rident])
        for i, nm in enumerate(["ret_dpos", "ret_dneg", "ret_diag", "ret_tp1", "ret_tr"]):
            P.dma("sp", cst[:, i, :], Dm[nm], w=[rcst])
        P.dma("sp", pcol[:], Dm["ret_pcol"], w=[rpcol])
        P.dma("sp", lg[:], Dm["ret_log_decay"][l].rearrange("a h -> (a h)").partition_broadcast(128), w=[rlg])
        P.op("dve", lambda e: e.memset(lnc[:, 0:1], LN8), w=[rlnc])
        P.op("dve", lambda e: e.memset(lnc[:, 1:2], EPS), w=[rlnc])
        P.op("act", lambda e: e.activation(out=lg[:], in_=lg[:], func=AF.Exp), r=[rlg], w=[rlg])
        P.op("dve", lambda e: e.tensor_scalar(out=lg[:], in0=lg[:], scalar1=-1.0, scalar2=None, op0=ALU.mult), r=[rlg], w=[rlg])
        for h in range(4):
            pb, c2 = (h % 2) * 64, h // 2
            P.op("dve", lambda e: e.tensor_scalar(out=tmp[:], in0=cst[:, 0, :], scalar1=lg[:, h:h + 1], scalar2=None, op0=ALU.mult), r=[rcst, rlg], w=[rtmp])
            P.op("dve", lambda e: e.scalar_tensor_tensor(out=tmp[:], in0=cst[:, 1, :], scalar=lg[:, 4 + h:5 + h], in1=tmp[:], op0=ALU.mult, op1=ALU.add),
                 r=[rcst, rlg, rtmp], w=[rtmp])
            P.op("dve", lambda e: e.tensor_tensor(out=tmp[:], in0=tmp[:], in1=cst[:, 2, :], op=ALU.add), r=[rtmp, rcst], w=[rtmp])
            P.op("act", lambda e: e.activation(out=DT[:, h, :], in_=tmp[:], func=AF.Exp), r=[rtmp], w=[rDT])
            P.op("act", lambda e: e.activation(out=QF[pb:pb + 64, c2, :], in_=cst[pb:pb + 64, 3, :], func=AF.Exp, scale=lg[pb:pb + 64, h:h + 1], bias=lnc[pb:pb + 64, 0:1]),
                 r=[rcst, rlg, rlnc], w=[rQF])
            P.op("act", lambda e: e.activation(out=QB[pb:pb + 64, c2, :], in_=cst[pb:pb + 64, 4, :], func=AF.Exp, scale=lg[pb:pb + 64, 4 + h:5 + h], bias=lnc[pb:pb + 64, 0:1]),
                 r=[rcst, rlg, rlnc], w=[rQB])
            P.op("act", lambda e: e.activation(out=KD[:, h:h + 1], in_=lg[:, h:h + 1], func=AF.Exp, scale=pcol[:, 0:1]), r=[rlg, rpcol], w=[rKD])
            P.op("act", lambda e: e.activation(out=KD[:, 4 + h:5 + h], in_=lg[:, 4 + h:5 + h], func=AF.Exp, scale=pcol[:, 1:2]), r=[rlg, rpcol], w=[rKD])
            P.op("act", lambda e: e.activation(out=CF[pb:pb + 64, c2, :], in_=lg[pb:pb + 64, h:h + 1].to_broadcast([64, 64]), func=AF.Exp, scale=128.0), r=[rlg], w=[rCF])
            P.op("act", lambda e: e.activation(out=CB[pb:pb + 64, c2, :], in_=lg[pb:pb + 64, 4 + h:5 + h].to_broadcast([64, 64]), func=AF.Exp, scale=128.0), r=[rlg], w=[rCB])

        def state_update(n, kdoff, Ctab, rCtab):
            P.op("dve", lambda e: e.tensor_tensor(out=kw[:].rearrange("p (h d) -> p h d", d=64), in0=Kt[:, n, :].rearrange("p (h d) -> p h d", d=64),
                                                  in1=KD[:, kdoff:kdoff + 4].unsqueeze(2).to_broadcast([128, 4, 64]), op=ALU.mult), r=[rKt, rKD], w=[rkw])
            for c2 in range(2):
                P.op("pe", lambda e: e.matmul(KVp[:, c2, :], lhsT=kw[:, c2 * 128:(c2 + 1) * 128], rhs=Vt[:, n, c2 * 128:(c2 + 1) * 128], start=True, stop=True),
                     r=[rkw, rVt], w=[rKVp])
            P.op("dve", lambda e: e.tensor_tensor(out=SM[:], in0=SM[:], in1=Ctab[:], op=ALU.mult), r=[rSM, rCtab], w=[rSM])
            for hp in (0, 64):
                P.op("dve", lambda e: e.tensor_tensor(out=SM[hp:hp + 64, :, :], in0=SM[hp:hp + 64, :, :], in1=KVp[hp:hp + 64, :, hp:hp + 64], op=ALU.add),
                     r=[rSM, rKVp], w=[rSM])

        fo = [64, 65] + list(range(NXT))
        P.op("dve", lambda e: e.memset(SM[:], 0.0), w=[rSM])
        for i, n in enumerate(fo):
            P.op("act", lambda e: e.copy(out=SF[:, n, :, :], in_=SM[:]), r=[rSM], w=[rSF])
            if i + 1 < len(fo):
                state_update(n, 0, CF, rCF)

        bo = [65, 64] + list(range(NXT - 1, -1, -1))
        outs = [n for n in bo if (n < NXT or with_ctx)]
        if out_chunks is not None:
            outs = [n for n in outs if n in out_chunks]
        P.op("dve", lambda e: e.memset(SM[:], 0.0), w=[rSM])
        oi = {n: i for i, n in enumerate(outs)}

        def emit_S(n):
            i = oi[n]
            sp, rsp = Sp[i % 2]
            cs = slice(n * 128, (n + 1) * 128)
            for h in range(4):
                pb, c2 = (h % 2) * 64, h // 2
                P.op("pe", lambda e: e.matmul(sp[:, h, :], lhsT=kT[pb:pb + 64, c2, cs], rhs=qT[pb:pb + 64, c2, cs], start=True, stop=True), r=[rkT, rqT], w=[rsp])
            pt, rpt = PT[i % 2]
            P.op("dve", lambda e: e.tensor_tensor(out=pt[:], in0=sp, in1=DT[:], op=ALU.mult), r=[rsp, rDT], w=[rpt])
            g_, rg_ = gt[i % 2]
            P.dma("sp", g_[:], Dm["rg"][cs, :], r=[R["mix"]], w=[rg_])

        def emit_O(n, sbc, rsbc):
            i = oi[n]
            cs = slice(n * 128, (n + 1) * 128)
            pt, rpt = PT[i % 2]
            op_, rop = Op[i % 2]
            g_, rg_ = gt[i % 2]
            P.op("dve", lambda e: e.tensor_tensor(out=qf[:], in0=qT[:, :, cs], in1=QF[:], op=ALU.mult), r=[rqT, rQF], w=[rqf])
            P.op("pool", lambda e: e.tensor_tensor(out=qb[:], in0=qT[:, :, cs], in1=QB[:], op=ALU.mult), r=[rqT, rQB], w=[rqb])
            for h in range(4):
                pb, c2 = (h % 2) * 64, h // 2
                P.op("pe", lambda e: e.matmul(op_[:, h, :], lhsT=pt[:, h, :], rhs=Vt[:, n, h * 64:(h + 1) * 64], start=True, stop=False), r=[rpt, rVt], w=[rop])
                P.op("pe", lambda e: e.matmul(op_[:, h, :], lhsT=qf[pb:pb + 64, c2, :], rhs=SF[pb:pb + 64, n, c2, :], start=False, stop=False), r=[rqf, rSF], w=[rop])
                P.op("pe", lambda e: e.matmul(op_[:, h, :], lhsT=qb[pb:pb + 64, c2, :], rhs=sbc[pb:pb + 64, c2, :], start=False, stop=True), r=[rqb, rsbc], w=[rop])
            P.op("act", lambda e: e.activation(out=sq[:].rearrange("p (h d) -> p h d", d=64), in_=op_, func=AF.Square), r=[rop], w=[rsq])
            P.op("dve", lambda e: e.tensor_reduce(out=ss4[:], in_=sq[:].rearrange("p (h d) -> p h d", d=64), axis=AX.X, op=ALU.add), r=[rsq], w=[rss4])
            P.op("act", lambda e: e.activation(out=ss4[:], in_=ss4[:], func=AF.Sqrt, scale=1.0 / 64, bias=lnc[:, 1:2]), r=[rss4, rlnc], w=[rss4])
            P.op("dve", lambda e: e.reciprocal(out=ss4[:], in_=ss4[:]), r=[rss4], w=[rss4])
            P.op("dve", lambda e: e.tensor_tensor(out=yf[:].rearrange("p (h d) -> p h d", d=64), in0=op_, in1=ss4[:].unsqueeze(2).to_broadcast([128, 4, 64]), op=ALU.mult),
                 r=[rop, rss4], w=[ryf])
            P.op("dve", lambda e: e.tensor_tensor(out=ybf[:], in0=yf[:], in1=g_[:], op=ALU.mult), r=[ryf, rg_], w=[rybf])
            for k in range(2):
                P.op("pe", lambda e: e.transpose(out=pT2[:, k, :], in_=ybf[:, k * 128:(k + 1) * 128], identity=ident[:]), r=[rybf, rident], w=[rpT2])
            P.op("act", lambda e: e.copy(out=oT[:], in_=pT2), r=[rpT2], w=[roT])
            P.dma("pool", Dm["yT"][0:256, :].rearrange("(k p) n -> p k n", p=128)[:, :, cs], oT[:], r=[roT], w=[R["y"]])

        if outs:
            emit_S(outs[0])
        for bi, n in enumerate(bo):
            sbc, rsbc = SBc[bi % 2]
            if n in oi:
                P.op("act", lambda e: e.copy(out=sbc[:], in_=SM[:]), r=[rSM], w=[rsbc])
                i = oi[n]
                if i + 1 < len(outs):
                    emit_S(outs[i + 1])
                emit_O(n, sbc, rsbc)
            if bi + 1 < len(bo):
                state_update(n, 4, CB, rCB)
    P.barrier()


def _sin_reduced(P, out, in_, shift, t1, rt1, t2, rt2, r_in, w_out):
    i2p = 1.0 / (2 * math.pi)
    P.op("dve", lambda e: e.tensor_scalar(out=t1, in0=in_, scalar1=i2p, scalar2=shift * i2p, op0=ALU.mult, op1=ALU.add), r=r_in, w=[rt1])
    P.op("dve", lambda e: e.tensor_scalar(out=t2, in0=t1, scalar1=MAGIC, scalar2=None, op0=ALU.add), r=[rt1], w=[rt2])
    P.op("dve", lambda e: e.tensor_scalar(out=t2, in0=t2, scalar1=MAGIC, scalar2=None, op0=ALU.subtract), r=[rt2], w=[rt2])
    P.op("dve", lambda e: e.tensor_tensor(out=t1, in0=t1, in1=t2, op=ALU.subtract), r=[rt1, rt2], w=[rt1])
    P.op("act", lambda e: e.activation(out=out, in_=t1, func=AF.Sin, scale=2 * math.pi), r=[rt1], w=w_out)


def stage_s5(K, l, with_ctx, epi_tiles=None):
    nc, P, Dm, R = K.nc, K.P, K.dram, K.R
    TC = S5_TC
    NCH = T // TC
    with contextlib.ExitStack() as es:
        uT, ruT = _tile(es, nc, "S_uT", [128, 2, T], BF16)
        ident, rident = _tile(es, nc, "S_ident", [128, 128], BF16)
        identf, ridentf = _tile(es, nc, "S_identf", [128, 128], F32)
        BbT, rBbT = _tile(es, nc, "S_BbT", [128, 2, 2, 8, 128], BF16)
        Cm, rCm = _tile(es, nc, "S_Cm", [128, 4, 2, 128], BF16)
        RD, rRD = _tile(es, nc, "S_RD", [128, 2, 8], F32)
        TH, rTH = _tile(es, nc, "S_TH", [128, 2, 8], F32)
        COS, rCOS = _tile(es, nc, "S_COS", [128, 8, TC], F32)
        SIN, rSIN = _tile(es, nc, "S_SIN", [128, 8, TC], F32)
        iota1, riota1 = _tile(es, nc, "S_iota1", [128, TC], F32)
        P.dma("sp", uT[:, 0, :], Dm["suT"][0:128, :], r=[R["mix"]], w=[ruT])
        P.dma("sp", uT[:, 1, :], Dm["suT"][128:256, :], r=[R["mix"]], w=[ruT])
        P.dma("sp", identf[:], Dm["ident"], w=[ridentf])
        P.dma("pool", ident[:], Dm["ident"], w=[rident])
        P.dma("sp", iota1[:], Dm["s5_iota1"], w=[riota1])

        with contextlib.ExitStack() as es2:
            LRt, rLR = _tile(es2, nc, "S_LR", [128, 8], F32)
            LIt, rLI = _tile(es2, nc, "S_LI", [128, 8], F32)
            DTt, rDTt = _tile(es2, nc, "S_DT", [128, 8], F32)
            w8 = [_tile(es2, nc, f"S_w8_{i}", [128, 8], F32) for i in range(8)]
            BRt, rBRt = _tile(es2, nc, "S_BR", [128, 8, 16], F32)
            BIt, rBIt = _tile(es2, nc, "S_BI", [128, 8, 16], F32)
            bb = [_tile(es2, nc, f"S_bb{i}", [128, 8, 16], F32) for i in range(4)]
            Zp, rZp = _tile(es2, nc, "S_Zp", [128, 8, 128], F32)
            Cn, rCn = _tile(es2, nc, "S_Cn", [128, 128], F32)
            bdm, rbdm = _tile(es2, nc, "S_bdm", [128, 128], F32)
            tp, rtp = _tile(es2, nc, "S_tp", [128, 128], F32, psum=True)
            P.dma("sp", bdm[:], Dm["s5_bdmask"], w=[rbdm])
            with nc.allow_non_contiguous_dma(reason="small parameter tables"):
                P.dma("sp", BRt[:], Dm["s5_b_re"][l].rearrange("(gp a) p c -> (a p) gp c", a=2), w=[rBRt])
                P.dma("sp", BIt[:], Dm["s5_b_im"][l].rearrange("(gp a) p c -> (a p) gp c", a=2), w=[rBIt])
            for dirn in range(2):
                with nc.allow_non_contiguous_dma(reason="small parameter tables"):
                    P.dma("sp", LRt[:], Dm["s5_lambda_re"][l, dirn].rearrange("(gp a) p -> (a p) gp", a=2), w=[rLR])
                    P.dma("sp", LIt[:], Dm["s5_lambda_im"][l, dirn].rearrange("(gp a) p -> (a p) gp", a=2), w=[rLI])
                    for a in range(2):
                        src = Dm["s5_log_step"][l, dirn].rearrange("(gp a) -> a gp", a=2)[a].partition_broadcast(64)
                        P.dma("sp", DTt[a * 64:(a + 1) * 64, :], src, w=[rDTt])
                (dt_, rdt), (mag, rmag), (sn, rsn), (cs_, rcs), (t1, rt1), (t2, rt2), (cr, rcr), (ci, rci) = w8
                P.op("act", lambda e: e.activation(out=dt_[:], in_=DTt[:], func=AF.Exp), r=[rDTt], w=[rdt])
                P.op("dve", lambda e: e.tensor_tensor(out=mag[:], in0=LRt[:], in1=dt_[:], op=ALU.mult), r=[rLR, rdt], w=[rmag])
                P.op("act", lambda e: e.activation(out=RD[:, dirn, :], in_=mag[:], func=AF.Exp), r=[rmag], w=[rRD])
                P.op("dve", lambda e: e.tensor_tensor(out=TH[:, dirn, :], in0=LIt[:], in1=dt_[:], op=ALU.mult), r=[rLI, rdt], w=[rTH])
                _sin_reduced(P, sn[:], TH[:, dirn, :], 0.0, t1[:], rt1, t2[:], rt2, [rTH], [rsn])
                _sin_reduced(P, cs_[:], TH[:, dirn, :], math.pi / 2, t1[:], rt1, t2[:], rt2, [rTH], [rcs])
                P.op("dve", lambda e: e.tensor_tensor(out=cs_[:], in0=cs_[:], in1=RD[:, dirn, :], op=ALU.mult), r=[rcs, rRD], w=[rcs])
                P.op("dve", lambda e: e.tensor_tensor(out=sn[:], in0=sn[:], in1=RD[:, dirn, :], op=ALU.mult), r=[rsn, rRD], w=[rsn])
                P.op("dve", lambda e: e.tensor_scalar(out=cs_[:], in0=cs_[:], scalar1=-1.0, scalar2=None, op0=ALU.add), r=[rcs], w=[rcs])
                P.op("dve", lambda e: e.tensor_tensor(out=t1[:], in0=LRt[:], in1=LRt[:], op=ALU.mult), r=[rLR], w=[rt1])
                P.op("dve", lambda e: e.tensor_tensor(out=t2[:], in0=LIt[:], in1=LIt[:], op=ALU.mult), r=[rLI], w=[rt2])
                P.op("dve", lambda e: e.tensor_tensor(out=t1[:], in0=t1[:], in1=t2[:], op=ALU.add), r=[rt1, rt2], w=[rt1])
                P.op("dve", lambda e: e.reciprocal(out=t1[:], in_=t1[:]), r=[rt1], w=[rt1])
                P.op("dve", lambda e: e.tensor_tensor(out=cr[:], in0=cs_[:], in1=LRt[:], op=ALU.mult), r=[rcs, rLR], w=[rcr])
                P.op("dve", lambda e: e.tensor_tensor(out=t2[:], in0=sn[:], in1=LIt[:], op=ALU.mult), r=[rsn, rLI], w=[rt2])
                P.op("dve", lambda e: e.tensor_tensor(out=cr[:], in0=cr[:], in1=t2[:], op=ALU.add), r=[rcr, rt2], w=[rcr])
                P.op("dve", lambda e: e.tensor_tensor(out=cr[:], in0=cr[:], in1=t1[:], op=ALU.mult), r=[rcr, rt1], w=[rcr])
                P.op("dve", lambda e: e.tensor_tensor(out=ci[:], in0=sn[:], in1=LRt[:], op=ALU.mult), r=[rsn, rLR], w=[rci])
                P.op("dve", lambda e: e.tensor_tensor(out=t2[:], in0=cs_[:], in1=LIt[:], op=ALU.mult), r=[rcs, rLI], w=[rt2])
                P.op("dve", lambda e: e.tensor_tensor(out=ci[:], in0=ci[:], in1=t2[:], op=ALU.subtract), r=[rci, rt2], w=[rci])
                P.op("dve", lambda e: e.tensor_tensor(out=ci[:], in0=ci[:], in1=t1[:], op=ALU.mult), r=[rci, rt1], w=[rci])
                crb = cr[:].unsqueeze(2).to_broadcast([128, 8, 16])
                cib = ci[:].unsqueeze(2).to_broadcast([128, 8, 16])
                (b0, rb0), (b1, rb1), (b2, rb2), (b3, rb3) = bb
                P.op("dve", lambda e: e.tensor_tensor(out=b0[:], in0=BRt[:], in1=crb, op=ALU.mult), r=[rBRt, rcr], w=[rb0])
                P.op("dve", lambda e: e.tensor_tensor(out=b1[:], in0=BIt[:], in1=cib, op=ALU.mult), r=[rBIt, rci], w=[rb1])
                P.op("dve", lambda e: e.tensor_tensor(out=b0[:], in0=b0[:], in1=b1[:], op=ALU.subtract), r=[rb0, rb1], w=[rb0])
                P.op("dve", lambda e: e.tensor_tensor(out=b2[:], in0=BIt[:], in1=crb, op=ALU.mult), r=[rBIt, rcr], w=[rb2])
                P.op("dve", lambda e: e.tensor_tensor(out=b3[:], in0=BRt[:], in1=cib, op=ALU.mult), r=[rBRt, rci], w=[rb3])
                P.op("dve", lambda e: e.tensor_tensor(out=b2[:], in0=b2[:], in1=b3[:], op=ALU.add), r=[rb2, rb3], w=[rb2])
                for ri_, (bsrc, rbsrc) in enumerate(((b0, rb0), (b2, rb2))):
                    P.op("dve", lambda e: e.memset(Zp[:], 0.0), w=[rZp])
                    for gp in range(8):
                        for a in range(2):
                            col0 = ((2 * gp + a) % 8) * 16
                            P.op("dve", lambda e: e.tensor_copy(out=Zp[a * 64:(a + 1) * 64, gp, col0:col0 + 16], in_=bsrc[a * 64:(a + 1) * 64, gp, :]), r=[rbsrc], w=[rZp])
                    for gp in range(8):
                        P.op("pe", lambda e: e.transpose(out=tp, in_=Zp[:, gp, :], identity=identf[:]), r=[rZp, ridentf], w=[rtp])
                        P.op("act", lambda e: e.copy(out=BbT[:, dirn, ri_, gp, :], in_=tp), r=[rtp], w=[rBbT])
            for half in range(2):
                for src_name, kinds in (("s5_c_re", ((0, 1.0), (1, -1.0))), ("s5_c_im", ((2, -1.0),))):
                    srcv = Dm[src_name][l].rearrange("g co p -> (g co) p")[half * 128:(half + 1) * 128, :]
                    P.dma("sp", Cn[:, 0:64], srcv, w=[rCn])
                    P.dma("sp", Cn[:, 64:128], srcv, w=[rCn])
                    P.op("dve", lambda e: e.tensor_tensor(out=Cn[:], in0=Cn[:], in1=bdm[:], op=ALU.mult), r=[rCn, rbdm], w=[rCn])
                    P.op("pe", lambda e: e.transpose(out=tp, in_=Cn[:], identity=identf[:]), r=[rCn, ridentf], w=[rtp])
                    for kidx, sgn in kinds:
                        P.op("act", lambda e: e.activation(out=Cm[:, kidx, half, :], in_=tp, func=AF.Copy, scale=sgn), r=[rtp], w=[rCm])
        P.barrier()

        with contextlib.ExitStack() as es3:
            Bp = [[_tile(es3, nc, f"S_Bp{i}{j}", [128, TC], F32, psum=True) for j in range(2)] for i in range(2)]
            Yp = [[_tile(es3, nc, f"S_Yp{i}{j}", [128, 256], F32, psum=True) for j in range(2)] for i in range(2)]
            tq = [[_tile(es3, nc, f"S_tq{i}{j}", [128, TC], F32) for j in range(4)] for i in range(2)]
            bp_ = [[_tile(es3, nc, f"S_bp{i}{j}", [128, TC], F32) for j in range(2)] for i in range(2)]
            Wt = [[_tile(es3, nc, f"S_W{i}{j}", [128, TC], F32) for j in range(2)] for i in range(2)]
            Pr = [[_tile(es3, nc, f"S_Pr{i}{j}", [128, TC], BF16) for j in range(4)] for i in range(2)]
            XR, rXR = _tile(es3, nc, "S_XR", [128, 8], F32)
            XI, rXI = _tile(es3, nc, "S_XI", [128, 8], F32)
            tc1, rtc1 = _tile(es3, nc, "S_tc1", [128, 1], F32)
            ysb = [_tile(es3, nc, f"S_ysb{i}", [128, 2, 256], F32) for i in range(2)]
            ang, rang = _tile(es3, nc, "S_ang", [128, 8, TC], F32)
            at2, rat2 = _tile(es3, nc, "S_at2", [128, 8, TC], F32)
            for dirn in range(2):
                for gp in range(8):
                    P.op("dve", lambda e: e.tensor_scalar(out=ang[:, gp, :], in0=iota1[:], scalar1=TH[:, dirn, gp:gp + 1], scalar2=None, op0=ALU.mult), r=[riota1, rTH], w=[rang])
                _sin_reduced(P, SIN[:].rearrange("p a b -> p (a b)"), ang[:].rearrange("p a b -> p (a b)"), 0.0,
                             COS[:].rearrange("p a b -> p (a b)"), rCOS, at2[:].rearrange("p a b -> p (a b)"), rat2, [rang], [rSIN])
                _sin_reduced(P, COS[:].rearrange("p a b -> p (a b)"), ang[:].rearrange("p a b -> p (a b)"), math.pi / 2,
                             ang[:].rearrange("p a b -> p (a b)"), rang, at2[:].rearrange("p a b -> p (a b)"), rat2, [rang], [rCOS])
                P.op("dve", lambda e: e.memset(XR[:], 0.0), w=[rXR])
                P.op("dve", lambda e: e.memset(XI[:], 0.0), w=[rXI])
                ydst = Dm["s5yf"] if dirn == 0 else Dm["s5yb"]
                for ck in range(NCH):
                    if dirn == 0:
                        c0 = L if ck == 0 else (ck - 1) * TC
                    else:
                        c0 = L if ck == 0 else L - ck * TC
                    yp = Yp[ck % 2]
                    for gp in range(8):
                        par = gp % 2
                        ct = gp // 4
                        half, gl = gp // 4, gp % 4
                        usl = uT[:, ct, c0:c0 + TC]
                        if dirn == 1:
                            usl = usl[:, ::-1]
                        (bre, rbre), (bim, rbim) = Bp[par]
                        P.op("pe", lambda e: e.matmul(bre, lhsT=BbT[:, dirn, 0, gp, :], rhs=usl, start=True, stop=True), r=[rBbT, ruT], w=[rbre])
                        P.op("pe", lambda e: e.matmul(bim, lhsT=BbT[:, dirn, 1, gp, :], rhs=usl, start=True, stop=True), r=[rBbT, ruT], w=[rbim])
                        (q1, rq1), (q2, rq2), (q3, rq3), (q4, rq4) = tq[par]
                        cosg, sing = COS[:, gp, :], SIN[:, gp, :]
                        P.op("dve", lambda e: e.tensor_tensor(out=q1[:], in0=bre, in1=cosg, op=ALU.mult), r=[rbre, rCOS], w=[rq1])
                        P.op("dve", lambda e: e.tensor_tensor(out=q2[:], in0=bim, in1=sing, op=ALU.mult), r=[rbim, rSIN], w=[rq2])
                        P.op("dve", lambda e: e.tensor_tensor(out=q3[:], in0=bim, in1=cosg, op=ALU.mult), r=[rbim, rCOS], w=[rq3])
                        P.op("dve", lambda e: e.tensor_tensor(out=q4[:], in0=bre, in1=sing, op=ALU.mult), r=[rbre, rSIN], w=[rq4])
                        (br2, rbr2), (bi2, rbi2) = bp_[par]
                        P.op("pool", lambda e: e.tensor_tensor(out=br2[:], in0=q1[:], in1=q2[:], op=ALU.add), r=[rq1, rq2], w=[rbr2])
                        P.op("pool", lambda e: e.tensor_tensor(out=bi2[:], in0=q3[:], in1=q4[:], op=ALU.subtract), r=[rq3, rq4], w=[rbi2])
                        (wr, rwr), (wi, rwi) = Wt[par]
                        rdb = RD[:, dirn, gp:gp + 1].to_broadcast([128, TC])
                        P.op("dve", lambda e: e.tensor_tensor_scan(out=wr[:], data0=rdb, data1=br2[:], initial=XR[:, gp:gp + 1], op0=ALU.mult, op1=ALU.add),
                             r=[rRD, rbr2, rXR], w=[rwr])
                        P.op("dve", lambda e: e.tensor_tensor_scan(out=wi[:], data0=rdb, data1=bi2[:], initial=XI[:, gp:gp + 1], op0=ALU.mult, op1=ALU.add),
                             r=[rRD, rbi2, rXI], w=[rwi])
                        cl, sl = COS[:, gp, TC - 1:TC], SIN[:, gp, TC - 1:TC]
                        P.op("dve", lambda e: e.tensor_tensor(out=tc1[:], in0=wi[:, TC - 1:TC], in1=sl, op=ALU.mult), r=[rwi, rSIN], w=[rtc1])
                        P.op("dve", lambda e: e.scalar_tensor_tensor(out=XR[:, gp:gp + 1], in0=wr[:, TC - 1:TC], scalar=cl, in1=tc1[:], op0=ALU.mult, op1=ALU.subtract),
                             r=[rwr, rCOS, rtc1], w=[rXR])
                        P.op("dve", lambda e: e.tensor_tensor(out=tc1[:], in0=wi[:, TC - 1:TC], in1=cl, op=ALU.mult), r=[rwi, rCOS], w=[rtc1])
                        P.op("dve", lambda e: e.scalar_tensor_tensor(out=XI[:, gp:gp + 1], in0=wr[:, TC - 1:TC], scalar=sl, in1=tc1[:], op0=ALU.mult, op1=ALU.add),
                             r=[rwr, rSIN, rtc1], w=[rXI])
                        (pcc, rpcc), (pis, rpis), (prs, rprs), (pic, rpic) = Pr[par]
                        ov = (lambda t_: t_[:, ::-1]) if dirn == 1 else (lambda t_: t_[:])
                        P.op("dve", lambda e: e.tensor_tensor(out=ov(pcc), in0=wr[:], in1=cosg, op=ALU.mult), r=[rwr, rCOS], w=[rpcc])
                        P.op("pool", lambda e: e.tensor_tensor(out=ov(pis), in0=wi[:], in1=sing, op=ALU.mult), r=[rwi, rSIN], w=[rpis])
                        P.op("dve", lambda e: e.tensor_tensor(out=ov(prs), in0=wr[:], in1=sing, op=ALU.mult), r=[rwr, rSIN], w=[rprs])
                        P.op("pool", lambda e: e.tensor_tensor(out=ov(pic), in0=wi[:], in1=cosg, op=ALU.mult), r=[rwi, rCOS], w=[rpic])
                        for sub in range(TC // 128):
                            ypt, rypt = yp[sub]
                            osl = ypt[:, gp * 32:(gp + 1) * 32]
                            cs_sl = slice(gl * 32, (gl + 1) * 32)
                            ssl = slice(sub * 128, (sub + 1) * 128)
                            P.op("pe", lambda e: e.matmul(osl, lhsT=pcc[:, ssl], rhs=Cm[:, 0, half, cs_sl], start=True, stop=False), r=[rpcc, rCm], w=[rypt])
                            P.op("pe", lambda e: e.matmul(osl, lhsT=pis[:, ssl], rhs=Cm[:, 1, half, cs_sl], start=False, stop=False), r=[rpis, rCm], w=[rypt])
                            P.op("pe", lambda e: e.matmul(osl, lhsT=prs[:, ssl], rhs=Cm[:, 2, half, cs_sl], start=False, stop=False), r=[rprs, rCm], w=[rypt])
                            P.op("pe", lambda e: e.matmul(osl, lhsT=pic[:, ssl], rhs=Cm[:, 2, half, cs_sl], start=False, stop=True), r=[rpic, rCm], w=[rypt])
                    ys, rys = ysb[ck % 2]
                    for sub in range(TC // 128):
                        ypt, rypt = yp[sub]
                        P.op("act", lambda e: e.copy(out=ys[:, sub, :], in_=ypt), r=[rypt], w=[rys])
                    P.dma("sp", ydst[c0:c0 + TC, :].rearrange("(s p) d -> p s d", p=128), ys[:], r=[rys], w=[R["s5y"]])
        P.barrier()

        with contextlib.ExitStack() as es4:
            dsk, rdsk = _tile(es4, nc, "S_dsk", [128, 256], F32)
            glb, rglb = _tile(es4, nc, "S_glb", [128, 256], F32)
            glw, rglw = _tile(es4, nc, "S_glw", [128, 2, 256], BF16)
            ut = [_tile(es4, nc, f"S_ut{i}", [128, 3, 256], F32) for i in range(2)]
            z, rz = _tile(es4, nc, "S_z", [128, 256], F32)
            zg, rzg = _tile(es4, nc, "S_zg", [128, 256], F32)
            zgb, rzgb = _tile(es4, nc, "S_zgb", [128, 256], BF16)
            zT, rzT = _tile(es4, nc, "S_zT", [128, 2, 128], BF16)
            sg, rsg = _tile(es4, nc, "S_sg", [128, 256], F32)
            ob, rob = _tile(es4, nc, "S_ob", [128, 256], BF16)
            oT, roT = _tile(es4, nc, "S_oT", [128, 2, 128], BF16)
            pT2, rpT2 = _tile(es4, nc, "S_pT2", [128, 2, 128], BF16, psum=True)
            pT3, rpT3 = _tile(es4, nc, "S_pT3", [128, 2, 128], BF16, psum=True)
            gp_, rgp_ = _tile(es4, nc, "S_gps", [128, 256], F32, psum=True)
            P.dma("sp", dsk[:], Dm["s5_d"][l].partition_broadcast(128), w=[rdsk])
            P.dma("sp", glb[:], Dm["s5_glu_b"][l].partition_broadcast(128), w=[rglb])
            P.dma("pool", glw[:], Dm["s5_glu_w"][l].rearrange("(k p) n -> p k n", p=128), w=[rglw])
            tiles = list(range(NXT)) + ([64, 65] if with_ctx else [])
            if epi_tiles is not None:
                tiles = [t for t in tiles if t in epi_tiles]

            def load(i):
                t = tiles[i]
                u_, ru_ = ut[i % 2]
                ts = slice(t * 128, (t + 1) * 128)
                P.dma("sp", u_[:, 0, :], Dm["su"][ts, :], r=[R["mix"]], w=[ru_])
                P.dma("sp", u_[:, 1, :], Dm["s5yf"][ts, :], r=[R["s5y"]], w=[ru_])
                P.dma("sp", u_[:, 2, :], Dm["s5yb"][ts, :], r=[R["s5y"]], w=[ru_])
            if tiles:
                load(0)
            for i, t in enumerate(tiles):
                if i + 1 < len(tiles):
                    load(i + 1)
                u_, ru_ = ut[i % 2]
                ts = slice(t * 128, (t + 1) * 128)
                P.op("dve", lambda e: e.tensor_tensor(out=z[:], in0=u_[:, 0, :], in1=dsk[:], op=ALU.mult), r=[ru_, rdsk], w=[rz])
                P.op("dve", lambda e: e.tensor_tensor(out=z[:], in0=z[:], in1=u_[:, 1, :], op=ALU.add), r=[rz, ru_], w=[rz])
                P.op("dve", lambda e: e.tensor_tensor(out=z[:], in0=z[:], in1=u_[:, 2, :], op=ALU.add), r=[rz, ru_], w=[rz])
                P.op("act", lambda e: e.activation(out=zg[:], in_=z[:], func=AF.Gelu), r=[rz], w=[rzg])
                P.op("dve", lambda e: e.tensor_copy(out=zgb[:], in_=zg[:]), r=[rzg], w=[rzgb])
                for k in range(2):
                    P.op("pe", lambda e: e.transpose(out=pT2[:, k, :], in_=zgb[:, k * 128:(k + 1) * 128], identity=ident[:]), r=[rzgb, rident], w=[rpT2])
                P.op("act", lambda e: e.copy(out=zT[:], in_=pT2), r=[rpT2], w=[rzT])
                for k in range(2):
                    P.op("pe", lambda e: e.matmul(gp_, lhsT=zT[:, k, :], rhs=glw[:, k, :], start=(k == 0), stop=(k == 1)), r=[rzT, rglw], w=[rgp_])
                P.op("dve", lambda e: e.tensor_tensor(out=sg[:], in0=gp_, in1=glb[:], op=ALU.add), r=[rgp_, rglb], w=[rsg])
                P.op("act", lambda e: e.activation(out=sg[:], in_=sg[:], func=AF.Sigmoid), r=[rsg], w=[rsg])
                P.op("dve", lambda e: e.tensor_tensor(out=ob[:], in0=sg[:], in1=zg[:], op=ALU.mult), r=[rsg, rzg], w=[rob])
                for k in range(2):
                    P.op("pe", lambda e: e.transpose(out=pT3[:, k, :], in_=ob[:, k * 128:(k + 1) * 128], identity=ident[:]), r=[rob, rident], w=[rpT3])
                P.op("act", lambda e: e.copy(out=oT[:], in_=pT3), r=[rpT3], w=[roT])
                P.dma("pool", Dm["yT"][512:768, :].rearrange("(k p) n -> p k n", p=128)[:, :, ts], oT[:], r=[roT], w=[R["y"]])
    P.barrier()


SCRATCH.update({"s5yf": ([T, 256], F32), "s5yb": ([T, 256], F32)})


def stage_merge(K, l, with_ctx, tiles=None):
    nc, P, Dm, R = K.nc, K.P, K.dram, K.R
    with contextlib.ExitStack() as es:
        wg, rwg = _tile(es, nc, "M_wg", [128, 8, 4096], BF16)
        wb, rwb = _tile(es, nc, "M_wb", [128, 8, 1024], BF16)
        wo, rwo = _tile(es, nc, "M_wo", [128, 8, 1024], BF16)
        m2, rm2 = _tile(es, nc, "M_m2", [128, 2, 1024], F32)
        ident, rident = _tile(es, nc, "M_ident", [128, 128], BF16)
        yTt = [_tile(es, nc, f"M_yT{i}", [128, 8, 128], BF16) for i in range(2)]
        aTt = [_tile(es, nc, f"M_aT{i}", [128, 8, 128], BF16) for i in range(2)]
        ht = [_tile(es, nc, f"M_h{i}", [128, 1024], F32) for i in range(2)]
        sig = [_tile(es, nc, f"M_sig{i}", [128, 512], F32) for i in range(2)]
        term, rterm = _tile(es, nc, "M_term", [128, 512], F32)
        mg, rmg = _tile(es, nc, "M_mg", [128, 1024], F32)
        mb, rmb = _tile(es, nc, "M_mb", [128, 1024], BF16)
        mT, rmT = _tile(es, nc, "M_mT", [128, 8, 128], BF16)
        hn, rhn = _tile(es, nc, "M_hn", [128, 1024], F32)
        Gp = [_tile(es, nc, f"M_Gp{i}", [128, 512], F32, psum=True) for i in range(2)]
        Zp = [_tile(es, nc, f"M_Zp{i}", [128, 512], F32, psum=True) for i in range(2)]
        pT, rpT = _tile(es, nc, "M_pT", [128, 8, 128], BF16, psum=True)
        Op = [_tile(es, nc, f"M_Op{i}", [128, 512], F32, psum=True) for i in range(2)]
        w_in_v = Dm["w_in"][l].rearrange("(k p) n -> p k n", p=128)
        for i in range(4):
            P.dma("pool", wg[:, :, i * 1024:(i + 1) * 1024], w_in_v[:, :, MIXC + i * 1024:MIXC + (i + 1) * 1024], w=[rwg])
        P.dma("pool", wb[:], Dm["w_branch"][l].rearrange("i (k p) n -> p (i k) n", p=128), w=[rwb])
        P.dma("pool", wo[:], Dm["w_out"][l].rearrange("(k p) n -> p k n", p=128), w=[rwo])
        P.dma("pool", ident[:], Dm["ident"], w=[rident])
        P.dma("sp", m2[:, 0, :], Dm["modv"][2].partition_broadcast(128), r=[R["modv"]], w=[rm2])
        P.dma("sp", m2[:, 1, :], Dm["modv"][8].partition_broadcast(128), r=[R["modv"]], w=[rm2])
        tiles = tiles if tiles is not None else list(range(NXT)) + ([64, 65] if with_ctx else [])
        hsrc = K.hsrc(l)

        def load(i):
            t = tiles[i]
            ts = slice(t * 128, (t + 1) * 128)
            P.dma("sp", yTt[i % 2][0][:], Dm["yT"].rearrange("(k p) n -> p k n", p=128)[:, :, ts], r=[R["y"]], w=[yTt[i % 2][1]])
            P.dma("sp", aTt[i % 2][0][:], Dm["aT"].rearrange("(k p) n -> p k n", p=128)[:, :, ts], r=[R["aT"]], w=[aTt[i % 2][1]])
            P.dma("sp", ht[i % 2][0][:], hsrc(t), r=[R["H"]], w=[ht[i % 2][1]])
        load(0)
        for i, t in enumerate(tiles):
            if i + 1 < len(tiles):
                load(i + 1)
            yt, ryt = yTt[i % 2]
            at, rat = aTt[i % 2]
            h_, rh_ = ht[i % 2]
            ts = slice(t * 128, (t + 1) * 128)
            cnt = 0
            for nh in range(2):
                for br in range(4):
                    gp, rgp = Gp[cnt % 2]
                    zp, rzp = Zp[cnt % 2]
                    sg, rsg = sig[cnt % 2]
                    cnt += 1
                    c0 = br * 1024 + nh * 512
                    for k in range(8):
                        P.op("pe", lambda e: e.matmul(gp, lhsT=at[:, k, :], rhs=wg[:, k, c0:c0 + 512], start=(k == 0), stop=(k == 7)), r=[rat, rwg], w=[rgp])
                    for k2 in range(2):
                        P.op("pe", lambda e: e.matmul(zp, lhsT=yt[:, 2 * br + k2, :], rhs=wb[:, 2 * br + k2, nh * 512:(nh + 1) * 512], start=(k2 == 0), stop=(k2 == 1)),
                             r=[ryt, rwb], w=[rzp])
                    P.op("act", lambda e: e.activation(out=sg[:], in_=gp, func=AF.Sigmoid), r=[rgp], w=[rsg])
                    dst = mg[:, nh * 512:(nh + 1) * 512]
                    if br == 0:
                        P.op("dve", lambda e: e.tensor_tensor(out=dst, in0=zp, in1=sg[:], op=ALU.mult), r=[rzp, rsg], w=[rmg])
                    else:
                        P.op("dve", lambda e: e.tensor_tensor(out=term[:], in0=zp, in1=sg[:], op=ALU.mult), r=[rzp, rsg], w=[rterm])
                        P.op("dve", lambda e: e.tensor_tensor(out=dst, in0=dst, in1=term[:], op=ALU.add), r=[rmg, rterm], w=[rmg])
            P.op("act", lambda e: e.copy(out=mb[:], in_=mg[:]), r=[rmg], w=[rmb])
            for k in range(8):
                P.op("pe", lambda e: e.transpose(out=pT[:, k, :], in_=mb[:, k * 128:(k + 1) * 128], identity=ident[:]), r=[rmb, rident], w=[rpT])
            P.op("act", lambda e: e.copy(out=mT[:], in_=pT), r=[rpT], w=[rmT])
            for nh in range(2):
                op_, rop = Op[nh]
                for k in range(8):
                    P.op("pe", lambda e: e.matmul(op_, lhsT=mT[:, k, :], rhs=wo[:, k, nh * 512:(nh + 1) * 512], start=(k == 0), stop=(k == 7)), r=[rmT, rwo], w=[rop])
                sl = slice(nh * 512, (nh + 1) * 512)
                P.op("dve", lambda e: e.tensor_tensor(out=hn[:, sl], in0=op_, in1=m2[:, 0 if t < NXT else 1, sl], op=ALU.mult), r=[rop, rm2], w=[rhn])
            P.op("dve", lambda e: e.tensor_tensor(out=hn[:], in0=hn[:], in1=h_[:], op=ALU.add), r=[rhn, rh_], w=[rhn])
            P.dma("pool", Dm["H"][ts, :], hn[:], r=[rhn], w=[R["H"]])
    P.barrier()


SGT = 12
BIG = 1.0e30


def stage_moe(K, l, with_ctx, last, tiles=None, experts=None):
    nc, P, Dm, R = K.nc, K.P, K.dram, K.R
    tiles = tiles if tiles is not None else list(range(NXT)) + ([64, 65] if with_ctx else [])
    experts = list(range(32)) if experts is None else experts
    with contextlib.ExitStack() as es:
        bc, rbc = _tile(es, nc, "E_bc", [128, 6, 1024], F32)
        identf, ridentf = _tile(es, nc, "E_identf", [128, 128], F32)
        rw, rrw = _tile(es, nc, "E_rw", [128, 8, 36], F32)
        rb, rrb = _tile(es, nc, "E_rb", [128, 36], F32)
        epsc, repsc = _tile(es, nc, "E_eps", [128, 1], F32)
        FT, rFT = _tile(es, nc, "E_FT", [128, 8, SGT * 128], BF16)
        Gall, rGall = _tile(es, nc, "E_G", [128, SGT, 32], F32)
        yacc, ryacc = _tile(es, nc, "E_yacc", [128, SGT, 1024], F32)
        for j, row in enumerate((3, 4, 5, 9, 10, 11)):
            P.dma("sp", bc[:, j, :], Dm["modv"][row].partition_broadcast(128), r=[R["modv"]], w=[rbc])
        P.dma("sp", identf[:], Dm["ident"], w=[ridentf])
        with nc.allow_non_contiguous_dma(reason="tiny router weights"):
            P.dma("sp", rw[:, :, 0:4], Dm["router_w1"][l].rearrange("(k p) n -> p k n", p=128), w=[rrw])
            P.dma("sp", rw[:, :, 4:36], Dm["router_w2"][l].rearrange("(k p) n -> p k n", p=128), w=[rrw])
        P.dma("sp", rb[:, 0:4], Dm["router_b1"][l].partition_broadcast(128), w=[rrb])
        P.dma("sp", rb[:, 4:36], Dm["router_b2"][l].partition_broadcast(128), w=[rrb])
        P.op("dve", lambda e: e.memset(epsc[:], EPS), w=[repsc])
        P.barrier()
        sgs = [tiles[i:i + SGT] for i in range(0, len(tiles), SGT)]
        for sg in sgs:
            with contextlib.ExitStack() as e1:
                ht = [_tile(e1, nc, f"E1_h{i}", [128, 1024], F32) for i in range(2)]
                junk, rjunk = _tile(e1, nc, "E1_junk", [128, 1024], BF16)
                ssq, rssq = _tile(e1, nc, "E1_ssq", [128, 1], F32)
                rstd, rrstd = _tile(e1, nc, "E1_rstd", [128, 1], F32)
                f_, rf_ = _tile(e1, nc, "E1_f", [128, 1024], F32)
                lgt, rlgt = _tile(e1, nc, "E1_lg", [128, 36], F32)
                sm = [_tile(e1, nc, f"E1_s{i}", [128, 8], F32) for i in range(4)]
                oh = [_tile(e1, nc, f"E1_oh{i}", [128, 32], F32) for i in range(3)]
                pTf = [_tile(e1, nc, f"E1_pT{i}", [128, 4, 128], F32, psum=True) for i in range(2)]
                lp, rlp = _tile(e1, nc, "E1_lp", [128, 36], F32, psum=True)

                def load(i):
                    t = sg[i]
                    P.dma("sp", ht[i % 2][0][:], Dm["H"][t * 128:(t + 1) * 128, :], r=[R["H"]], w=[ht[i % 2][1]])
                load(0)
                for i, t in enumerate(sg):
                    if i + 1 < len(sg):
                        load(i + 1)
                    h_, rh_ = ht[i % 2]
                    o = 0 if t < NXT else 3
                    P.op("act", lambda e: e.activation(out=junk[:], in_=h_[:], func=AF.Square, accum_out=ssq[:]), r=[rh_], w=[rjunk, rssq])
                    P.op("act", lambda e: e.activation(out=rstd[:], in_=ssq[:], func=AF.Sqrt, scale=1.0 / D, bias=epsc[:, 0:1]), r=[rssq, repsc], w=[rrstd])
                    P.op("dve", lambda e: e.reciprocal(out=rstd[:], in_=rstd[:]), r=[rrstd], w=[rrstd])
                    P.op("dve", lambda e: e.scalar_tensor_tensor(out=f_[:], in0=h_[:], scalar=rstd[:, 0:1], in1=bc[:, o, :], op0=ALU.mult, op1=ALU.mult),
                         r=[rh_, rrstd, rbc], w=[rf_])
                    P.op("dve", lambda e: e.tensor_tensor(out=f_[:], in0=f_[:], in1=bc[:, o + 1, :], op=ALU.add), r=[rf_, rbc], w=[rf_])
                    for hf in range(2):
                        pt, rpt = pTf[hf]
                        for k in range(4):
                            kk = hf * 4 + k
                            P.op("pe", lambda e: e.transpose(out=pt[:, k, :], in_=f_[:, kk * 128:(kk + 1) * 128], identity=identf[:]), r=[rf_, ridentf], w=[rpt])
                    fTf, rfTf = f_, rf_
                    for hf in range(2):
                        pt, rpt = pTf[hf]
                        P.op("act", lambda e: e.copy(out=fTf[:, hf * 512:(hf + 1) * 512].rearrange("p (k n) -> p k n", n=128), in_=pt), r=[rpt], w=[rfTf])
                    P.op("dve", lambda e: e.tensor_copy(out=FT[:, :, i * 128:(i + 1) * 128], in_=fTf[:].rearrange("p (k n) -> p k n", n=128)), r=[rfTf], w=[rFT])
                    for k in range(8):
                        P.op("pe", lambda e: e.matmul(lp, lhsT=fTf[:, k * 128:(k + 1) * 128], rhs=rw[:, k, :], start=(k == 0), stop=(k == 7)), r=[rfTf, rrw], w=[rlp])
                    P.op("dve", lambda e: e.tensor_tensor(out=lgt[:], in0=lp, in1=rb[:], op=ALU.add), r=[rlp, rrb], w=[rlgt])
                    (s0, rs0), (s1, rs1), (s2, rs2), (s3, rs3) = sm
                    (oh1, roh1), (oh2, roh2), (l2, rl2) = oh
                    P.op("dve", lambda e: e.tensor_reduce(out=s0[:, 0:1], in_=lgt[:, 0:4], axis=AX.X, op=ALU.max), r=[rlgt], w=[rs0])
                    P.op("dve", lambda e: e.tensor_scalar(out=s0[:, 1:2], in0=s0[:, 0:1], scalar1=-1.0, scalar2=None, op0=ALU.mult), r=[rs0], w=[rs0])
                    P.op("act", lambda e: e.activation(out=s1[:, 0:4], in_=lgt[:, 0:4], func=AF.Exp, bias=s0[:, 1:2], accum_out=s0[:, 2:3]), r=[rlgt, rs0], w=[rs1, rs0])
                    P.op("dve", lambda e: e.reciprocal(out=s0[:, 3:4], in_=s0[:, 2:3]), r=[rs0], w=[rs0])
                    P.op("dve", lambda e: e.tensor_scalar(out=s2[:, 0:4], in0=lgt[:, 0:4], scalar1=s0[:, 0:1], scalar2=None, op0=ALU.is_equal), r=[rlgt, rs0], w=[rs2])
                    P.op("dve", lambda e: e.tensor_scalar(out=s2[:, 0:4], in0=s2[:, 0:4], scalar1=BIG, scalar2=-BIG, op0=ALU.mult, op1=ALU.add), r=[rs2], w=[rs2])
                    P.op("dve", lambda e: e.tensor_tensor(out=l2[:].rearrange("p (g e) -> p g e", e=8), in0=lgt[:, 4:36].rearrange("p (g e) -> p g e", e=8),
                                                          in1=s2[:, 0:4].unsqueeze(2).to_broadcast([128, 4, 8]), op=ALU.add), r=[rlgt, rs2], w=[rl2])
                    P.op("dve", lambda e: e.tensor_reduce(out=s3[:, 0:1], in_=l2[:], axis=AX.X, op=ALU.max), r=[rl2], w=[rs3])
                    P.op("dve", lambda e: e.tensor_scalar(out=oh1[:], in0=l2[:], scalar1=s3[:, 0:1], scalar2=None, op0=ALU.is_equal), r=[rl2, rs3], w=[roh1])
                    P.op("dve", lambda e: e.scalar_tensor_tensor(out=l2[:], in0=oh1[:], scalar=-BIG, in1=l2[:], op0=ALU.mult, op1=ALU.add), r=[roh1, rl2], w=[rl2])
                    P.op("dve", lambda e: e.tensor_reduce(out=s3[:, 1:2], in_=l2[:], axis=AX.X, op=ALU.max), r=[rl2], w=[rs3])
                    P.op("dve", lambda e: e.tensor_scalar(out=oh2[:], in0=l2[:], scalar1=s3[:, 1:2], scalar2=None, op0=ALU.is_equal), r=[rl2, rs3], w=[roh2])
                    P.op("dve", lambda e: e.tensor_tensor(out=s3[:, 2:3], in0=s3[:, 1:2], in1=s3[:, 0:1], op=ALU.subtract), r=[rs3], w=[rs3])
                    P.op("act", lambda e: e.activation(out=s3[:, 3:4], in_=s3[:, 2:3], func=AF.Exp), r=[rs3], w=[rs3])
                    P.op("dve", lambda e: e.tensor_scalar(out=s3[:, 4:5], in0=s3[:, 3:4], scalar1=1.0, scalar2=None, op0=ALU.add), r=[rs3], w=[rs3])
                    P.op("dve", lambda e: e.reciprocal(out=s3[:, 4:5], in_=s3[:, 4:5]), r=[rs3], w=[rs3])
                    P.op("dve", lambda e: e.tensor_tensor(out=s3[:, 5:6], in0=s3[:, 4:5], in1=s0[:, 3:4], op=ALU.mult), r=[rs3, rs0], w=[rs3])
                    P.op("dve", lambda e: e.tensor_tensor(out=s3[:, 6:7], in0=s3[:, 5:6], in1=s3[:, 3:4], op=ALU.mult), r=[rs3], w=[rs3])
                    P.op("dve", lambda e: e.tensor_scalar(out=Gall[:, i, :], in0=oh1[:], scalar1=s3[:, 5:6], scalar2=None, op0=ALU.mult), r=[roh1, rs3], w=[rGall])
                    P.op("dve", lambda e: e.scalar_tensor_tensor(out=Gall[:, i, :], in0=oh2[:], scalar=s3[:, 6:7], in1=Gall[:, i, :], op0=ALU.mult, op1=ALU.add),
                         r=[roh2, rs3, rGall], w=[rGall])
            P.barrier()
            with contextlib.ExitStack() as e2:
                w1 = [_tile(e2, nc, f"E2_w1_{i}", [128, 8, 512], BF16) for i in range(2)]
                w3 = [_tile(e2, nc, f"E2_w3_{i}", [128, 8, 512], BF16) for i in range(2)]
                w2 = [_tile(e2, nc, f"E2_w2_{i}", [128, 4, 1024], BF16) for i in range(2)]
                sl = [_tile(e2, nc, f"E2_sl{i}", [128, 512], F32) for i in range(2)]
                hid = [_tile(e2, nc, f"E2_hid{i}", [128, 4, 512], BF16) for i in range(2)]
                H1 = [_tile(e2, nc, f"E2_H1{i}", [128, 512], F32, psum=True) for i in range(2)]
                H3 = [_tile(e2, nc, f"E2_H3{i}", [128, 512], F32, psum=True) for i in range(2)]
                Yp = [[_tile(e2, nc, f"E2_Y{i}{j}", [128, 512], F32, psum=True) for j in range(2)] for i in range(2)]
                groups = [list(range(g0, min(g0 + 4, len(sg)))) for g0 in range(0, len(sg), 4)]

                def loadw(ei):
                    e_ = experts[ei]
                    P.dma("pool", w1[ei % 2][0][:], Dm["exp_w1"][l, e_].rearrange("(k p) n -> p k n", p=128), w=[w1[ei % 2][1]])
                    P.dma("pool", w3[ei % 2][0][:], Dm["exp_w3"][l, e_].rearrange("(k p) n -> p k n", p=128), w=[w3[ei % 2][1]])
                    P.dma("pool", w2[ei % 2][0][:], Dm["exp_w2"][l, e_].rearrange("(k p) n -> p k n", p=128), w=[w2[ei % 2][1]])
                loadw(0)
                cnt = 0
                ycnt = 0
                gi_ = 0
                for ei, e_ in enumerate(experts):
                    if ei + 1 < len(experts):
                        loadw(ei + 1)
                    (w1t, rw1), (w3t, rw3), (w2t, rw2) = w1[ei % 2], w3[ei % 2], w2[ei % 2]
                    for grp in groups:
                        ntok = len(grp) * 128
                        tsl = slice(grp[0] * 128, grp[0] * 128 + ntok)
                        hd, rhd = hid[gi_ % 2]
                        gi_ += 1
                        for fc in range(4):
                            h1, rh1 = H1[cnt % 2]
                            h3, rh3 = H3[cnt % 2]
                            s_, rs_ = sl[cnt % 2]
                            cnt += 1
                            for k in range(8):
                                P.op("pe", lambda e: e.matmul(h1[:, 0:ntok], lhsT=w1t[:, k, fc * 128:(fc + 1) * 128], rhs=FT[:, k, tsl], start=(k == 0), stop=(k == 7)),
                                     r=[rw1, rFT], w=[rh1])
                            for k in range(8):
                                P.op("pe", lambda e: e.matmul(h3[:, 0:ntok], lhsT=w3t[:, k, fc * 128:(fc + 1) * 128], rhs=FT[:, k, tsl], start=(k == 0), stop=(k == 7)),
                                     r=[rw3, rFT], w=[rh3])
                            P.op("act", lambda e: e.activation(out=s_[:, 0:ntok], in_=h1[:, 0:ntok], func=AF.Silu), r=[rh1], w=[rs_])
                            P.op("dve", lambda e: e.tensor_tensor(out=hd[:, fc, 0:ntok], in0=h3[:, 0:ntok], in1=s_[:, 0:ntok], op=ALU.mult), r=[rh3, rs_], w=[rhd])
                        for ti, tt in enumerate(grp):
                            yp = Yp[ycnt % 2]
                            ycnt += 1
                            for nh in range(2):
                                ypt, rypt = yp[nh]
                                for fc in range(4):
                                    P.op("pe", lambda e: e.matmul(ypt, lhsT=hd[:, fc, ti * 128:(ti + 1) * 128], rhs=w2t[:, fc, nh * 512:(nh + 1) * 512], start=(fc == 0), stop=(fc == 3)),
                                         r=[rhd, rw2], w=[rypt])
                                dst = yacc[:, tt, nh * 512:(nh + 1) * 512]
                                if ei == 0:
                                    P.op("dve", lambda e: e.tensor_scalar(out=dst, in0=ypt, scalar1=Gall[:, tt, e_:e_ + 1], scalar2=None, op0=ALU.mult), r=[rypt, rGall], w=[ryacc])
                                else:
                                    P.op("dve", lambda e: e.scalar_tensor_tensor(out=dst, in0=ypt, scalar=Gall[:, tt, e_:e_ + 1], in1=dst, op0=ALU.mult, op1=ALU.add),
                                         r=[rypt, rGall, ryacc], w=[ryacc])
            P.barrier()
            with contextlib.ExitStack() as e3:
                ht = [_tile(e3, nc, f"E3_h{i}", [128, 1024], F32) for i in range(2)]
                hn = [_tile(e3, nc, f"E3_hn{i}", [128, 1024], F32) for i in range(2)]
                for i, t in enumerate(sg):
                    h_, rh_ = ht[i % 2]
                    n_, rn_ = hn[i % 2]
                    ts = slice(t * 128, (t + 1) * 128)
                    P.dma("sp", h_[:], Dm["H"][ts, :], r=[R["H"]], w=[rh_])
                    P.op("dve", lambda e: e.tensor_tensor(out=n_[:], in0=yacc[:, i, :], in1=bc[:, 2 if t < NXT else 5, :], op=ALU.mult), r=[ryacc, rbc], w=[rn_])
                    P.op("dve", lambda e: e.tensor_tensor(out=n_[:], in0=n_[:], in1=h_[:], op=ALU.add), r=[rn_, rh_], w=[rn_])
                    if last:
                        P.dma("pool", Dm["out"][ts, :], n_[:], r=[rn_], w=[R["out"]])
                    else:
                        P.dma("pool", Dm["H"][ts, :], n_[:], r=[rn_], w=[R["H"]])
            P.barrier()
    P.barrier()


def full_plan(nlayers=DEPTH):
    def plan(K):
        for l in range(nlayers):
            with_ctx = l < DEPTH - 1
            last = l == DEPTH - 1
            stage_prep(K, l)
            stage_A(K, l)
            stage_ret(K, l, with_ctx)
            stage_na(K, l, with_ctx)
            stage_s5(K, l, with_ctx)
            stage_gqa(K, l, with_ctx)
            stage_merge(K, l, with_ctx)
            stage_moe_sparse(K, l, with_ctx, last)
    return plan


_CACHE = {}


def kernel(**inputs):
    consts = make_consts()
    n = 8
    x = np.asarray(inputs["x"], np.float32)
    in_maps = []
    shared = {k: np.ascontiguousarray(np.asarray(inputs[k], np.float32)) for k in INPUT_NAMES if k not in ("x", "c", "ctx")}
    for b in range(n):
        m = dict(shared)
        m["x"] = np.ascontiguousarray(x[b])
        m["ctx"] = np.ascontiguousarray(np.asarray(inputs["ctx"], np.float32)[b])
        m["c"] = np.ascontiguousarray(np.asarray(inputs["c"], np.float32)[b:b + 1])
        m.update(consts)
        in_maps.append(m)
    shapes = {k: (v.shape, F32) for k, v in in_maps[0].items() if k not in consts}
    if "nc" not in _CACHE:
        _CACHE["nc"] = build(shapes, consts, full_plan())[0]
    res = run_bass_kernel_spmd(_CACHE["nc"], in_maps, core_ids=list(range(n)))
    return np.stack([np.asarray(r["out"], np.float32) for r in res.results], axis=0)


def _idma(P, out, out_off, in_, in_off, r=(), w=()):
    q = "pool"
    P._deps(q, r, w)
    i = P.dnext[q]
    P.dnext[q] = (i + 1) % P.NDS
    key = ("d", q, i)
    P._wait(q, key, 16 * P.duse[q][i])
    ins = P.nc.gpsimd.indirect_dma_start(out=out, out_offset=out_off, in_=in_, in_offset=in_off)
    P.duse[q][i] += 1
    ins.then_inc(P.semh[key], 16)
    P._mark((key, 16 * P.duse[q][i]), r, w)
    P.ninst += 1
    return ins


def stage_moe_sparse(K, l, with_ctx, last, precast=True, nblocks=None):
    nc, P, Dm, R = K.nc, K.P, K.dram, K.R
    tiles = list(range(NXT)) + ([64, 65] if with_ctx else [])
    NTL = len(tiles)
    B = MOE_B
    NB = -(-(2 * NTL * 128 + 32 * (B - 1)) // B)
    IOA = bass.IndirectOffsetOnAxis
    rwb = Res("wbf")
    rfb = Res("fb")
    rxs = Res("xs")
    rys = Res("ys")
    if precast:
        with contextlib.ExitStack() as e0:
            st = [_tile(e0, nc, f"E0_st{i}", [128, 4096], F32) for i in range(3)]
            sb = [_tile(e0, nc, f"E0_sb{i}", [128, 4096], BF16) for i in range(3)]
            cnt = 0
            for e_ in range(32):
                for src, dst, kk in (("exp_w1", "wb1", 8), ("exp_w3", "wb3", 8), ("exp_w2", "wb2", 4)):
                    s_, rs_ = st[cnt % 3]
                    b_, rb_ = sb[cnt % 3]
                    eng = ("act", "pool", "dve")[cnt % 3]
                    cnt += 1
                    P.dma("sp", s_[:].rearrange("p (k n) -> p k n", k=kk), Dm[src][l, e_].rearrange("(k p) n -> p k n", p=128), w=[rs_])
                    if eng == "act":
                        P.op("act", lambda e: e.copy(out=b_[:], in_=s_[:]), r=[rs_], w=[rb_])
                    else:
                        P.op(eng, lambda e: e.tensor_copy(out=b_[:], in_=s_[:]), r=[rs_], w=[rb_])
                    P.dma("act", Dm[dst][e_ * 128:(e_ + 1) * 128, :], b_[:], r=[rb_], w=[rwb])
        P.barrier()
    with contextlib.ExitStack() as es:
        bc, rbc = _tile(es, nc, "Q_bc", [128, 6, 1024], F32)
        OH1, rOH1 = _tile(es, nc, "Q_OH1", [128, NTL, 32], F32)
        OH2, rOH2 = _tile(es, nc, "Q_OH2", [128, NTL, 32], F32)
        OH12, rOH12 = _tile(es, nc, "Q_OH12", [128, NTL, 32], BF16)
        GA, rGA = _tile(es, nc, "Q_GA", [128, NTL, 2], F32)
        DST, rDST = _tile(es, nc, "Q_DST", [128, NTL, 2], I32)
        IDXW, rIDXW = _tile(es, nc, "Q_IDXW", [128, NB], I32)
        pstart, rpstart = _tile(es, nc, "Q_pstart", [128, 32], F32)
        onesb, ronesb = _tile(es, nc, "Q_ones", [128, 128], BF16)
        ustr, rustr = _tile(es, nc, "Q_ustr", [128, 128], BF16)
        identb, ridentb = _tile(es, nc, "Q_identb", [128, 128], BF16)
        for j, row in enumerate((3, 4, 5, 9, 10, 11)):
            P.dma("sp", bc[:, j, :], Dm["modv"][row].partition_broadcast(128), r=[R["modv"]], w=[rbc])
        P.dma("pool", onesb[:], Dm["moe_ones"], w=[ronesb])
        P.dma("pool", ustr[:], Dm["moe_ustrict"], w=[rustr])
        P.dma("pool", identb[:], Dm["ident"], w=[ridentb])
        with contextlib.ExitStack() as e1:
            identf, ridentf = _tile(e1, nc, "Q1_identf", [128, 128], F32)
            rw, rrw = _tile(e1, nc, "Q1_rw", [128, 8, 36], F32)
            rb, rrb = _tile(e1, nc, "Q1_rb", [128, 36], F32)
            epsc, repsc = _tile(e1, nc, "Q1_eps", [128, 1], F32)
            ht = [_tile(e1, nc, f"Q1_h{i}", [128, 1024], F32) for i in range(2)]
            junk, rjunk = _tile(e1, nc, "Q1_junk", [128, 1024], BF16)
            ssq, rssq = _tile(e1, nc, "Q1_ssq", [128, 1], F32)
            rstd, rrstd = _tile(e1, nc, "Q1_rstd", [128, 1], F32)
            f_, rf_ = _tile(e1, nc, "Q1_f", [128, 1024], F32)
            fb = [_tile(e1, nc, f"Q1_fb{i}", [128, 1024], BF16) for i in range(2)]
            lgt, rlgt = _tile(e1, nc, "Q1_lg", [128, 36], F32)
            sm = [_tile(e1, nc, f"Q1_s{i}", [128, 8], F32) for i in range(4)]
            l2, rl2 = _tile(e1, nc, "Q1_l2", [128, 32], F32)
            cnts, rcnts = _tile(e1, nc, "Q1_cnt", [128, 32], F32)
            wk = [_tile(e1, nc, f"Q1_wk{i}", [128, 32], F32) for i in range(3)]
            cmpt, rcmpt = _tile(e1, nc, "Q1_cmp", [128, NB, 32], F32)
            bbt, rbbt = _tile(e1, nc, "Q1_bb", [128, NB, 32], F32)
            eblk, reblk = _tile(e1, nc, "Q1_eblk", [128, NB], F32)
            pcol, rpcol = _tile(e1, nc, "Q1_pcol", [128, 1], F32)
            pTf = [_tile(e1, nc, f"Q1_pT{i}", [128, 4, 128], F32, psum=True) for i in range(2)]
            lp, rlp = _tile(e1, nc, "Q1_lp", [128, 36], F32, psum=True)
            cp, rcp = _tile(e1, nc, "Q1_cp", [128, 32], F32, psum=True)
            P.dma("sp", identf[:], Dm["ident"], w=[ridentf])
            with nc.allow_non_contiguous_dma(reason="tiny router weights"):
                P.dma("sp", rw[:, :, 0:4], Dm["router_w1"][l].rearrange("(k p) n -> p k n", p=128), w=[rrw])
                P.dma("sp", rw[:, :, 4:36], Dm["router_w2"][l].rearrange("(k p) n -> p k n", p=128), w=[rrw])
            P.dma("sp", rb[:, 0:4], Dm["router_b1"][l].partition_broadcast(128), w=[rrb])
            P.dma("sp", rb[:, 4:36], Dm["router_b2"][l].partition_broadcast(128), w=[rrb])
            P.dma("sp", bbt[:].rearrange("p a b -> p (a b)"), Dm["moe_bb"][:, 0:NB * 32], w=[rbbt])
            P.dma("sp", pcol[:], Dm["moe_pcol"], w=[rpcol])
            P.op("dve", lambda e: e.memset(epsc[:], EPS), w=[repsc])

            def load(i):
                t = tiles[i]
                P.dma("sp", ht[i % 2][0][:], Dm["H"][t * 128:(t + 1) * 128, :], r=[R["H"]], w=[ht[i % 2][1]])
            load(0)
            for i, t in enumerate(tiles):
                if i + 1 < NTL:
                    load(i + 1)
                h_, rh_ = ht[i % 2]
                fb_, rfb_ = fb[i % 2]
                o = 0 if t < NXT else 3
                P.op("act", lambda e: e.activation(out=junk[:], in_=h_[:], func=AF.Square, accum_out=ssq[:]), r=[rh_], w=[rjunk, rssq])
                P.op("act", lambda e: e.activation(out=rstd[:], in_=ssq[:], func=AF.Sqrt, scale=1.0 / D, bias=epsc[:, 0:1]), r=[rssq, repsc], w=[rrstd])
                P.op("dve", lambda e: e.reciprocal(out=rstd[:], in_=rstd[:]), r=[rrstd], w=[rrstd])
                P.op("dve", lambda e: e.scalar_tensor_tensor(out=f_[:], in0=h_[:], scalar=rstd[:, 0:1], in1=bc[:, o, :], op0=ALU.mult, op1=ALU.mult),
                     r=[rh_, rrstd, rbc], w=[rf_])
                P.op("dve", lambda e: e.tensor_tensor(out=f_[:], in0=f_[:], in1=bc[:, o + 1, :], op=ALU.add), r=[rf_, rbc], w=[rf_])
                P.op("act", lambda e: e.copy(out=fb_[:], in_=f_[:]), r=[rf_], w=[rfb_])
                P.dma("act", Dm["fb"][i * 128:(i + 1) * 128, :], fb_[:], r=[rfb_], w=[rfb])
                for hf in range(2):
                    pt, rpt = pTf[hf]
                    for k in range(4):
                        kk = hf * 4 + k
                        P.op("pe", lambda e: e.transpose(out=pt[:, k, :], in_=f_[:, kk * 128:(kk + 1) * 128], identity=identf[:]), r=[rf_, ridentf], w=[rpt])
                for hf in range(2):
                    pt, rpt = pTf[hf]
                    P.op("act", lambda e: e.copy(out=f_[:, hf * 512:(hf + 1) * 512].rearrange("p (k n) -> p k n", n=128), in_=pt), r=[rpt], w=[rf_])
                for k in range(8):
                    P.op("pe", lambda e: e.matmul(lp, lhsT=f_[:, k * 128:(k + 1) * 128], rhs=rw[:, k, :], start=(k == 0), stop=(k == 7)), r=[rf_, rrw], w=[rlp])
                P.op("dve", lambda e: e.tensor_tensor(out=lgt[:], in0=lp, in1=rb[:], op=ALU.add), r=[rlp, rrb], w=[rlgt])
                (s0, rs0), (s1, rs1), (s2, rs2), (s3, rs3) = sm
                oh1, oh2 = OH1[:, i, :], OH2[:, i, :]
                P.op("dve", lambda e: e.tensor_reduce(out=s0[:, 0:1], in_=lgt[:, 0:4], axis=AX.X, op=ALU.max), r=[rlgt], w=[rs0])
                P.op("dve", lambda e: e.tensor_scalar(out=s0[:, 1:2], in0=s0[:, 0:1], scalar1=-1.0, scalar2=None, op0=ALU.mult), r=[rs0], w=[rs0])
                P.op("act", lambda e: e.activation(out=s1[:, 0:4], in_=lgt[:, 0:4], func=AF.Exp, bias=s0[:, 1:2], accum_out=s0[:, 2:3]), r=[rlgt, rs0], w=[rs1, rs0])
                P.op("dve", lambda e: e.reciprocal(out=s0[:, 3:4], in_=s0[:, 2:3]), r=[rs0], w=[rs0])
                P.op("dve", lambda e: e.tensor_scalar(out=s2[:, 0:4], in0=lgt[:, 0:4], scalar1=s0[:, 0:1], scalar2=None, op0=ALU.is_equal), r=[rlgt, rs0], w=[rs2])
                P.op("dve", lambda e: e.tensor_scalar(out=s2[:, 0:4], in0=s2[:, 0:4], scalar1=BIG, scalar2=-BIG, op0=ALU.mult, op1=ALU.add), r=[rs2], w=[rs2])
                P.op("dve", lambda e: e.tensor_tensor(out=l2[:].rearrange("p (g e) -> p g e", e=8), in0=lgt[:, 4:36].rearrange("p (g e) -> p g e", e=8),
                                                      in1=s2[:, 0:4].unsqueeze(2).to_broadcast([128, 4, 8]), op=ALU.add), r=[rlgt, rs2], w=[rl2])
                P.op("dve", lambda e: e.tensor_reduce(out=s3[:, 0:1], in_=l2[:], axis=AX.X, op=ALU.max), r=[rl2], w=[rs3])
                P.op("dve", lambda e: e.tensor_scalar(out=oh1, in0=l2[:], scalar1=s3[:, 0:1], scalar2=None, op0=ALU.is_equal), r=[rl2, rs3], w=[rOH1])
                P.op("dve", lambda e: e.scalar_tensor_tensor(out=l2[:], in0=oh1, scalar=-BIG, in1=l2[:], op0=ALU.mult, op1=ALU.add), r=[rOH1, rl2], w=[rl2])
                P.op("dve", lambda e: e.tensor_reduce(out=s3[:, 1:2], in_=l2[:], axis=AX.X, op=ALU.max), r=[rl2], w=[rs3])
                P.op("dve", lambda e: e.tensor_scalar(out=oh2, in0=l2[:], scalar1=s3[:, 1:2], scalar2=None, op0=ALU.is_equal), r=[rl2, rs3], w=[rOH2])
                P.op("dve", lambda e: e.tensor_tensor(out=OH12[:, i, :], in0=oh1, in1=oh2, op=ALU.add), r=[rOH1, rOH2], w=[rOH12])
                P.op("dve", lambda e: e.tensor_tensor(out=s3[:, 2:3], in0=s3[:, 1:2], in1=s3[:, 0:1], op=ALU.subtract), r=[rs3], w=[rs3])
                P.op("act", lambda e: e.activation(out=s3[:, 3:4], in_=s3[:, 2:3], func=AF.Exp), r=[rs3], w=[rs3])
                P.op("dve", lambda e: e.tensor_scalar(out=s3[:, 4:5], in0=s3[:, 3:4], scalar1=1.0, scalar2=None, op0=ALU.add), r=[rs3], w=[rs3])
                P.op("dve", lambda e: e.reciprocal(out=s3[:, 4:5], in_=s3[:, 4:5]), r=[rs3], w=[rs3])
                P.op("dve", lambda e: e.tensor_tensor(out=GA[:, i, 0:1], in0=s3[:, 4:5], in1=s0[:, 3:4], op=ALU.mult), r=[rs3, rs0], w=[rGA])
                P.op("dve", lambda e: e.tensor_tensor(out=GA[:, i, 1:2], in0=GA[:, i, 0:1], in1=s3[:, 3:4], op=ALU.mult), r=[rGA, rs3], w=[rGA])
                P.op("pe", lambda e: e.matmul(cp, lhsT=onesb[:], rhs=OH12[:, i, :], start=(i == 0), stop=(i == NTL - 1)), r=[ronesb, rOH12], w=[rcp])
            (a0, ra0), (a1, ra1), (a2, ra2) = wk
            P.op("dve", lambda e: e.tensor_copy(out=cnts[:], in_=cp), r=[rcp], w=[rcnts])
            P.op("dve", lambda e: e.tensor_scalar(out=a0[:], in0=cnts[:], scalar1=float(B - 1), scalar2=1.0 / B, op0=ALU.add, op1=ALU.mult), r=[rcnts], w=[ra0])
            P.op("dve", lambda e: e.tensor_scalar(out=a0[:], in0=a0[:], scalar1=-(B - 1) / (2.0 * B), scalar2=None, op0=ALU.add), r=[ra0], w=[ra0])
            P.op("dve", lambda e: e.tensor_scalar(out=a0[:], in0=a0[:], scalar1=MAGIC, scalar2=None, op0=ALU.add), r=[ra0], w=[ra0])
            P.op("dve", lambda e: e.tensor_scalar(out=a0[:], in0=a0[:], scalar1=MAGIC, scalar2=None, op0=ALU.subtract), r=[ra0], w=[ra0])
            P.op("dve", lambda e: e.tensor_scalar(out=a0[:], in0=a0[:], scalar1=float(B), scalar2=None, op0=ALU.mult), r=[ra0], w=[ra0])
            P.op("dve", lambda e: e.memset(a2[:], 1.0), w=[ra2])
            P.op("dve", lambda e: e.tensor_tensor_scan(out=a1[:], data0=a2[:], data1=a0[:], initial=0.0, op0=ALU.mult, op1=ALU.add), r=[ra2, ra0], w=[ra1])
            P.op("dve", lambda e: e.tensor_tensor(out=pstart[:], in0=a1[:], in1=a0[:], op=ALU.subtract), r=[ra1, ra0], w=[rpstart])
            P.op("dve", lambda e: e.tensor_tensor(out=cmpt[:], in0=a1[:].unsqueeze(1).to_broadcast([128, NB, 32]), in1=bbt[:], op=ALU.is_le), r=[ra1, rbbt], w=[rcmpt])
            P.op("dve", lambda e: e.tensor_reduce(out=eblk[:], in_=cmpt[:], axis=AX.X, op=ALU.add), r=[rcmpt], w=[reblk])
            P.op("dve", lambda e: e.tensor_scalar(out=eblk[:], in0=eblk[:], scalar1=31.0, scalar2=128.0, op0=ALU.min, op1=ALU.mult), r=[reblk], w=[reblk])
            P.op("dve", lambda e: e.tensor_scalar(out=eblk[:], in0=eblk[:], scalar1=pcol[:, 0:1], scalar2=None, op0=ALU.add), r=[reblk, rpcol], w=[reblk])
            P.op("dve", lambda e: e.tensor_copy(out=IDXW[:], in_=eblk[:]), r=[reblk], w=[rIDXW])
        P.barrier()
        with contextlib.ExitStack() as e3:
            base, rbase = _tile(e3, nc, "Q3_base", [128, 32], F32)
            sb_, rsb_ = _tile(e3, nc, "Q3_sb", [128, 32], F32)
            pr, rpr = _tile(e3, nc, "Q3_pr", [128, 32], F32)
            dd, rdd = _tile(e3, nc, "Q3_dd", [128, 2], F32)
            fbt = [_tile(e3, nc, f"Q3_fb{i}", [128, 1024], BF16) for i in range(2)]
            rp_, rrp_ = _tile(e3, nc, "Q3_rp", [128, 32], F32, psum=True)
            csp, rcsp = _tile(e3, nc, "Q3_cs", [128, 32], F32, psum=True)
            P.op("dve", lambda e: e.memset(base[:], 0.0), w=[rbase])
            zt, rzt = _tile(e3, nc, "Q3_zero", [128, 7, 1024], BF16)
            P.op("pool", lambda e: e.memset(zt[:], 0.0), w=[rzt])
            xsv = Dm["xs"][0:NB * B, :].rearrange("(p a) d -> p a d", p=128)
            na = NB * B // 128
            for a0_ in range(0, na, 7):
                a1_ = min(a0_ + 7, na)
                P.dma("sp" if (a0_ // 7) % 2 == 0 else "act", xsv[:, a0_:a1_, :], zt[:, 0:a1_ - a0_, :], r=[rzt], w=[rxs])
            for i in range(NTL):
                f2, rf2 = fbt[i % 2]
                P.dma("sp", f2[:], Dm["fb"][i * 128:(i + 1) * 128, :], r=[rfb], w=[rf2])
                P.op("pe", lambda e: e.matmul(rp_, lhsT=ustr[:], rhs=OH12[:, i, :], start=True, stop=True), r=[rustr, rOH12], w=[rrp_])
                P.op("pe", lambda e: e.matmul(csp, lhsT=onesb[:], rhs=OH12[:, i, :], start=True, stop=True), r=[ronesb, rOH12], w=[rcsp])
                P.op("dve", lambda e: e.tensor_tensor(out=sb_[:], in0=rp_, in1=base[:], op=ALU.add), r=[rrp_, rbase], w=[rsb_])
                P.op("dve", lambda e: e.tensor_tensor(out=sb_[:], in0=sb_[:], in1=pstart[:], op=ALU.add), r=[rsb_, rpstart], w=[rsb_])
                for k, (OHk, rOHk) in enumerate(((OH1, rOH1), (OH2, rOH2))):
                    P.op("dve", lambda e: e.tensor_tensor(out=pr[:], in0=sb_[:], in1=OHk[:, i, :], op=ALU.mult), r=[rsb_, rOHk], w=[rpr])
                    P.op("dve", lambda e: e.tensor_reduce(out=dd[:, k:k + 1], in_=pr[:], axis=AX.X, op=ALU.add), r=[rpr], w=[rdd])
                P.op("dve", lambda e: e.tensor_copy(out=DST[:, i, :], in_=dd[:]), r=[rdd], w=[rDST])
                P.op("dve", lambda e: e.tensor_tensor(out=base[:], in0=base[:], in1=csp, op=ALU.add), r=[rbase, rcsp], w=[rbase])
                for k in range(2):
                    _idma(P, Dm["xs"][0:NB * B, :], IOA(DST[:, i, k:k + 1], 0), f2[:], None, r=[rf2, rDST], w=[rxs])
        P.barrier()
        with contextlib.ExitStack() as e4:
            w1 = [_tile(e4, nc, f"Q4_w1_{i}", [128, 8, 512], BF16) for i in range(2)]
            w3 = [_tile(e4, nc, f"Q4_w3_{i}", [128, 8, 512], BF16) for i in range(2)]
            w2 = [_tile(e4, nc, f"Q4_w2_{i}", [128, 4, 1024], BF16) for i in range(2)]
            xsb = [_tile(e4, nc, f"Q4_xs{i}", [128, 2, 1024], BF16) for i in range(2)]
            xT = [_tile(e4, nc, f"Q4_xT{i}", [128, 8, B], BF16) for i in range(2)]
            sl = [_tile(e4, nc, f"Q4_sl{i}", [128, B], F32) for i in range(2)]
            hid = [_tile(e4, nc, f"Q4_hid{i}", [128, 4, B], BF16) for i in range(2)]
            ysb = [_tile(e4, nc, f"Q4_ys{i}", [128, 1024], F32) for i in range(2)]
            pT, rpT = _tile(e4, nc, "Q4_pT", [128, 8, 128], BF16, psum=True)
            H1 = [_tile(e4, nc, f"Q4_H1{i}", [128, B], F32, psum=True) for i in range(2)]
            H3 = [_tile(e4, nc, f"Q4_H3{i}", [128, B], F32, psum=True) for i in range(2)]
            Yp = [_tile(e4, nc, f"Q4_Y{i}", [128, 512], F32, psum=True) for i in range(2)]
            nbl = NB if nblocks is None else nblocks

            def loadb(b):
                _idma(P, w1[b % 2][0][:].rearrange("p k n -> p (k n)"), None, Dm["wb1"], IOA(IDXW[:, b:b + 1], 0), r=[rwb, rIDXW], w=[w1[b % 2][1]])
                _idma(P, w3[b % 2][0][:].rearrange("p k n -> p (k n)"), None, Dm["wb3"], IOA(IDXW[:, b:b + 1], 0), r=[rwb, rIDXW], w=[w3[b % 2][1]])
                _idma(P, w2[b % 2][0][:].rearrange("p k n -> p (k n)"), None, Dm["wb2"], IOA(IDXW[:, b:b + 1], 0), r=[rwb, rIDXW], w=[w2[b % 2][1]])
                P.dma("sp", xsb[b % 2][0][:], Dm["xs"][b * B:(b + 1) * B, :].rearrange("(s p) d -> p s d", p=128), r=[rxs], w=[xsb[b % 2][1]])
            loadb(0)
            cnt = 0
            yc = 0
            for b in range(nbl):
                if b + 1 < nbl:
                    loadb(b + 1)
                (w1t, rw1), (w3t, rw3), (w2t, rw2) = w1[b % 2], w3[b % 2], w2[b % 2]
                xb, rxb = xsb[b % 2]
                xt, rxt = xT[b % 2]
                hd, rhd = hid[b % 2]
                for s_ in range(2):
                    for k in range(8):
                        P.op("pe", lambda e: e.transpose(out=pT[:, k, :], in_=xb[:, s_, k * 128:(k + 1) * 128], identity=identb[:]), r=[rxb, ridentb], w=[rpT])
                    P.op("act", lambda e: e.copy(out=xt[:, :, s_ * 128:(s_ + 1) * 128], in_=pT), r=[rpT], w=[rxt])
                for fc in range(4):
                    h1, rh1 = H1[cnt % 2]
                    h3, rh3 = H3[cnt % 2]
                    sl_, rsl_ = sl[cnt % 2]
                    cnt += 1
                    for k in range(8):
                        P.op("pe", lambda e: e.matmul(h1, lhsT=w1t[:, k, fc * 128:(fc + 1) * 128], rhs=xt[:, k, :], start=(k == 0), stop=(k == 7)), r=[rw1, rxt], w=[rh1])
                    for k in range(8):
                        P.op("pe", lambda e: e.matmul(h3, lhsT=w3t[:, k, fc * 128:(fc + 1) * 128], rhs=xt[:, k, :], start=(k == 0), stop=(k == 7)), r=[rw3, rxt], w=[rh3])
                    P.op("act", lambda e: e.activation(out=sl_[:], in_=h1, func=AF.Silu), r=[rh1], w=[rsl_])
                    P.op("dve", lambda e: e.tensor_tensor(out=hd[:, fc, :], in0=h3, in1=sl_[:], op=ALU.mult), r=[rh3, rsl_], w=[rhd])
                for s_ in range(2):
                    ys_, rys_ = ysb[yc % 2]
                    yc += 1
                    for nh in range(2):
                        ypt, rypt = Yp[nh]
                        for fc in range(4):
                            P.op("pe", lambda e: e.matmul(ypt, lhsT=hd[:, fc, s_ * 128:(s_ + 1) * 128], rhs=w2t[:, fc, nh * 512:(nh + 1) * 512], start=(fc == 0), stop=(fc == 3)),
                                 r=[rhd, rw2], w=[rypt])
                        if nh == 0:
                            P.op("act", lambda e: e.copy(out=ys_[:, 0:512], in_=ypt), r=[rypt], w=[rys_])
                        else:
                            P.op("dve", lambda e: e.tensor_copy(out=ys_[:, 512:1024], in_=ypt), r=[rypt], w=[rys_])
                    P.dma("sp", Dm["ys"][b * B + s_ * 128:b * B + (s_ + 1) * 128, :], ys_[:], r=[rys_], w=[rys])
        P.barrier()
        with contextlib.ExitStack() as e5:
            y1 = [_tile(e5, nc, f"Q5_y1{i}", [128, 1024], F32) for i in range(2)]
            y2 = [_tile(e5, nc, f"Q5_y2{i}", [128, 1024], F32) for i in range(2)]
            ht = [_tile(e5, nc, f"Q5_h{i}", [128, 1024], F32) for i in range(2)]
            hn = [_tile(e5, nc, f"Q5_hn{i}", [128, 1024], F32) for i in range(2)]

            def load5(i):
                t = tiles[i]
                _idma(P, y1[i % 2][0][:], None, Dm["ys"][0:NB * B, :], IOA(DST[:, i, 0:1], 0), r=[rys, rDST], w=[y1[i % 2][1]])
                _idma(P, y2[i % 2][0][:], None, Dm["ys"][0:NB * B, :], IOA(DST[:, i, 1:2], 0), r=[rys, rDST], w=[y2[i % 2][1]])
                P.dma("sp", ht[i % 2][0][:], Dm["H"][t * 128:(t + 1) * 128, :], r=[R["H"]], w=[ht[i % 2][1]])
            load5(0)
            for i, t in enumerate(tiles):
                if i + 1 < NTL:
                    load5(i + 1)
                (a_, ra_), (b_, rb_), (h_, rh_), (n_, rn_) = y1[i % 2], y2[i % 2], ht[i % 2], hn[i % 2]
                ts = slice(t * 128, (t + 1) * 128)
                P.op("dve", lambda e: e.tensor_scalar(out=n_[:], in0=a_[:], scalar1=GA[:, i, 0:1], scalar2=None, op0=ALU.mult), r=[ra_, rGA], w=[rn_])
                P.op("dve", lambda e: e.scalar_tensor_tensor(out=n_[:], in0=b_[:], scalar=GA[:, i, 1:2], in1=n_[:], op0=ALU.mult, op1=ALU.add), r=[rb_, rGA, rn_], w=[rn_])
                P.op("pool", lambda e: e.tensor_tensor(out=n_[:], in0=n_[:], in1=bc[:, 2 if t < NXT else 5, :], op=ALU.mult), r=[rn_, rbc], w=[rn_])
                P.op("dve", lambda e: e.tensor_tensor(out=n_[:], in0=n_[:], in1=h_[:], op=ALU.add), r=[rn_, rh_], w=[rn_])
                if last:
                    P.dma("act", Dm["out"][ts, :], n_[:], r=[rn_], w=[R["out"]])
                else:
                    P.dma("act", Dm["H"][ts, :], n_[:], r=[rn_], w=[R["H"]])
    P.barrier()


SCRATCH.update({"wb1": ([32 * 128, 4096], BF16), "wb3": ([32 * 128, 4096], BF16), "wb2": ([32 * 128, 4096], BF16),
                "fb": ([T, 1024], BF16), "xs": ([MOE_NBMAX * MOE_B, 1024], BF16), "ys": ([MOE_NBMAX * MOE_B, 1024], F32)})
```

```python
import contextlib
import math
import numpy as np
import ml_dtypes
import concourse.bass as bass
import concourse.mybir as mybir
from concourse.bass_utils import run_bass_kernel_spmd

F32 = mybir.dt.float32
BF16 = mybir.dt.bfloat16
I32 = mybir.dt.int32
AF = mybir.ActivationFunctionType
ALU = mybir.AluOpType
AX = mybir.AxisListType

D = 1024
L = 8192
NCTX = 256
T = L + NCTX
NT = T // 128
NXT = L // 128
DEPTH = 4
MIXC = 2560
EPS = 1e-6
SAME_SYNC = True
STQ = "pool"
NEGM = -240000.0


class Res:
    __slots__ = ("name", "w", "rd")

    def __init__(self, name=""):
        self.name = name
        self.w = None
        self.rd = {}


class Prog:
    NDS = 12

    def __init__(self, nc, same_sync=True, dma_queues=("sp", "pool", "act")):
        self.nc = nc
        self.E = {"pe": nc.tensor, "dve": nc.vector, "act": nc.scalar, "pool": nc.gpsimd, "sp": nc.sync}
        self.same_sync = same_sync
        self.semh = {}
        self.cnt = {}
        for k in self.E:
            self.semh[("c", k)] = nc.alloc_semaphore(f"sc_{k}")
            self.cnt[k] = 0
        self.seen = {k: {} for k in self.E}
        self.duse = {}
        self.dnext = {}
        for q in dma_queues:
            self.duse[q] = [0] * self.NDS
            self.dnext[q] = 0
            for i in range(self.NDS):
                self.semh[("d", q, i)] = nc.alloc_semaphore(f"sd_{q}_{i}")
        self.ninst = 0
        self.pe_relaxed = False

    def _wait(self, eng, key, val):
        if val <= 0 or self.seen[eng].get(key, 0) >= val:
            return
        self.E[eng].wait_ge(self.semh[key], val)
        self.seen[eng][key] = val

    def _deps(self, eng, r, w):
        deps = {}
        for res in r:
            if res.w is not None:
                k, v = res.w
                if deps.get(k, 0) < v:
                    deps[k] = v
        for res in w:
            if res.w is not None:
                k, v = res.w
                if deps.get(k, 0) < v:
                    deps[k] = v
            for k, v in res.rd.items():
                if deps.get(k, 0) < v:
                    deps[k] = v
        for k, v in deps.items():
            if k == ("c", eng) and ((eng == "pe" and self.pe_relaxed) or not self.same_sync):
                continue
            self._wait(eng, k, v)

    def _mark(self, tok, r, w):
        k, v = tok
        for res in r:
            if res.rd.get(k, 0) < v:
                res.rd[k] = v
        for res in w:
            res.w = tok
            res.rd = {}

    def op(self, eng, fn, r=(), w=()):
        self._deps(eng, r, w)
        ins = fn(self.E[eng])
        self.cnt[eng] += 1
        ins.then_inc(self.semh[("c", eng)], 1)
        self._mark((("c", eng), self.cnt[eng]), r, w)
        self.ninst += 1
        return ins

    def dma(self, q, out, in_, r=(), w=(), **kw):
        self._deps(q, r, w)
        i = self.dnext[q]
        self.dnext[q] = (i + 1) % self.NDS
        key = ("d", q, i)
        self._wait(q, key, 16 * self.duse[q][i])
        ins = self.E[q].dma_start(out=out, in_=in_, **kw)
        self.duse[q][i] += 1
        ins.then_inc(self.semh[key], 16)
        self._mark((key, 16 * self.duse[q][i]), r, w)
        self.ninst += 1
        return ins

    def barrier(self, engines=None):
        engines = engines or list(self.E)
        for eng in engines:
            for k in self.E:
                if k != eng:
                    self._wait(eng, ("c", k), self.cnt[k])
            for q in self.duse:
                for i in range(self.NDS):
                    self._wait(eng, ("d", q, i), 16 * self.duse[q][i])


class Ctx:
    pass


_TCNT = [0]


def _tile(es, nc, name, shape, dt, psum=False):
    _TCNT[0] += 1
    name = f"{name}_{_TCNT[0]}"
    if not psum:
        t = es.enter_context(nc.sbuf_tensor(name, shape, dt))
        return t, Res(name)
    esz = 2 if dt == BF16 else 4
    n = int(np.prod(shape[1:]))
    per_bank = 2048 // esz
    nb = (n + per_bank - 1) // per_bank
    t = es.enter_context(nc.psum_tensor(name, [128, nb * per_bank], dt))
    ap = t[0:shape[0], 0:n]
    if len(shape) == 3:
        ap = ap.rearrange("p (a b) -> p a b", b=shape[2])
    elif len(shape) == 4:
        ap = ap.rearrange("p (a b c) -> p a b c", b=shape[2], c=shape[3])
    return ap, Res(name)


NA_NCLS = 21
MOE_B = 256
MOE_NBMAX = 98
S5_TC = 256
MAGIC = 12582912.0


def na_class_list():
    lst = [(10, 10 + dc) for dc in (-2, -1, 0, 1, 2)]
    for j in (0, 1):
        lst += [(j, c) for c in range(4)]
    for j in (62, 63):
        lst += [(j, c) for c in range(60, 64)]
    return lst


def na_chunks(j):
    if 2 <= j <= 61:
        return [(j + dc, dc + 2) for dc in (-2, -1, 0, 1, 2)]
    base = {0: 5, 1: 9, 62: 13, 63: 17}[j]
    c0 = 0 if j < 2 else 60
    return [(c0 + i, base + i) for i in range(4)]


def make_consts():
    c = {}
    pos = np.arange(L)
    inv = (10000.0 ** (-np.arange(16, dtype=np.float32) / 16)).astype(np.float32)
    ang_r = (pos // 64).astype(np.float32)[:, None] * inv
    ang_c = (pos % 64).astype(np.float32)[:, None] * inv
    cr, sr, cc, sc = np.cos(ang_r), np.sin(ang_r), np.cos(ang_c), np.sin(ang_c)
    cosf = np.concatenate([cr, cr, cc, cc], axis=1)
    sinf = np.concatenate([-sr, sr, -sc, sc], axis=1)
    cosf = np.concatenate([cosf, np.ones((NCTX, 64))], axis=0)
    sinf = np.concatenate([sinf, np.zeros((NCTX, 64))], axis=0)
    c["ropecs"] = np.concatenate([cosf, sinf], axis=1).astype(np.float32)
    c["ident"] = np.eye(128, dtype=np.float32)
    kl = np.arange(128)[:, None]
    ql = np.arange(128)[None, :]
    lo = np.where(kl >= ql, 0.0, NEGM)
    hi = np.where(kl <= ql, 0.0, NEGM)
    c["na_jx"] = np.zeros((128, 128), np.float32)
    for q in range(128):
        c["na_jx"][(q // 64) * 64 + 63 - q % 64, q] = 1.0
    rm = np.zeros((NA_NCLS, 128, 128), np.float32)
    for cls, (j, cch) in enumerate(na_class_list()):
        for qp in range(128):
            rq = 2 * j + qp // 64
            cq = 63 - qp % 64
            r0 = min(max(rq - 4, 0), 120)
            ws = min(max(cq - 8, 0), 48)
            for key in range(128):
                rk = 2 * cch + key // 64
                ck = key % 64
                ok = (r0 <= rk < r0 + 8) and (ws <= ck < ws + 16)
                rm[cls, qp, key] = 0.0 if ok else NEGM
    c["na_rm"] = rm
    si = np.arange(128, dtype=np.float32)[:, None]
    ti = np.arange(128, dtype=np.float32)[None, :]
    c["ret_dpos"] = np.maximum(ti - si, 0.0).astype(np.float32)
    c["ret_dneg"] = np.maximum(si - ti, 0.0).astype(np.float32)
    c["ret_diag"] = ((si == ti) * math.log(2.0) + math.log(0.125)).astype(np.float32)
    c["ret_tp1"] = np.broadcast_to(ti + 1.0, (128, 128)).astype(np.float32).copy()
    c["ret_tr"] = np.broadcast_to(128.0 - ti, (128, 128)).astype(np.float32).copy()
    c["ret_pcol"] = np.concatenate([127.0 - si, si], axis=1).astype(np.float32)
    c["s5_iota1"] = np.broadcast_to(np.arange(1, S5_TC + 1, dtype=np.float32)[None, :], (128, S5_TC)).copy()
    bm = np.zeros((128, 128), np.float32)
    for r in range(128):
        a = (r // 16) % 2
        bm[r, a * 64:(a + 1) * 64] = 1.0
    c["s5_bdmask"] = bm
    c["moe_bb"] = np.broadcast_to((np.arange(MOE_NBMAX, dtype=np.float32) * MOE_B)[None, :, None], (128, MOE_NBMAX, 32)).reshape(128, MOE_NBMAX * 32).copy()
    c["moe_pcol"] = np.arange(128, dtype=np.float32)[:, None].copy()
    c["moe_ustrict"] = (np.arange(128)[:, None] < np.arange(128)[None, :]).astype(np.float32)
    c["moe_ones"] = np.ones((128, 128), np.float32)
    c["gqa_mask"] = np.stack([np.tile(lo, (1, 2)), np.tile(hi, (1, 2))], axis=1).astype(np.float32)
    return c


def stage_prep(K, l):
    nc, P, Dm = K.nc, K.P, K.dram
    with contextlib.ExitStack() as es:
        cc, rcc = _tile(es, nc, "pp_cc", [128, 8, 2], F32)
        sc, rsc = _tile(es, nc, "pp_sc", [128, 8, 2], F32)
        mw, rmw = _tile(es, nc, "pp_mw", [128, 8, 512], F32)
        mw2, rmw2 = _tile(es, nc, "pp_mw2", [128, 8, 512], F32)
        mws = [(mw, rmw), (mw2, rmw2)]
        mv, rmv = _tile(es, nc, "pp_mv", [2, 6144], F32)
        mb, rmb = _tile(es, nc, "pp_mb", [2, 6144], F32)
        g12, rg12 = _tile(es, nc, "pp_g", [2, 2048], F32)
        ps, rps = _tile(es, nc, "pp_ps", [2, 512], F32, psum=True)
        ps2, rps2 = _tile(es, nc, "pp_ps2", [2, 512], F32, psum=True)
        pss = [(ps, rps), (ps2, rps2)]
        with nc.allow_non_contiguous_dma(reason="tiny"):
            P.dma("sp", cc[:, :, 0], Dm["c"].rearrange("o (k p) -> p (o k)", p=128), w=[rcc])
            P.dma("sp", cc[:, :, 1], Dm["c_ctx"].rearrange("(k p) -> p k", p=128), w=[rcc])
        P.dma("sp", mb[:], Dm["mod_b"][l].partition_broadcast(2), w=[rmb])
        P.dma("sp", g12[:, 0:1024], Dm["norm1_g"][l].partition_broadcast(2), w=[rg12])
        P.dma("sp", g12[:, 1024:2048], Dm["norm2_g"][l].partition_broadcast(2), w=[rg12])
        P.op("act", lambda e: e.activation(out=sc[:], in_=cc[:], func=AF.Silu), r=[rcc], w=[rsc])
        mwv = Dm["mod_w"][l].rearrange("(k p) n -> p k n", p=128)
        for n in range(12):
            w_, rw_ = mws[n % 2]
            p_, rp_ = pss[n % 2]
            P.dma("sp", w_[:], mwv[:, :, n * 512:(n + 1) * 512], w=[rw_])
            for k in range(8):
                P.op("pe", lambda e: e.matmul(p_[:], lhsT=sc[:, k, :], rhs=w_[:, k, :], start=(k == 0), stop=(k == 7)),
                     r=[rsc, rw_], w=[rp_])
            P.op("dve", lambda e: e.tensor_tensor(out=mv[:, n * 512:(n + 1) * 512], in0=p_[:], in1=mb[:, n * 512:(n + 1) * 512], op=ALU.add),
                 r=[rp_, rmb], w=[rmv])
        for slot, goff in ((1, 0), (4, 1024)):
            P.op("dve", lambda e: e.scalar_tensor_tensor(out=mv[:, slot * 1024:(slot + 1) * 1024], in0=mv[:, slot * 1024:(slot + 1) * 1024],
                                                         scalar=1.0, in1=g12[:, goff:goff + 1024], op0=ALU.add, op1=ALU.mult),
                 r=[rmv, rg12], w=[rmv])
        order = [1, 0, 2, 4, 3, 5]
        mvd = Dm["modv"].rearrange("(a r) d -> a r d", a=2)
        for j, slot in enumerate(order):
            P.dma("sp", mvd[:, j, :], mv[:, slot * 1024:(slot + 1) * 1024], r=[rmv], w=[K.R["modv"]])
    P.barrier()


def stage_A(K, l, tiles=None):
    nc, P, Dm, R = K.nc, K.P, K.dram, K.R
    tiles = list(range(NT)) if tiles is None else tiles
    P.pe_relaxed = True
    with contextlib.ExitStack() as es:
        win, rwin = _tile(es, nc, "A_win", [128, 8, MIXC], BF16)
        ident, rident = _tile(es, nc, "A_ident", [128, 128], BF16)
        identf, ridentf = _tile(es, nc, "A_identf", [128, 128], F32)
        bc, rbc = _tile(es, nc, "A_bc", [128, 4, 1024], F32)
        gains, rgains = _tile(es, nc, "A_gains", [128, 4, 64], F32)
        epsc, repsc = _tile(es, nc, "A_eps", [128, 1], F32)
        hts = [_tile(es, nc, f"A_h{i}", [128, 1024], F32) for i in range(2)]
        rps_ = [_tile(es, nc, f"A_rope{i}", [128, 128], F32) for i in range(2)]
        junk, rjunk = _tile(es, nc, "A_junk", [128, 1024], BF16)
        ssq, rssq = _tile(es, nc, "A_ssq", [128, 1], F32)
        rstd, rrstd = _tile(es, nc, "A_rstd", [128, 1], F32)
        t1, rt1 = _tile(es, nc, "A_t1", [128, 1024], F32)
        abf, rabf = _tile(es, nc, "A_abf", [128, 1024], BF16)
        aT, raT = _tile(es, nc, "A_aT", [128, 8, 128], BF16)
        pT, rpT = _tile(es, nc, "A_pT", [128, 8, 128], BF16, psum=True)
        pm = [_tile(es, nc, f"A_pm{i}", [128, 512], F32, psum=True) for i in range(5)]
        pT2, rpT2 = _tile(es, nc, "A_pT2", [128, 4, 128], BF16, psum=True)
        sA, rsA = _tile(es, nc, "A_sA", [128, 512], F32)
        sB, rsB = _tile(es, nc, "A_sB", [128, 512], F32)
        o_rqk, ro_rqk = _tile(es, nc, "A_orqk", [128, 512], BF16)
        o_rv, ro_rv = _tile(es, nc, "A_orv", [128, 256], BF16)
        o_rg, ro_rg = _tile(es, nc, "A_org", [128, 256], F32)
        o_nqk, ro_nqk = _tile(es, nc, "A_onqk", [128, 512], BF16)
        o_nv, ro_nv = _tile(es, nc, "A_onv", [128, 256], BF16)
        o_su, ro_su = _tile(es, nc, "A_osu", [128, 256], F32)
        o_sub, ro_sub = _tile(es, nc, "A_osub", [128, 256], BF16)
        o_gqk, ro_gqk = _tile(es, nc, "A_ogqk", [128, 384], BF16)
        o_gv, ro_gv = _tile(es, nc, "A_ogv", [128, 128], BF16)
        oT, roT = _tile(es, nc, "A_oT", [128, 4, 128], BF16)
        ss8, rss8 = _tile(es, nc, "A_ss8", [128, 8], F32)
        rs8, rrs8 = _tile(es, nc, "A_rs8", [128, 8], F32)

        P.dma("pool", win[:], Dm["w_in"][l].rearrange("(k p) n -> p k n", p=128)[:, :, 0:MIXC], w=[rwin])
        P.dma("sp", identf[:], Dm["ident"], w=[ridentf])
        P.op("dve", lambda e: e.tensor_copy(ident[:], identf[:]), r=[ridentf], w=[rident])
        for j, row in enumerate((0, 1, 6, 7)):
            P.dma("sp", bc[:, j, :], Dm["modv"][row].partition_broadcast(128), r=[R["modv"]], w=[rbc])
        P.dma("sp", gains[:, 0:2, :], Dm["na_qk_gain"][l].partition_broadcast(128), w=[rgains])
        P.dma("sp", gains[:, 2:4, :], Dm["gqa_qk_gain"][l].partition_broadcast(128), w=[rgains])
        P.op("dve", lambda e: e.memset(epsc[:], EPS), w=[repsc])

        hsrc = K.hsrc(l)

        def load(t, par):
            ht, rht = hts[par]
            rp, rrp = rps_[par]
            P.dma("sp", ht[:], hsrc(t), r=[R["H"]], w=[rht])
            P.dma("sp", rp[:], Dm["ropecs"][t * 128:(t + 1) * 128, :], w=[rrp])

        def rmsn(src, nh, gidx, dst, rdst_list, rsrc_list):
            P.op("act", lambda e: e.activation(out=sB[:, 0:nh * 64], in_=src, func=AF.Square), r=rsrc_list, w=[rsB])
            P.op("dve", lambda e: e.tensor_reduce(out=ss8[:, 0:nh], in_=sB[:, 0:nh * 64].rearrange("p (h d) -> p h d", d=64), axis=AX.X, op=ALU.add),
                 r=[rsB], w=[rss8])
            P.op("act", lambda e: e.activation(out=rs8[:, 0:nh], in_=ss8[:, 0:nh], func=AF.Sqrt, scale=1.0 / 64, bias=epsc[:, 0:1]),
                 r=[rss8, repsc], w=[rrs8])
            P.op("dve", lambda e: e.reciprocal(out=rs8[:, 0:nh], in_=rs8[:, 0:nh]), r=[rrs8], w=[rrs8])
            P.op("dve", lambda e: e.tensor_tensor(out=dst.rearrange("p (h d) -> p h d", d=64), in0=src.rearrange("p (h d) -> p h d", d=64),
                                                  in1=rs8[:, 0:nh].unsqueeze(2).to_broadcast([128, nh, 64]), op=ALU.mult),
                 r=rsrc_list + [rrs8], w=rdst_list)
            P.op("dve", lambda e: e.tensor_tensor(out=dst.rearrange("p (h d) -> p h d", d=64), in0=dst.rearrange("p (h d) -> p h d", d=64),
                                                  in1=gains[:, gidx, :].unsqueeze(1).to_broadcast([128, nh, 64]), op=ALU.mult),
                 r=rdst_list + [rgains], w=rdst_list)

        def rope(src, nh, rp, rrp, dst_bf, rsrc_list, rdst_list):
            v5 = lambda ap: ap.rearrange("p (h a b c) -> p h a b c", a=2, b=2, c=16)
            cosb = rp[:, 0:64].rearrange("p (a b c) -> p a b c", a=2, b=2).unsqueeze(1).to_broadcast([128, nh, 2, 2, 16])
            sinb = rp[:, 64:128].rearrange("p (a b c) -> p a b c", a=2, b=2).unsqueeze(1).to_broadcast([128, nh, 2, 2, 16])
            P.op("dve", lambda e: e.tensor_tensor(out=v5(sA[:, 0:nh * 64]), in0=v5(src), in1=cosb, op=ALU.mult), r=rsrc_list + [rrp], w=[rsA])
            P.op("dve", lambda e: e.tensor_tensor(out=v5(sB[:, 0:nh * 64]), in0=v5(src)[:, :, :, ::-1, :], in1=sinb, op=ALU.mult),
                 r=rsrc_list + [rrp], w=[rsB])
            P.op("dve", lambda e: e.tensor_tensor(out=dst_bf, in0=sA[:, 0:nh * 64], in1=sB[:, 0:nh * 64], op=ALU.add), r=[rsA, rsB], w=rdst_list)

        def transp_out(src_bf, rsrc, nchunk, dram_rows, t):
            for k in range(nchunk):
                P.op("pe", lambda e: e.transpose(out=pT2[:, k, :], in_=src_bf[:, k * 128:(k + 1) * 128], identity=ident[:]),
                     r=[rsrc, rident], w=[rpT2])
            P.op("act", lambda e: e.copy(out=oT[:, 0:nchunk, :], in_=pT2[:, 0:nchunk, :]), r=[rpT2], w=[roT])
            P.dma(STQ, dram_rows.rearrange("(k p) n -> p k n", p=128)[:, :, t * 128:(t + 1) * 128], oT[:, 0:nchunk, :], r=[roT], w=[R["mix"]])

        load(tiles[0], 0)
        for idx, t in enumerate(tiles):
            if idx + 1 < len(tiles):
                load(tiles[idx + 1], (idx + 1) % 2)
            ht, rht = hts[idx % 2]
            rp, rrp = rps_[idx % 2]
            isx = t < NXT
            g1 = bc[:, 0 if isx else 2, :]
            sh1 = bc[:, 1 if isx else 3, :]
            ts = slice(t * 128, (t + 1) * 128)
            P.op("act", lambda e: e.activation(out=junk[:], in_=ht[:], func=AF.Square, accum_out=ssq[:]), r=[rht], w=[rjunk, rssq])
            P.op("act", lambda e: e.activation(out=rstd[:], in_=ssq[:], func=AF.Sqrt, scale=1.0 / D, bias=epsc[:, 0:1]), r=[rssq, repsc], w=[rrstd])
            P.op("dve", lambda e: e.reciprocal(out=rstd[:], in_=rstd[:]), r=[rrstd], w=[rrstd])
            P.op("dve", lambda e: e.scalar_tensor_tensor(out=t1[:], in0=ht[:], scalar=rstd[:, 0:1], in1=g1, op0=ALU.mult, op1=ALU.mult),
                 r=[rht, rrstd, rbc], w=[rt1])
            P.op("dve", lambda e: e.tensor_tensor(out=abf[:], in0=t1[:], in1=sh1, op=ALU.add), r=[rt1, rbc], w=[rabf])
            for k in range(8):
                P.op("pe", lambda e: e.transpose(out=pT[:, k, :], in_=abf[:, k * 128:(k + 1) * 128], identity=ident[:]), r=[rabf, rident], w=[rpT])
            P.op("act", lambda e: e.copy(out=aT[:], in_=pT[:]), r=[rpT], w=[raT])
            P.dma(STQ, Dm["aT"].rearrange("(k p) n -> p k n", p=128)[:, :, ts], aT[:], r=[raT], w=[R["aT"]])
            for n in range(5):
                pmn, rpmn = pm[n]
                for k in range(8):
                    P.op("pe", lambda e: e.matmul(pmn[:], lhsT=aT[:, k, :], rhs=win[:, k, n * 512:(n + 1) * 512], start=(k == 0), stop=(k == 7)),
                         r=[raT, rwin], w=[rpmn])
            rope(pm[0][0][:], 8, rp, rrp, o_rqk[:], [pm[0][1]], [ro_rqk])
            P.dma(STQ, Dm["rk"][ts, :], o_rqk[:, 256:512], r=[ro_rqk], w=[R["mix"]])
            transp_out(o_rqk, ro_rqk, 4, Dm["rqkT"], t)
            P.op("act", lambda e: e.copy(out=o_rv[:], in_=pm[1][0][:, 0:256]), r=[pm[1][1]], w=[ro_rv])
            P.op("act", lambda e: e.activation(out=o_rg[:], in_=pm[1][0][:, 256:512], func=AF.Silu), r=[pm[1][1]], w=[ro_rg])
            P.dma(STQ, Dm["rv"][ts, :], o_rv[:], r=[ro_rv], w=[R["mix"]])
            P.dma(STQ, Dm["rg"][ts, :], o_rg[:], r=[ro_rg], w=[R["mix"]])
            rmsn(pm[2][0][:, 0:256], 4, 0, t1[:, 0:256], [rt1], [pm[2][1]])
            rmsn(pm[2][0][:, 256:512], 4, 1, t1[:, 256:512], [rt1], [pm[2][1]])
            P.op("act", lambda e: e.copy(out=o_nqk[:], in_=t1[:, 0:512]), r=[rt1], w=[ro_nqk])
            transp_out(o_nqk, ro_nqk, 4, Dm["nqkT"], t)
            P.op("act", lambda e: e.copy(out=o_nv[:], in_=pm[3][0][:, 0:256]), r=[pm[3][1]], w=[ro_nv])
            P.dma(STQ, Dm["nv"][ts, :], o_nv[:], r=[ro_nv], w=[R["mix"]])
            P.op("act", lambda e: e.copy(out=o_su[:], in_=pm[3][0][:, 256:512]), r=[pm[3][1]], w=[ro_su])
            P.op("dve", lambda e: e.tensor_copy(out=o_sub[:], in_=pm[3][0][:, 256:512]), r=[pm[3][1]], w=[ro_sub])
            P.dma(STQ, Dm["su"][ts, :], o_su[:], r=[ro_su], w=[R["mix"]])
            transp_out(o_sub, ro_sub, 2, Dm["suT"], t)
            rmsn(pm[4][0][:, 0:256], 4, 2, t1[:, 512:768], [rt1], [pm[4][1]])
            rmsn(pm[4][0][:, 256:384], 2, 3, t1[:, 768:896], [rt1], [pm[4][1]])
            rope(t1[:, 512:896], 6, rp, rrp, o_gqk[:], [rt1], [ro_gqk])
            transp_out(o_gqk, ro_gqk, 3, Dm["gqkT"], t)
            P.op("act", lambda e: e.copy(out=o_gv[:], in_=pm[4][0][:, 384:512]), r=[pm[4][1]], w=[ro_gv])
            P.dma(STQ, Dm["gv"][ts, :], o_gv[:], r=[ro_gv], w=[R["mix"]])
    P.pe_relaxed = False
    P.barrier()


INPUT_NAMES = ["x", "c", "ctx", "c_ctx", "mod_w", "mod_b", "norm1_g", "norm2_g", "w_in", "w_branch", "w_out", "ret_log_decay",
               "na_qk_gain", "na_rpb", "s5_lambda_re", "s5_lambda_im", "s5_log_step", "s5_b_re", "s5_b_im", "s5_c_re",
               "s5_c_im", "s5_d", "s5_glu_w", "s5_glu_b", "gqa_qk_gain", "gqa_sink", "router_w1", "router_b1", "router_w2",
               "router_b2", "exp_w1", "exp_w3", "exp_w2"]

SCRATCH = {
    "modv": ([12, 1024], F32),
    "H": ([T, D], F32),
    "aT": ([D, T], BF16),
    "rqkT": ([512, T], BF16), "rk": ([T, 256], BF16), "rv": ([T, 256], BF16), "rg": ([T, 256], F32),
    "nqkT": ([512, T], BF16), "nv": ([T, 256], BF16),
    "su": ([T, 256], F32), "suT": ([256, T], BF16),
    "gqkT": ([384, T], BF16), "gv": ([T, 128], BF16),
}


def build(in_shapes, consts, plan, expose=()):
    nc = bass.Bass("TRN2", target_bir_lowering=False)
    K = Ctx()
    K.nc = nc
    K.dram = {}
    for name, (shape, dt) in in_shapes.items():
        K.dram[name] = nc.dram_tensor(name, list(shape), dt, kind="ExternalInput").ap()
    for name, arr in consts.items():
        K.dram[name] = nc.dram_tensor(name, list(arr.shape), F32, kind="ExternalInput").ap()
    for name, (shape, dt) in SCRATCH.items():
        if name in K.dram:
            continue
        kind = "ExternalOutput" if name in expose else "Internal"
        K.dram[name] = nc.dram_tensor(name, list(shape), dt, kind=kind).ap()
    K.dram["out"] = nc.dram_tensor("out", [L, D], F32, kind="ExternalOutput").ap()
    K.R = {k: Res(k) for k in ["modv", "H", "aT", "mix", "y", "out", "s5y"]}
    K.P = Prog(nc, same_sync=SAME_SYNC)

    def hsrc(l):
        def f(t):
            if l == 0:
                if t < NXT:
                    return K.dram["x"][t * 128:(t + 1) * 128, :]
                return K.dram["ctx"][(t - NXT) * 128:(t - NXT + 1) * 128, :]
            return K.dram["H"][t * 128:(t + 1) * 128, :]
        return f
    K.hsrc = hsrc
    plan(K)
    K.P.barrier(["sp"])
    return nc, K


def run_attention(K, es, pfx, units, rd_res, epilogue, maxc):
    nc, P = K.nc, K.P
    wmax = max(u["nq"] for u in units) * 128
    spb = 512 // wmax
    nbank = (maxc + spb - 1) // spb
    S = [[_tile(es, nc, f"{pfx}_S{a}_{b}", [128, spb, wmax], F32, psum=True) for b in range(nbank)] for a in range(2)]
    O = _tile(es, nc, f"{pfx}_O", [128, 4, 65], F32, psum=True)
    pT = [[_tile(es, nc, f"{pfx}_pT{a}_{b}", [128, wmax], BF16) for b in range(maxc)] for a in range(2)]

    def emit_S(ui):
        u = units[ui]
        a = ui % 2
        w = u["nq"] * 128
        for ci, (kT, bias, _v) in enumerate(u["chunks"]):
            st, rst = S[a][ci // spb]
            sp = st[:, ci % spb, 0:w]
            P.op("pe", lambda e: e.matmul(sp, lhsT=kT, rhs=u["q"], start=True, stop=(bias is None)), r=rd_res, w=[rst])
            if bias is not None:
                P.op("pe", lambda e: e.matmul(sp, lhsT=bias[0], rhs=bias[1], start=False, stop=True), r=rd_res, w=[rst])
        for ci in range(len(u["chunks"])):
            st, rst = S[a][ci // spb]
            sp = st[:, ci % spb, 0:w]
            pt, rpt = pT[a][ci]
            P.op("act", lambda e: e.activation(out=pt[:, 0:w], in_=sp, func=AF.Exp, scale=0.125), r=[rst], w=[rpt])

    def emit_PV(ui):
        u = units[ui]
        a = ui % 2
        ot, rot = O
        nch = len(u["chunks"])
        for g, h in enumerate(u["heads"]):
            for ci, (_k, _b, v) in enumerate(u["chunks"]):
                pt, rpt = pT[a][ci]
                P.op("pe", lambda e: e.matmul(ot[:, h, :], lhsT=pt[:, g * 128:(g + 1) * 128], rhs=v, start=(ci == 0), stop=(ci == nch - 1)),
                     r=[rpt] + rd_res, w=[rot])
        if u["final"]:
            epilogue(u["j"], ot, rot)

    emit_S(0)
    for ui in range(len(units)):
        if ui + 1 < len(units):
            emit_S(ui + 1)
        emit_PV(ui)


def attn_epilogue_factory(K, es, pfx, ident, rident, yrow0, extra_den=None):
    nc, P, Dm, R = K.nc, K.P, K.dram, K.R
    den, rden = _tile(es, nc, pfx + "_den", [128, 4], F32)
    ybf, rybf = _tile(es, nc, pfx + "_ybf", [128, 256], BF16)
    pT2, rpT2 = _tile(es, nc, pfx + "_pT2", [128, 2, 128], BF16, psum=True)
    oT, roT = _tile(es, nc, pfx + "_oT", [128, 2, 128], BF16)

    def epi(j, ot, rot):
        if extra_den is not None:
            P.op("dve", lambda e: e.tensor_tensor(out=den[:], in0=ot[:, :, 64], in1=extra_den[0][:], op=ALU.add), r=[rot, extra_den[1]], w=[rden])
            P.op("dve", lambda e: e.reciprocal(out=den[:], in_=den[:]), r=[rden], w=[rden])
        else:
            P.op("dve", lambda e: e.reciprocal(out=den[:], in_=ot[:, :, 64]), r=[rot], w=[rden])
        P.op("dve", lambda e: e.tensor_tensor(out=ybf[:].rearrange("p (h d) -> p h d", d=64), in0=ot[:, :, 0:64],
                                              in1=den[:].unsqueeze(2).to_broadcast([128, 4, 64]), op=ALU.mult), r=[rot, rden], w=[rybf])
        for k in range(2):
            P.op("pe", lambda e: e.transpose(out=pT2[:, k, :], in_=ybf[:, k * 128:(k + 1) * 128], identity=ident[:]), r=[rybf, rident], w=[rpT2])
        P.op("act", lambda e: e.copy(out=oT[:], in_=pT2[:]), r=[rpT2], w=[roT])
        P.dma(STQ, Dm["yT"][yrow0:yrow0 + 256, :].rearrange("(k p) n -> p k n", p=128)[:, :, j * 128:(j + 1) * 128], oT[:], r=[roT], w=[R["y"]])
    return epi


def stage_gqa(K, l, with_ctx, qtiles=None):
    nc, P, Dm, R = K.nc, K.P, K.dram, K.R
    with contextlib.ExitStack() as es:
        qT, rqT = _tile(es, nc, "G_qT", [64, 4, T], BF16)
        kT, rkT = _tile(es, nc, "G_kT", [64, 2, T], BF16)
        V, rV = _tile(es, nc, "G_V", [128, NT, 2, 65], BF16)
        mk, rmk = _tile(es, nc, "G_mask", [128, 2, 256], BF16)
        ident, rident = _tile(es, nc, "G_ident", [128, 128], BF16)
        esk, resk = _tile(es, nc, "G_esink", [128, 4], F32)
        for h in range(4):
            P.dma("sp", qT[:, h, :], Dm["gqkT"][h * 64:(h + 1) * 64, :], r=[R["mix"]], w=[rqT])
        for kv in range(2):
            P.dma("sp", kT[:, kv, :], Dm["gqkT"][256 + kv * 64:256 + (kv + 1) * 64, :], r=[R["mix"]], w=[rkT])
            P.dma("sp", V[:, :, kv, 0:64], Dm["gv"][:, kv * 64:(kv + 1) * 64].rearrange("(c p) d -> p c d", p=128), r=[R["mix"]], w=[rV])
        P.op("pool", lambda e: e.memset(V[:, :, :, 64:65], 1.0), w=[rV])
        P.dma("pool", mk[:], Dm["gqa_mask"], w=[rmk])
        P.dma("pool", ident[:], Dm["ident"], w=[rident])
        P.dma("sp", esk[:], Dm["gqa_sink"][l].partition_broadcast(128), w=[resk])
        P.op("act", lambda e: e.activation(out=esk[:], in_=esk[:], func=AF.Exp), r=[resk], w=[resk])
        rd = [rqT, rkT, rV, rmk, rident]
        epi = attn_epilogue_factory(K, es, "G", ident, rident, 768, extra_den=(esk, resk))
        qtiles = qtiles if qtiles is not None else list(range(NXT)) + ([64, 65] if with_ctx else [])
        units = []
        for j in qtiles:
            if j < NXT:
                ch = [(c, tag) for c, tag in ((j - 1, 0), (j, None), (j + 1, 1)) if 0 <= c < NXT] + [(64, None), (65, None)]
            else:
                ch = [(64, None), (65, None)]
            for kv in range(2):
                chunks = []
                for c, tag in ch:
                    bias = None if tag is None else (ident[:], mk[:, tag, :])
                    chunks.append((kT[:, kv, c * 128:(c + 1) * 128], bias, V[:, c, kv, :]))
                units.append(dict(j=j, q=qT[:, 2 * kv:2 * kv + 2, j * 128:(j + 1) * 128], nq=2, heads=[2 * kv, 2 * kv + 1], chunks=chunks, final=(kv == 1)))
        run_attention(K, es, "G", units, rd, epi, 5)
    P.barrier()


SCRATCH.update({"yT": ([1024, T], BF16)})


def stage_na(K, l, with_ctx, qtiles=None):
    nc, P, Dm, R = K.nc, K.P, K.dram, K.R
    with contextlib.ExitStack() as es:
        qT, rqT = _tile(es, nc, "N_qT", [128, 2, T], BF16)
        kT, rkT = _tile(es, nc, "N_kT", [128, 2, T], BF16)
        V, rV = _tile(es, nc, "N_V", [128, NT, 4, 65], BF16)
        Bp, rBp = _tile(es, nc, "N_Bp", [128, NA_NCLS, 4, 128], BF16)
        jx, rjx = _tile(es, nc, "N_jx", [128, 128], BF16)
        ident, rident = _tile(es, nc, "N_ident", [128, 128], BF16)
        zt, rzt = _tile(es, nc, "N_zero", [64, 128], F32)
        rp, rrp = _tile(es, nc, "N_rpb", [15, 4, 31], F32)
        stg = [_tile(es, nc, f"N_stg{i}", [128, 2, 64], F32) for i in range(2)]
        rms_ = [_tile(es, nc, f"N_rm{i}", [128, 128], F32) for i in range(2)]
        rpad = Res("rpbpad")
        for c2 in range(2):
            P.dma("sp", qT[:, c2, :], Dm["nqkT"][c2 * 128:(c2 + 1) * 128, :], r=[R["mix"]], w=[rqT])
            P.dma("sp", kT[:, c2, :], Dm["nqkT"][256 + c2 * 128:256 + (c2 + 1) * 128, :], r=[R["mix"]], w=[rkT])
        for h in range(4):
            P.dma("sp", V[:, :, h, 0:64], Dm["nv"][:, h * 64:(h + 1) * 64].rearrange("(c p) d -> p c d", p=128), r=[R["mix"]], w=[rV])
        P.op("pool", lambda e: e.memset(V[:, :, :, 64:65], 1.0), w=[rV])
        P.dma("pool", jx[:], Dm["na_jx"], w=[rjx])
        P.dma("pool", ident[:], Dm["ident"], w=[rident])
        P.op("dve", lambda e: e.memset(zt[:], 0.0), w=[rzt])
        P.dma("sp", Dm["rpbpad"].rearrange("h r j -> (h r) j"), zt[:], r=[rzt], w=[rpad])
        P.dma("sp", rp[:], Dm["na_rpb"][l].rearrange("h r j -> r h j"), w=[rrp])
        for h in range(4):
            P.dma("sp", Dm["rpbpad"][h, 0:15, 48:79], rp[:, h, :], r=[rrp], w=[rpad])
        padt = Dm["rpbpad"].tensor
        cl = na_class_list()
        for cls, (j, cch) in enumerate(cl):
            rmt, rrmt = rms_[cls % 2]
            P.dma("sp", rmt[:], Dm["na_rm"][cls], w=[rrmt])
            for h in range(4):
                st, rst = stg[(cls * 4 + h) % 2]
                for rq in range(2):
                    dr0 = 2 * (cch - j) + 0 - rq + 7
                    src = bass.AP(tensor=padt, offset=(h * 16 + dr0) * 128, ap=[[1, 64], [128, 2], [1, 64]])
                    P.dma("sp" if rq == 0 else "act", st[rq * 64:(rq + 1) * 64, :, :], src, r=[rpad], w=[rst])
                P.op("dve", lambda e: e.scalar_tensor_tensor(out=Bp[:, cls, h, :], in0=st[:].rearrange("p a b -> p (a b)"), scalar=8.0, in1=rmt[:],
                                                             op0=ALU.mult, op1=ALU.add), r=[rst, rrmt], w=[rBp])
        rd = [rqT, rkT, rV, rBp, rjx, rident]
        epi = attn_epilogue_factory(K, es, "N", ident, rident, 256)
        qtiles = qtiles if qtiles is not None else list(range(NXT)) + ([64, 65] if with_ctx else [])
        units = []
        for j in qtiles:
            ch = (na_chunks(j) if j < NXT else []) + [(64, None), (65, None)]
            for h in range(4):
                pb, c2 = (h % 2) * 64, h // 2
                chunks = []
                for c, cls in ch:
                    bias = None if cls is None else (Bp[:, cls, h, :], jx[:])
                    chunks.append((kT[pb:pb + 64, c2, c * 128:(c + 1) * 128], bias, V[:, c, h, :]))
                units.append(dict(j=j, q=qT[pb:pb + 64, c2, j * 128:(j + 1) * 128], nq=1, heads=[h], chunks=chunks, final=(h == 3)))
        run_attention(K, es, "N", units, rd, epi, 7)
    P.barrier()


SCRATCH.update({"rpbpad": ([4, 16, 128], F32)})


def stage_ret(K, l, with_ctx, out_chunks=None):
    nc, P, Dm, R = K.nc, K.P, K.dram, K.R
    LN8 = math.log(0.125)
    with contextlib.ExitStack() as es:
        qT, rqT = _tile(es, nc, "R_qT", [128, 2, T], BF16)
        kT, rkT = _tile(es, nc, "R_kT", [128, 2, T], BF16)
        Kt, rKt = _tile(es, nc, "R_Kt", [128, NT, 256], BF16)
        Vt, rVt = _tile(es, nc, "R_Vt", [128, NT, 256], BF16)
        SF, rSF = _tile(es, nc, "R_SF", [128, NT, 2, 64], BF16)
        ident, rident = _tile(es, nc, "R_ident", [128, 128], BF16)
        lg, rlg = _tile(es, nc, "R_lg", [128, 8], F32)
        cst, rcst = _tile(es, nc, "R_cst", [128, 5, 128], F32)
        pcol, rpcol = _tile(es, nc, "R_pcol", [128, 2], F32)
        lnc, rlnc = _tile(es, nc, "R_lnc", [128, 2], F32)
        tmp, rtmp = _tile(es, nc, "R_tmp", [128, 128], F32)
        DT, rDT = _tile(es, nc, "R_DT", [128, 4, 128], F32)
        QF, rQF = _tile(es, nc, "R_QF", [128, 2, 128], F32)
        QB, rQB = _tile(es, nc, "R_QB", [128, 2, 128], F32)
        KD, rKD = _tile(es, nc, "R_KD", [128, 8], F32)
        CF, rCF = _tile(es, nc, "R_CF", [128, 2, 64], F32)
        CB, rCB = _tile(es, nc, "R_CB", [128, 2, 64], F32)
        SM, rSM = _tile(es, nc, "R_SM", [128, 2, 64], F32)
        SBc = [_tile(es, nc, f"R_SBc{i}", [128, 2, 64], BF16) for i in range(2)]
        kw, rkw = _tile(es, nc, "R_kw", [128, 256], BF16)
        PT = [_tile(es, nc, f"R_PT{i}", [128, 4, 128], BF16) for i in range(2)]
        qf, rqf = _tile(es, nc, "R_qf", [128, 2, 128], BF16)
        qb, rqb = _tile(es, nc, "R_qb", [128, 2, 128], BF16)
        gt = [_tile(es, nc, f"R_g{i}", [128, 256], F32) for i in range(2)]
        sq, rsq = _tile(es, nc, "R_sq", [128, 256], F32)
        ss4, rss4 = _tile(es, nc, "R_ss4", [128, 4], F32)
        yf, ryf = _tile(es, nc, "R_yf", [128, 256], F32)
        ybf, rybf = _tile(es, nc, "R_ybf", [128, 256], BF16)
        oT, roT = _tile(es, nc, "R_oT", [128, 2, 128], BF16)
        Sp = [_tile(es, nc, f"R_Sp{i}", [128, 4, 128], F32, psum=True) for i in range(2)]
        Op = [_tile(es, nc, f"R_Op{i}", [128, 4, 64], F32, psum=True) for i in range(2)]
        KVp, rKVp = _tile(es, nc, "R_KVp", [128, 2, 128], F32, psum=True)
        pT2, rpT2 = _tile(es, nc, "R_pT2", [128, 2, 128], BF16, psum=True)

        for c2 in range(2):
            P.dma("sp", qT[:, c2, :], Dm["rqkT"][c2 * 128:(c2 + 1) * 128, :], r=[R["mix"]], w=[rqT])
            P.dma("sp", kT[:, c2, :], Dm["rqkT"][256 + c2 * 128:256 + (c2 + 1) * 128, :], r=[R["mix"]], w=[rkT])
        P.dma("act", Kt[:], Dm["rk"].rearrange("(c p) d -> p c d", p=128), r=[R["mix"]], w=[rKt])
        P.dma("act", Vt[:], Dm["rv"].rearrange("(c p) d -> p c d", p=128), r=[R["mix"]], w=[rVt])
        P.dma("pool", ident[:], Dm["ident"], w=[rident])
        for i, nm in enumerate(["ret_dpos", "ret_dneg", "ret_diag", "ret_tp1", "ret_tr"]):
            P.dma("sp", cst[:, i, :], Dm[nm], w=[rcst])
        P.dma("sp", pcol[:], Dm["ret_pcol"], w=[rpcol])
        P.dma("sp", lg[:], Dm["ret_log_decay"][l].rearrange("a h -> (a h)").partition_broadcast(128), w=[rlg])
        P.op("dve", lambda e: e.memset(lnc[:, 0:1], LN8), w=[rlnc])
        P.op("dve", lambda e: e.memset(lnc[:, 1:2], EPS), w=[rlnc])
        P.op("act", lambda e: e.activation(out=lg[:], in_=lg[:], func=AF.Exp), r=[rlg], w=[rlg])
        P.op("dve", lambda e: e.tensor_scalar(out=lg[:], in0=lg[:], scalar1=-1.0, scalar2=None, op0=ALU.mult), r=[rlg], w=[rlg])
        for h in range(4):
            pb, c2 = (h % 2) * 64, h // 2
            P.op("dve", lambda e: e.tensor_scalar(out=tmp[:], in0=cst[:, 0, :], scalar1=lg[:, h:h + 1], scalar2=None, op0=ALU.mult), r=[rcst, rlg], w=[rtmp])
            P.op("dve", lambda e: e.scalar_tensor_tensor(out=tmp[:], in0=cst[:, 1, :], scalar=lg[:, 4 + h:5 + h], in1=tmp[:], op0=ALU.mult, op1=ALU.add),
                 r=[rcst, rlg, rtmp], w=[rtmp])
            P.op("dve", lambda e: e.tensor_tensor(out=tmp[:], in0=tmp[:], in1=cst[:, 2, :], op=ALU.add), r=[rtmp, rcst], w=[rtmp])
            P.op("act", lambda e: e.activation(out=DT[:, h, :], in_=tmp[:], func=AF.Exp), r=[rtmp], w=[rDT])
            P.op("act", lambda e: e.activation(out=QF[pb:pb + 64, c2, :], in_=cst[pb:pb + 64, 3, :], func=AF.Exp, scale=lg[pb:pb + 64, h:h + 1], bias=lnc[pb:pb + 64, 0:1]),
                 r=[rcst, rlg, rlnc], w=[rQF])
            P.op("act", lambda e: e.activation(out=QB[pb:pb + 64, c2, :], in_=cst[pb:pb + 64, 4, :], func=AF.Exp, scale=lg[pb:pb + 64, 4 + h:5 + h], bias=lnc[pb:pb + 64, 0:1]),
                 r=[rcst, rlg, rlnc], w=[rQB])
            P.op("act", lambda e: e.activation(out=KD[:, h:h + 1], in_=lg[:, h:h + 1], func=AF.Exp, scale=pcol[:, 0:1]), r=[rlg, rpcol], w=[rKD])
            P.op("act", lambda e: e.activation(out=KD[:, 4 + h:5 + h], in_=lg[:, 4 + h:5 + h], func=AF.Exp, scale=pcol[:, 1:2]), r=[rlg, rpcol], w=[rKD])
            P.op("act", lambda e: e.activation(out=CF[pb:pb + 64, c2, :], in_=lg[pb:pb + 64, h:h + 1].to_broadcast([64, 64]), func=AF.Exp, scale=128.0), r=[rlg], w=[rCF])
            P.op("act", lambda e: e.activation(out=CB[pb:pb + 64, c2, :], in_=lg[pb:pb + 64, 4 + h:5 + h].to_broadcast([64, 64]), func=AF.Exp, scale=128.0), r=[rlg], w=[rCB])

        def state_update(n, kdoff, Ctab, rCtab):
            P.op("dve", lambda e: e.tensor_tensor(out=kw[:].rearrange("p (h d) -> p h d", d=64), in0=Kt[:, n, :].rearrange("p (h d) -> p h d", d=64),
                                                  in1=KD[:, kdoff:kdoff + 4].unsqueeze(2).to_broadcast([128, 4, 64]), op=ALU.mult), r=[rKt, rKD], w=[rkw])
            for c2 in range(2):
                P.op("pe", lambda e: e.matmul(KVp[:, c2, :], lhsT=kw[:, c2 * 128:(c2 + 1) * 128], rhs=Vt[:, n, c2 * 128:(c2 + 1) * 128], start=True, stop=True),
                     r=[rkw, rVt], w=[rKVp])
            P.op("dve", lambda e: e.tensor_tensor(out=SM[:], in0=SM[:], in1=Ctab[:], op=ALU.mult), r=[rSM, rCtab], w=[rSM])
            for hp in (0, 64):
                P.op("dve", lambda e: e.tensor_tensor(out=SM[hp:hp + 64, :, :], in0=SM[hp:hp + 64, :, :], in1=KVp[hp:hp + 64, :, hp:hp + 64], op=ALU.add),
                     r=[rSM, rKVp], w=[rSM])

        fo = [64, 65] + list(range(NXT))
        P.op("dve", lambda e: e.memset(SM[:], 0.0), w=[rSM])
        for i, n in enumerate(fo):
            P.op("act", lambda e: e.copy(out=SF[:, n, :, :], in_=SM[:]), r=[rSM], w=[rSF])
            if i + 1 < len(fo):
                state_update(n, 0, CF, rCF)

        bo = [65, 64] + list(range(NXT - 1, -1, -1))
        outs = [n for n in bo if (n < NXT or with_ctx)]
        if out_chunks is not None:
            outs = [n for n in outs if n in out_chunks]
        P.op("dve", lambda e: e.memset(SM[:], 0.0), w=[rSM])
        oi = {n: i for i, n in enumerate(outs)}

        def emit_S(n):
            i = oi[n]
            sp, rsp = Sp[i % 2]
            cs = slice(n * 128, (n + 1) * 128)
            for h in range(4):
                pb, c2 = (h % 2) * 64, h // 2
                P.op("pe", lambda e: e.matmul(sp[:, h, :], lhsT=kT[pb:pb + 64, c2, cs], rhs=qT[pb:pb + 64, c2, cs], start=True, stop=True), r=[rkT, rqT], w=[rsp])
            pt, rpt = PT[i % 2]
            P.op("dve", lambda e: e.tensor_tensor(out=pt[:], in0=sp, in1=DT[:], op=ALU.mult), r=[rsp, rDT], w=[rpt])
            g_, rg_ = gt[i % 2]
            P.dma("sp", g_[:], Dm["rg"][cs, :], r=[R["mix"]], w=[rg_])

        def emit_O(n, sbc, rsbc):
            i = oi[n]
            cs = slice(n * 128, (n + 1) * 128)
            pt, rpt = PT[i % 2]
            op_, rop = Op[i % 2]
            g_, rg_ = gt[i % 2]
            P.op("dve", lambda e: e.tensor_tensor(out=qf[:], in0=qT[:, :, cs], in1=QF[:], op=ALU.mult), r=[rqT, rQF], w=[rqf])
            P.op("pool", lambda e: e.tensor_tensor(out=qb[:], in0=qT[:, :, cs], in1=QB[:], op=ALU.mult), r=[rqT, rQB], w=[rqb])
            for h in range(4):
                pb, c2 = (h % 2) * 64, h // 2
                P.op("pe", lambda e: e.matmul(op_[:, h, :], lhsT=pt[:, h, :], rhs=Vt[:, n, h * 64:(h + 1) * 64], start=True, stop=False), r=[rpt, rVt], w=[rop])
                P.op("pe", lambda e: e.matmul(op_[:, h, :], lhsT=qf[pb:pb + 64, c2, :], rhs=SF[pb:pb + 64, n, c2, :], start=False, stop=False), r=[rqf, rSF], w=[rop])
                P.op("pe", lambda e: e.matmul(op_[:, h, :], lhsT=qb[pb:pb + 64, c2, :], rhs=sbc[pb:pb + 64, c2, :], start=False, stop=True), r=[rqb, rsbc], w=[rop])
            P.op("act", lambda e: e.activation(out=sq[:].rearrange("p (h d) -> p h d", d=64), in_=op_, func=AF.Square), r=[rop], w=[rsq])
            P.op("dve", lambda e: e.tensor_reduce(out=ss4[:], in_=sq[:].rearrange("p (h d) -> p h d", d=64), axis=AX.X, op=ALU.add), r=[rsq], w=[rss4])
            P.op("act", lambda e: e.activation(out=ss4[:], in_=ss4[:], func=AF.Sqrt, scale=1.0 / 64, bias=lnc[:, 1:2]), r=[rss4, rlnc], w=[rss4])
            P.op("dve", lambda e: e.reciprocal(out=ss4[:], in_=ss4[:]), r=[rss4], w=[rss4])
            P.op("dve", lambda e: e.tensor_tensor(out=yf[:].rearrange("p (h d) -> p h d", d=64), in0=op_, in1=ss4[:].unsqueeze(2).to_broadcast([128, 4, 64]), op=ALU.mult),
                 r=[rop, rss4], w=[ryf])
            P.op("dve", lambda e: e.tensor_tensor(out=ybf[:], in0=yf[:], in1=g_[:], op=ALU.mult), r=[ryf, rg_], w=[rybf])
            for k in range(2):
                P.op("pe", lambda e: e.transpose(out=pT2[:, k, :], in_=ybf[:, k * 128:(k + 1) * 128], identity=ident[:]), r=[rybf, rident], w=[rpT2])
            P.op("act", lambda e: e.copy(out=oT[:], in_=pT2), r=[rpT2], w=[roT])
            P.dma(STQ, Dm["yT"][0:256, :].rearrange("(k p) n -> p k n", p=128)[:, :, cs], oT[:], r=[roT], w=[R["y"]])

        if outs:
            emit_S(outs[0])
        for bi, n in enumerate(bo):
            sbc, rsbc = SBc[bi % 2]
            if n in oi:
                P.op("act", lambda e: e.copy(out=sbc[:], in_=SM[:]), r=[rSM], w=[rsbc])
                i = oi[n]
                if i + 1 < len(outs):
                    emit_S(outs[i + 1])
                emit_O(n, sbc, rsbc)
            if bi + 1 < len(bo):
                state_update(n, 4, CB, rCB)
    P.barrier()


def _sin_reduced(P, out, in_, shift, t1, rt1, t2, rt2, r_in, w_out):
    i2p = 1.0 / (2 * math.pi)
    P.op("dve", lambda e: e.tensor_scalar(out=t1, in0=in_, scalar1=i2p, scalar2=shift * i2p, op0=ALU.mult, op1=ALU.add), r=r_in, w=[rt1])
    P.op("dve", lambda e: e.tensor_scalar(out=t2, in0=t1, scalar1=MAGIC, scalar2=None, op0=ALU.add), r=[rt1], w=[rt2])
    P.op("dve", lambda e: e.tensor_scalar(out=t2, in0=t2, scalar1=MAGIC, scalar2=None, op0=ALU.subtract), r=[rt2], w=[rt2])
    P.op("dve", lambda e: e.tensor_tensor(out=t1, in0=t1, in1=t2, op=ALU.subtract), r=[rt1, rt2], w=[rt1])
    P.op("act", lambda e: e.activation(out=out, in_=t1, func=AF.Sin, scale=2 * math.pi), r=[rt1], w=w_out)


def stage_s5(K, l, with_ctx, epi_tiles=None):
    nc, P, Dm, R = K.nc, K.P, K.dram, K.R
    TC = S5_TC
    NCH = T // TC
    with contextlib.ExitStack() as es:
        uT, ruT = _tile(es, nc, "S_uT", [128, 2, T], BF16)
        ident, rident = _tile(es, nc, "S_ident", [128, 128], BF16)
        identf, ridentf = _tile(es, nc, "S_identf", [128, 128], F32)
        BbT, rBbT = _tile(es, nc, "S_BbT", [128, 2, 2, 8, 128], BF16)
        Cm, rCm = _tile(es, nc, "S_Cm", [128, 4, 2, 128], BF16)
        RD, rRD = _tile(es, nc, "S_RD", [128, 2, 8], F32)
        TH, rTH = _tile(es, nc, "S_TH", [128, 2, 8], F32)
        COS, rCOS = _tile(es, nc, "S_COS", [128, 8, TC], F32)
        SIN, rSIN = _tile(es, nc, "S_SIN", [128, 8, TC], F32)
        iota1, riota1 = _tile(es, nc, "S_iota1", [128, TC], F32)
        P.dma("sp", uT[:, 0, :], Dm["suT"][0:128, :], r=[R["mix"]], w=[ruT])
        P.dma("sp", uT[:, 1, :], Dm["suT"][128:256, :], r=[R["mix"]], w=[ruT])
        P.dma("sp", identf[:], Dm["ident"], w=[ridentf])
        P.dma("pool", ident[:], Dm["ident"], w=[rident])
        P.dma("sp", iota1[:], Dm["s5_iota1"], w=[riota1])

        with contextlib.ExitStack() as es2:
            LRt, rLR = _tile(es2, nc, "S_LR", [128, 8], F32)
            LIt, rLI = _tile(es2, nc, "S_LI", [128, 8], F32)
            DTt, rDTt = _tile(es2, nc, "S_DT", [128, 8], F32)
            w8 = [_tile(es2, nc, f"S_w8_{i}", [128, 8], F32) for i in range(8)]
            BRt, rBRt = _tile(es2, nc, "S_BR", [128, 8, 16], F32)
            BIt, rBIt = _tile(es2, nc, "S_BI", [128, 8, 16], F32)
            bb = [_tile(es2, nc, f"S_bb{i}", [128, 8, 16], F32) for i in range(4)]
            Zp, rZp = _tile(es2, nc, "S_Zp", [128, 8, 128], F32)
            Cn, rCn = _tile(es2, nc, "S_Cn", [128, 128], F32)
            bdm, rbdm = _tile(es2, nc, "S_bdm", [128, 128], F32)
            tp, rtp = _tile(es2, nc, "S_tp", [128, 128], F32, psum=True)
            P.dma("sp", bdm[:], Dm["s5_bdmask"], w=[rbdm])
            with nc.allow_non_contiguous_dma(reason="small parameter tables"):
                P.dma("sp", BRt[:], Dm["s5_b_re"][l].rearrange("(gp a) p c -> (a p) gp c", a=2), w=[rBRt])
                P.dma("sp", BIt[:], Dm["s5_b_im"][l].rearrange("(gp a) p c -> (a p) gp c", a=2), w=[rBIt])
            for dirn in range(2):
                with nc.allow_non_contiguous_dma(reason="small parameter tables"):
                    P.dma("sp", LRt[:], Dm["s5_lambda_re"][l, dirn].rearrange("(gp a) p -> (a p) gp", a=2), w=[rLR])
                    P.dma("sp", LIt[:], Dm["s5_lambda_im"][l, dirn].rearrange("(gp a) p -> (a p) gp", a=2), w=[rLI])
                    for a in range(2):
                        src = Dm["s5_log_step"][l, dirn].rearrange("(gp a) -> a gp", a=2)[a].partition_broadcast(64)
                        P.dma("sp", DTt[a * 64:(a + 1) * 64, :], src, w=[rDTt])
                (dt_, rdt), (mag, rmag), (sn, rsn), (cs_, rcs), (t1, rt1), (t2, rt2), (cr, rcr), (ci, rci) = w8
                P.op("act", lambda e: e.activation(out=dt_[:], in_=DTt[:], func=AF.Exp), r=[rDTt], w=[rdt])
                P.op("dve", lambda e: e.tensor_tensor(out=mag[:], in0=LRt[:], in1=dt_[:], op=ALU.mult), r=[rLR, rdt], w=[rmag])
                P.op("act", lambda e: e.activation(out=RD[:, dirn, :], in_=mag[:], func=AF.Exp), r=[rmag], w=[rRD])
                P.op("dve", lambda e: e.tensor_tensor(out=TH[:, dirn, :], in0=LIt[:], in1=dt_[:], op=ALU.mult), r=[rLI, rdt], w=[rTH])
                _sin_reduced(P, sn[:], TH[:, dirn, :], 0.0, t1[:], rt1, t2[:], rt2, [rTH], [rsn])
                _sin_reduced(P, cs_[:], TH[:, dirn, :], math.pi / 2, t1[:], rt1, t2[:], rt2, [rTH], [rcs])
                P.op("dve", lambda e: e.tensor_tensor(out=cs_[:], in0=cs_[:], in1=RD[:, dirn, :], op=ALU.mult), r=[rcs, rRD], w=[rcs])
                P.op("dve", lambda e: e.tensor_tensor(out=sn[:], in0=sn[:], in1=RD[:, dirn, :], op=ALU.mult), r=[rsn, rRD], w=[rsn])
                P.op("dve", lambda e: e.tensor_scalar(out=cs_[:], in0=cs_[:], scalar1=-1.0, scalar2=None, op0=ALU.add), r=[rcs], w=[rcs])
                P.op("dve", lambda e: e.tensor_tensor(out=t1[:], in0=LRt[:], in1=LRt[:], op=ALU.mult), r=[rLR], w=[rt1])
                P.op("dve", lambda e: e.tensor_tensor(out=t2[:], in0=LIt[:], in1=LIt[:], op=ALU.mult), r=[rLI], w=[rt2])
                P.op("dve", lambda e: e.tensor_tensor(out=t1[:], in0=t1[:], in1=t2[:], op=ALU.add), r=[rt1, rt2], w=[rt1])
                P.op("dve", lambda e: e.reciprocal(out=t1[:], in_=t1[:]), r=[rt1], w=[rt1])
                P.op("dve", lambda e: e.tensor_tensor(out=cr[:], in0=cs_[:], in1=LRt[:], op=ALU.mult), r=[rcs, rLR], w=[rcr])
                P.op("dve", lambda e: e.tensor_tensor(out=t2[:], in0=sn[:], in1=LIt[:], op=ALU.mult), r=[rsn, rLI], w=[rt2])
                P.op("dve", lambda e: e.tensor_tensor(out=cr[:], in0=cr[:], in1=t2[:], op=ALU.add), r=[rcr, rt2], w=[rcr])
                P.op("dve", lambda e: e.tensor_tensor(out=cr[:], in0=cr[:], in1=t1[:], op=ALU.mult), r=[rcr, rt1], w=[rcr])
                P.op("dve", lambda e: e.tensor_tensor(out=ci[:], in0=sn[:], in1=LRt[:], op=ALU.mult), r=[rsn, rLR], w=[rci])
                P.op("dve", lambda e: e.tensor_tensor(out=t2[:], in0=cs_[:], in1=LIt[:], op=ALU.mult), r=[rcs, rLI], w=[rt2])
                P.op("dve", lambda e: e.tensor_tensor(out=ci[:], in0=ci[:], in1=t2[:], op=ALU.subtract), r=[rci, rt2], w=[rci])
                P.op("dve", lambda e: e.tensor_tensor(out=ci[:], in0=ci[:], in1=t1[:], op=ALU.mult), r=[rci, rt1], w=[rci])
                crb = cr[:].unsqueeze(2).to_broadcast([128, 8, 16])
                cib = ci[:].unsqueeze(2).to_broadcast([128, 8, 16])
                (b0, rb0), (b1, rb1), (b2, rb2), (b3, rb3) = bb
                P.op("dve", lambda e: e.tensor_tensor(out=b0[:], in0=BRt[:], in1=crb, op=ALU.mult), r=[rBRt, rcr], w=[rb0])
                P.op("dve", lambda e: e.tensor_tensor(out=b1[:], in0=BIt[:], in1=cib, op=ALU.mult), r=[rBIt, rci], w=[rb1])
                P.op("dve", lambda e: e.tensor_tensor(out=b0[:], in0=b0[:], in1=b1[:], op=ALU.subtract), r=[rb0, rb1], w=[rb0])
                P.op("dve", lambda e: e.tensor_tensor(out=b2[:], in0=BIt[:], in1=crb, op=ALU.mult), r=[rBIt, rcr], w=[rb2])
                P.op("dve", lambda e: e.tensor_tensor(out=b3[:], in0=BRt[:], in1=cib, op=ALU.mult), r=[rBRt, rci], w=[rb3])
                P.op("dve", lambda e: e.tensor_tensor(out=b2[:], in0=b2[:], in1=b3[:], op=ALU.add), r=[rb2, rb3], w=[rb2])
                for ri_, (bsrc, rbsrc) in enumerate(((b0, rb0), (b2, rb2))):
                    P.op("dve", lambda e: e.memset(Zp[:], 0.0), w=[rZp])
                    for gp in range(8):
                        for a in range(2):
                            col0 = ((2 * gp + a) % 8) * 16
                            P.op("dve", lambda e: e.tensor_copy(out=Zp[a * 64:(a + 1) * 64, gp, col0:col0 + 16], in_=bsrc[a * 64:(a + 1) * 64, gp, :]), r=[rbsrc], w=[rZp])
                    for gp in range(8):
                        P.op("pe", lambda e: e.transpose(out=tp, in_=Zp[:, gp, :], identity=identf[:]), r=[rZp, ridentf], w=[rtp])
                        P.op("act", lambda e: e.copy(out=BbT[:, dirn, ri_, gp, :], in_=tp), r=[rtp], w=[rBbT])
            for half in range(2):
                for src_name, kinds in (("s5_c_re", ((0, 1.0), (1, -1.0))), ("s5_c_im", ((2, -1.0),))):
                    srcv = Dm[src_name][l].rearrange("g co p -> (g co) p")[half * 128:(half + 1) * 128, :]
                    P.dma("sp", Cn[:, 0:64], srcv, w=[rCn])
                    P.dma("sp", Cn[:, 64:128], srcv, w=[rCn])
                    P.op("dve", lambda e: e.tensor_tensor(out=Cn[:], in0=Cn[:], in1=bdm[:], op=ALU.mult), r=[rCn, rbdm], w=[rCn])
                    P.op("pe", lambda e: e.transpose(out=tp, in_=Cn[:], identity=identf[:]), r=[rCn, ridentf], w=[rtp])
                    for kidx, sgn in kinds:
                        P.op("act", lambda e: e.activation(out=Cm[:, kidx, half, :], in_=tp, func=AF.Copy, scale=sgn), r=[rtp], w=[rCm])
        P.barrier()

        with contextlib.ExitStack() as es3:
            Bp = [[_tile(es3, nc, f"S_Bp{i}{j}", [128, TC], F32, psum=True) for j in range(2)] for i in range(2)]
            Yp = [[_tile(es3, nc, f"S_Yp{i}{j}", [128, 256], F32, psum=True) for j in range(2)] for i in range(2)]
            tq = [[_tile(es3, nc, f"S_tq{i}{j}", [128, TC], F32) for j in range(4)] for i in range(2)]
            bp_ = [[_tile(es3, nc, f"S_bp{i}{j}", [128, TC], F32) for j in range(2)] for i in range(2)]
            Wt = [[_tile(es3, nc, f"S_W{i}{j}", [128, TC], F32) for j in range(2)] for i in range(2)]
            Pr = [[_tile(es3, nc, f"S_Pr{i}{j}", [128, TC], BF16) for j in range(4)] for i in range(2)]
            XR, rXR = _tile(es3, nc, "S_XR", [128, 8], F32)
            XI, rXI = _tile(es3, nc, "S_XI", [128, 8], F32)
            tc1, rtc1 = _tile(es3, nc, "S_tc1", [128, 1], F32)
            ysb = [_tile(es3, nc, f"S_ysb{i}", [128, 2, 256], F32) for i in range(2)]
            ang, rang = _tile(es3, nc, "S_ang", [128, 8, TC], F32)
            at2, rat2 = _tile(es3, nc, "S_at2", [128, 8, TC], F32)
            for dirn in range(2):
                for gp in range(8):
                    P.op("dve", lambda e: e.tensor_scalar(out=ang[:, gp, :], in0=iota1[:], scalar1=TH[:, dirn, gp:gp + 1], scalar2=None, op0=ALU.mult), r=[riota1, rTH], w=[rang])
                _sin_reduced(P, SIN[:].rearrange("p a b -> p (a b)"), ang[:].rearrange("p a b -> p (a b)"), 0.0,
                             COS[:].rearrange("p a b -> p (a b)"), rCOS, at2[:].rearrange("p a b -> p (a b)"), rat2, [rang], [rSIN])
                _sin_reduced(P, COS[:].rearrange("p a b -> p (a b)"), ang[:].rearrange("p a b -> p (a b)"), math.pi / 2,
                             ang[:].rearrange("p a b -> p (a b)"), rang, at2[:].rearrange("p a b -> p (a b)"), rat2, [rang], [rCOS])
                P.op("dve", lambda e: e.memset(XR[:], 0.0), w=[rXR])
                P.op("dve", lambda e: e.memset(XI[:], 0.0), w=[rXI])
                ydst = Dm["s5yf"] if dirn == 0 else Dm["s5yb"]
                for ck in range(NCH):
                    if dirn == 0:
                        c0 = L if ck == 0 else (ck - 1) * TC
                    else:
                        c0 = L if ck == 0 else L - ck * TC
                    yp = Yp[ck % 2]
                    for gp in range(8):
                        par = gp % 2
                        ct = gp // 4
                        half, gl = gp // 4, gp % 4
                        usl = uT[:, ct, c0:c0 + TC]
                        if dirn == 1:
                            usl = usl[:, ::-1]
                        (bre, rbre), (bim, rbim) = Bp[par]
                        P.op("pe", lambda e: e.matmul(bre, lhsT=BbT[:, dirn, 0, gp, :], rhs=usl, start=True, stop=True), r=[rBbT, ruT], w=[rbre])
                        P.op("pe", lambda e: e.matmul(bim, lhsT=BbT[:, dirn, 1, gp, :], rhs=usl, start=True, stop=True), r=[rBbT, ruT], w=[rbim])
                        (q1, rq1), (q2, rq2), (q3, rq3), (q4, rq4) = tq[par]
                        cosg, sing = COS[:, gp, :], SIN[:, gp, :]
                        P.op("dve", lambda e: e.tensor_tensor(out=q1[:], in0=bre, in1=cosg, op=ALU.mult), r=[rbre, rCOS], w=[rq1])
                        P.op("dve", lambda e: e.tensor_tensor(out=q2[:], in0=bim, in1=sing, op=ALU.mult), r=[rbim, rSIN], w=[rq2])
                        P.op("dve", lambda e: e.tensor_tensor(out=q3[:], in0=bim, in1=cosg, op=ALU.mult), r=[rbim, rCOS], w=[rq3])
                        P.op("dve", lambda e: e.tensor_tensor(out=q4[:], in0=bre, in1=sing, op=ALU.mult), r=[rbre, rSIN], w=[rq4])
                        (br2, rbr2), (bi2, rbi2) = bp_[par]
                        P.op("pool", lambda e: e.tensor_tensor(out=br2[:], in0=q1[:], in1=q2[:], op=ALU.add), r=[rq1, rq2], w=[rbr2])
                        P.op("pool", lambda e: e.tensor_tensor(out=bi2[:], in0=q3[:], in1=q4[:], op=ALU.subtract), r=[rq3, rq4], w=[rbi2])
                        (wr, rwr), (wi, rwi) = Wt[par]
                        rdb = RD[:, dirn, gp:gp + 1].to_broadcast([128, TC])
                        P.op("dve", lambda e: e.tensor_tensor_scan(out=wr[:], data0=rdb, data1=br2[:], initial=XR[:, gp:gp + 1], op0=ALU.mult, op1=ALU.add),
                             r=[rRD, rbr2, rXR], w=[rwr])
                        P.op("dve", lambda e: e.tensor_tensor_scan(out=wi[:], data0=rdb, data1=bi2[:], initial=XI[:, gp:gp + 1], op0=ALU.mult, op1=ALU.add),
                             r=[rRD, rbi2, rXI], w=[rwi])
                        cl, sl = COS[:, gp, TC - 1:TC], SIN[:, gp, TC - 1:TC]
                        P.op("dve", lambda e: e.tensor_tensor(out=tc1[:], in0=wi[:, TC - 1:TC], in1=sl, op=ALU.mult), r=[rwi, rSIN], w=[rtc1])
                        P.op("dve", lambda e: e.scalar_tensor_tensor(out=XR[:, gp:gp + 1], in0=wr[:, TC - 1:TC], scalar=cl, in1=tc1[:], op0=ALU.mult, op1=ALU.subtract),
                             r=[rwr, rCOS, rtc1], w=[rXR])
                        P.op("dve", lambda e: e.tensor_tensor(out=tc1[:], in0=wi[:, TC - 1:TC], in1=cl, op=ALU.mult), r=[rwi, rCOS], w=[rtc1])
                        P.op("dve", lambda e: e.scalar_tensor_tensor(out=XI[:, gp:gp + 1], in0=wr[:, TC - 1:TC], scalar=sl, in1=tc1[:], op0=ALU.mult, op1=ALU.add),
                             r=[rwr, rSIN, rtc1], w=[rXI])
                        (pcc, rpcc), (pis, rpis), (prs, rprs), (pic, rpic) = Pr[par]
                        ov = (lambda t_: t_[:, ::-1]) if dirn == 1 else (lambda t_: t_[:])
                        P.op("dve", lambda e: e.tensor_tensor(out=ov(pcc), in0=wr[:], in1=cosg, op=ALU.mult), r=[rwr, rCOS], w=[rpcc])
                        P.op("pool", lambda e: e.tensor_tensor(out=ov(pis), in0=wi[:], in1=sing, op=ALU.mult), r=[rwi, rSIN], w=[rpis])
                        P.op("dve", lambda e: e.tensor_tensor(out=ov(prs), in0=wr[:], in1=sing, op=ALU.mult), r=[rwr, rSIN], w=[rprs])
                        P.op("pool", lambda e: e.tensor_tensor(out=ov(pic), in0=wi[:], in1=cosg, op=ALU.mult), r=[rwi, rCOS], w=[rpic])
                        for sub in range(TC // 128):
                            ypt, rypt = yp[sub]
                            osl = ypt[:, gp * 32:(gp + 1) * 32]
                            cs_sl = slice(gl * 32, (gl + 1) * 32)
                            ssl = slice(sub * 128, (sub + 1) * 128)
                            P.op("pe", lambda e: e.matmul(osl, lhsT=pcc[:, ssl], rhs=Cm[:, 0, half, cs_sl], start=True, stop=False), r=[rpcc, rCm], w=[rypt])
                            P.op("pe", lambda e: e.matmul(osl, lhsT=pis[:, ssl], rhs=Cm[:, 1, half, cs_sl], start=False, stop=False), r=[rpis, rCm], w=[rypt])
                            P.op("pe", lambda e: e.matmul(osl, lhsT=prs[:, ssl], rhs=Cm[:, 2, half, cs_sl], start=False, stop=False), r=[rprs, rCm], w=[rypt])
                            P.op("pe", lambda e: e.matmul(osl, lhsT=pic[:, ssl], rhs=Cm[:, 2, half, cs_sl], start=False, stop=True), r=[rpic, rCm], w=[rypt])
                    ys, rys = ysb[ck % 2]
                    for sub in range(TC // 128):
                        ypt, rypt = yp[sub]
                        P.op("act", lambda e: e.copy(out=ys[:, sub, :], in_=ypt), r=[rypt], w=[rys])
                    P.dma("sp", ydst[c0:c0 + TC, :].rearrange("(s p) d -> p s d", p=128), ys[:], r=[rys], w=[R["s5y"]])
        P.barrier()

        with contextlib.ExitStack() as es4:
            dsk, rdsk = _tile(es4, nc, "S_dsk", [128, 256], F32)
            glb, rglb = _tile(es4, nc, "S_glb", [128, 256], F32)
            glw, rglw = _tile(es4, nc, "S_glw", [128, 2, 256], BF16)
            ut = [_tile(es4, nc, f"S_ut{i}", [128, 3, 256], F32) for i in range(2)]
            z, rz = _tile(es4, nc, "S_z", [128, 256], F32)
            zg, rzg = _tile(es4, nc, "S_zg", [128, 256], F32)
            zgb, rzgb = _tile(es4, nc, "S_zgb", [128, 256], BF16)
            zT, rzT = _tile(es4, nc, "S_zT", [128, 2, 128], BF16)
            sg, rsg = _tile(es4, nc, "S_sg", [128, 256], F32)
            ob, rob = _tile(es4, nc, "S_ob", [128, 256], BF16)
            oT, roT = _tile(es4, nc, "S_oT", [128, 2, 128], BF16)
            pT2, rpT2 = _tile(es4, nc, "S_pT2", [128, 2, 128], BF16, psum=True)
            pT3, rpT3 = _tile(es4, nc, "S_pT3", [128, 2, 128], BF16, psum=True)
            gp_, rgp_ = _tile(es4, nc, "S_gps", [128, 256], F32, psum=True)
            P.dma("sp", dsk[:], Dm["s5_d"][l].partition_broadcast(128), w=[rdsk])
            P.dma("sp", glb[:], Dm["s5_glu_b"][l].partition_broadcast(128), w=[rglb])
            P.dma("pool", glw[:], Dm["s5_glu_w"][l].rearrange("(k p) n -> p k n", p=128), w=[rglw])
            tiles = list(range(NXT)) + ([64, 65] if with_ctx else [])
            if epi_tiles is not None:
                tiles = [t for t in tiles if t in epi_tiles]

            def load(i):
                t = tiles[i]
                u_, ru_ = ut[i % 2]
                ts = slice(t * 128, (t + 1) * 128)
                P.dma("sp", u_[:, 0, :], Dm["su"][ts, :], r=[R["mix"]], w=[ru_])
                P.dma("sp", u_[:, 1, :], Dm["s5yf"][ts, :], r=[R["s5y"]], w=[ru_])
                P.dma("sp", u_[:, 2, :], Dm["s5yb"][ts, :], r=[R["s5y"]], w=[ru_])
            if tiles:
                load(0)
            for i, t in enumerate(tiles):
                if i + 1 < len(tiles):
                    load(i + 1)
                u_, ru_ = ut[i % 2]
                ts = slice(t * 128, (t + 1) * 128)
                P.op("dve", lambda e: e.tensor_tensor(out=z[:], in0=u_[:, 0, :], in1=dsk[:], op=ALU.mult), r=[ru_, rdsk], w=[rz])
                P.op("dve", lambda e: e.tensor_tensor(out=z[:], in0=z[:], in1=u_[:, 1, :], op=ALU.add), r=[rz, ru_], w=[rz])
                P.op("dve", lambda e: e.tensor_tensor(out=z[:], in0=z[:], in1=u_[:, 2, :], op=ALU.add), r=[rz, ru_], w=[rz])
                P.op("act", lambda e: e.activation(out=zg[:], in_=z[:], func=AF.Gelu), r=[rz], w=[rzg])
                P.op("dve", lambda e: e.tensor_copy(out=zgb[:], in_=zg[:]), r=[rzg], w=[rzgb])
                for k in range(2):
                    P.op("pe", lambda e: e.transpose(out=pT2[:, k, :], in_=zgb[:, k * 128:(k + 1) * 128], identity=ident[:]), r=[rzgb, rident], w=[rpT2])
                P.op("act", lambda e: e.copy(out=zT[:], in_=pT2), r=[rpT2], w=[rzT])
                for k in range(2):
                    P.op("pe", lambda e: e.matmul(gp_, lhsT=zT[:, k, :], rhs=glw[:, k, :], start=(k == 0), stop=(k == 1)), r=[rzT, rglw], w=[rgp_])
                P.op("dve", lambda e: e.tensor_tensor(out=sg[:], in0=gp_, in1=glb[:], op=ALU.add), r=[rgp_, rglb], w=[rsg])
                P.op("act", lambda e: e.activation(out=sg[:], in_=sg[:], func=AF.Sigmoid), r=[rsg], w=[rsg])
                P.op("dve", lambda e: e.tensor_tensor(out=ob[:], in0=sg[:], in1=zg[:], op=ALU.mult), r=[rsg, rzg], w=[rob])
                for k in range(2):
                    P.op("pe", lambda e: e.transpose(out=pT3[:, k, :], in_=ob[:, k * 128:(k + 1) * 128], identity=ident[:]), r=[rob, rident], w=[rpT3])
                P.op("act", lambda e: e.copy(out=oT[:], in_=pT3), r=[rpT3], w=[roT])
                P.dma(STQ, Dm["yT"][512:768, :].rearrange("(k p) n -> p k n", p=128)[:, :, ts], oT[:], r=[roT], w=[R["y"]])
    P.barrier()


SCRATCH.update({"s5yf": ([T, 256], F32), "s5yb": ([T, 256], F32)})


def stage_merge(K, l, with_ctx, tiles=None):
    nc, P, Dm, R = K.nc, K.P, K.dram, K.R
    P.pe_relaxed = True
    with contextlib.ExitStack() as es:
        wg, rwg = _tile(es, nc, "M_wg", [128, 8, 4096], BF16)
        wb, rwb = _tile(es, nc, "M_wb", [128, 8, 1024], BF16)
        wo, rwo = _tile(es, nc, "M_wo", [128, 8, 1024], BF16)
        m2, rm2 = _tile(es, nc, "M_m2", [128, 2, 1024], F32)
        ident, rident = _tile(es, nc, "M_ident", [128, 128], BF16)
        yTt = [_tile(es, nc, f"M_yT{i}", [128, 8, 128], BF16) for i in range(2)]
        aTt = [_tile(es, nc, f"M_aT{i}", [128, 8, 128], BF16) for i in range(2)]
        ht = [_tile(es, nc, f"M_h{i}", [128, 1024], F32) for i in range(2)]
        sig = [_tile(es, nc, f"M_sig{i}", [128, 512], F32) for i in range(2)]
        term, rterm = _tile(es, nc, "M_term", [128, 512], F32)
        mg, rmg = _tile(es, nc, "M_mg", [128, 1024], F32)
        mb, rmb = _tile(es, nc, "M_mb", [128, 1024], BF16)
        mT, rmT = _tile(es, nc, "M_mT", [128, 8, 128], BF16)
        hn, rhn = _tile(es, nc, "M_hn", [128, 1024], F32)
        Gp = [_tile(es, nc, f"M_Gp{i}", [128, 512], F32, psum=True) for i in range(2)]
        Zp = [_tile(es, nc, f"M_Zp{i}", [128, 512], F32, psum=True) for i in range(2)]
        pT, rpT = _tile(es, nc, "M_pT", [128, 8, 128], BF16, psum=True)
        Op = [_tile(es, nc, f"M_Op{i}", [128, 512], F32, psum=True) for i in range(2)]
        w_in_v = Dm["w_in"][l].rearrange("(k p) n -> p k n", p=128)
        for i in range(4):
            P.dma("pool", wg[:, :, i * 1024:(i + 1) * 1024], w_in_v[:, :, MIXC + i * 1024:MIXC + (i + 1) * 1024], w=[rwg])
        P.dma("pool", wb[:], Dm["w_branch"][l].rearrange("i (k p) n -> p (i k) n", p=128), w=[rwb])
        P.dma("pool", wo[:], Dm["w_out"][l].rearrange("(k p) n -> p k n", p=128), w=[rwo])
        P.dma("pool", ident[:], Dm["ident"], w=[rident])
        P.dma("sp", m2[:, 0, :], Dm["modv"][2].partition_broadcast(128), r=[R["modv"]], w=[rm2])
        P.dma("sp", m2[:, 1, :], Dm["modv"][8].partition_broadcast(128), r=[R["modv"]], w=[rm2])
        tiles = tiles if tiles is not None else list(range(NXT)) + ([64, 65] if with_ctx else [])
        hsrc = K.hsrc(l)

        def load(i):
            t = tiles[i]
            ts = slice(t * 128, (t + 1) * 128)
            P.dma("sp", yTt[i % 2][0][:], Dm["yT"].rearrange("(k p) n -> p k n", p=128)[:, :, ts], r=[R["y"]], w=[yTt[i % 2][1]])
            P.dma("sp", aTt[i % 2][0][:], Dm["aT"].rearrange("(k p) n -> p k n", p=128)[:, :, ts], r=[R["aT"]], w=[aTt[i % 2][1]])
            P.dma("sp", ht[i % 2][0][:], hsrc(t), r=[R["H"]], w=[ht[i % 2][1]])
        load(0)
        for i, t in enumerate(tiles):
            if i + 1 < len(tiles):
                load(i + 1)
            yt, ryt = yTt[i % 2]
            at, rat = aTt[i % 2]
            h_, rh_ = ht[i % 2]
            ts = slice(t * 128, (t + 1) * 128)
            cnt = 0
            for nh in range(2):
                for br in range(4):
                    gp, rgp = Gp[cnt % 2]
                    zp, rzp = Zp[cnt % 2]
                    sg, rsg = sig[cnt % 2]
                    cnt += 1
                    c0 = br * 1024 + nh * 512
                    for k in range(8):
                        P.op("pe", lambda e: e.matmul(gp, lhsT=at[:, k, :], rhs=wg[:, k, c0:c0 + 512], start=(k == 0), stop=(k == 7)), r=[rat, rwg], w=[rgp])
                    for k2 in range(2):
                        P.op("pe", lambda e: e.matmul(zp, lhsT=yt[:, 2 * br + k2, :], rhs=wb[:, 2 * br + k2, nh * 512:(nh + 1) * 512], start=(k2 == 0), stop=(k2 == 1)),
                             r=[ryt, rwb], w=[rzp])
                    P.op("act", lambda e: e.activation(out=sg[:], in_=gp, func=AF.Sigmoid), r=[rgp], w=[rsg])
                    dst = mg[:, nh * 512:(nh + 1) * 512]
                    if br == 0:
                        P.op("dve", lambda e: e.tensor_tensor(out=dst, in0=zp, in1=sg[:], op=ALU.mult), r=[rzp, rsg], w=[rmg])
                    else:
                        P.op("dve", lambda e: e.tensor_tensor(out=term[:], in0=zp, in1=sg[:], op=ALU.mult), r=[rzp, rsg], w=[rterm])
                        P.op("dve", lambda e: e.tensor_tensor(out=dst, in0=dst, in1=term[:], op=ALU.add), r=[rmg, rterm], w=[rmg])
            P.op("act", lambda e: e.copy(out=mb[:], in_=mg[:]), r=[rmg], w=[rmb])
            for k in range(8):
                P.op("pe", lambda e: e.transpose(out=pT[:, k, :], in_=mb[:, k * 128:(k + 1) * 128], identity=ident[:]), r=[rmb, rident], w=[rpT])
            P.op("act", lambda e: e.copy(out=mT[:], in_=pT), r=[rpT], w=[rmT])
            for nh in range(2):
                op_, rop = Op[nh]
                for k in range(8):
                    P.op("pe", lambda e: e.matmul(op_, lhsT=mT[:, k, :], rhs=wo[:, k, nh * 512:(nh + 1) * 512], start=(k == 0), stop=(k == 7)), r=[rmT, rwo], w=[rop])
                sl = slice(nh * 512, (nh + 1) * 512)
                P.op("dve", lambda e: e.tensor_tensor(out=hn[:, sl], in0=op_, in1=m2[:, 0 if t < NXT else 1, sl], op=ALU.mult), r=[rop, rm2], w=[rhn])
            P.op("dve", lambda e: e.tensor_tensor(out=hn[:], in0=hn[:], in1=h_[:], op=ALU.add), r=[rhn, rh_], w=[rhn])
            P.dma(STQ, Dm["H"][ts, :], hn[:], r=[rhn], w=[R["H"]])
    P.pe_relaxed = False
    P.barrier()


SGT = 12
BIG = 1.0e30


def stage_moe(K, l, with_ctx, last, tiles=None, experts=None):
    nc, P, Dm, R = K.nc, K.P, K.dram, K.R
    tiles = tiles if tiles is not None else list(range(NXT)) + ([64, 65] if with_ctx else [])
    experts = list(range(32)) if experts is None else experts
    with contextlib.ExitStack() as es:
        bc, rbc = _tile(es, nc, "E_bc", [128, 6, 1024], F32)
        identf, ridentf = _tile(es, nc, "E_identf", [128, 128], F32)
        rw, rrw = _tile(es, nc, "E_rw", [128, 8, 36], F32)
        rb, rrb = _tile(es, nc, "E_rb", [128, 36], F32)
        epsc, repsc = _tile(es, nc, "E_eps", [128, 1], F32)
        FT, rFT = _tile(es, nc, "E_FT", [128, 8, SGT * 128], BF16)
        Gall, rGall = _tile(es, nc, "E_G", [128, SGT, 32], F32)
        yacc, ryacc = _tile(es, nc, "E_yacc", [128, SGT, 1024], F32)
        for j, row in enumerate((3, 4, 5, 9, 10, 11)):
            P.dma("sp", bc[:, j, :], Dm["modv"][row].partition_broadcast(128), r=[R["modv"]], w=[rbc])
        P.dma("sp", identf[:], Dm["ident"], w=[ridentf])
        with nc.allow_non_contiguous_dma(reason="tiny router weights"):
            P.dma("sp", rw[:, :, 0:4], Dm["router_w1"][l].rearrange("(k p) n -> p k n", p=128), w=[rrw])
            P.dma("sp", rw[:, :, 4:36], Dm["router_w2"][l].rearrange("(k p) n -> p k n", p=128), w=[rrw])
        P.dma("sp", rb[:, 0:4], Dm["router_b1"][l].partition_broadcast(128), w=[rrb])
        P.dma("sp", rb[:, 4:36], Dm["router_b2"][l].partition_broadcast(128), w=[rrb])
        P.op("dve", lambda e: e.memset(epsc[:], EPS), w=[repsc])
        P.barrier()
        sgs = [tiles[i:i + SGT] for i in range(0, len(tiles), SGT)]
        for sg in sgs:
            with contextlib.ExitStack() as e1:
                ht = [_tile(e1, nc, f"E1_h{i}", [128, 1024], F32) for i in range(2)]
                junk, rjunk = _tile(e1, nc, "E1_junk", [128, 1024], BF16)
                ssq, rssq = _tile(e1, nc, "E1_ssq", [128, 1], F32)
                rstd, rrstd = _tile(e1, nc, "E1_rstd", [128, 1], F32)
                f_, rf_ = _tile(e1, nc, "E1_f", [128, 1024], F32)
                lgt, rlgt = _tile(e1, nc, "E1_lg", [128, 36], F32)
                sm = [_tile(e1, nc, f"E1_s{i}", [128, 8], F32) for i in range(4)]
                oh = [_tile(e1, nc, f"E1_oh{i}", [128, 32], F32) for i in range(3)]
                pTf = [_tile(e1, nc, f"E1_pT{i}", [128, 4, 128], F32, psum=True) for i in range(2)]
                lp, rlp = _tile(e1, nc, "E1_lp", [128, 36], F32, psum=True)

                def load(i):
                    t = sg[i]
                    P.dma("sp", ht[i % 2][0][:], Dm["H"][t * 128:(t + 1) * 128, :], r=[R["H"]], w=[ht[i % 2][1]])
                load(0)
                for i, t in enumerate(sg):
                    if i + 1 < len(sg):
                        load(i + 1)
                    h_, rh_ = ht[i % 2]
                    o = 0 if t < NXT else 3
                    P.op("act", lambda e: e.activation(out=junk[:], in_=h_[:], func=AF.Square, accum_out=ssq[:]), r=[rh_], w=[rjunk, rssq])
                    P.op("act", lambda e: e.activation(out=rstd[:], in_=ssq[:], func=AF.Sqrt, scale=1.0 / D, bias=epsc[:, 0:1]), r=[rssq, repsc], w=[rrstd])
                    P.op("dve", lambda e: e.reciprocal(out=rstd[:], in_=rstd[:]), r=[rrstd], w=[rrstd])
                    P.op("dve", lambda e: e.scalar_tensor_tensor(out=f_[:], in0=h_[:], scalar=rstd[:, 0:1], in1=bc[:, o, :], op0=ALU.mult, op1=ALU.mult),
                         r=[rh_, rrstd, rbc], w=[rf_])
                    P.op("dve", lambda e: e.tensor_tensor(out=f_[:], in0=f_[:], in1=bc[:, o + 1, :], op=ALU.add), r=[rf_, rbc], w=[rf_])
                    for hf in range(2):
                        pt, rpt = pTf[hf]
                        for k in range(4):
                            kk = hf * 4 + k
                            P.op("pe", lambda e: e.transpose(out=pt[:, k, :], in_=f_[:, kk * 128:(kk + 1) * 128], identity=identf[:]), r=[rf_, ridentf], w=[rpt])
                    fTf, rfTf = f_, rf_
                    for hf in range(2):
                        pt, rpt = pTf[hf]
                        P.op("act", lambda e: e.copy(out=fTf[:, hf * 512:(hf + 1) * 512].rearrange("p (k n) -> p k n", n=128), in_=pt), r=[rpt], w=[rfTf])
                    P.op("dve", lambda e: e.tensor_copy(out=FT[:, :, i * 128:(i + 1) * 128], in_=fTf[:].rearrange("p (k n) -> p k n", n=128)), r=[rfTf], w=[rFT])
                    for k in range(8):
                        P.op("pe", lambda e: e.matmul(lp, lhsT=fTf[:, k * 128:(k + 1) * 128], rhs=rw[:, k, :], start=(k == 0), stop=(k == 7)), r=[rfTf, rrw], w=[rlp])
                    P.op("dve", lambda e: e.tensor_tensor(out=lgt[:], in0=lp, in1=rb[:], op=ALU.add), r=[rlp, rrb], w=[rlgt])
                    (s0, rs0), (s1, rs1), (s2, rs2), (s3, rs3) = sm
                    (oh1, roh1), (oh2, roh2), (l2, rl2) = oh
                    P.op("dve", lambda e: e.tensor_reduce(out=s0[:, 0:1], in_=lgt[:, 0:4], axis=AX.X, op=ALU.max), r=[rlgt], w=[rs0])
                    P.op("dve", lambda e: e.tensor_scalar(out=s0[:, 1:2], in0=s0[:, 0:1], scalar1=-1.0, scalar2=None, op0=ALU.mult), r=[rs0], w=[rs0])
                    P.op("act", lambda e: e.activation(out=s1[:, 0:4], in_=lgt[:, 0:4], func=AF.Exp, bias=s0[:, 1:2], accum_out=s0[:, 2:3]), r=[rlgt, rs0], w=[rs1, rs0])
                    P.op("dve", lambda e: e.reciprocal(out=s0[:, 3:4], in_=s0[:, 2:3]), r=[rs0], w=[rs0])
                    P.op("dve", lambda e: e.tensor_scalar(out=s2[:, 0:4], in0=lgt[:, 0:4], scalar1=s0[:, 0:1], scalar2=None, op0=ALU.is_equal), r=[rlgt, rs0], w=[rs2])
                    P.op("dve", lambda e: e.tensor_scalar(out=s2[:, 0:4], in0=s2[:, 0:4], scalar1=BIG, scalar2=-BIG, op0=ALU.mult, op1=ALU.add), r=[rs2], w=[rs2])
                    P.op("dve", lambda e: e.tensor_tensor(out=l2[:].rearrange("p (g e) -> p g e", e=8), in0=lgt[:, 4:36].rearrange("p (g e) -> p g e", e=8),
                                                          in1=s2[:, 0:4].unsqueeze(2).to_broadcast([128, 4, 8]), op=ALU.add), r=[rlgt, rs2], w=[rl2])
                    P.op("dve", lambda e: e.tensor_reduce(out=s3[:, 0:1], in_=l2[:], axis=AX.X, op=ALU.max), r=[rl2], w=[rs3])
                    P.op("dve", lambda e: e.tensor_scalar(out=oh1[:], in0=l2[:], scalar1=s3[:, 0:1], scalar2=None, op0=ALU.is_equal), r=[rl2, rs3], w=[roh1])
                    P.op("dve", lambda e: e.scalar_tensor_tensor(out=l2[:], in0=oh1[:], scalar=-BIG, in1=l2[:], op0=ALU.mult, op1=ALU.add), r=[roh1, rl2], w=[rl2])
                    P.op("dve", lambda e: e.tensor_reduce(out=s3[:, 1:2], in_=l2[:], axis=AX.X, op=ALU.max), r=[rl2], w=[rs3])
                    P.op("dve", lambda e: e.tensor_scalar(out=oh2[:], in0=l2[:], scalar1=s3[:, 1:2], scalar2=None, op0=ALU.is_equal), r=[rl2, rs3], w=[roh2])
                    P.op("dve", lambda e: e.tensor_tensor(out=s3[:, 2:3], in0=s3[:, 1:2], in1=s3[:, 0:1], op=ALU.subtract), r=[rs3], w=[rs3])
                    P.op("act", lambda e: e.activation(out=s3[:, 3:4], in_=s3[:, 2:3], func=AF.Exp), r=[rs3], w=[rs3])
                    P.op("dve", lambda e: e.tensor_scalar(out=s3[:, 4:5], in0=s3[:, 3:4], scalar1=1.0, scalar2=None, op0=ALU.add), r=[rs3], w=[rs3])
                    P.op("dve", lambda e: e.reciprocal(out=s3[:, 4:5], in_=s3[:, 4:5]), r=[rs3], w=[rs3])
                    P.op("dve", lambda e: e.tensor_tensor(out=s3[:, 5:6], in0=s3[:, 4:5], in1=s0[:, 3:4], op=ALU.mult), r=[rs3, rs0], w=[rs3])
                    P.op("dve", lambda e: e.tensor_tensor(out=s3[:, 6:7], in0=s3[:, 5:6], in1=s3[:, 3:4], op=ALU.mult), r=[rs3], w=[rs3])
                    P.op("dve", lambda e: e.tensor_scalar(out=Gall[:, i, :], in0=oh1[:], scalar1=s3[:, 5:6], scalar2=None, op0=ALU.mult), r=[roh1, rs3], w=[rGall])
                    P.op("dve", lambda e: e.scalar_tensor_tensor(out=Gall[:, i, :], in0=oh2[:], scalar=s3[:, 6:7], in1=Gall[:, i, :], op0=ALU.mult, op1=ALU.add),
                         r=[roh2, rs3, rGall], w=[rGall])
            P.barrier()
            with contextlib.ExitStack() as e2:
                w1 = [_tile(e2, nc, f"E2_w1_{i}", [128, 8, 512], BF16) for i in range(2)]
                w3 = [_tile(e2, nc, f"E2_w3_{i}", [128, 8, 512], BF16) for i in range(2)]
                w2 = [_tile(e2, nc, f"E2_w2_{i}", [128, 4, 1024], BF16) for i in range(2)]
                sl = [_tile(e2, nc, f"E2_sl{i}", [128, 512], F32) for i in range(2)]
                hid = [_tile(e2, nc, f"E2_hid{i}", [128, 4, 512], BF16) for i in range(2)]
                H1 = [_tile(e2, nc, f"E2_H1{i}", [128, 512], F32, psum=True) for i in range(2)]
                H3 = [_tile(e2, nc, f"E2_H3{i}", [128, 512], F32, psum=True) for i in range(2)]
                Yp = [[_tile(e2, nc, f"E2_Y{i}{j}", [128, 512], F32, psum=True) for j in range(2)] for i in range(2)]
                groups = [list(range(g0, min(g0 + 4, len(sg)))) for g0 in range(0, len(sg), 4)]

                def loadw(ei):
                    e_ = experts[ei]
                    P.dma("pool", w1[ei % 2][0][:], Dm["exp_w1"][l, e_].rearrange("(k p) n -> p k n", p=128), w=[w1[ei % 2][1]])
                    P.dma("pool", w3[ei % 2][0][:], Dm["exp_w3"][l, e_].rearrange("(k p) n -> p k n", p=128), w=[w3[ei % 2][1]])
                    P.dma("pool", w2[ei % 2][0][:], Dm["exp_w2"][l, e_].rearrange("(k p) n -> p k n", p=128), w=[w2[ei % 2][1]])
                loadw(0)
                cnt = 0
                ycnt = 0
                gi_ = 0
                for ei, e_ in enumerate(experts):
                    if ei + 1 < len(experts):
                        loadw(ei + 1)
                    (w1t, rw1), (w3t, rw3), (w2t, rw2) = w1[ei % 2], w3[ei % 2], w2[ei % 2]
                    for grp in groups:
                        ntok = len(grp) * 128
                        tsl = slice(grp[0] * 128, grp[0] * 128 + ntok)
                        hd, rhd = hid[gi_ % 2]
                        gi_ += 1
                        for fc in range(4):
                            h1, rh1 = H1[cnt % 2]
                            h3, rh3 = H3[cnt % 2]
                            s_, rs_ = sl[cnt % 2]
                            cnt += 1
                            for k in range(8):
                                P.op("pe", lambda e: e.matmul(h1[:, 0:ntok], lhsT=w1t[:, k, fc * 128:(fc + 1) * 128], rhs=FT[:, k, tsl], start=(k == 0), stop=(k == 7)),
                                     r=[rw1, rFT], w=[rh1])
                            for k in range(8):
                                P.op("pe", lambda e: e.matmul(h3[:, 0:ntok], lhsT=w3t[:, k, fc * 128:(fc + 1) * 128], rhs=FT[:, k, tsl], start=(k == 0), stop=(k == 7)),
                                     r=[rw3, rFT], w=[rh3])
                            P.op("act", lambda e: e.activation(out=s_[:, 0:ntok], in_=h1[:, 0:ntok], func=AF.Silu), r=[rh1], w=[rs_])
                            P.op("dve", lambda e: e.tensor_tensor(out=hd[:, fc, 0:ntok], in0=h3[:, 0:ntok], in1=s_[:, 0:ntok], op=ALU.mult), r=[rh3, rs_], w=[rhd])
                        for ti, tt in enumerate(grp):
                            yp = Yp[ycnt % 2]
                            ycnt += 1
                            for nh in range(2):
                                ypt, rypt = yp[nh]
                                for fc in range(4):
                                    P.op("pe", lambda e: e.matmul(ypt, lhsT=hd[:, fc, ti * 128:(ti + 1) * 128], rhs=w2t[:, fc, nh * 512:(nh + 1) * 512], start=(fc == 0), stop=(fc == 3)),
                                         r=[rhd, rw2], w=[rypt])
                                dst = yacc[:, tt, nh * 512:(nh + 1) * 512]
                                if ei == 0:
                                    P.op("dve", lambda e: e.tensor_scalar(out=dst, in0=ypt, scalar1=Gall[:, tt, e_:e_ + 1], scalar2=None, op0=ALU.mult), r=[rypt, rGall], w=[ryacc])
                                else:
                                    P.op("dve", lambda e: e.scalar_tensor_tensor(out=dst, in0=ypt, scalar=Gall[:, tt, e_:e_ + 1], in1=dst, op0=ALU.mult, op1=ALU.add),
                                         r=[rypt, rGall, ryacc], w=[ryacc])
            P.barrier()
            with contextlib.ExitStack() as e3:
                ht = [_tile(e3, nc, f"E3_h{i}", [128, 1024], F32) for i in range(2)]
                hn = [_tile(e3, nc, f"E3_hn{i}", [128, 1024], F32) for i in range(2)]
                for i, t in enumerate(sg):
                    h_, rh_ = ht[i % 2]
                    n_, rn_ = hn[i % 2]
                    ts = slice(t * 128, (t + 1) * 128)
                    P.dma("sp", h_[:], Dm["H"][ts, :], r=[R["H"]], w=[rh_])
                    P.op("dve", lambda e: e.tensor_tensor(out=n_[:], in0=yacc[:, i, :], in1=bc[:, 2 if t < NXT else 5, :], op=ALU.mult), r=[ryacc, rbc], w=[rn_])
                    P.op("dve", lambda e: e.tensor_tensor(out=n_[:], in0=n_[:], in1=h_[:], op=ALU.add), r=[rn_, rh_], w=[rn_])
                    if last:
                        P.dma("pool", Dm["out"][ts, :], n_[:], r=[rn_], w=[R["out"]])
                    else:
                        P.dma("pool", Dm["H"][ts, :], n_[:], r=[rn_], w=[R["H"]])
            P.barrier()
    P.barrier()


def full_plan(nlayers=DEPTH):
    def plan(K):
        for l in range(nlayers):
            with_ctx = l < DEPTH - 1
            last = l == DEPTH - 1
            stage_prep(K, l)
            stage_A(K, l)
            stage_ret(K, l, with_ctx)
            stage_na(K, l, with_ctx)
            stage_s5(K, l, with_ctx)
            stage_gqa(K, l, with_ctx)
            stage_merge(K, l, with_ctx)
            stage_moe_sparse(K, l, with_ctx, last)
    return plan


_CACHE = {}


def kernel(**inputs):
    consts = make_consts()
    n = 8
    x = np.asarray(inputs["x"], np.float32)
    in_maps = []
    shared = {k: np.ascontiguousarray(np.asarray(inputs[k], np.float32)) for k in INPUT_NAMES if k not in ("x", "c", "ctx")}
    for b in range(n):
        m = dict(shared)
        m["x"] = np.ascontiguousarray(x[b])
        m["ctx"] = np.ascontiguousarray(np.asarray(inputs["ctx"], np.float32)[b])
        m["c"] = np.ascontiguousarray(np.asarray(inputs["c"], np.float32)[b:b + 1])
        m.update(consts)
        in_maps.append(m)
    shapes = {k: (v.shape, F32) for k, v in in_maps[0].items() if k not in consts}
    if "nc" not in _CACHE:
        _CACHE["nc"] = build(shapes, consts, full_plan())[0]
    res = run_bass_kernel_spmd(_CACHE["nc"], in_maps, core_ids=list(range(n)))
    return np.stack([np.asarray(r["out"], np.float32) for r in res.results], axis=0)


def _idma(P, out, out_off, in_, in_off, r=(), w=()):
    q = "pool"
    P._deps(q, r, w)
    i = P.dnext[q]
    P.dnext[q] = (i + 1) % P.NDS
    key = ("d", q, i)
    P._wait(q, key, 16 * P.duse[q][i])
    ins = P.nc.gpsimd.indirect_dma_start(out=out, out_offset=out_off, in_=in_, in_offset=in_off)
    P.duse[q][i] += 1
    ins.then_inc(P.semh[key], 16)
    P._mark((key, 16 * P.duse[q][i]), r, w)
    P.ninst += 1
    return ins


def stage_moe_sparse(K, l, with_ctx, last, precast=True, nblocks=None):
    nc, P, Dm, R = K.nc, K.P, K.dram, K.R
    tiles = list(range(NXT)) + ([64, 65] if with_ctx else [])
    NTL = len(tiles)
    B = MOE_B
    NB = -(-(2 * NTL * 128 + 32 * (B - 1)) // B)
    IOA = bass.IndirectOffsetOnAxis
    rwb = Res("wbf")
    rfb = Res("fb")
    rxs = Res("xs")
    rys = Res("ys")
    P.pe_relaxed = True
    if precast:
        with contextlib.ExitStack() as e0:
            st = [_tile(e0, nc, f"E0_st{i}", [128, 4096], F32) for i in range(3)]
            sb = [_tile(e0, nc, f"E0_sb{i}", [128, 4096], BF16) for i in range(3)]
            cnt = 0
            for e_ in range(32):
                for src, dst, kk in (("exp_w1", "wb1", 8), ("exp_w3", "wb3", 8), ("exp_w2", "wb2", 4)):
                    s_, rs_ = st[cnt % 3]
                    b_, rb_ = sb[cnt % 3]
                    eng = ("act", "pool", "dve")[cnt % 3]
                    cnt += 1
                    P.dma("sp", s_[:].rearrange("p (k n) -> p k n", k=kk), Dm[src][l, e_].rearrange("(k p) n -> p k n", p=128), w=[rs_])
                    if eng == "act":
                        P.op("act", lambda e: e.copy(out=b_[:], in_=s_[:]), r=[rs_], w=[rb_])
                    else:
                        P.op(eng, lambda e: e.tensor_copy(out=b_[:], in_=s_[:]), r=[rs_], w=[rb_])
                    P.dma("act", Dm[dst][e_ * 128:(e_ + 1) * 128, :], b_[:], r=[rb_], w=[rwb])
        P.barrier()
    with contextlib.ExitStack() as es:
        bc, rbc = _tile(es, nc, "Q_bc", [128, 6, 1024], F32)
        OH1, rOH1 = _tile(es, nc, "Q_OH1", [128, NTL, 32], F32)
        OH2, rOH2 = _tile(es, nc, "Q_OH2", [128, NTL, 32], F32)
        OH12, rOH12 = _tile(es, nc, "Q_OH12", [128, NTL, 32], BF16)
        GA, rGA = _tile(es, nc, "Q_GA", [128, NTL, 2], F32)
        DST, rDST = _tile(es, nc, "Q_DST", [128, NTL, 2], I32)
        IDXW, rIDXW = _tile(es, nc, "Q_IDXW", [128, NB], I32)
        pstart, rpstart = _tile(es, nc, "Q_pstart", [128, 32], F32)
        onesb, ronesb = _tile(es, nc, "Q_ones", [128, 128], BF16)
        ustr, rustr = _tile(es, nc, "Q_ustr", [128, 128], BF16)
        identb, ridentb = _tile(es, nc, "Q_identb", [128, 128], BF16)
        for j, row in enumerate((3, 4, 5, 9, 10, 11)):
            P.dma("sp", bc[:, j, :], Dm["modv"][row].partition_broadcast(128), r=[R["modv"]], w=[rbc])
        P.dma("pool", onesb[:], Dm["moe_ones"], w=[ronesb])
        P.dma("pool", ustr[:], Dm["moe_ustrict"], w=[rustr])
        P.dma("pool", identb[:], Dm["ident"], w=[ridentb])
        with contextlib.ExitStack() as e1:
            identf, ridentf = _tile(e1, nc, "Q1_identf", [128, 128], F32)
            rw, rrw = _tile(e1, nc, "Q1_rw", [128, 8, 36], F32)
            rb, rrb = _tile(e1, nc, "Q1_rb", [128, 36], F32)
            epsc, repsc = _tile(e1, nc, "Q1_eps", [128, 1], F32)
            ht = [_tile(e1, nc, f"Q1_h{i}", [128, 1024], F32) for i in range(2)]
            junk, rjunk = _tile(e1, nc, "Q1_junk", [128, 1024], BF16)
            ssq, rssq = _tile(e1, nc, "Q1_ssq", [128, 1], F32)
            rstd, rrstd = _tile(e1, nc, "Q1_rstd", [128, 1], F32)
            f_, rf_ = _tile(e1, nc, "Q1_f", [128, 1024], F32)
            fb = [_tile(e1, nc, f"Q1_fb{i}", [128, 1024], BF16) for i in range(2)]
            lgt, rlgt = _tile(e1, nc, "Q1_lg", [128, 36], F32)
            sm = [_tile(e1, nc, f"Q1_s{i}", [128, 8], F32) for i in range(4)]
            l2, rl2 = _tile(e1, nc, "Q1_l2", [128, 32], F32)
            cnts, rcnts = _tile(e1, nc, "Q1_cnt", [128, 32], F32)
            wk = [_tile(e1, nc, f"Q1_wk{i}", [128, 32], F32) for i in range(3)]
            cmpt, rcmpt = _tile(e1, nc, "Q1_cmp", [128, NB, 32], F32)
            bbt, rbbt = _tile(e1, nc, "Q1_bb", [128, NB, 32], F32)
            eblk, reblk = _tile(e1, nc, "Q1_eblk", [128, NB], F32)
            pcol, rpcol = _tile(e1, nc, "Q1_pcol", [128, 1], F32)
            pTf = [_tile(e1, nc, f"Q1_pT{i}", [128, 4, 128], F32, psum=True) for i in range(2)]
            lp, rlp = _tile(e1, nc, "Q1_lp", [128, 36], F32, psum=True)
            cp, rcp = _tile(e1, nc, "Q1_cp", [128, 32], F32, psum=True)
            P.dma("sp", identf[:], Dm["ident"], w=[ridentf])
            with nc.allow_non_contiguous_dma(reason="tiny router weights"):
                P.dma("sp", rw[:, :, 0:4], Dm["router_w1"][l].rearrange("(k p) n -> p k n", p=128), w=[rrw])
                P.dma("sp", rw[:, :, 4:36], Dm["router_w2"][l].rearrange("(k p) n -> p k n", p=128), w=[rrw])
            P.dma("sp", rb[:, 0:4], Dm["router_b1"][l].partition_broadcast(128), w=[rrb])
            P.dma("sp", rb[:, 4:36], Dm["router_b2"][l].partition_broadcast(128), w=[rrb])
            P.dma("sp", bbt[:].rearrange("p a b -> p (a b)"), Dm["moe_bb"][:, 0:NB * 32], w=[rbbt])
            P.dma("sp", pcol[:], Dm["moe_pcol"], w=[rpcol])
            P.op("dve", lambda e: e.memset(epsc[:], EPS), w=[repsc])

            def load(i):
                t = tiles[i]
                P.dma("sp", ht[i % 2][0][:], Dm["H"][t * 128:(t + 1) * 128, :], r=[R["H"]], w=[ht[i % 2][1]])
            load(0)
            for i, t in enumerate(tiles):
                if i + 1 < NTL:
                    load(i + 1)
                h_, rh_ = ht[i % 2]
                fb_, rfb_ = fb[i % 2]
                o = 0 if t < NXT else 3
                P.op("act", lambda e: e.activation(out=junk[:], in_=h_[:], func=AF.Square, accum_out=ssq[:]), r=[rh_], w=[rjunk, rssq])
                P.op("act", lambda e: e.activation(out=rstd[:], in_=ssq[:], func=AF.Sqrt, scale=1.0 / D, bias=epsc[:, 0:1]), r=[rssq, repsc], w=[rrstd])
                P.op("dve", lambda e: e.reciprocal(out=rstd[:], in_=rstd[:]), r=[rrstd], w=[rrstd])
                P.op("dve", lambda e: e.scalar_tensor_tensor(out=f_[:], in0=h_[:], scalar=rstd[:, 0:1], in1=bc[:, o, :], op0=ALU.mult, op1=ALU.mult),
                     r=[rh_, rrstd, rbc], w=[rf_])
                P.op("dve", lambda e: e.tensor_tensor(out=f_[:], in0=f_[:], in1=bc[:, o + 1, :], op=ALU.add), r=[rf_, rbc], w=[rf_])
                P.op("act", lambda e: e.copy(out=fb_[:], in_=f_[:]), r=[rf_], w=[rfb_])
                P.dma("act", Dm["fb"][i * 128:(i + 1) * 128, :], fb_[:], r=[rfb_], w=[rfb])
                for hf in range(2):
                    pt, rpt = pTf[hf]
                    for k in range(4):
                        kk = hf * 4 + k
                        P.op("pe", lambda e: e.transpose(out=pt[:, k, :], in_=f_[:, kk * 128:(kk + 1) * 128], identity=identf[:]), r=[rf_, ridentf], w=[rpt])
                for hf in range(2):
                    pt, rpt = pTf[hf]
                    P.op("act", lambda e: e.copy(out=f_[:, hf * 512:(hf + 1) * 512].rearrange("p (k n) -> p k n", n=128), in_=pt), r=[rpt], w=[rf_])
                for k in range(8):
                    P.op("pe", lambda e: e.matmul(lp, lhsT=f_[:, k * 128:(k + 1) * 128], rhs=rw[:, k, :], start=(k == 0), stop=(k == 7)), r=[rf_, rrw], w=[rlp])
                P.op("dve", lambda e: e.tensor_tensor(out=lgt[:], in0=lp, in1=rb[:], op=ALU.add), r=[rlp, rrb], w=[rlgt])
                (s0, rs0), (s1, rs1), (s2, rs2), (s3, rs3) = sm
                oh1, oh2 = OH1[:, i, :], OH2[:, i, :]
                P.op("dve", lambda e: e.tensor_reduce(out=s0[:, 0:1], in_=lgt[:, 0:4], axis=AX.X, op=ALU.max), r=[rlgt], w=[rs0])
                P.op("dve", lambda e: e.tensor_scalar(out=s0[:, 1:2], in0=s0[:, 0:1], scalar1=-1.0, scalar2=None, op0=ALU.mult), r=[rs0], w=[rs0])
                P.op("act", lambda e: e.activation(out=s1[:, 0:4], in_=lgt[:, 0:4], func=AF.Exp, bias=s0[:, 1:2], accum_out=s0[:, 2:3]), r=[rlgt, rs0], w=[rs1, rs0])
                P.op("dve", lambda e: e.reciprocal(out=s0[:, 3:4], in_=s0[:, 2:3]), r=[rs0], w=[rs0])
                P.op("dve", lambda e: e.tensor_scalar(out=s2[:, 0:4], in0=lgt[:, 0:4], scalar1=s0[:, 0:1], scalar2=None, op0=ALU.is_equal), r=[rlgt, rs0], w=[rs2])
                P.op("dve", lambda e: e.tensor_scalar(out=s2[:, 0:4], in0=s2[:, 0:4], scalar1=BIG, scalar2=-BIG, op0=ALU.mult, op1=ALU.add), r=[rs2], w=[rs2])
                P.op("dve", lambda e: e.tensor_tensor(out=l2[:].rearrange("p (g e) -> p g e", e=8), in0=lgt[:, 4:36].rearrange("p (g e) -> p g e", e=8),
                                                      in1=s2[:, 0:4].unsqueeze(2).to_broadcast([128, 4, 8]), op=ALU.add), r=[rlgt, rs2], w=[rl2])
                P.op("dve", lambda e: e.tensor_reduce(out=s3[:, 0:1], in_=l2[:], axis=AX.X, op=ALU.max), r=[rl2], w=[rs3])
                P.op("dve", lambda e: e.tensor_scalar(out=oh1, in0=l2[:], scalar1=s3[:, 0:1], scalar2=None, op0=ALU.is_equal), r=[rl2, rs3], w=[rOH1])
                P.op("dve", lambda e: e.scalar_tensor_tensor(out=l2[:], in0=oh1, scalar=-BIG, in1=l2[:], op0=ALU.mult, op1=ALU.add), r=[rOH1, rl2], w=[rl2])
                P.op("dve", lambda e: e.tensor_reduce(out=s3[:, 1:2], in_=l2[:], axis=AX.X, op=ALU.max), r=[rl2], w=[rs3])
                P.op("dve", lambda e: e.tensor_scalar(out=oh2, in0=l2[:], scalar1=s3[:, 1:2], scalar2=None, op0=ALU.is_equal), r=[rl2, rs3], w=[rOH2])
                P.op("dve", lambda e: e.tensor_tensor(out=OH12[:, i, :], in0=oh1, in1=oh2, op=ALU.add), r=[rOH1, rOH2], w=[rOH12])
                P.op("dve", lambda e: e.tensor_tensor(out=s3[:, 2:3], in0=s3[:, 1:2], in1=s3[:, 0:1], op=ALU.subtract), r=[rs3], w=[rs3])
                P.op("act", lambda e: e.activation(out=s3[:, 3:4], in_=s3[:, 2:3], func=AF.Exp), r=[rs3], w=[rs3])
                P.op("dve", lambda e: e.tensor_scalar(out=s3[:, 4:5], in0=s3[:, 3:4], scalar1=1.0, scalar2=None, op0=ALU.add), r=[rs3], w=[rs3])
                P.op("dve", lambda e: e.reciprocal(out=s3[:, 4:5], in_=s3[:, 4:5]), r=[rs3], w=[rs3])
                P.op("dve", lambda e: e.tensor_tensor(out=GA[:, i, 0:1], in0=s3[:, 4:5], in1=s0[:, 3:4], op=ALU.mult), r=[rs3, rs0], w=[rGA])
                P.op("dve", lambda e: e.tensor_tensor(out=GA[:, i, 1:2], in0=GA[:, i, 0:1], in1=s3[:, 3:4], op=ALU.mult), r=[rGA, rs3], w=[rGA])
                P.op("pe", lambda e: e.matmul(cp, lhsT=onesb[:], rhs=OH12[:, i, :], start=(i == 0), stop=(i == NTL - 1)), r=[ronesb, rOH12], w=[rcp])
            (a0, ra0), (a1, ra1), (a2, ra2) = wk
            P.op("dve", lambda e: e.tensor_copy(out=cnts[:], in_=cp), r=[rcp], w=[rcnts])
            P.op("dve", lambda e: e.tensor_scalar(out=a0[:], in0=cnts[:], scalar1=float(B - 1), scalar2=1.0 / B, op0=ALU.add, op1=ALU.mult), r=[rcnts], w=[ra0])
            P.op("dve", lambda e: e.tensor_scalar(out=a0[:], in0=a0[:], scalar1=-(B - 1) / (2.0 * B), scalar2=None, op0=ALU.add), r=[ra0], w=[ra0])
            P.op("dve", lambda e: e.tensor_scalar(out=a0[:], in0=a0[:], scalar1=MAGIC, scalar2=None, op0=ALU.add), r=[ra0], w=[ra0])
            P.op("dve", lambda e: e.tensor_scalar(out=a0[:], in0=a0[:], scalar1=MAGIC, scalar2=None, op0=ALU.subtract), r=[ra0], w=[ra0])
            P.op("dve", lambda e: e.tensor_scalar(out=a0[:], in0=a0[:], scalar1=float(B), scalar2=None, op0=ALU.mult), r=[ra0], w=[ra0])
            P.op("dve", lambda e: e.memset(a2[:], 1.0), w=[ra2])
            P.op("dve", lambda e: e.tensor_tensor_scan(out=a1[:], data0=a2[:], data1=a0[:], initial=0.0, op0=ALU.mult, op1=ALU.add), r=[ra2, ra0], w=[ra1])
            P.op("dve", lambda e: e.tensor_tensor(out=pstart[:], in0=a1[:], in1=a0[:], op=ALU.subtract), r=[ra1, ra0], w=[rpstart])
            P.op("dve", lambda e: e.tensor_tensor(out=cmpt[:], in0=a1[:].unsqueeze(1).to_broadcast([128, NB, 32]), in1=bbt[:], op=ALU.is_le), r=[ra1, rbbt], w=[rcmpt])
            P.op("dve", lambda e: e.tensor_reduce(out=eblk[:], in_=cmpt[:], axis=AX.X, op=ALU.add), r=[rcmpt], w=[reblk])
            P.op("dve", lambda e: e.tensor_scalar(out=eblk[:], in0=eblk[:], scalar1=31.0, scalar2=128.0, op0=ALU.min, op1=ALU.mult), r=[reblk], w=[reblk])
            P.op("dve", lambda e: e.tensor_scalar(out=eblk[:], in0=eblk[:], scalar1=pcol[:, 0:1], scalar2=None, op0=ALU.add), r=[reblk, rpcol], w=[reblk])
            P.op("dve", lambda e: e.tensor_copy(out=IDXW[:], in_=eblk[:]), r=[reblk], w=[rIDXW])
        P.barrier()
        with contextlib.ExitStack() as e3:
            base, rbase = _tile(e3, nc, "Q3_base", [128, 32], F32)
            sb_, rsb_ = _tile(e3, nc, "Q3_sb", [128, 32], F32)
            pr, rpr = _tile(e3, nc, "Q3_pr", [128, 32], F32)
            dd, rdd = _tile(e3, nc, "Q3_dd", [128, 2], F32)
            fbt = [_tile(e3, nc, f"Q3_fb{i}", [128, 1024], BF16) for i in range(2)]
            rp_, rrp_ = _tile(e3, nc, "Q3_rp", [128, 32], F32, psum=True)
            csp, rcsp = _tile(e3, nc, "Q3_cs", [128, 32], F32, psum=True)
            P.op("dve", lambda e: e.memset(base[:], 0.0), w=[rbase])
            zt, rzt = _tile(e3, nc, "Q3_zero", [128, 7, 1024], BF16)
            P.op("pool", lambda e: e.memset(zt[:], 0.0), w=[rzt])
            xsv = Dm["xs"][0:NB * B, :].rearrange("(p a) d -> p a d", p=128)
            na = NB * B // 128
            for a0_ in range(0, na, 7):
                a1_ = min(a0_ + 7, na)
                P.dma("sp" if (a0_ // 7) % 2 == 0 else "act", xsv[:, a0_:a1_, :], zt[:, 0:a1_ - a0_, :], r=[rzt], w=[rxs])
            for i in range(NTL):
                f2, rf2 = fbt[i % 2]
                P.dma("sp", f2[:], Dm["fb"][i * 128:(i + 1) * 128, :], r=[rfb], w=[rf2])
                P.op("pe", lambda e: e.matmul(rp_, lhsT=ustr[:], rhs=OH12[:, i, :], start=True, stop=True), r=[rustr, rOH12], w=[rrp_])
                P.op("pe", lambda e: e.matmul(csp, lhsT=onesb[:], rhs=OH12[:, i, :], start=True, stop=True), r=[ronesb, rOH12], w=[rcsp])
                P.op("dve", lambda e: e.tensor_tensor(out=sb_[:], in0=rp_, in1=base[:], op=ALU.add), r=[rrp_, rbase], w=[rsb_])
                P.op("dve", lambda e: e.tensor_tensor(out=sb_[:], in0=sb_[:], in1=pstart[:], op=ALU.add), r=[rsb_, rpstart], w=[rsb_])
                for k, (OHk, rOHk) in enumerate(((OH1, rOH1), (OH2, rOH2))):
                    P.op("dve", lambda e: e.tensor_tensor(out=pr[:], in0=sb_[:], in1=OHk[:, i, :], op=ALU.mult), r=[rsb_, rOHk], w=[rpr])
                    P.op("dve", lambda e: e.tensor_reduce(out=dd[:, k:k + 1], in_=pr[:], axis=AX.X, op=ALU.add), r=[rpr], w=[rdd])
                P.op("dve", lambda e: e.tensor_copy(out=DST[:, i, :], in_=dd[:]), r=[rdd], w=[rDST])
                P.op("dve", lambda e: e.tensor_tensor(out=base[:], in0=base[:], in1=csp, op=ALU.add), r=[rbase, rcsp], w=[rbase])
                for k in range(2):
                    _idma(P, Dm["xs"][0:NB * B, :], IOA(DST[:, i, k:k + 1], 0), f2[:], None, r=[rf2, rDST], w=[rxs])
        P.barrier()
        with contextlib.ExitStack() as e4:
            w1 = [_tile(e4, nc, f"Q4_w1_{i}", [128, 8, 512], BF16) for i in range(2)]
            w3 = [_tile(e4, nc, f"Q4_w3_{i}", [128, 8, 512], BF16) for i in range(2)]
            w2 = [_tile(e4, nc, f"Q4_w2_{i}", [128, 4, 1024], BF16) for i in range(2)]
            xsb = [_tile(e4, nc, f"Q4_xs{i}", [128, 2, 1024], BF16) for i in range(2)]
            xT = [_tile(e4, nc, f"Q4_xT{i}", [128, 8, B], BF16) for i in range(2)]
            sl = [_tile(e4, nc, f"Q4_sl{i}", [128, B], F32) for i in range(2)]
            hid = [_tile(e4, nc, f"Q4_hid{i}", [128, 4, B], BF16) for i in range(2)]
            ysb = [_tile(e4, nc, f"Q4_ys{i}", [128, 1024], F32) for i in range(2)]
            pT, rpT = _tile(e4, nc, "Q4_pT", [128, 8, 128], BF16, psum=True)
            H1 = [_tile(e4, nc, f"Q4_H1{i}", [128, B], F32, psum=True) for i in range(2)]
            H3 = [_tile(e4, nc, f"Q4_H3{i}", [128, B], F32, psum=True) for i in range(2)]
            Yp = [_tile(e4, nc, f"Q4_Y{i}", [128, 512], F32, psum=True) for i in range(2)]
            nbl = NB if nblocks is None else nblocks

            def loadb(b):
                _idma(P, w1[b % 2][0][:].rearrange("p k n -> p (k n)"), None, Dm["wb1"], IOA(IDXW[:, b:b + 1], 0), r=[rwb, rIDXW], w=[w1[b % 2][1]])
                _idma(P, w3[b % 2][0][:].rearrange("p k n -> p (k n)"), None, Dm["wb3"], IOA(IDXW[:, b:b + 1], 0), r=[rwb, rIDXW], w=[w3[b % 2][1]])
                _idma(P, w2[b % 2][0][:].rearrange("p k n -> p (k n)"), None, Dm["wb2"], IOA(IDXW[:, b:b + 1], 0), r=[rwb, rIDXW], w=[w2[b % 2][1]])
                P.dma("sp", xsb[b % 2][0][:], Dm["xs"][b * B:(b + 1) * B, :].rearrange("(s p) d -> p s d", p=128), r=[rxs], w=[xsb[b % 2][1]])
            loadb(0)
            cnt = 0
            yc = 0
            for b in range(nbl):
                if b + 1 < nbl:
                    loadb(b + 1)
                (w1t, rw1), (w3t, rw3), (w2t, rw2) = w1[b % 2], w3[b % 2], w2[b % 2]
                xb, rxb = xsb[b % 2]
                xt, rxt = xT[b % 2]
                hd, rhd = hid[b % 2]
                for s_ in range(2):
                    for k in range(8):
                        P.op("pe", lambda e: e.transpose(out=pT[:, k, :], in_=xb[:, s_, k * 128:(k + 1) * 128], identity=identb[:]), r=[rxb, ridentb], w=[rpT])
                    P.op("act", lambda e: e.copy(out=xt[:, :, s_ * 128:(s_ + 1) * 128], in_=pT), r=[rpT], w=[rxt])
                for fc in range(4):
                    h1, rh1 = H1[cnt % 2]
                    h3, rh3 = H3[cnt % 2]
                    sl_, rsl_ = sl[cnt % 2]
                    cnt += 1
                    for k in range(8):
                        P.op("pe", lambda e: e.matmul(h1, lhsT=w1t[:, k, fc * 128:(fc + 1) * 128], rhs=xt[:, k, :], start=(k == 0), stop=(k == 7)), r=[rw1, rxt], w=[rh1])
                    for k in range(8):
                        P.op("pe", lambda e: e.matmul(h3, lhsT=w3t[:, k, fc * 128:(fc + 1) * 128], rhs=xt[:, k, :], start=(k == 0), stop=(k == 7)), r=[rw3, rxt], w=[rh3])
                    P.op("act", lambda e: e.activation(out=sl_[:], in_=h1, func=AF.Silu), r=[rh1], w=[rsl_])
                    P.op("dve", lambda e: e.tensor_tensor(out=hd[:, fc, :], in0=h3, in1=sl_[:], op=ALU.mult), r=[rh3, rsl_], w=[rhd])
                for s_ in range(2):
                    ys_, rys_ = ysb[yc % 2]
                    yc += 1
                    for nh in range(2):
                        ypt, rypt = Yp[nh]
                        for fc in range(4):
                            P.op("pe", lambda e: e.matmul(ypt, lhsT=hd[:, fc, s_ * 128:(s_ + 1) * 128], rhs=w2t[:, fc, nh * 512:(nh + 1) * 512], start=(fc == 0), stop=(fc == 3)),
                                 r=[rhd, rw2], w=[rypt])
                        if nh == 0:
                            P.op("act", lambda e: e.copy(out=ys_[:, 0:512], in_=ypt), r=[rypt], w=[rys_])
                        else:
                            P.op("dve", lambda e: e.tensor_copy(out=ys_[:, 512:1024], in_=ypt), r=[rypt], w=[rys_])
                    P.dma("sp", Dm["ys"][b * B + s_ * 128:b * B + (s_ + 1) * 128, :], ys_[:], r=[rys_], w=[rys])
        P.barrier()
        with contextlib.ExitStack() as e5:
            y1 = [_tile(e5, nc, f"Q5_y1{i}", [128, 1024], F32) for i in range(2)]
            y2 = [_tile(e5, nc, f"Q5_y2{i}", [128, 1024], F32) for i in range(2)]
            ht = [_tile(e5, nc, f"Q5_h{i}", [128, 1024], F32) for i in range(2)]
            hn = [_tile(e5, nc, f"Q5_hn{i}", [128, 1024], F32) for i in range(2)]

            def load5(i):
                t = tiles[i]
                _idma(P, y1[i % 2][0][:], None, Dm["ys"][0:NB * B, :], IOA(DST[:, i, 0:1], 0), r=[rys, rDST], w=[y1[i % 2][1]])
                _idma(P, y2[i % 2][0][:], None, Dm["ys"][0:NB * B, :], IOA(DST[:, i, 1:2], 0), r=[rys, rDST], w=[y2[i % 2][1]])
                P.dma("sp", ht[i % 2][0][:], Dm["H"][t * 128:(t + 1) * 128, :], r=[R["H"]], w=[ht[i % 2][1]])
            load5(0)
            for i, t in enumerate(tiles):
                if i + 1 < NTL:
                    load5(i + 1)
                (a_, ra_), (b_, rb_), (h_, rh_), (n_, rn_) = y1[i % 2], y2[i % 2], ht[i % 2], hn[i % 2]
                ts = slice(t * 128, (t + 1) * 128)
                P.op("dve", lambda e: e.tensor_scalar(out=n_[:], in0=a_[:], scalar1=GA[:, i, 0:1], scalar2=None, op0=ALU.mult), r=[ra_, rGA], w=[rn_])
                P.op("dve", lambda e: e.scalar_tensor_tensor(out=n_[:], in0=b_[:], scalar=GA[:, i, 1:2], in1=n_[:], op0=ALU.mult, op1=ALU.add), r=[rb_, rGA, rn_], w=[rn_])
                P.op("pool", lambda e: e.tensor_tensor(out=n_[:], in0=n_[:], in1=bc[:, 2 if t < NXT else 5, :], op=ALU.mult), r=[rn_, rbc], w=[rn_])
                P.op("dve", lambda e: e.tensor_tensor(out=n_[:], in0=n_[:], in1=h_[:], op=ALU.add), r=[rn_, rh_], w=[rn_])
                if last:
                    P.dma("act", Dm["out"][ts, :], n_[:], r=[rn_], w=[R["out"]])
                else:
                    P.dma("act", Dm["H"][ts, :], n_[:], r=[rn_], w=[R["H"]])
    P.pe_relaxed = False
    P.barrier()


SCRATCH.update({"wb1": ([32 * 128, 4096], BF16), "wb3": ([32 * 128, 4096], BF16), "wb2": ([32 * 128, 4096], BF16),
                "fb": ([T, 1024], BF16), "xs": ([MOE_NBMAX * MOE_B, 1024], BF16), "ys": ([MOE_NBMAX * MOE_B, 1024], F32)})
```

```python
import contextlib
import math
import numpy as np
import ml_dtypes
import concourse.bass as bass
import concourse.mybir as mybir
from concourse.bass_utils import run_bass_kernel_spmd

F32 = mybir.dt.float32
BF16 = mybir.dt.bfloat16
I32 = mybir.dt.int32
AF = mybir.ActivationFunctionType
ALU = mybir.AluOpType
AX = mybir.AxisListType

D = 1024
L = 8192
NCTX = 256
T = L + NCTX
NT = T // 128
NXT = L // 128
DEPTH = 4
MIXC = 2560
EPS = 1e-6
SAME_SYNC = True
ATT_RELAX = True
STQ = "pool"
NEGM = -240000.0


class Res:
    __slots__ = ("name", "w", "rd")

    def __init__(self, name=""):
        self.name = name
        self.w = None
        self.rd = {}


class Prog:
    NDS = 12

    def __init__(self, nc, same_sync=True, dma_queues=("sp", "pool", "act")):
        self.nc = nc
        self.E = {"pe": nc.tensor, "dve": nc.vector, "act": nc.scalar, "pool": nc.gpsimd, "sp": nc.sync}
        self.same_sync = same_sync
        self.semh = {}
        self.cnt = {}
        for k in self.E:
            self.semh[("c", k)] = nc.alloc_semaphore(f"sc_{k}")
            self.cnt[k] = 0
        self.seen = {k: {} for k in self.E}
        self.duse = {}
        self.dnext = {}
        for q in dma_queues:
            self.duse[q] = [0] * self.NDS
            self.dnext[q] = 0
            for i in range(self.NDS):
                self.semh[("d", q, i)] = nc.alloc_semaphore(f"sd_{q}_{i}")
        self.ninst = 0
        self.pe_relaxed = False

    def _wait(self, eng, key, val):
        if val <= 0 or self.seen[eng].get(key, 0) >= val:
            return
        self.E[eng].wait_ge(self.semh[key], val)
        self.seen[eng][key] = val

    def _deps(self, eng, r, w):
        deps = {}
        for res in r:
            if res.w is not None:
                k, v = res.w
                if deps.get(k, 0) < v:
                    deps[k] = v
        for res in w:
            if res.w is not None:
                k, v = res.w
                if deps.get(k, 0) < v:
                    deps[k] = v
            for k, v in res.rd.items():
                if deps.get(k, 0) < v:
                    deps[k] = v
        for k, v in deps.items():
            if k == ("c", eng) and ((eng == "pe" and self.pe_relaxed) or not self.same_sync):
                continue
            self._wait(eng, k, v)

    def _mark(self, tok, r, w):
        k, v = tok
        for res in r:
            if res.rd.get(k, 0) < v:
                res.rd[k] = v
        for res in w:
            res.w = tok
            res.rd = {}

    def op(self, eng, fn, r=(), w=()):
        self._deps(eng, r, w)
        ins = fn(self.E[eng])
        self.cnt[eng] += 1
        ins.then_inc(self.semh[("c", eng)], 1)
        self._mark((("c", eng), self.cnt[eng]), r, w)
        self.ninst += 1
        return ins

    def dma(self, q, out, in_, r=(), w=(), **kw):
        self._deps(q, r, w)
        i = self.dnext[q]
        self.dnext[q] = (i + 1) % self.NDS
        key = ("d", q, i)
        self._wait(q, key, 16 * self.duse[q][i])
        ins = self.E[q].dma_start(out=out, in_=in_, **kw)
        self.duse[q][i] += 1
        ins.then_inc(self.semh[key], 16)
        self._mark((key, 16 * self.duse[q][i]), r, w)
        self.ninst += 1
        return ins

    def barrier(self, engines=None):
        engines = engines or list(self.E)
        for eng in engines:
            for k in self.E:
                if k != eng:
                    self._wait(eng, ("c", k), self.cnt[k])
            for q in self.duse:
                for i in range(self.NDS):
                    self._wait(eng, ("d", q, i), 16 * self.duse[q][i])


class Ctx:
    pass


_TCNT = [0]


def _tile(es, nc, name, shape, dt, psum=False):
    _TCNT[0] += 1
    name = f"{name}_{_TCNT[0]}"
    if not psum:
        t = es.enter_context(nc.sbuf_tensor(name, shape, dt))
        return t, Res(name)
    esz = 2 if dt == BF16 else 4
    n = int(np.prod(shape[1:]))
    per_bank = 2048 // esz
    nb = (n + per_bank - 1) // per_bank
    t = es.enter_context(nc.psum_tensor(name, [128, nb * per_bank], dt))
    ap = t[0:shape[0], 0:n]
    if len(shape) == 3:
        ap = ap.rearrange("p (a b) -> p a b", b=shape[2])
    elif len(shape) == 4:
        ap = ap.rearrange("p (a b c) -> p a b c", b=shape[2], c=shape[3])
    return ap, Res(name)


NA_NCLS = 21
MOE_B = 256
MOE_NBMAX = 98
S5_TC = 256
MAGIC = 12582912.0


def na_class_list():
    lst = [(10, 10 + dc) for dc in (-2, -1, 0, 1, 2)]
    for j in (0, 1):
        lst += [(j, c) for c in range(4)]
    for j in (62, 63):
        lst += [(j, c) for c in range(60, 64)]
    return lst


def na_chunks(j):
    if 2 <= j <= 61:
        return [(j + dc, dc + 2) for dc in (-2, -1, 0, 1, 2)]
    base = {0: 5, 1: 9, 62: 13, 63: 17}[j]
    c0 = 0 if j < 2 else 60
    return [(c0 + i, base + i) for i in range(4)]


def make_consts():
    c = {}
    pos = np.arange(L)
    inv = (10000.0 ** (-np.arange(16, dtype=np.float32) / 16)).astype(np.float32)
    ang_r = (pos // 64).astype(np.float32)[:, None] * inv
    ang_c = (pos % 64).astype(np.float32)[:, None] * inv
    cr, sr, cc, sc = np.cos(ang_r), np.sin(ang_r), np.cos(ang_c), np.sin(ang_c)
    cosf = np.concatenate([cr, cr, cc, cc], axis=1)
    sinf = np.concatenate([-sr, sr, -sc, sc], axis=1)
    cosf = np.concatenate([cosf, np.ones((NCTX, 64))], axis=0)
    sinf = np.concatenate([sinf, np.zeros((NCTX, 64))], axis=0)
    c["ropecs"] = np.concatenate([cosf, sinf], axis=1).astype(np.float32)
    c["ident"] = np.eye(128, dtype=np.float32)
    kl = np.arange(128)[:, None]
    ql = np.arange(128)[None, :]
    lo = np.where(kl >= ql, 0.0, NEGM)
    hi = np.where(kl <= ql, 0.0, NEGM)
    c["na_jx"] = np.zeros((128, 128), np.float32)
    for q in range(128):
        c["na_jx"][(q // 64) * 64 + 63 - q % 64, q] = 1.0
    rm = np.zeros((NA_NCLS, 128, 128), np.float32)
    for cls, (j, cch) in enumerate(na_class_list()):
        for qp in range(128):
            rq = 2 * j + qp // 64
            cq = 63 - qp % 64
            r0 = min(max(rq - 4, 0), 120)
            ws = min(max(cq - 8, 0), 48)
            for key in range(128):
                rk = 2 * cch + key // 64
                ck = key % 64
                ok = (r0 <= rk < r0 + 8) and (ws <= ck < ws + 16)
                rm[cls, qp, key] = 0.0 if ok else NEGM
    c["na_rm"] = rm
    si = np.arange(128, dtype=np.float32)[:, None]
    ti = np.arange(128, dtype=np.float32)[None, :]
    c["ret_dpos"] = np.maximum(ti - si, 0.0).astype(np.float32)
    c["ret_dneg"] = np.maximum(si - ti, 0.0).astype(np.float32)
    c["ret_diag"] = ((si == ti) * math.log(2.0) + math.log(0.125)).astype(np.float32)
    c["ret_tp1"] = np.broadcast_to(ti + 1.0, (128, 128)).astype(np.float32).copy()
    c["ret_tr"] = np.broadcast_to(128.0 - ti, (128, 128)).astype(np.float32).copy()
    c["ret_pcol"] = np.concatenate([127.0 - si, si], axis=1).astype(np.float32)
    c["s5_iota1"] = np.broadcast_to(np.arange(1, S5_TC + 1, dtype=np.float32)[None, :], (128, S5_TC)).copy()
    bm = np.zeros((128, 128), np.float32)
    for r in range(128):
        a = (r // 16) % 2
        bm[r, a * 64:(a + 1) * 64] = 1.0
    c["s5_bdmask"] = bm
    c["moe_bb"] = np.broadcast_to((np.arange(MOE_NBMAX, dtype=np.float32) * MOE_B)[None, :, None], (128, MOE_NBMAX, 32)).reshape(128, MOE_NBMAX * 32).copy()
    c["moe_pcol"] = np.arange(128, dtype=np.float32)[:, None].copy()
    c["moe_ustrict"] = (np.arange(128)[:, None] < np.arange(128)[None, :]).astype(np.float32)
    c["moe_ones"] = np.ones((128, 128), np.float32)
    c["gqa_mask"] = np.stack([np.tile(lo, (1, 2)), np.tile(hi, (1, 2))], axis=1).astype(np.float32)
    return c


def stage_prep(K, l):
    nc, P, Dm = K.nc, K.P, K.dram
    with contextlib.ExitStack() as es:
        cc, rcc = _tile(es, nc, "pp_cc", [128, 8, 2], F32)
        sc, rsc = _tile(es, nc, "pp_sc", [128, 8, 2], F32)
        mw, rmw = _tile(es, nc, "pp_mw", [128, 8, 512], F32)
        mw2, rmw2 = _tile(es, nc, "pp_mw2", [128, 8, 512], F32)
        mws = [(mw, rmw), (mw2, rmw2)]
        mv, rmv = _tile(es, nc, "pp_mv", [2, 6144], F32)
        mb, rmb = _tile(es, nc, "pp_mb", [2, 6144], F32)
        g12, rg12 = _tile(es, nc, "pp_g", [2, 2048], F32)
        ps, rps = _tile(es, nc, "pp_ps", [2, 512], F32, psum=True)
        ps2, rps2 = _tile(es, nc, "pp_ps2", [2, 512], F32, psum=True)
        pss = [(ps, rps), (ps2, rps2)]
        with nc.allow_non_contiguous_dma(reason="tiny"):
            P.dma("sp", cc[:, :, 0], Dm["c"].rearrange("o (k p) -> p (o k)", p=128), w=[rcc])
            P.dma("sp", cc[:, :, 1], Dm["c_ctx"].rearrange("(k p) -> p k", p=128), w=[rcc])
        P.dma("sp", mb[:], Dm["mod_b"][l].partition_broadcast(2), w=[rmb])
        P.dma("sp", g12[:, 0:1024], Dm["norm1_g"][l].partition_broadcast(2), w=[rg12])
        P.dma("sp", g12[:, 1024:2048], Dm["norm2_g"][l].partition_broadcast(2), w=[rg12])
        P.op("act", lambda e: e.activation(out=sc[:], in_=cc[:], func=AF.Silu), r=[rcc], w=[rsc])
        mwv = Dm["mod_w"][l].rearrange("(k p) n -> p k n", p=128)
        for n in range(12):
            w_, rw_ = mws[n % 2]
            p_, rp_ = pss[n % 2]
            P.dma("sp", w_[:], mwv[:, :, n * 512:(n + 1) * 512], w=[rw_])
            for k in range(8):
                P.op("pe", lambda e: e.matmul(p_[:], lhsT=sc[:, k, :], rhs=w_[:, k, :], start=(k == 0), stop=(k == 7)),
                     r=[rsc, rw_], w=[rp_])
            P.op("dve", lambda e: e.tensor_tensor(out=mv[:, n * 512:(n + 1) * 512], in0=p_[:], in1=mb[:, n * 512:(n + 1) * 512], op=ALU.add),
                 r=[rp_, rmb], w=[rmv])
        for slot, goff in ((1, 0), (4, 1024)):
            P.op("dve", lambda e: e.scalar_tensor_tensor(out=mv[:, slot * 1024:(slot + 1) * 1024], in0=mv[:, slot * 1024:(slot + 1) * 1024],
                                                         scalar=1.0, in1=g12[:, goff:goff + 1024], op0=ALU.add, op1=ALU.mult),
                 r=[rmv, rg12], w=[rmv])
        order = [1, 0, 2, 4, 3, 5]
        mvd = Dm["modv"].rearrange("(a r) d -> a r d", a=2)
        for j, slot in enumerate(order):
            P.dma("sp", mvd[:, j, :], mv[:, slot * 1024:(slot + 1) * 1024], r=[rmv], w=[K.R["modv"]])
    P.barrier()


def stage_A(K, l, tiles=None):
    nc, P, Dm, R = K.nc, K.P, K.dram, K.R
    tiles = list(range(NT)) if tiles is None else tiles
    P.pe_relaxed = True
    with contextlib.ExitStack() as es:
        win, rwin = _tile(es, nc, "A_win", [128, 8, MIXC], BF16)
        ident, rident = _tile(es, nc, "A_ident", [128, 128], BF16)
        identf, ridentf = _tile(es, nc, "A_identf", [128, 128], F32)
        bc, rbc = _tile(es, nc, "A_bc", [128, 4, 1024], F32)
        gains, rgains = _tile(es, nc, "A_gains", [128, 4, 64], F32)
        epsc, repsc = _tile(es, nc, "A_eps", [128, 1], F32)
        hts = [_tile(es, nc, f"A_h{i}", [128, 1024], F32) for i in range(2)]
        rps_ = [_tile(es, nc, f"A_rope{i}", [128, 128], F32) for i in range(2)]
        junk, rjunk = _tile(es, nc, "A_junk", [128, 1024], BF16)
        ssq, rssq = _tile(es, nc, "A_ssq", [128, 1], F32)
        rstd, rrstd = _tile(es, nc, "A_rstd", [128, 1], F32)
        t1, rt1 = _tile(es, nc, "A_t1", [128, 1024], F32)
        abf, rabf = _tile(es, nc, "A_abf", [128, 1024], BF16)
        aT, raT = _tile(es, nc, "A_aT", [128, 8, 128], BF16)
        pT, rpT = _tile(es, nc, "A_pT", [128, 8, 128], BF16, psum=True)
        pm = [_tile(es, nc, f"A_pm{i}", [128, 512], F32, psum=True) for i in range(5)]
        pT2, rpT2 = _tile(es, nc, "A_pT2", [128, 4, 128], BF16, psum=True)
        sA, rsA = _tile(es, nc, "A_sA", [128, 512], F32)
        sB, rsB = _tile(es, nc, "A_sB", [128, 512], F32)
        o_rqk, ro_rqk = _tile(es, nc, "A_orqk", [128, 512], BF16)
        o_rv, ro_rv = _tile(es, nc, "A_orv", [128, 256], BF16)
        o_rg, ro_rg = _tile(es, nc, "A_org", [128, 256], F32)
        o_nqk, ro_nqk = _tile(es, nc, "A_onqk", [128, 512], BF16)
        o_nv, ro_nv = _tile(es, nc, "A_onv", [128, 256], BF16)
        o_su, ro_su = _tile(es, nc, "A_osu", [128, 256], F32)
        o_sub, ro_sub = _tile(es, nc, "A_osub", [128, 256], BF16)
        o_gqk, ro_gqk = _tile(es, nc, "A_ogqk", [128, 384], BF16)
        o_gv, ro_gv = _tile(es, nc, "A_ogv", [128, 128], BF16)
        oT, roT = _tile(es, nc, "A_oT", [128, 4, 128], BF16)
        ss8, rss8 = _tile(es, nc, "A_ss8", [128, 8], F32)
        rs8, rrs8 = _tile(es, nc, "A_rs8", [128, 8], F32)

        P.dma("pool", win[:], Dm["w_in"][l].rearrange("(k p) n -> p k n", p=128)[:, :, 0:MIXC], w=[rwin])
        P.dma("sp", identf[:], Dm["ident"], w=[ridentf])
        P.op("dve", lambda e: e.tensor_copy(ident[:], identf[:]), r=[ridentf], w=[rident])
        for j, row in enumerate((0, 1, 6, 7)):
            P.dma("sp", bc[:, j, :], Dm["modv"][row].partition_broadcast(128), r=[R["modv"]], w=[rbc])
        P.dma("sp", gains[:, 0:2, :], Dm["na_qk_gain"][l].partition_broadcast(128), w=[rgains])
        P.dma("sp", gains[:, 2:4, :], Dm["gqa_qk_gain"][l].partition_broadcast(128), w=[rgains])
        P.op("dve", lambda e: e.memset(epsc[:], EPS), w=[repsc])

        hsrc = K.hsrc(l)

        def load(t, par):
            ht, rht = hts[par]
            rp, rrp = rps_[par]
            P.dma("sp", ht[:], hsrc(t), r=[R["H"]], w=[rht])
            P.dma("sp", rp[:], Dm["ropecs"][t * 128:(t + 1) * 128, :], w=[rrp])

        def rmsn(src, nh, gidx, dst, rdst_list, rsrc_list):
            P.op("act", lambda e: e.activation(out=sB[:, 0:nh * 64], in_=src, func=AF.Square), r=rsrc_list, w=[rsB])
            P.op("dve", lambda e: e.tensor_reduce(out=ss8[:, 0:nh], in_=sB[:, 0:nh * 64].rearrange("p (h d) -> p h d", d=64), axis=AX.X, op=ALU.add),
                 r=[rsB], w=[rss8])
            P.op("act", lambda e: e.activation(out=rs8[:, 0:nh], in_=ss8[:, 0:nh], func=AF.Sqrt, scale=1.0 / 64, bias=epsc[:, 0:1]),
                 r=[rss8, repsc], w=[rrs8])
            P.op("dve", lambda e: e.reciprocal(out=rs8[:, 0:nh], in_=rs8[:, 0:nh]), r=[rrs8], w=[rrs8])
            P.op("dve", lambda e: e.tensor_tensor(out=dst.rearrange("p (h d) -> p h d", d=64), in0=src.rearrange("p (h d) -> p h d", d=64),
                                                  in1=rs8[:, 0:nh].unsqueeze(2).to_broadcast([128, nh, 64]), op=ALU.mult),
                 r=rsrc_list + [rrs8], w=rdst_list)
            P.op("dve", lambda e: e.tensor_tensor(out=dst.rearrange("p (h d) -> p h d", d=64), in0=dst.rearrange("p (h d) -> p h d", d=64),
                                                  in1=gains[:, gidx, :].unsqueeze(1).to_broadcast([128, nh, 64]), op=ALU.mult),
                 r=rdst_list + [rgains], w=rdst_list)

        def rope(src, nh, rp, rrp, dst_bf, rsrc_list, rdst_list):
            v5 = lambda ap: ap.rearrange("p (h a b c) -> p h a b c", a=2, b=2, c=16)
            cosb = rp[:, 0:64].rearrange("p (a b c) -> p a b c", a=2, b=2).unsqueeze(1).to_broadcast([128, nh, 2, 2, 16])
            sinb = rp[:, 64:128].rearrange("p (a b c) -> p a b c", a=2, b=2).unsqueeze(1).to_broadcast([128, nh, 2, 2, 16])
            P.op("dve", lambda e: e.tensor_tensor(out=v5(sA[:, 0:nh * 64]), in0=v5(src), in1=cosb, op=ALU.mult), r=rsrc_list + [rrp], w=[rsA])
            P.op("dve", lambda e: e.tensor_tensor(out=v5(sB[:, 0:nh * 64]), in0=v5(src)[:, :, :, ::-1, :], in1=sinb, op=ALU.mult),
                 r=rsrc_list + [rrp], w=[rsB])
            P.op("dve", lambda e: e.tensor_tensor(out=dst_bf, in0=sA[:, 0:nh * 64], in1=sB[:, 0:nh * 64], op=ALU.add), r=[rsA, rsB], w=rdst_list)

        def transp_out(src_bf, rsrc, nchunk, dram_rows, t):
            for k in range(nchunk):
                P.op("pe", lambda e: e.transpose(out=pT2[:, k, :], in_=src_bf[:, k * 128:(k + 1) * 128], identity=ident[:]),
                     r=[rsrc, rident], w=[rpT2])
            P.op("act", lambda e: e.copy(out=oT[:, 0:nchunk, :], in_=pT2[:, 0:nchunk, :]), r=[rpT2], w=[roT])
            P.dma(STQ, dram_rows.rearrange("(k p) n -> p k n", p=128)[:, :, t * 128:(t + 1) * 128], oT[:, 0:nchunk, :], r=[roT], w=[R["mix"]])

        load(tiles[0], 0)
        for idx, t in enumerate(tiles):
            if idx + 1 < len(tiles):
                load(tiles[idx + 1], (idx + 1) % 2)
            ht, rht = hts[idx % 2]
            rp, rrp = rps_[idx % 2]
            isx = t < NXT
            g1 = bc[:, 0 if isx else 2, :]
            sh1 = bc[:, 1 if isx else 3, :]
            ts = slice(t * 128, (t + 1) * 128)
            P.op("act", lambda e: e.activation(out=junk[:], in_=ht[:], func=AF.Square, accum_out=ssq[:]), r=[rht], w=[rjunk, rssq])
            P.op("act", lambda e: e.activation(out=rstd[:], in_=ssq[:], func=AF.Sqrt, scale=1.0 / D, bias=epsc[:, 0:1]), r=[rssq, repsc], w=[rrstd])
            P.op("dve", lambda e: e.reciprocal(out=rstd[:], in_=rstd[:]), r=[rrstd], w=[rrstd])
            P.op("dve", lambda e: e.scalar_tensor_tensor(out=t1[:], in0=ht[:], scalar=rstd[:, 0:1], in1=g1, op0=ALU.mult, op1=ALU.mult),
                 r=[rht, rrstd, rbc], w=[rt1])
            P.op("dve", lambda e: e.tensor_tensor(out=abf[:], in0=t1[:], in1=sh1, op=ALU.add), r=[rt1, rbc], w=[rabf])
            for k in range(8):
                P.op("pe", lambda e: e.transpose(out=pT[:, k, :], in_=abf[:, k * 128:(k + 1) * 128], identity=ident[:]), r=[rabf, rident], w=[rpT])
            P.op("act", lambda e: e.copy(out=aT[:], in_=pT[:]), r=[rpT], w=[raT])
            P.dma(STQ, Dm["aT"].rearrange("(k p) n -> p k n", p=128)[:, :, ts], aT[:], r=[raT], w=[R["aT"]])
            for n in range(5):
                pmn, rpmn = pm[n]
                for k in range(8):
                    P.op("pe", lambda e: e.matmul(pmn[:], lhsT=aT[:, k, :], rhs=win[:, k, n * 512:(n + 1) * 512], start=(k == 0), stop=(k == 7)),
                         r=[raT, rwin], w=[rpmn])
            rope(pm[0][0][:], 8, rp, rrp, o_rqk[:], [pm[0][1]], [ro_rqk])
            P.dma(STQ, Dm["rk"][ts, :], o_rqk[:, 256:512], r=[ro_rqk], w=[R["mix"]])
            transp_out(o_rqk, ro_rqk, 4, Dm["rqkT"], t)
            P.op("act", lambda e: e.copy(out=o_rv[:], in_=pm[1][0][:, 0:256]), r=[pm[1][1]], w=[ro_rv])
            P.op("act", lambda e: e.activation(out=o_rg[:], in_=pm[1][0][:, 256:512], func=AF.Silu), r=[pm[1][1]], w=[ro_rg])
            P.dma(STQ, Dm["rv"][ts, :], o_rv[:], r=[ro_rv], w=[R["mix"]])
            P.dma(STQ, Dm["rg"][ts, :], o_rg[:], r=[ro_rg], w=[R["mix"]])
            rmsn(pm[2][0][:, 0:256], 4, 0, t1[:, 0:256], [rt1], [pm[2][1]])
            rmsn(pm[2][0][:, 256:512], 4, 1, t1[:, 256:512], [rt1], [pm[2][1]])
            P.op("act", lambda e: e.copy(out=o_nqk[:], in_=t1[:, 0:512]), r=[rt1], w=[ro_nqk])
            transp_out(o_nqk, ro_nqk, 4, Dm["nqkT"], t)
            P.op("act", lambda e: e.copy(out=o_nv[:], in_=pm[3][0][:, 0:256]), r=[pm[3][1]], w=[ro_nv])
            P.dma(STQ, Dm["nv"][ts, :], o_nv[:], r=[ro_nv], w=[R["mix"]])
            P.op("act", lambda e: e.copy(out=o_su[:], in_=pm[3][0][:, 256:512]), r=[pm[3][1]], w=[ro_su])
            P.op("dve", lambda e: e.tensor_copy(out=o_sub[:], in_=pm[3][0][:, 256:512]), r=[pm[3][1]], w=[ro_sub])
            P.dma(STQ, Dm["su"][ts, :], o_su[:], r=[ro_su], w=[R["mix"]])
            transp_out(o_sub, ro_sub, 2, Dm["suT"], t)
            rmsn(pm[4][0][:, 0:256], 4, 2, t1[:, 512:768], [rt1], [pm[4][1]])
            rmsn(pm[4][0][:, 256:384], 2, 3, t1[:, 768:896], [rt1], [pm[4][1]])
            rope(t1[:, 512:896], 6, rp, rrp, o_gqk[:], [rt1], [ro_gqk])
            transp_out(o_gqk, ro_gqk, 3, Dm["gqkT"], t)
            P.op("act", lambda e: e.copy(out=o_gv[:], in_=pm[4][0][:, 384:512]), r=[pm[4][1]], w=[ro_gv])
            P.dma(STQ, Dm["gv"][ts, :], o_gv[:], r=[ro_gv], w=[R["mix"]])
    P.pe_relaxed = False
    P.barrier()


INPUT_NAMES = ["x", "c", "ctx", "c_ctx", "mod_w", "mod_b", "norm1_g", "norm2_g", "w_in", "w_branch", "w_out", "ret_log_decay",
               "na_qk_gain", "na_rpb", "s5_lambda_re", "s5_lambda_im", "s5_log_step", "s5_b_re", "s5_b_im", "s5_c_re",
               "s5_c_im", "s5_d", "s5_glu_w", "s5_glu_b", "gqa_qk_gain", "gqa_sink", "router_w1", "router_b1", "router_w2",
               "router_b2", "exp_w1", "exp_w3", "exp_w2"]

SCRATCH = {
    "modv": ([12, 1024], F32),
    "H": ([T, D], F32),
    "aT": ([D, T], BF16),
    "rqkT": ([512, T], BF16), "rk": ([T, 256], BF16), "rv": ([T, 256], BF16), "rg": ([T, 256], F32),
    "nqkT": ([512, T], BF16), "nv": ([T, 256], BF16),
    "su": ([T, 256], F32), "suT": ([256, T], BF16),
    "gqkT": ([384, T], BF16), "gv": ([T, 128], BF16),
}


def build(in_shapes, consts, plan, expose=()):
    nc = bass.Bass("TRN2", target_bir_lowering=False)
    K = Ctx()
    K.nc = nc
    K.dram = {}
    for name, (shape, dt) in in_shapes.items():
        K.dram[name] = nc.dram_tensor(name, list(shape), dt, kind="ExternalInput").ap()
    for name, arr in consts.items():
        K.dram[name] = nc.dram_tensor(name, list(arr.shape), F32, kind="ExternalInput").ap()
    for name, (shape, dt) in SCRATCH.items():
        if name in K.dram:
            continue
        kind = "ExternalOutput" if name in expose else "Internal"
        K.dram[name] = nc.dram_tensor(name, list(shape), dt, kind=kind).ap()
    K.dram["out"] = nc.dram_tensor("out", [L, D], F32, kind="ExternalOutput").ap()
    K.R = {k: Res(k) for k in ["modv", "H", "aT", "mix", "y", "out", "s5y", "wbf"]}
    K.P = Prog(nc, same_sync=SAME_SYNC)

    def hsrc(l):
        def f(t):
            if l == 0:
                if t < NXT:
                    return K.dram["x"][t * 128:(t + 1) * 128, :]
                return K.dram["ctx"][(t - NXT) * 128:(t - NXT + 1) * 128, :]
            return K.dram["H"][t * 128:(t + 1) * 128, :]
        return f
    K.hsrc = hsrc
    plan(K)
    K.P.barrier(["sp"])
    return nc, K


def run_attention(K, es, pfx, units, rd_res, epilogue, maxc):
    nc, P = K.nc, K.P
    P.pe_relaxed = ATT_RELAX
    wmax = max(u["nq"] for u in units) * 128
    spb = 512 // wmax
    nbank = (maxc + spb - 1) // spb
    S = [[_tile(es, nc, f"{pfx}_S{a}_{b}", [128, spb, wmax], F32, psum=True) for b in range(nbank)] for a in range(2)]
    O = _tile(es, nc, f"{pfx}_O", [128, 4, 65], F32, psum=True)
    pT = [[_tile(es, nc, f"{pfx}_pT{a}_{b}", [128, wmax], BF16) for b in range(maxc)] for a in range(2)]

    def emit_S(ui):
        u = units[ui]
        a = ui % 2
        w = u["nq"] * 128
        for ci, (kT, bias, _v) in enumerate(u["chunks"]):
            st, rst = S[a][ci // spb]
            sp = st[:, ci % spb, 0:w]
            P.op("pe", lambda e: e.matmul(sp, lhsT=kT, rhs=u["q"], start=True, stop=(bias is None)), r=rd_res, w=[rst])
            if bias is not None:
                P.op("pe", lambda e: e.matmul(sp, lhsT=bias[0], rhs=bias[1], start=False, stop=True), r=rd_res, w=[rst])
        for ci in range(len(u["chunks"])):
            st, rst = S[a][ci // spb]
            sp = st[:, ci % spb, 0:w]
            pt, rpt = pT[a][ci]
            P.op("act", lambda e: e.activation(out=pt[:, 0:w], in_=sp, func=AF.Exp, scale=0.125), r=[rst], w=[rpt])

    def emit_PV(ui):
        u = units[ui]
        a = ui % 2
        ot, rot = O
        nch = len(u["chunks"])
        for g, h in enumerate(u["heads"]):
            for ci, (_k, _b, v) in enumerate(u["chunks"]):
                pt, rpt = pT[a][ci]
                P.op("pe", lambda e: e.matmul(ot[:, h, :], lhsT=pt[:, g * 128:(g + 1) * 128], rhs=v, start=(ci == 0), stop=(ci == nch - 1)),
                     r=[rpt] + rd_res, w=[rot])
        if u["final"]:
            epilogue(u["j"], ot, rot)

    emit_S(0)
    for ui in range(len(units)):
        if ui + 1 < len(units):
            emit_S(ui + 1)
        emit_PV(ui)
    P.pe_relaxed = False


def attn_epilogue_factory(K, es, pfx, ident, rident, yrow0, extra_den=None):
    nc, P, Dm, R = K.nc, K.P, K.dram, K.R
    den, rden = _tile(es, nc, pfx + "_den", [128, 4], F32)
    ybf, rybf = _tile(es, nc, pfx + "_ybf", [128, 256], BF16)
    pT2, rpT2 = _tile(es, nc, pfx + "_pT2", [128, 2, 128], BF16, psum=True)
    oT, roT = _tile(es, nc, pfx + "_oT", [128, 2, 128], BF16)

    def epi(j, ot, rot):
        if extra_den is not None:
            P.op("dve", lambda e: e.tensor_tensor(out=den[:], in0=ot[:, :, 64], in1=extra_den[0][:], op=ALU.add), r=[rot, extra_den[1]], w=[rden])
            P.op("dve", lambda e: e.reciprocal(out=den[:], in_=den[:]), r=[rden], w=[rden])
        else:
            P.op("dve", lambda e: e.reciprocal(out=den[:], in_=ot[:, :, 64]), r=[rot], w=[rden])
        P.op("dve", lambda e: e.tensor_tensor(out=ybf[:].rearrange("p (h d) -> p h d", d=64), in0=ot[:, :, 0:64],
                                              in1=den[:].unsqueeze(2).to_broadcast([128, 4, 64]), op=ALU.mult), r=[rot, rden], w=[rybf])
        for k in range(2):
            P.op("pe", lambda e: e.transpose(out=pT2[:, k, :], in_=ybf[:, k * 128:(k + 1) * 128], identity=ident[:]), r=[rybf, rident], w=[rpT2])
        P.op("act", lambda e: e.copy(out=oT[:], in_=pT2[:]), r=[rpT2], w=[roT])
        P.dma(STQ, Dm["yT"][yrow0:yrow0 + 256, :].rearrange("(k p) n -> p k n", p=128)[:, :, j * 128:(j + 1) * 128], oT[:], r=[roT], w=[R["y"]])
    return epi


def stage_gqa(K, l, with_ctx, qtiles=None):
    nc, P, Dm, R = K.nc, K.P, K.dram, K.R
    with contextlib.ExitStack() as es:
        qT, rqT = _tile(es, nc, "G_qT", [64, 4, T], BF16)
        kT, rkT = _tile(es, nc, "G_kT", [64, 2, T], BF16)
        V, rV = _tile(es, nc, "G_V", [128, NT, 2, 65], BF16)
        mk, rmk = _tile(es, nc, "G_mask", [128, 2, 256], BF16)
        ident, rident = _tile(es, nc, "G_ident", [128, 128], BF16)
        esk, resk = _tile(es, nc, "G_esink", [128, 4], F32)
        for h in range(4):
            P.dma("sp", qT[:, h, :], Dm["gqkT"][h * 64:(h + 1) * 64, :], r=[R["mix"]], w=[rqT])
        for kv in range(2):
            P.dma("sp", kT[:, kv, :], Dm["gqkT"][256 + kv * 64:256 + (kv + 1) * 64, :], r=[R["mix"]], w=[rkT])
            P.dma("sp", V[:, :, kv, 0:64], Dm["gv"][:, kv * 64:(kv + 1) * 64].rearrange("(c p) d -> p c d", p=128), r=[R["mix"]], w=[rV])
        P.op("pool", lambda e: e.memset(V[:, :, :, 64:65], 1.0), w=[rV])
        P.dma("pool", mk[:], Dm["gqa_mask"], w=[rmk])
        P.dma("pool", ident[:], Dm["ident"], w=[rident])
        P.dma("sp", esk[:], Dm["gqa_sink"][l].partition_broadcast(128), w=[resk])
        P.op("act", lambda e: e.activation(out=esk[:], in_=esk[:], func=AF.Exp), r=[resk], w=[resk])
        rd = [rqT, rkT, rV, rmk, rident]
        epi = attn_epilogue_factory(K, es, "G", ident, rident, 768, extra_den=(esk, resk))
        qtiles = qtiles if qtiles is not None else list(range(NXT)) + ([64, 65] if with_ctx else [])
        units = []
        for j in qtiles:
            if j < NXT:
                ch = [(c, tag) for c, tag in ((j - 1, 0), (j, None), (j + 1, 1)) if 0 <= c < NXT] + [(64, None), (65, None)]
            else:
                ch = [(64, None), (65, None)]
            for kv in range(2):
                chunks = []
                for c, tag in ch:
                    bias = None if tag is None else (ident[:], mk[:, tag, :])
                    chunks.append((kT[:, kv, c * 128:(c + 1) * 128], bias, V[:, c, kv, :]))
                units.append(dict(j=j, q=qT[:, 2 * kv:2 * kv + 2, j * 128:(j + 1) * 128], nq=2, heads=[2 * kv, 2 * kv + 1], chunks=chunks, final=(kv == 1)))
        run_attention(K, es, "G", units, rd, epi, 5)
    P.barrier()


SCRATCH.update({"yT": ([1024, T], BF16)})


def stage_na(K, l, with_ctx, qtiles=None):
    nc, P, Dm, R = K.nc, K.P, K.dram, K.R
    with contextlib.ExitStack() as es:
        qT, rqT = _tile(es, nc, "N_qT", [128, 2, T], BF16)
        kT, rkT = _tile(es, nc, "N_kT", [128, 2, T], BF16)
        V, rV = _tile(es, nc, "N_V", [128, NT, 4, 65], BF16)
        Bp, rBp = _tile(es, nc, "N_Bp", [128, NA_NCLS, 4, 128], BF16)
        jx, rjx = _tile(es, nc, "N_jx", [128, 128], BF16)
        ident, rident = _tile(es, nc, "N_ident", [128, 128], BF16)
        zt, rzt = _tile(es, nc, "N_zero", [64, 128], F32)
        rp, rrp = _tile(es, nc, "N_rpb", [15, 4, 31], F32)
        stg = [_tile(es, nc, f"N_stg{i}", [128, 2, 64], F32) for i in range(2)]
        rms_ = [_tile(es, nc, f"N_rm{i}", [128, 128], F32) for i in range(2)]
        rpad = Res("rpbpad")
        for c2 in range(2):
            P.dma("sp", qT[:, c2, :], Dm["nqkT"][c2 * 128:(c2 + 1) * 128, :], r=[R["mix"]], w=[rqT])
            P.dma("sp", kT[:, c2, :], Dm["nqkT"][256 + c2 * 128:256 + (c2 + 1) * 128, :], r=[R["mix"]], w=[rkT])
        for h in range(4):
            P.dma("sp", V[:, :, h, 0:64], Dm["nv"][:, h * 64:(h + 1) * 64].rearrange("(c p) d -> p c d", p=128), r=[R["mix"]], w=[rV])
        P.op("pool", lambda e: e.memset(V[:, :, :, 64:65], 1.0), w=[rV])
        P.dma("pool", jx[:], Dm["na_jx"], w=[rjx])
        P.dma("pool", ident[:], Dm["ident"], w=[rident])
        P.op("dve", lambda e: e.memset(zt[:], 0.0), w=[rzt])
        P.dma("sp", Dm["rpbpad"].rearrange("h r j -> (h r) j"), zt[:], r=[rzt], w=[rpad])
        P.dma("sp", rp[:], Dm["na_rpb"][l].rearrange("h r j -> r h j"), w=[rrp])
        for h in range(4):
            P.dma("sp", Dm["rpbpad"][h, 0:15, 48:79], rp[:, h, :], r=[rrp], w=[rpad])
        padt = Dm["rpbpad"].tensor
        cl = na_class_list()
        for cls, (j, cch) in enumerate(cl):
            rmt, rrmt = rms_[cls % 2]
            P.dma("sp", rmt[:], Dm["na_rm"][cls], w=[rrmt])
            for h in range(4):
                st, rst = stg[(cls * 4 + h) % 2]
                for rq in range(2):
                    dr0 = 2 * (cch - j) + 0 - rq + 7
                    src = bass.AP(tensor=padt, offset=(h * 16 + dr0) * 128, ap=[[1, 64], [128, 2], [1, 64]])
                    P.dma("sp" if rq == 0 else "act", st[rq * 64:(rq + 1) * 64, :, :], src, r=[rpad], w=[rst])
                P.op("dve", lambda e: e.scalar_tensor_tensor(out=Bp[:, cls, h, :], in0=st[:].rearrange("p a b -> p (a b)"), scalar=8.0, in1=rmt[:],
                                                             op0=ALU.mult, op1=ALU.add), r=[rst, rrmt], w=[rBp])
        rd = [rqT, rkT, rV, rBp, rjx, rident]
        epi = attn_epilogue_factory(K, es, "N", ident, rident, 256)
        qtiles = qtiles if qtiles is not None else list(range(NXT)) + ([64, 65] if with_ctx else [])
        units = []
        for j in qtiles:
            ch = (na_chunks(j) if j < NXT else []) + [(64, None), (65, None)]
            for h in range(4):
                pb, c2 = (h % 2) * 64, h // 2
                chunks = []
                for c, cls in ch:
                    bias = None if cls is None else (Bp[:, cls, h, :], jx[:])
                    chunks.append((kT[pb:pb + 64, c2, c * 128:(c + 1) * 128], bias, V[:, c, h, :]))
                units.append(dict(j=j, q=qT[pb:pb + 64, c2, j * 128:(j + 1) * 128], nq=1, heads=[h], chunks=chunks, final=(h == 3)))
        run_attention(K, es, "N", units, rd, epi, 7)
    P.barrier()


SCRATCH.update({"rpbpad": ([4, 16, 128], F32)})


def stage_ret(K, l, with_ctx, out_chunks=None):
    nc, P, Dm, R = K.nc, K.P, K.dram, K.R
    LN8 = math.log(0.125)
    with contextlib.ExitStack() as es:
        qT, rqT = _tile(es, nc, "R_qT", [128, 2, T], BF16)
        kT, rkT = _tile(es, nc, "R_kT", [128, 2, T], BF16)
        Kt, rKt = _tile(es, nc, "R_Kt", [128, NT, 256], BF16)
        Vt, rVt = _tile(es, nc, "R_Vt", [128, NT, 256], BF16)
        SF, rSF = _tile(es, nc, "R_SF", [128, NT, 2, 64], BF16)
        ident, rident = _tile(es, nc, "R_ident", [128, 128], BF16)
        lg, rlg = _tile(es, nc, "R_lg", [128, 8], F32)
        cst, rcst = _tile(es, nc, "R_cst", [128, 5, 128], F32)
        pcol, rpcol = _tile(es, nc, "R_pcol", [128, 2], F32)
        lnc, rlnc = _tile(es, nc, "R_lnc", [128, 2], F32)
        tmp, rtmp = _tile(es, nc, "R_tmp", [128, 128], F32)
        DT, rDT = _tile(es, nc, "R_DT", [128, 4, 128], F32)
        QF, rQF = _tile(es, nc, "R_QF", [128, 2, 128], F32)
        QB, rQB = _tile(es, nc, "R_QB", [128, 2, 128], F32)
        KD, rKD = _tile(es, nc, "R_KD", [128, 8], F32)
        CF, rCF = _tile(es, nc, "R_CF", [128, 2, 64], F32)
        CB, rCB = _tile(es, nc, "R_CB", [128, 2, 64], F32)
        SM, rSM = _tile(es, nc, "R_SM", [128, 2, 64], F32)
        SBc = [_tile(es, nc, f"R_SBc{i}", [128, 2, 64], BF16) for i in range(2)]
        kw, rkw = _tile(es, nc, "R_kw", [128, 256], BF16)
        PT = [_tile(es, nc, f"R_PT{i}", [128, 4, 128], BF16) for i in range(2)]
        qf, rqf = _tile(es, nc, "R_qf", [128, 2, 128], BF16)
        qb, rqb = _tile(es, nc, "R_qb", [128, 2, 128], BF16)
        gt = [_tile(es, nc, f"R_g{i}", [128, 256], F32) for i in range(2)]
        sq, rsq = _tile(es, nc, "R_sq", [128, 256], F32)
        ss4, rss4 = _tile(es, nc, "R_ss4", [128, 4], F32)
        yf, ryf = _tile(es, nc, "R_yf", [128, 256], F32)
        ybf, rybf = _tile(es, nc, "R_ybf", [128, 256], BF16)
        oT, roT = _tile(es, nc, "R_oT", [128, 2, 128], BF16)
        Sp = [_tile(es, nc, f"R_Sp{i}", [128, 4, 128], F32, psum=True) for i in range(2)]
        Op = [_tile(es, nc, f"R_Op{i}", [128, 4, 64], F32, psum=True) for i in range(2)]
        KVp, rKVp = _tile(es, nc, "R_KVp", [128, 2, 128], F32, psum=True)
        pT2, rpT2 = _tile(es, nc, "R_pT2", [128, 2, 128], BF16, psum=True)

        for c2 in range(2):
            P.dma("sp", qT[:, c2, :], Dm["rqkT"][c2 * 128:(c2 + 1) * 128, :], r=[R["mix"]], w=[rqT])
            P.dma("sp", kT[:, c2, :], Dm["rqkT"][256 + c2 * 128:256 + (c2 + 1) * 128, :], r=[R["mix"]], w=[rkT])
        P.dma("act", Kt[:], Dm["rk"].rearrange("(c p) d -> p c d", p=128), r=[R["mix"]], w=[rKt])
        P.dma("act", Vt[:], Dm["rv"].rearrange("(c p) d -> p c d", p=128), r=[R["mix"]], w=[rVt])
        P.dma("pool", ident[:], Dm["ident"], w=[rident])
        for i, nm in enumerate(["ret_dpos", "ret_dneg", "ret_diag", "ret_tp1", "ret_tr"]):
            P.dma("sp", cst[:, i, :], Dm[nm], w=[rcst])
        P.dma("sp", pcol[:], Dm["ret_pcol"], w=[rpcol])
        P.dma("sp", lg[:], Dm["ret_log_decay"][l].rearrange("a h -> (a h)").partition_broadcast(128), w=[rlg])
        P.op("dve", lambda e: e.memset(lnc[:, 0:1], LN8), w=[rlnc])
        P.op("dve", lambda e: e.memset(lnc[:, 1:2], EPS), w=[rlnc])
        P.op("act", lambda e: e.activation(out=lg[:], in_=lg[:], func=AF.Exp), r=[rlg], w=[rlg])
        P.op("dve", lambda e: e.tensor_scalar(out=lg[:], in0=lg[:], scalar1=-1.0, scalar2=None, op0=ALU.mult), r=[rlg], w=[rlg])
        for h in range(4):
            pb, c2 = (h % 2) * 64, h // 2
            P.op("dve", lambda e: e.tensor_scalar(out=tmp[:], in0=cst[:, 0, :], scalar1=lg[:, h:h + 1], scalar2=None, op0=ALU.mult), r=[rcst, rlg], w=[rtmp])
            P.op("dve", lambda e: e.scalar_tensor_tensor(out=tmp[:], in0=cst[:, 1, :], scalar=lg[:, 4 + h:5 + h], in1=tmp[:], op0=ALU.mult, op1=ALU.add),
                 r=[rcst, rlg, rtmp], w=[rtmp])
            P.op("dve", lambda e: e.tensor_tensor(out=tmp[:], in0=tmp[:], in1=cst[:, 2, :], op=ALU.add), r=[rtmp, rcst], w=[rtmp])
            P.op("act", lambda e: e.activation(out=DT[:, h, :], in_=tmp[:], func=AF.Exp), r=[rtmp], w=[rDT])
            P.op("act", lambda e: e.activation(out=QF[pb:pb + 64, c2, :], in_=cst[pb:pb + 64, 3, :], func=AF.Exp, scale=lg[pb:pb + 64, h:h + 1], bias=lnc[pb:pb + 64, 0:1]),
                 r=[rcst, rlg, rlnc], w=[rQF])
            P.op("act", lambda e: e.activation(out=QB[pb:pb + 64, c2, :], in_=cst[pb:pb + 64, 4, :], func=AF.Exp, scale=lg[pb:pb + 64, 4 + h:5 + h], bias=lnc[pb:pb + 64, 0:1]),
                 r=[rcst, rlg, rlnc], w=[rQB])
            P.op("act", lambda e: e.activation(out=KD[:, h:h + 1], in_=lg[:, h:h + 1], func=AF.Exp, scale=pcol[:, 0:1]), r=[rlg, rpcol], w=[rKD])
            P.op("act", lambda e: e.activation(out=KD[:, 4 + h:5 + h], in_=lg[:, 4 + h:5 + h], func=AF.Exp, scale=pcol[:, 1:2]), r=[rlg, rpcol], w=[rKD])
            P.op("act", lambda e: e.activation(out=CF[pb:pb + 64, c2, :], in_=lg[pb:pb + 64, h:h + 1].to_broadcast([64, 64]), func=AF.Exp, scale=128.0), r=[rlg], w=[rCF])
            P.op("act", lambda e: e.activation(out=CB[pb:pb + 64, c2, :], in_=lg[pb:pb + 64, 4 + h:5 + h].to_broadcast([64, 64]), func=AF.Exp, scale=128.0), r=[rlg], w=[rCB])

        def state_update(n, kdoff, Ctab, rCtab):
            P.op("dve", lambda e: e.tensor_tensor(out=kw[:].rearrange("p (h d) -> p h d", d=64), in0=Kt[:, n, :].rearrange("p (h d) -> p h d", d=64),
                                                  in1=KD[:, kdoff:kdoff + 4].unsqueeze(2).to_broadcast([128, 4, 64]), op=ALU.mult), r=[rKt, rKD], w=[rkw])
            for c2 in range(2):
                P.op("pe", lambda e: e.matmul(KVp[:, c2, :], lhsT=kw[:, c2 * 128:(c2 + 1) * 128], rhs=Vt[:, n, c2 * 128:(c2 + 1) * 128], start=True, stop=True),
                     r=[rkw, rVt], w=[rKVp])
            P.op("dve", lambda e: e.tensor_tensor(out=SM[:], in0=SM[:], in1=Ctab[:], op=ALU.mult), r=[rSM, rCtab], w=[rSM])
            for hp in (0, 64):
                P.op("dve", lambda e: e.tensor_tensor(out=SM[hp:hp + 64, :, :], in0=SM[hp:hp + 64, :, :], in1=KVp[hp:hp + 64, :, hp:hp + 64], op=ALU.add),
                     r=[rSM, rKVp], w=[rSM])

        fo = [64, 65] + list(range(NXT))
        P.op("dve", lambda e: e.memset(SM[:], 0.0), w=[rSM])
        for i, n in enumerate(fo):
            P.op("act", lambda e: e.copy(out=SF[:, n, :, :], in_=SM[:]), r=[rSM], w=[rSF])
            if i + 1 < len(fo):
                state_update(n, 0, CF, rCF)

        bo = [65, 64] + list(range(NXT - 1, -1, -1))
        outs = [n for n in bo if (n < NXT or with_ctx)]
        if out_chunks is not None:
            outs = [n for n in outs if n in out_chunks]
        P.op("dve", lambda e: e.memset(SM[:], 0.0), w=[rSM])
        oi = {n: i for i, n in enumerate(outs)}

        def emit_S(n):
            i = oi[n]
            sp, rsp = Sp[i % 2]
            cs = slice(n * 128, (n + 1) * 128)
            for h in range(4):
                pb, c2 = (h % 2) * 64, h // 2
                P.op("pe", lambda e: e.matmul(sp[:, h, :], lhsT=kT[pb:pb + 64, c2, cs], rhs=qT[pb:pb + 64, c2, cs], start=True, stop=True), r=[rkT, rqT], w=[rsp])
            pt, rpt = PT[i % 2]
            P.op("dve", lambda e: e.tensor_tensor(out=pt[:], in0=sp, in1=DT[:], op=ALU.mult), r=[rsp, rDT], w=[rpt])
            g_, rg_ = gt[i % 2]
            P.dma("sp", g_[:], Dm["rg"][cs, :], r=[R["mix"]], w=[rg_])

        def emit_O(n, sbc, rsbc):
            i = oi[n]
            cs = slice(n * 128, (n + 1) * 128)
            pt, rpt = PT[i % 2]
            op_, rop = Op[i % 2]
            g_, rg_ = gt[i % 2]
            P.op("dve", lambda e: e.tensor_tensor(out=qf[:], in0=qT[:, :, cs], in1=QF[:], op=ALU.mult), r=[rqT, rQF], w=[rqf])
            P.op("pool", lambda e: e.tensor_tensor(out=qb[:], in0=qT[:, :, cs], in1=QB[:], op=ALU.mult), r=[rqT, rQB], w=[rqb])
            for h in range(4):
                pb, c2 = (h % 2) * 64, h // 2
                P.op("pe", lambda e: e.matmul(op_[:, h, :], lhsT=pt[:, h, :], rhs=Vt[:, n, h * 64:(h + 1) * 64], start=True, stop=False), r=[rpt, rVt], w=[rop])
                P.op("pe", lambda e: e.matmul(op_[:, h, :], lhsT=qf[pb:pb + 64, c2, :], rhs=SF[pb:pb + 64, n, c2, :], start=False, stop=False), r=[rqf, rSF], w=[rop])
                P.op("pe", lambda e: e.matmul(op_[:, h, :], lhsT=qb[pb:pb + 64, c2, :], rhs=sbc[pb:pb + 64, c2, :], start=False, stop=True), r=[rqb, rsbc], w=[rop])
            P.op("act", lambda e: e.activation(out=sq[:].rearrange("p (h d) -> p h d", d=64), in_=op_, func=AF.Square), r=[rop], w=[rsq])
            P.op("dve", lambda e: e.tensor_reduce(out=ss4[:], in_=sq[:].rearrange("p (h d) -> p h d", d=64), axis=AX.X, op=ALU.add), r=[rsq], w=[rss4])
            P.op("act", lambda e: e.activation(out=ss4[:], in_=ss4[:], func=AF.Sqrt, scale=1.0 / 64, bias=lnc[:, 1:2]), r=[rss4, rlnc], w=[rss4])
            P.op("dve", lambda e: e.reciprocal(out=ss4[:], in_=ss4[:]), r=[rss4], w=[rss4])
            P.op("dve", lambda e: e.tensor_tensor(out=yf[:].rearrange("p (h d) -> p h d", d=64), in0=op_, in1=ss4[:].unsqueeze(2).to_broadcast([128, 4, 64]), op=ALU.mult),
                 r=[rop, rss4], w=[ryf])
            P.op("dve", lambda e: e.tensor_tensor(out=ybf[:], in0=yf[:], in1=g_[:], op=ALU.mult), r=[ryf, rg_], w=[rybf])
            for k in range(2):
                P.op("pe", lambda e: e.transpose(out=pT2[:, k, :], in_=ybf[:, k * 128:(k + 1) * 128], identity=ident[:]), r=[rybf, rident], w=[rpT2])
            P.op("act", lambda e: e.copy(out=oT[:], in_=pT2), r=[rpT2], w=[roT])
            P.dma(STQ, Dm["yT"][0:256, :].rearrange("(k p) n -> p k n", p=128)[:, :, cs], oT[:], r=[roT], w=[R["y"]])

        if outs:
            emit_S(outs[0])
        for bi, n in enumerate(bo):
            sbc, rsbc = SBc[bi % 2]
            if n in oi:
                P.op("act", lambda e: e.copy(out=sbc[:], in_=SM[:]), r=[rSM], w=[rsbc])
                i = oi[n]
                if i + 1 < len(outs):
                    emit_S(outs[i + 1])
                emit_O(n, sbc, rsbc)
            if bi + 1 < len(bo):
                state_update(n, 4, CB, rCB)
    P.barrier()


def _sin_reduced(P, out, in_, shift, t1, rt1, t2, rt2, r_in, w_out):
    i2p = 1.0 / (2 * math.pi)
    P.op("dve", lambda e: e.tensor_scalar(out=t1, in0=in_, scalar1=i2p, scalar2=shift * i2p, op0=ALU.mult, op1=ALU.add), r=r_in, w=[rt1])
    P.op("dve", lambda e: e.tensor_scalar(out=t2, in0=t1, scalar1=MAGIC, scalar2=None, op0=ALU.add), r=[rt1], w=[rt2])
    P.op("dve", lambda e: e.tensor_scalar(out=t2, in0=t2, scalar1=MAGIC, scalar2=None, op0=ALU.subtract), r=[rt2], w=[rt2])
    P.op("dve", lambda e: e.tensor_tensor(out=t1, in0=t1, in1=t2, op=ALU.subtract), r=[rt1, rt2], w=[rt1])
    P.op("act", lambda e: e.activation(out=out, in_=t1, func=AF.Sin, scale=2 * math.pi), r=[rt1], w=w_out)


def stage_s5(K, l, with_ctx, epi_tiles=None, precast=False):
    nc, P, Dm, R = K.nc, K.P, K.dram, K.R
    TC = S5_TC
    NCH = T // TC
    P.pe_relaxed = True
    with contextlib.ExitStack() as es:
        uT, ruT = _tile(es, nc, "S_uT", [128, 2, T], BF16)
        ident, rident = _tile(es, nc, "S_ident", [128, 128], BF16)
        identf, ridentf = _tile(es, nc, "S_identf", [128, 128], F32)
        BbT, rBbT = _tile(es, nc, "S_BbT", [128, 2, 2, 8, 128], BF16)
        Cm, rCm = _tile(es, nc, "S_Cm", [128, 4, 2, 128], BF16)
        RD, rRD = _tile(es, nc, "S_RD", [128, 2, 8], F32)
        TH, rTH = _tile(es, nc, "S_TH", [128, 2, 8], F32)
        COS, rCOS = _tile(es, nc, "S_COS", [128, 8, TC], F32)
        SIN, rSIN = _tile(es, nc, "S_SIN", [128, 8, TC], F32)
        iota1, riota1 = _tile(es, nc, "S_iota1", [128, TC], F32)
        P.dma("sp", uT[:, 0, :], Dm["suT"][0:128, :], r=[R["mix"]], w=[ruT])
        P.dma("sp", uT[:, 1, :], Dm["suT"][128:256, :], r=[R["mix"]], w=[ruT])
        P.dma("sp", identf[:], Dm["ident"], w=[ridentf])
        P.dma("pool", ident[:], Dm["ident"], w=[rident])
        P.dma("sp", iota1[:], Dm["s5_iota1"], w=[riota1])

        with contextlib.ExitStack() as es2:
            LRt, rLR = _tile(es2, nc, "S_LR", [128, 8], F32)
            LIt, rLI = _tile(es2, nc, "S_LI", [128, 8], F32)
            DTt, rDTt = _tile(es2, nc, "S_DT", [128, 8], F32)
            w8 = [_tile(es2, nc, f"S_w8_{i}", [128, 8], F32) for i in range(8)]
            BRt, rBRt = _tile(es2, nc, "S_BR", [128, 8, 16], F32)
            BIt, rBIt = _tile(es2, nc, "S_BI", [128, 8, 16], F32)
            bb = [_tile(es2, nc, f"S_bb{i}", [128, 8, 16], F32) for i in range(4)]
            Zp, rZp = _tile(es2, nc, "S_Zp", [128, 8, 128], F32)
            Cn, rCn = _tile(es2, nc, "S_Cn", [128, 128], F32)
            bdm, rbdm = _tile(es2, nc, "S_bdm", [128, 128], F32)
            tp, rtp = _tile(es2, nc, "S_tp", [128, 128], F32, psum=True)
            P.dma("sp", bdm[:], Dm["s5_bdmask"], w=[rbdm])
            with nc.allow_non_contiguous_dma(reason="small parameter tables"):
                P.dma("sp", BRt[:], Dm["s5_b_re"][l].rearrange("(gp a) p c -> (a p) gp c", a=2), w=[rBRt])
                P.dma("sp", BIt[:], Dm["s5_b_im"][l].rearrange("(gp a) p c -> (a p) gp c", a=2), w=[rBIt])
            for dirn in range(2):
                with nc.allow_non_contiguous_dma(reason="small parameter tables"):
                    P.dma("sp", LRt[:], Dm["s5_lambda_re"][l, dirn].rearrange("(gp a) p -> (a p) gp", a=2), w=[rLR])
                    P.dma("sp", LIt[:], Dm["s5_lambda_im"][l, dirn].rearrange("(gp a) p -> (a p) gp", a=2), w=[rLI])
                    for a in range(2):
                        src = Dm["s5_log_step"][l, dirn].rearrange("(gp a) -> a gp", a=2)[a].partition_broadcast(64)
                        P.dma("sp", DTt[a * 64:(a + 1) * 64, :], src, w=[rDTt])
                (dt_, rdt), (mag, rmag), (sn, rsn), (cs_, rcs), (t1, rt1), (t2, rt2), (cr, rcr), (ci, rci) = w8
                P.op("act", lambda e: e.activation(out=dt_[:], in_=DTt[:], func=AF.Exp), r=[rDTt], w=[rdt])
                P.op("dve", lambda e: e.tensor_tensor(out=mag[:], in0=LRt[:], in1=dt_[:], op=ALU.mult), r=[rLR, rdt], w=[rmag])
                P.op("act", lambda e: e.activation(out=RD[:, dirn, :], in_=mag[:], func=AF.Exp), r=[rmag], w=[rRD])
                P.op("dve", lambda e: e.tensor_tensor(out=TH[:, dirn, :], in0=LIt[:], in1=dt_[:], op=ALU.mult), r=[rLI, rdt], w=[rTH])
                _sin_reduced(P, sn[:], TH[:, dirn, :], 0.0, t1[:], rt1, t2[:], rt2, [rTH], [rsn])
                _sin_reduced(P, cs_[:], TH[:, dirn, :], math.pi / 2, t1[:], rt1, t2[:], rt2, [rTH], [rcs])
                P.op("dve", lambda e: e.tensor_tensor(out=cs_[:], in0=cs_[:], in1=RD[:, dirn, :], op=ALU.mult), r=[rcs, rRD], w=[rcs])
                P.op("dve", lambda e: e.tensor_tensor(out=sn[:], in0=sn[:], in1=RD[:, dirn, :], op=ALU.mult), r=[rsn, rRD], w=[rsn])
                P.op("dve", lambda e: e.tensor_scalar(out=cs_[:], in0=cs_[:], scalar1=-1.0, scalar2=None, op0=ALU.add), r=[rcs], w=[rcs])
                P.op("dve", lambda e: e.tensor_tensor(out=t1[:], in0=LRt[:], in1=LRt[:], op=ALU.mult), r=[rLR], w=[rt1])
                P.op("dve", lambda e: e.tensor_tensor(out=t2[:], in0=LIt[:], in1=LIt[:], op=ALU.mult), r=[rLI], w=[rt2])
                P.op("dve", lambda e: e.tensor_tensor(out=t1[:], in0=t1[:], in1=t2[:], op=ALU.add), r=[rt1, rt2], w=[rt1])
                P.op("dve", lambda e: e.reciprocal(out=t1[:], in_=t1[:]), r=[rt1], w=[rt1])
                P.op("dve", lambda e: e.tensor_tensor(out=cr[:], in0=cs_[:], in1=LRt[:], op=ALU.mult), r=[rcs, rLR], w=[rcr])
                P.op("dve", lambda e: e.tensor_tensor(out=t2[:], in0=sn[:], in1=LIt[:], op=ALU.mult), r=[rsn, rLI], w=[rt2])
                P.op("dve", lambda e: e.tensor_tensor(out=cr[:], in0=cr[:], in1=t2[:], op=ALU.add), r=[rcr, rt2], w=[rcr])
                P.op("dve", lambda e: e.tensor_tensor(out=cr[:], in0=cr[:], in1=t1[:], op=ALU.mult), r=[rcr, rt1], w=[rcr])
                P.op("dve", lambda e: e.tensor_tensor(out=ci[:], in0=sn[:], in1=LRt[:], op=ALU.mult), r=[rsn, rLR], w=[rci])
                P.op("dve", lambda e: e.tensor_tensor(out=t2[:], in0=cs_[:], in1=LIt[:], op=ALU.mult), r=[rcs, rLI], w=[rt2])
                P.op("dve", lambda e: e.tensor_tensor(out=ci[:], in0=ci[:], in1=t2[:], op=ALU.subtract), r=[rci, rt2], w=[rci])
                P.op("dve", lambda e: e.tensor_tensor(out=ci[:], in0=ci[:], in1=t1[:], op=ALU.mult), r=[rci, rt1], w=[rci])
                crb = cr[:].unsqueeze(2).to_broadcast([128, 8, 16])
                cib = ci[:].unsqueeze(2).to_broadcast([128, 8, 16])
                (b0, rb0), (b1, rb1), (b2, rb2), (b3, rb3) = bb
                P.op("dve", lambda e: e.tensor_tensor(out=b0[:], in0=BRt[:], in1=crb, op=ALU.mult), r=[rBRt, rcr], w=[rb0])
                P.op("dve", lambda e: e.tensor_tensor(out=b1[:], in0=BIt[:], in1=cib, op=ALU.mult), r=[rBIt, rci], w=[rb1])
                P.op("dve", lambda e: e.tensor_tensor(out=b0[:], in0=b0[:], in1=b1[:], op=ALU.subtract), r=[rb0, rb1], w=[rb0])
                P.op("dve", lambda e: e.tensor_tensor(out=b2[:], in0=BIt[:], in1=crb, op=ALU.mult), r=[rBIt, rcr], w=[rb2])
                P.op("dve", lambda e: e.tensor_tensor(out=b3[:], in0=BRt[:], in1=cib, op=ALU.mult), r=[rBRt, rci], w=[rb3])
                P.op("dve", lambda e: e.tensor_tensor(out=b2[:], in0=b2[:], in1=b3[:], op=ALU.add), r=[rb2, rb3], w=[rb2])
                for ri_, (bsrc, rbsrc) in enumerate(((b0, rb0), (b2, rb2))):
                    P.op("dve", lambda e: e.memset(Zp[:], 0.0), w=[rZp])
                    for gp in range(8):
                        for a in range(2):
                            col0 = ((2 * gp + a) % 8) * 16
                            P.op("dve", lambda e: e.tensor_copy(out=Zp[a * 64:(a + 1) * 64, gp, col0:col0 + 16], in_=bsrc[a * 64:(a + 1) * 64, gp, :]), r=[rbsrc], w=[rZp])
                    for gp in range(8):
                        P.op("pe", lambda e: e.transpose(out=tp, in_=Zp[:, gp, :], identity=identf[:]), r=[rZp, ridentf], w=[rtp])
                        P.op("act", lambda e: e.copy(out=BbT[:, dirn, ri_, gp, :], in_=tp), r=[rtp], w=[rBbT])
            for half in range(2):
                for src_name, kinds in (("s5_c_re", ((0, 1.0), (1, -1.0))), ("s5_c_im", ((2, -1.0),))):
                    srcv = Dm[src_name][l].rearrange("g co p -> (g co) p")[half * 128:(half + 1) * 128, :]
                    P.dma("sp", Cn[:, 0:64], srcv, w=[rCn])
                    P.dma("sp", Cn[:, 64:128], srcv, w=[rCn])
                    P.op("dve", lambda e: e.tensor_tensor(out=Cn[:], in0=Cn[:], in1=bdm[:], op=ALU.mult), r=[rCn, rbdm], w=[rCn])
                    P.op("pe", lambda e: e.transpose(out=tp, in_=Cn[:], identity=identf[:]), r=[rCn, ridentf], w=[rtp])
                    for kidx, sgn in kinds:
                        P.op("act", lambda e: e.activation(out=Cm[:, kidx, half, :], in_=tp, func=AF.Copy, scale=sgn), r=[rtp], w=[rCm])
        P.barrier()

        with contextlib.ExitStack() as es3:
            Bp = [[_tile(es3, nc, f"S_Bp{i}{j}", [128, TC], F32, psum=True) for j in range(2)] for i in range(2)]
            Yp = [[_tile(es3, nc, f"S_Yp{i}{j}", [128, 256], F32, psum=True) for j in range(2)] for i in range(2)]
            tq = [[_tile(es3, nc, f"S_tq{i}{j}", [128, TC], F32) for j in range(4)] for i in range(2)]
            bp_ = [[_tile(es3, nc, f"S_bp{i}{j}", [128, TC], F32) for j in range(2)] for i in range(2)]
            Wt = [[_tile(es3, nc, f"S_W{i}{j}", [128, TC], F32) for j in range(2)] for i in range(2)]
            Pr = [[_tile(es3, nc, f"S_Pr{i}{j}", [128, TC], BF16) for j in range(4)] for i in range(2)]
            XR, rXR = _tile(es3, nc, "S_XR", [128, 8], F32)
            XI, rXI = _tile(es3, nc, "S_XI", [128, 8], F32)
            tc1, rtc1 = _tile(es3, nc, "S_tc1", [128, 1], F32)
            tc2, rtc2 = _tile(es3, nc, "S_tc2", [128, 1], F32)
            ysb = [_tile(es3, nc, f"S_ysb{i}", [128, 2, 256], F32) for i in range(2)]
            ang, rang = _tile(es3, nc, "S_ang", [128, 8, TC], F32)
            at2, rat2 = _tile(es3, nc, "S_at2", [128, 8, TC], F32)
            pc_step = make_precast(K, l, es3) if precast else None
            for dirn in range(2):
                for gp in range(8):
                    P.op("dve", lambda e: e.tensor_scalar(out=ang[:, gp, :], in0=iota1[:], scalar1=TH[:, dirn, gp:gp + 1], scalar2=None, op0=ALU.mult), r=[riota1, rTH], w=[rang])
                _sin_reduced(P, SIN[:].rearrange("p a b -> p (a b)"), ang[:].rearrange("p a b -> p (a b)"), 0.0,
                             COS[:].rearrange("p a b -> p (a b)"), rCOS, at2[:].rearrange("p a b -> p (a b)"), rat2, [rang], [rSIN])
                _sin_reduced(P, COS[:].rearrange("p a b -> p (a b)"), ang[:].rearrange("p a b -> p (a b)"), math.pi / 2,
                             ang[:].rearrange("p a b -> p (a b)"), rang, at2[:].rearrange("p a b -> p (a b)"), rat2, [rang], [rCOS])
                P.op("dve", lambda e: e.memset(XR[:], 0.0), w=[rXR])
                P.op("dve", lambda e: e.memset(XI[:], 0.0), w=[rXI])
                ydst = Dm["s5yf"] if dirn == 0 else Dm["s5yb"]
                for ck in range(NCH):
                    if dirn == 0:
                        c0 = L if ck == 0 else (ck - 1) * TC
                    else:
                        c0 = L if ck == 0 else L - ck * TC
                    yp = Yp[ck % 2]
                    if pc_step is not None:
                        pc_step(2)
                    def phase_a(gp):
                        par = gp % 2
                        ct = gp // 4
                        usl = uT[:, ct, c0:c0 + TC]
                        if dirn == 1:
                            usl = usl[:, ::-1]
                        (bre, rbre), (bim, rbim) = Bp[par]
                        P.op("pe", lambda e: e.matmul(bre, lhsT=BbT[:, dirn, 0, gp, :], rhs=usl, start=True, stop=True), r=[rBbT, ruT], w=[rbre])
                        P.op("pe", lambda e: e.matmul(bim, lhsT=BbT[:, dirn, 1, gp, :], rhs=usl, start=True, stop=True), r=[rBbT, ruT], w=[rbim])
                        (q1, rq1), (q2, rq2), (q3, rq3), (q4, rq4) = tq[par]
                        cosg, sing = COS[:, gp, :], SIN[:, gp, :]
                        P.op("dve", lambda e: e.tensor_tensor(out=q1[:], in0=bre, in1=cosg, op=ALU.mult), r=[rbre, rCOS], w=[rq1])
                        P.op("dve", lambda e: e.tensor_tensor(out=q2[:], in0=bim, in1=sing, op=ALU.mult), r=[rbim, rSIN], w=[rq2])
                        P.op("dve", lambda e: e.tensor_tensor(out=q3[:], in0=bim, in1=cosg, op=ALU.mult), r=[rbim, rCOS], w=[rq3])
                        P.op("dve", lambda e: e.tensor_tensor(out=q4[:], in0=bre, in1=sing, op=ALU.mult), r=[rbre, rSIN], w=[rq4])
                        (br2, rbr2), (bi2, rbi2) = bp_[par]
                        P.op("pool", lambda e: e.tensor_tensor(out=br2[:], in0=q1[:], in1=q2[:], op=ALU.add), r=[rq1, rq2], w=[rbr2])
                        P.op("pool", lambda e: e.tensor_tensor(out=bi2[:], in0=q3[:], in1=q4[:], op=ALU.subtract), r=[rq3, rq4], w=[rbi2])

                    def phase_b(gp):
                        par = gp % 2
                        half, gl = gp // 4, gp % 4
                        cosg, sing = COS[:, gp, :], SIN[:, gp, :]
                        (br2, rbr2), (bi2, rbi2) = bp_[par]
                        (wr, rwr), (wi, rwi) = Wt[par]
                        rdb = RD[:, dirn, gp:gp + 1].to_broadcast([128, TC])
                        P.op("dve", lambda e: e.tensor_tensor_scan(out=wr[:], data0=rdb, data1=br2[:], initial=XR[:, gp:gp + 1], op0=ALU.mult, op1=ALU.add),
                             r=[rRD, rbr2, rXR], w=[rwr])
                        P.op("dve", lambda e: e.tensor_tensor_scan(out=wi[:], data0=rdb, data1=bi2[:], initial=XI[:, gp:gp + 1], op0=ALU.mult, op1=ALU.add),
                             r=[rRD, rbi2, rXI], w=[rwi])
                        cl, sl = COS[:, gp, TC - 1:TC], SIN[:, gp, TC - 1:TC]
                        P.op("dve", lambda e: e.tensor_tensor(out=tc1[:], in0=wi[:, TC - 1:TC], in1=sl, op=ALU.mult), r=[rwi, rSIN], w=[rtc1])
                        P.op("dve", lambda e: e.tensor_tensor(out=tc2[:], in0=wi[:, TC - 1:TC], in1=cl, op=ALU.mult), r=[rwi, rCOS], w=[rtc2])
                        P.op("dve", lambda e: e.scalar_tensor_tensor(out=XR[:, gp:gp + 1], in0=wr[:, TC - 1:TC], scalar=cl, in1=tc1[:], op0=ALU.mult, op1=ALU.subtract),
                             r=[rwr, rCOS, rtc1], w=[rXR])
                        P.op("dve", lambda e: e.scalar_tensor_tensor(out=XI[:, gp:gp + 1], in0=wr[:, TC - 1:TC], scalar=sl, in1=tc2[:], op0=ALU.mult, op1=ALU.add),
                             r=[rwr, rSIN, rtc2], w=[rXI])
                        (pcc, rpcc), (pis, rpis), (prs, rprs), (pic, rpic) = Pr[par]
                        ov = (lambda t_: t_[:, ::-1]) if dirn == 1 else (lambda t_: t_[:])
                        P.op("dve", lambda e: e.tensor_tensor(out=ov(pcc), in0=wr[:], in1=cosg, op=ALU.mult), r=[rwr, rCOS], w=[rpcc])
                        P.op("pool", lambda e: e.tensor_tensor(out=ov(pis), in0=wi[:], in1=sing, op=ALU.mult), r=[rwi, rSIN], w=[rpis])
                        P.op("dve", lambda e: e.tensor_tensor(out=ov(prs), in0=wr[:], in1=sing, op=ALU.mult), r=[rwr, rSIN], w=[rprs])
                        P.op("pool", lambda e: e.tensor_tensor(out=ov(pic), in0=wi[:], in1=cosg, op=ALU.mult), r=[rwi, rCOS], w=[rpic])
                        for sub in range(TC // 128):
                            ypt, rypt = yp[sub]
                            osl = ypt[:, gp * 32:(gp + 1) * 32]
                            cs_sl = slice(gl * 32, (gl + 1) * 32)
                            ssl = slice(sub * 128, (sub + 1) * 128)
                            P.op("pe", lambda e: e.matmul(osl, lhsT=pcc[:, ssl], rhs=Cm[:, 0, half, cs_sl], start=True, stop=False), r=[rpcc, rCm], w=[rypt])
                            P.op("pe", lambda e: e.matmul(osl, lhsT=pis[:, ssl], rhs=Cm[:, 1, half, cs_sl], start=False, stop=False), r=[rpis, rCm], w=[rypt])
                            P.op("pe", lambda e: e.matmul(osl, lhsT=prs[:, ssl], rhs=Cm[:, 2, half, cs_sl], start=False, stop=False), r=[rprs, rCm], w=[rypt])
                            P.op("pe", lambda e: e.matmul(osl, lhsT=pic[:, ssl], rhs=Cm[:, 2, half, cs_sl], start=False, stop=True), r=[rpic, rCm], w=[rypt])

                    phase_a(0)
                    for gp in range(8):
                        if gp + 1 < 8:
                            phase_a(gp + 1)
                        phase_b(gp)
                    ys, rys = ysb[ck % 2]
                    for sub in range(TC // 128):
                        ypt, rypt = yp[sub]
                        P.op("act", lambda e: e.copy(out=ys[:, sub, :], in_=ypt), r=[rypt], w=[rys])
                    P.dma("sp", ydst[c0:c0 + TC, :].rearrange("(s p) d -> p s d", p=128), ys[:], r=[rys], w=[R["s5y"]])
            if pc_step is not None:
                pc_step(96)
        P.barrier()

        with contextlib.ExitStack() as es4:
            dsk, rdsk = _tile(es4, nc, "S_dsk", [128, 256], F32)
            glb, rglb = _tile(es4, nc, "S_glb", [128, 256], F32)
            glw, rglw = _tile(es4, nc, "S_glw", [128, 2, 256], BF16)
            ut = [_tile(es4, nc, f"S_ut{i}", [128, 3, 256], F32) for i in range(2)]
            z, rz = _tile(es4, nc, "S_z", [128, 256], F32)
            zg, rzg = _tile(es4, nc, "S_zg", [128, 256], F32)
            zgb, rzgb = _tile(es4, nc, "S_zgb", [128, 256], BF16)
            zT, rzT = _tile(es4, nc, "S_zT", [128, 2, 128], BF16)
            sg, rsg = _tile(es4, nc, "S_sg", [128, 256], F32)
            ob, rob = _tile(es4, nc, "S_ob", [128, 256], BF16)
            oT, roT = _tile(es4, nc, "S_oT", [128, 2, 128], BF16)
            pT2, rpT2 = _tile(es4, nc, "S_pT2", [128, 2, 128], BF16, psum=True)
            pT3, rpT3 = _tile(es4, nc, "S_pT3", [128, 2, 128], BF16, psum=True)
            gp_, rgp_ = _tile(es4, nc, "S_gps", [128, 256], F32, psum=True)
            P.dma("sp", dsk[:], Dm["s5_d"][l].partition_broadcast(128), w=[rdsk])
            P.dma("sp", glb[:], Dm["s5_glu_b"][l].partition_broadcast(128), w=[rglb])
            P.dma("pool", glw[:], Dm["s5_glu_w"][l].rearrange("(k p) n -> p k n", p=128), w=[rglw])
            tiles = list(range(NXT)) + ([64, 65] if with_ctx else [])
            if epi_tiles is not None:
                tiles = [t for t in tiles if t in epi_tiles]

            def load(i):
                t = tiles[i]
                u_, ru_ = ut[i % 2]
                ts = slice(t * 128, (t + 1) * 128)
                P.dma("sp", u_[:, 0, :], Dm["su"][ts, :], r=[R["mix"]], w=[ru_])
                P.dma("sp", u_[:, 1, :], Dm["s5yf"][ts, :], r=[R["s5y"]], w=[ru_])
                P.dma("sp", u_[:, 2, :], Dm["s5yb"][ts, :], r=[R["s5y"]], w=[ru_])
            if tiles:
                load(0)
            for i, t in enumerate(tiles):
                if i + 1 < len(tiles):
                    load(i + 1)
                u_, ru_ = ut[i % 2]
                ts = slice(t * 128, (t + 1) * 128)
                P.op("dve", lambda e: e.tensor_tensor(out=z[:], in0=u_[:, 0, :], in1=dsk[:], op=ALU.mult), r=[ru_, rdsk], w=[rz])
                P.op("dve", lambda e: e.tensor_tensor(out=z[:], in0=z[:], in1=u_[:, 1, :], op=ALU.add), r=[rz, ru_], w=[rz])
                P.op("dve", lambda e: e.tensor_tensor(out=z[:], in0=z[:], in1=u_[:, 2, :], op=ALU.add), r=[rz, ru_], w=[rz])
                P.op("act", lambda e: e.activation(out=zg[:], in_=z[:], func=AF.Gelu), r=[rz], w=[rzg])
                P.op("dve", lambda e: e.tensor_copy(out=zgb[:], in_=zg[:]), r=[rzg], w=[rzgb])
                for k in range(2):
                    P.op("pe", lambda e: e.transpose(out=pT2[:, k, :], in_=zgb[:, k * 128:(k + 1) * 128], identity=ident[:]), r=[rzgb, rident], w=[rpT2])
                P.op("act", lambda e: e.copy(out=zT[:], in_=pT2), r=[rpT2], w=[rzT])
                for k in range(2):
                    P.op("pe", lambda e: e.matmul(gp_, lhsT=zT[:, k, :], rhs=glw[:, k, :], start=(k == 0), stop=(k == 1)), r=[rzT, rglw], w=[rgp_])
                P.op("dve", lambda e: e.tensor_tensor(out=sg[:], in0=gp_, in1=glb[:], op=ALU.add), r=[rgp_, rglb], w=[rsg])
                P.op("act", lambda e: e.activation(out=sg[:], in_=sg[:], func=AF.Sigmoid), r=[rsg], w=[rsg])
                P.op("dve", lambda e: e.tensor_tensor(out=ob[:], in0=sg[:], in1=zg[:], op=ALU.mult), r=[rsg, rzg], w=[rob])
                for k in range(2):
                    P.op("pe", lambda e: e.transpose(out=pT3[:, k, :], in_=ob[:, k * 128:(k + 1) * 128], identity=ident[:]), r=[rob, rident], w=[rpT3])
                P.op("act", lambda e: e.copy(out=oT[:], in_=pT3), r=[rpT3], w=[roT])
                P.dma(STQ, Dm["yT"][512:768, :].rearrange("(k p) n -> p k n", p=128)[:, :, ts], oT[:], r=[roT], w=[R["y"]])
    P.pe_relaxed = False
    P.barrier()


SCRATCH.update({"s5yf": ([T, 256], F32), "s5yb": ([T, 256], F32)})


def stage_merge(K, l, with_ctx, tiles=None):
    nc, P, Dm, R = K.nc, K.P, K.dram, K.R
    P.pe_relaxed = True
    with contextlib.ExitStack() as es:
        wg, rwg = _tile(es, nc, "M_wg", [128, 8, 4096], BF16)
        wb, rwb = _tile(es, nc, "M_wb", [128, 8, 1024], BF16)
        wo, rwo = _tile(es, nc, "M_wo", [128, 8, 1024], BF16)
        m2, rm2 = _tile(es, nc, "M_m2", [128, 2, 1024], F32)
        ident, rident = _tile(es, nc, "M_ident", [128, 128], BF16)
        yTt = [_tile(es, nc, f"M_yT{i}", [128, 8, 128], BF16) for i in range(2)]
        aTt = [_tile(es, nc, f"M_aT{i}", [128, 8, 128], BF16) for i in range(2)]
        ht = [_tile(es, nc, f"M_h{i}", [128, 1024], F32) for i in range(2)]
        sig = [_tile(es, nc, f"M_sig{i}", [128, 512], F32) for i in range(2)]
        term, rterm = _tile(es, nc, "M_term", [128, 512], F32)
        mg, rmg = _tile(es, nc, "M_mg", [128, 1024], F32)
        mb, rmb = _tile(es, nc, "M_mb", [128, 1024], BF16)
        mT, rmT = _tile(es, nc, "M_mT", [128, 8, 128], BF16)
        hn, rhn = _tile(es, nc, "M_hn", [128, 1024], F32)
        Gp = [_tile(es, nc, f"M_Gp{i}", [128, 512], F32, psum=True) for i in range(2)]
        Zp = [_tile(es, nc, f"M_Zp{i}", [128, 512], F32, psum=True) for i in range(2)]
        pT, rpT = _tile(es, nc, "M_pT", [128, 8, 128], BF16, psum=True)
        Op = [_tile(es, nc, f"M_Op{i}", [128, 512], F32, psum=True) for i in range(2)]
        w_in_v = Dm["w_in"][l].rearrange("(k p) n -> p k n", p=128)
        for i in range(4):
            P.dma("pool", wg[:, :, i * 1024:(i + 1) * 1024], w_in_v[:, :, MIXC + i * 1024:MIXC + (i + 1) * 1024], w=[rwg])
        P.dma("pool", wb[:], Dm["w_branch"][l].rearrange("i (k p) n -> p (i k) n", p=128), w=[rwb])
        P.dma("pool", wo[:], Dm["w_out"][l].rearrange("(k p) n -> p k n", p=128), w=[rwo])
        P.dma("pool", ident[:], Dm["ident"], w=[rident])
        P.dma("sp", m2[:, 0, :], Dm["modv"][2].partition_broadcast(128), r=[R["modv"]], w=[rm2])
        P.dma("sp", m2[:, 1, :], Dm["modv"][8].partition_broadcast(128), r=[R["modv"]], w=[rm2])
        tiles = tiles if tiles is not None else list(range(NXT)) + ([64, 65] if with_ctx else [])
        hsrc = K.hsrc(l)

        def load(i):
            t = tiles[i]
            ts = slice(t * 128, (t + 1) * 128)
            P.dma("sp", yTt[i % 2][0][:], Dm["yT"].rearrange("(k p) n -> p k n", p=128)[:, :, ts], r=[R["y"]], w=[yTt[i % 2][1]])
            P.dma("sp", aTt[i % 2][0][:], Dm["aT"].rearrange("(k p) n -> p k n", p=128)[:, :, ts], r=[R["aT"]], w=[aTt[i % 2][1]])
            P.dma("sp", ht[i % 2][0][:], hsrc(t), r=[R["H"]], w=[ht[i % 2][1]])
        load(0)
        for i, t in enumerate(tiles):
            if i + 1 < len(tiles):
                load(i + 1)
            yt, ryt = yTt[i % 2]
            at, rat = aTt[i % 2]
            h_, rh_ = ht[i % 2]
            ts = slice(t * 128, (t + 1) * 128)
            cnt = 0
            for nh in range(2):
                for br in range(4):
                    gp, rgp = Gp[cnt % 2]
                    zp, rzp = Zp[cnt % 2]
                    sg, rsg = sig[cnt % 2]
                    cnt += 1
                    c0 = br * 1024 + nh * 512
                    for k in range(8):
                        P.op("pe", lambda e: e.matmul(gp, lhsT=at[:, k, :], rhs=wg[:, k, c0:c0 + 512], start=(k == 0), stop=(k == 7)), r=[rat, rwg], w=[rgp])
                    for k2 in range(2):
                        P.op("pe", lambda e: e.matmul(zp, lhsT=yt[:, 2 * br + k2, :], rhs=wb[:, 2 * br + k2, nh * 512:(nh + 1) * 512], start=(k2 == 0), stop=(k2 == 1)),
                             r=[ryt, rwb], w=[rzp])
                    P.op("act", lambda e: e.activation(out=sg[:], in_=gp, func=AF.Sigmoid), r=[rgp], w=[rsg])
                    dst = mg[:, nh * 512:(nh + 1) * 512]
                    if br == 0:
                        P.op("dve", lambda e: e.tensor_tensor(out=dst, in0=zp, in1=sg[:], op=ALU.mult), r=[rzp, rsg], w=[rmg])
                    else:
                        P.op("dve", lambda e: e.tensor_tensor(out=term[:], in0=zp, in1=sg[:], op=ALU.mult), r=[rzp, rsg], w=[rterm])
                        P.op("dve", lambda e: e.tensor_tensor(out=dst, in0=dst, in1=term[:], op=ALU.add), r=[rmg, rterm], w=[rmg])
            P.op("act", lambda e: e.copy(out=mb[:], in_=mg[:]), r=[rmg], w=[rmb])
            for k in range(8):
                P.op("pe", lambda e: e.transpose(out=pT[:, k, :], in_=mb[:, k * 128:(k + 1) * 128], identity=ident[:]), r=[rmb, rident], w=[rpT])
            P.op("act", lambda e: e.copy(out=mT[:], in_=pT), r=[rpT], w=[rmT])
            for nh in range(2):
                op_, rop = Op[nh]
                for k in range(8):
                    P.op("pe", lambda e: e.matmul(op_, lhsT=mT[:, k, :], rhs=wo[:, k, nh * 512:(nh + 1) * 512], start=(k == 0), stop=(k == 7)), r=[rmT, rwo], w=[rop])
                sl = slice(nh * 512, (nh + 1) * 512)
                P.op("dve", lambda e: e.tensor_tensor(out=hn[:, sl], in0=op_, in1=m2[:, 0 if t < NXT else 1, sl], op=ALU.mult), r=[rop, rm2], w=[rhn])
            P.op("dve", lambda e: e.tensor_tensor(out=hn[:], in0=hn[:], in1=h_[:], op=ALU.add), r=[rhn, rh_], w=[rhn])
            P.dma(STQ, Dm["H"][ts, :], hn[:], r=[rhn], w=[R["H"]])
    P.pe_relaxed = False
    P.barrier()


SGT = 12
BIG = 1.0e30


def stage_moe(K, l, with_ctx, last, tiles=None, experts=None):
    nc, P, Dm, R = K.nc, K.P, K.dram, K.R
    tiles = tiles if tiles is not None else list(range(NXT)) + ([64, 65] if with_ctx else [])
    experts = list(range(32)) if experts is None else experts
    with contextlib.ExitStack() as es:
        bc, rbc = _tile(es, nc, "E_bc", [128, 6, 1024], F32)
        identf, ridentf = _tile(es, nc, "E_identf", [128, 128], F32)
        rw, rrw = _tile(es, nc, "E_rw", [128, 8, 36], F32)
        rb, rrb = _tile(es, nc, "E_rb", [128, 36], F32)
        epsc, repsc = _tile(es, nc, "E_eps", [128, 1], F32)
        FT, rFT = _tile(es, nc, "E_FT", [128, 8, SGT * 128], BF16)
        Gall, rGall = _tile(es, nc, "E_G", [128, SGT, 32], F32)
        yacc, ryacc = _tile(es, nc, "E_yacc", [128, SGT, 1024], F32)
        for j, row in enumerate((3, 4, 5, 9, 10, 11)):
            P.dma("sp", bc[:, j, :], Dm["modv"][row].partition_broadcast(128), r=[R["modv"]], w=[rbc])
        P.dma("sp", identf[:], Dm["ident"], w=[ridentf])
        with nc.allow_non_contiguous_dma(reason="tiny router weights"):
            P.dma("sp", rw[:, :, 0:4], Dm["router_w1"][l].rearrange("(k p) n -> p k n", p=128), w=[rrw])
            P.dma("sp", rw[:, :, 4:36], Dm["router_w2"][l].rearrange("(k p) n -> p k n", p=128), w=[rrw])
        P.dma("sp", rb[:, 0:4], Dm["router_b1"][l].partition_broadcast(128), w=[rrb])
        P.dma("sp", rb[:, 4:36], Dm["router_b2"][l].partition_broadcast(128), w=[rrb])
        P.op("dve", lambda e: e.memset(epsc[:], EPS), w=[repsc])
        P.barrier()
        sgs = [tiles[i:i + SGT] for i in range(0, len(tiles), SGT)]
        for sg in sgs:
            with contextlib.ExitStack() as e1:
                ht = [_tile(e1, nc, f"E1_h{i}", [128, 1024], F32) for i in range(2)]
                junk, rjunk = _tile(e1, nc, "E1_junk", [128, 1024], BF16)
                ssq, rssq = _tile(e1, nc, "E1_ssq", [128, 1], F32)
                rstd, rrstd = _tile(e1, nc, "E1_rstd", [128, 1], F32)
                f_, rf_ = _tile(e1, nc, "E1_f", [128, 1024], F32)
                lgt, rlgt = _tile(e1, nc, "E1_lg", [128, 36], F32)
                sm = [_tile(e1, nc, f"E1_s{i}", [128, 8], F32) for i in range(4)]
                oh = [_tile(e1, nc, f"E1_oh{i}", [128, 32], F32) for i in range(3)]
                pTf = [_tile(e1, nc, f"E1_pT{i}", [128, 4, 128], F32, psum=True) for i in range(2)]
                lp, rlp = _tile(e1, nc, "E1_lp", [128, 36], F32, psum=True)

                def load(i):
                    t = sg[i]
                    P.dma("sp", ht[i % 2][0][:], Dm["H"][t * 128:(t + 1) * 128, :], r=[R["H"]], w=[ht[i % 2][1]])
                load(0)
                for i, t in enumerate(sg):
                    if i + 1 < len(sg):
                        load(i + 1)
                    h_, rh_ = ht[i % 2]
                    o = 0 if t < NXT else 3
                    P.op("act", lambda e: e.activation(out=junk[:], in_=h_[:], func=AF.Square, accum_out=ssq[:]), r=[rh_], w=[rjunk, rssq])
                    P.op("act", lambda e: e.activation(out=rstd[:], in_=ssq[:], func=AF.Sqrt, scale=1.0 / D, bias=epsc[:, 0:1]), r=[rssq, repsc], w=[rrstd])
                    P.op("dve", lambda e: e.reciprocal(out=rstd[:], in_=rstd[:]), r=[rrstd], w=[rrstd])
                    P.op("dve", lambda e: e.scalar_tensor_tensor(out=f_[:], in0=h_[:], scalar=rstd[:, 0:1], in1=bc[:, o, :], op0=ALU.mult, op1=ALU.mult),
                         r=[rh_, rrstd, rbc], w=[rf_])
                    P.op("dve", lambda e: e.tensor_tensor(out=f_[:], in0=f_[:], in1=bc[:, o + 1, :], op=ALU.add), r=[rf_, rbc], w=[rf_])
                    for hf in range(2):
                        pt, rpt = pTf[hf]
                        for k in range(4):
                            kk = hf * 4 + k
                            P.op("pe", lambda e: e.transpose(out=pt[:, k, :], in_=f_[:, kk * 128:(kk + 1) * 128], identity=identf[:]), r=[rf_, ridentf], w=[rpt])
                    fTf, rfTf = f_, rf_
                    for hf in range(2):
                        pt, rpt = pTf[hf]
                        P.op("act", lambda e: e.copy(out=fTf[:, hf * 512:(hf + 1) * 512].rearrange("p (k n) -> p k n", n=128), in_=pt), r=[rpt], w=[rfTf])
                    P.op("dve", lambda e: e.tensor_copy(out=FT[:, :, i * 128:(i + 1) * 128], in_=fTf[:].rearrange("p (k n) -> p k n", n=128)), r=[rfTf], w=[rFT])
                    for k in range(8):
                        P.op("pe", lambda e: e.matmul(lp, lhsT=fTf[:, k * 128:(k + 1) * 128], rhs=rw[:, k, :], start=(k == 0), stop=(k == 7)), r=[rfTf, rrw], w=[rlp])
                    P.op("dve", lambda e: e.tensor_tensor(out=lgt[:], in0=lp, in1=rb[:], op=ALU.add), r=[rlp, rrb], w=[rlgt])
                    (s0, rs0), (s1, rs1), (s2, rs2), (s3, rs3) = sm
                    (oh1, roh1), (oh2, roh2), (l2, rl2) = oh
                    P.op("dve", lambda e: e.tensor_reduce(out=s0[:, 0:1], in_=lgt[:, 0:4], axis=AX.X, op=ALU.max), r=[rlgt], w=[rs0])
                    P.op("dve", lambda e: e.tensor_scalar(out=s0[:, 1:2], in0=s0[:, 0:1], scalar1=-1.0, scalar2=None, op0=ALU.mult), r=[rs0], w=[rs0])
                    P.op("act", lambda e: e.activation(out=s1[:, 0:4], in_=lgt[:, 0:4], func=AF.Exp, bias=s0[:, 1:2], accum_out=s0[:, 2:3]), r=[rlgt, rs0], w=[rs1, rs0])
                    P.op("dve", lambda e: e.reciprocal(out=s0[:, 3:4], in_=s0[:, 2:3]), r=[rs0], w=[rs0])
                    P.op("dve", lambda e: e.tensor_scalar(out=s2[:, 0:4], in0=lgt[:, 0:4], scalar1=s0[:, 0:1], scalar2=None, op0=ALU.is_equal), r=[rlgt, rs0], w=[rs2])
                    P.op("dve", lambda e: e.tensor_scalar(out=s2[:, 0:4], in0=s2[:, 0:4], scalar1=BIG, scalar2=-BIG, op0=ALU.mult, op1=ALU.add), r=[rs2], w=[rs2])
                    P.op("dve", lambda e: e.tensor_tensor(out=l2[:].rearrange("p (g e) -> p g e", e=8), in0=lgt[:, 4:36].rearrange("p (g e) -> p g e", e=8),
                                                          in1=s2[:, 0:4].unsqueeze(2).to_broadcast([128, 4, 8]), op=ALU.add), r=[rlgt, rs2], w=[rl2])
                    P.op("dve", lambda e: e.tensor_reduce(out=s3[:, 0:1], in_=l2[:], axis=AX.X, op=ALU.max), r=[rl2], w=[rs3])
                    P.op("dve", lambda e: e.tensor_scalar(out=oh1[:], in0=l2[:], scalar1=s3[:, 0:1], scalar2=None, op0=ALU.is_equal), r=[rl2, rs3], w=[roh1])
                    P.op("dve", lambda e: e.scalar_tensor_tensor(out=l2[:], in0=oh1[:], scalar=-BIG, in1=l2[:], op0=ALU.mult, op1=ALU.add), r=[roh1, rl2], w=[rl2])
                    P.op("dve", lambda e: e.tensor_reduce(out=s3[:, 1:2], in_=l2[:], axis=AX.X, op=ALU.max), r=[rl2], w=[rs3])
                    P.op("dve", lambda e: e.tensor_scalar(out=oh2[:], in0=l2[:], scalar1=s3[:, 1:2], scalar2=None, op0=ALU.is_equal), r=[rl2, rs3], w=[roh2])
                    P.op("dve", lambda e: e.tensor_tensor(out=s3[:, 2:3], in0=s3[:, 1:2], in1=s3[:, 0:1], op=ALU.subtract), r=[rs3], w=[rs3])
                    P.op("act", lambda e: e.activation(out=s3[:, 3:4], in_=s3[:, 2:3], func=AF.Exp), r=[rs3], w=[rs3])
                    P.op("dve", lambda e: e.tensor_scalar(out=s3[:, 4:5], in0=s3[:, 3:4], scalar1=1.0, scalar2=None, op0=ALU.add), r=[rs3], w=[rs3])
                    P.op("dve", lambda e: e.reciprocal(out=s3[:, 4:5], in_=s3[:, 4:5]), r=[rs3], w=[rs3])
                    P.op("dve", lambda e: e.tensor_tensor(out=s3[:, 5:6], in0=s3[:, 4:5], in1=s0[:, 3:4], op=ALU.mult), r=[rs3, rs0], w=[rs3])
                    P.op("dve", lambda e: e.tensor_tensor(out=s3[:, 6:7], in0=s3[:, 5:6], in1=s3[:, 3:4], op=ALU.mult), r=[rs3], w=[rs3])
                    P.op("dve", lambda e: e.tensor_scalar(out=Gall[:, i, :], in0=oh1[:], scalar1=s3[:, 5:6], scalar2=None, op0=ALU.mult), r=[roh1, rs3], w=[rGall])
                    P.op("dve", lambda e: e.scalar_tensor_tensor(out=Gall[:, i, :], in0=oh2[:], scalar=s3[:, 6:7], in1=Gall[:, i, :], op0=ALU.mult, op1=ALU.add),
                         r=[roh2, rs3, rGall], w=[rGall])
            P.barrier()
            with contextlib.ExitStack() as e2:
                w1 = [_tile(e2, nc, f"E2_w1_{i}", [128, 8, 512], BF16) for i in range(2)]
                w3 = [_tile(e2, nc, f"E2_w3_{i}", [128, 8, 512], BF16) for i in range(2)]
                w2 = [_tile(e2, nc, f"E2_w2_{i}", [128, 4, 1024], BF16) for i in range(2)]
                sl = [_tile(e2, nc, f"E2_sl{i}", [128, 512], F32) for i in range(2)]
                hid = [_tile(e2, nc, f"E2_hid{i}", [128, 4, 512], BF16) for i in range(2)]
                H1 = [_tile(e2, nc, f"E2_H1{i}", [128, 512], F32, psum=True) for i in range(2)]
                H3 = [_tile(e2, nc, f"E2_H3{i}", [128, 512], F32, psum=True) for i in range(2)]
                Yp = [[_tile(e2, nc, f"E2_Y{i}{j}", [128, 512], F32, psum=True) for j in range(2)] for i in range(2)]
                groups = [list(range(g0, min(g0 + 4, len(sg)))) for g0 in range(0, len(sg), 4)]

                def loadw(ei):
                    e_ = experts[ei]
                    P.dma("pool", w1[ei % 2][0][:], Dm["exp_w1"][l, e_].rearrange("(k p) n -> p k n", p=128), w=[w1[ei % 2][1]])
                    P.dma("pool", w3[ei % 2][0][:], Dm["exp_w3"][l, e_].rearrange("(k p) n -> p k n", p=128), w=[w3[ei % 2][1]])
                    P.dma("pool", w2[ei % 2][0][:], Dm["exp_w2"][l, e_].rearrange("(k p) n -> p k n", p=128), w=[w2[ei % 2][1]])
                loadw(0)
                cnt = 0
                ycnt = 0
                gi_ = 0
                for ei, e_ in enumerate(experts):
                    if ei + 1 < len(experts):
                        loadw(ei + 1)
                    (w1t, rw1), (w3t, rw3), (w2t, rw2) = w1[ei % 2], w3[ei % 2], w2[ei % 2]
                    for grp in groups:
                        ntok = len(grp) * 128
                        tsl = slice(grp[0] * 128, grp[0] * 128 + ntok)
                        hd, rhd = hid[gi_ % 2]
                        gi_ += 1
                        for fc in range(4):
                            h1, rh1 = H1[cnt % 2]
                            h3, rh3 = H3[cnt % 2]
                            s_, rs_ = sl[cnt % 2]
                            cnt += 1
                            for k in range(8):
                                P.op("pe", lambda e: e.matmul(h1[:, 0:ntok], lhsT=w1t[:, k, fc * 128:(fc + 1) * 128], rhs=FT[:, k, tsl], start=(k == 0), stop=(k == 7)),
                                     r=[rw1, rFT], w=[rh1])
                            for k in range(8):
                                P.op("pe", lambda e: e.matmul(h3[:, 0:ntok], lhsT=w3t[:, k, fc * 128:(fc + 1) * 128], rhs=FT[:, k, tsl], start=(k == 0), stop=(k == 7)),
                                     r=[rw3, rFT], w=[rh3])
                            P.op("act", lambda e: e.activation(out=s_[:, 0:ntok], in_=h1[:, 0:ntok], func=AF.Silu), r=[rh1], w=[rs_])
                            P.op("dve", lambda e: e.tensor_tensor(out=hd[:, fc, 0:ntok], in0=h3[:, 0:ntok], in1=s_[:, 0:ntok], op=ALU.mult), r=[rh3, rs_], w=[rhd])
                        for ti, tt in enumerate(grp):
                            yp = Yp[ycnt % 2]
                            ycnt += 1
                            for nh in range(2):
                                ypt, rypt = yp[nh]
                                for fc in range(4):
                                    P.op("pe", lambda e: e.matmul(ypt, lhsT=hd[:, fc, ti * 128:(ti + 1) * 128], rhs=w2t[:, fc, nh * 512:(nh + 1) * 512], start=(fc == 0), stop=(fc == 3)),
                                         r=[rhd, rw2], w=[rypt])
                                dst = yacc[:, tt, nh * 512:(nh + 1) * 512]
                                if ei == 0:
                                    P.op("dve", lambda e: e.tensor_scalar(out=dst, in0=ypt, scalar1=Gall[:, tt, e_:e_ + 1], scalar2=None, op0=ALU.mult), r=[rypt, rGall], w=[ryacc])
                                else:
                                    P.op("dve", lambda e: e.scalar_tensor_tensor(out=dst, in0=ypt, scalar=Gall[:, tt, e_:e_ + 1], in1=dst, op0=ALU.mult, op1=ALU.add),
                                         r=[rypt, rGall, ryacc], w=[ryacc])
            P.barrier()
            with contextlib.ExitStack() as e3:
                ht = [_tile(e3, nc, f"E3_h{i}", [128, 1024], F32) for i in range(2)]
                hn = [_tile(e3, nc, f"E3_hn{i}", [128, 1024], F32) for i in range(2)]
                for i, t in enumerate(sg):
                    h_, rh_ = ht[i % 2]
                    n_, rn_ = hn[i % 2]
                    ts = slice(t * 128, (t + 1) * 128)
                    P.dma("sp", h_[:], Dm["H"][ts, :], r=[R["H"]], w=[rh_])
                    P.op("dve", lambda e: e.tensor_tensor(out=n_[:], in0=yacc[:, i, :], in1=bc[:, 2 if t < NXT else 5, :], op=ALU.mult), r=[ryacc, rbc], w=[rn_])
                    P.op("dve", lambda e: e.tensor_tensor(out=n_[:], in0=n_[:], in1=h_[:], op=ALU.add), r=[rn_, rh_], w=[rn_])
                    if last:
                        P.dma("pool", Dm["out"][ts, :], n_[:], r=[rn_], w=[R["out"]])
                    else:
                        P.dma("pool", Dm["H"][ts, :], n_[:], r=[rn_], w=[R["H"]])
            P.barrier()
    P.barrier()


def full_plan(nlayers=DEPTH):
    def plan(K):
        for l in range(nlayers):
            with_ctx = l < DEPTH - 1
            last = l == DEPTH - 1
            stage_prep(K, l)
            stage_A(K, l)
            stage_ret(K, l, with_ctx)
            stage_na(K, l, with_ctx)
            stage_s5(K, l, with_ctx, precast=True)
            stage_gqa(K, l, with_ctx)
            stage_merge(K, l, with_ctx)
            stage_moe_sparse(K, l, with_ctx, last, precast=False)
    return plan


_CACHE = {}


def kernel(**inputs):
    consts = make_consts()
    n = 8
    x = np.asarray(inputs["x"], np.float32)
    in_maps = []
    shared = {k: np.ascontiguousarray(np.asarray(inputs[k], np.float32)) for k in INPUT_NAMES if k not in ("x", "c", "ctx")}
    for b in range(n):
        m = dict(shared)
        m["x"] = np.ascontiguousarray(x[b])
        m["ctx"] = np.ascontiguousarray(np.asarray(inputs["ctx"], np.float32)[b])
        m["c"] = np.ascontiguousarray(np.asarray(inputs["c"], np.float32)[b:b + 1])
        m.update(consts)
        in_maps.append(m)
    shapes = {k: (v.shape, F32) for k, v in in_maps[0].items() if k not in consts}
    if "nc" not in _CACHE:
        _CACHE["nc"] = build(shapes, consts, full_plan())[0]
    res = run_bass_kernel_spmd(_CACHE["nc"], in_maps, core_ids=list(range(n)))
    return np.stack([np.asarray(r["out"], np.float32) for r in res.results], axis=0)


def _idma(P, out, out_off, in_, in_off, r=(), w=()):
    q = "pool"
    P._deps(q, r, w)
    i = P.dnext[q]
    P.dnext[q] = (i + 1) % P.NDS
    key = ("d", q, i)
    P._wait(q, key, 16 * P.duse[q][i])
    ins = P.nc.gpsimd.indirect_dma_start(out=out, out_offset=out_off, in_=in_, in_offset=in_off)
    P.duse[q][i] += 1
    ins.then_inc(P.semh[key], 16)
    P._mark((key, 16 * P.duse[q][i]), r, w)
    P.ninst += 1
    return ins


def make_precast(K, l, es):
    nc, P, Dm = K.nc, K.P, K.dram
    st = [_tile(es, nc, f"PC_st{i}", [128, 4096], F32) for i in range(2)]
    sb = [_tile(es, nc, f"PC_sb{i}", [128, 4096], BF16) for i in range(2)]
    jobs = [(e_, src, dst, kk) for e_ in range(32) for (src, dst, kk) in (("exp_w1", "wb1", 8), ("exp_w3", "wb3", 8), ("exp_w2", "wb2", 4))]
    state = {"i": 0}
    rwb = K.R["wbf"]

    def step(n):
        for _ in range(n):
            i = state["i"]
            if i >= len(jobs):
                return
            state["i"] = i + 1
            e_, src, dst, kk = jobs[i]
            s_, rs_ = st[i % 2]
            b_, rb_ = sb[i % 2]
            P.dma("sp", s_[:].rearrange("p (k n) -> p k n", k=kk), Dm[src][l, e_].rearrange("(k p) n -> p k n", p=128), w=[rs_])
            P.op("act", lambda e: e.copy(out=b_[:], in_=s_[:]), r=[rs_], w=[rb_])
            P.dma("act", Dm[dst][e_ * 128:(e_ + 1) * 128, :], b_[:], r=[rb_], w=[rwb])
    return step


def stage_moe_sparse(K, l, with_ctx, last, precast=True, nblocks=None):
    nc, P, Dm, R = K.nc, K.P, K.dram, K.R
    tiles = list(range(NXT)) + ([64, 65] if with_ctx else [])
    NTL = len(tiles)
    B = MOE_B
    NB = -(-(2 * NTL * 128 + 32 * (B - 1)) // B)
    IOA = bass.IndirectOffsetOnAxis
    rwb = K.R["wbf"]
    rfb = Res("fb")
    rxs = Res("xs")
    rys = Res("ys")
    P.pe_relaxed = True
    if precast:
        with contextlib.ExitStack() as e0:
            st = [_tile(e0, nc, f"E0_st{i}", [128, 4096], F32) for i in range(3)]
            sb = [_tile(e0, nc, f"E0_sb{i}", [128, 4096], BF16) for i in range(3)]
            cnt = 0
            for e_ in range(32):
                for src, dst, kk in (("exp_w1", "wb1", 8), ("exp_w3", "wb3", 8), ("exp_w2", "wb2", 4)):
                    s_, rs_ = st[cnt % 3]
                    b_, rb_ = sb[cnt % 3]
                    eng = ("act", "pool", "dve")[cnt % 3]
                    cnt += 1
                    P.dma("sp", s_[:].rearrange("p (k n) -> p k n", k=kk), Dm[src][l, e_].rearrange("(k p) n -> p k n", p=128), w=[rs_])
                    if eng == "act":
                        P.op("act", lambda e: e.copy(out=b_[:], in_=s_[:]), r=[rs_], w=[rb_])
                    else:
                        P.op(eng, lambda e: e.tensor_copy(out=b_[:], in_=s_[:]), r=[rs_], w=[rb_])
                    P.dma("act", Dm[dst][e_ * 128:(e_ + 1) * 128, :], b_[:], r=[rb_], w=[rwb])
        P.barrier()
    with contextlib.ExitStack() as es:
        bc, rbc = _tile(es, nc, "Q_bc", [128, 6, 1024], F32)
        OH1, rOH1 = _tile(es, nc, "Q_OH1", [128, NTL, 32], F32)
        OH2, rOH2 = _tile(es, nc, "Q_OH2", [128, NTL, 32], F32)
        OH12, rOH12 = _tile(es, nc, "Q_OH12", [128, NTL, 32], BF16)
        GA, rGA = _tile(es, nc, "Q_GA", [128, NTL, 2], F32)
        DST, rDST = _tile(es, nc, "Q_DST", [128, NTL, 2], I32)
        IDXW, rIDXW = _tile(es, nc, "Q_IDXW", [128, NB], I32)
        pstart, rpstart = _tile(es, nc, "Q_pstart", [128, 32], F32)
        onesb, ronesb = _tile(es, nc, "Q_ones", [128, 128], BF16)
        ustr, rustr = _tile(es, nc, "Q_ustr", [128, 128], BF16)
        identb, ridentb = _tile(es, nc, "Q_identb", [128, 128], BF16)
        for j, row in enumerate((3, 4, 5, 9, 10, 11)):
            P.dma("sp", bc[:, j, :], Dm["modv"][row].partition_broadcast(128), r=[R["modv"]], w=[rbc])
        P.dma("pool", onesb[:], Dm["moe_ones"], w=[ronesb])
        P.dma("pool", ustr[:], Dm["moe_ustrict"], w=[rustr])
        P.dma("pool", identb[:], Dm["ident"], w=[ridentb])
        with contextlib.ExitStack() as e1:
            identf, ridentf = _tile(e1, nc, "Q1_identf", [128, 128], F32)
            rw, rrw = _tile(e1, nc, "Q1_rw", [128, 8, 36], F32)
            rb, rrb = _tile(e1, nc, "Q1_rb", [128, 36], F32)
            epsc, repsc = _tile(e1, nc, "Q1_eps", [128, 1], F32)
            ht = [_tile(e1, nc, f"Q1_h{i}", [128, 1024], F32) for i in range(2)]
            junk, rjunk = _tile(e1, nc, "Q1_junk", [128, 1024], BF16)
            ssq, rssq = _tile(e1, nc, "Q1_ssq", [128, 1], F32)
            rstd, rrstd = _tile(e1, nc, "Q1_rstd", [128, 1], F32)
            f_, rf_ = _tile(e1, nc, "Q1_f", [128, 1024], F32)
            fb = [_tile(e1, nc, f"Q1_fb{i}", [128, 1024], BF16) for i in range(2)]
            lgt, rlgt = _tile(e1, nc, "Q1_lg", [128, 36], F32)
            sm = [_tile(e1, nc, f"Q1_s{i}", [128, 8], F32) for i in range(4)]
            l2, rl2 = _tile(e1, nc, "Q1_l2", [128, 32], F32)
            cnts, rcnts = _tile(e1, nc, "Q1_cnt", [128, 32], F32)
            wk = [_tile(e1, nc, f"Q1_wk{i}", [128, 32], F32) for i in range(3)]
            cmpt, rcmpt = _tile(e1, nc, "Q1_cmp", [128, NB, 32], F32)
            bbt, rbbt = _tile(e1, nc, "Q1_bb", [128, NB, 32], F32)
            eblk, reblk = _tile(e1, nc, "Q1_eblk", [128, NB], F32)
            pcol, rpcol = _tile(e1, nc, "Q1_pcol", [128, 1], F32)
            pTf = [_tile(e1, nc, f"Q1_pT{i}", [128, 4, 128], F32, psum=True) for i in range(2)]
            lp, rlp = _tile(e1, nc, "Q1_lp", [128, 36], F32, psum=True)
            cp, rcp = _tile(e1, nc, "Q1_cp", [128, 32], F32, psum=True)
            P.dma("sp", identf[:], Dm["ident"], w=[ridentf])
            with nc.allow_non_contiguous_dma(reason="tiny router weights"):
                P.dma("sp", rw[:, :, 0:4], Dm["router_w1"][l].rearrange("(k p) n -> p k n", p=128), w=[rrw])
                P.dma("sp", rw[:, :, 4:36], Dm["router_w2"][l].rearrange("(k p) n -> p k n", p=128), w=[rrw])
            P.dma("sp", rb[:, 0:4], Dm["router_b1"][l].partition_broadcast(128), w=[rrb])
            P.dma("sp", rb[:, 4:36], Dm["router_b2"][l].partition_broadcast(128), w=[rrb])
            P.dma("sp", bbt[:].rearrange("p a b -> p (a b)"), Dm["moe_bb"][:, 0:NB * 32], w=[rbbt])
            P.dma("sp", pcol[:], Dm["moe_pcol"], w=[rpcol])
            P.op("dve", lambda e: e.memset(epsc[:], EPS), w=[repsc])

            def load(i):
                t = tiles[i]
                P.dma("sp", ht[i % 2][0][:], Dm["H"][t * 128:(t + 1) * 128, :], r=[R["H"]], w=[ht[i % 2][1]])
            load(0)
            for i, t in enumerate(tiles):
                if i + 1 < NTL:
                    load(i + 1)
                h_, rh_ = ht[i % 2]
                fb_, rfb_ = fb[i % 2]
                o = 0 if t < NXT else 3
                P.op("act", lambda e: e.activation(out=junk[:], in_=h_[:], func=AF.Square, accum_out=ssq[:]), r=[rh_], w=[rjunk, rssq])
                P.op("act", lambda e: e.activation(out=rstd[:], in_=ssq[:], func=AF.Sqrt, scale=1.0 / D, bias=epsc[:, 0:1]), r=[rssq, repsc], w=[rrstd])
                P.op("dve", lambda e: e.reciprocal(out=rstd[:], in_=rstd[:]), r=[rrstd], w=[rrstd])
                P.op("dve", lambda e: e.scalar_tensor_tensor(out=f_[:], in0=h_[:], scalar=rstd[:, 0:1], in1=bc[:, o, :], op0=ALU.mult, op1=ALU.mult),
                     r=[rh_, rrstd, rbc], w=[rf_])
                P.op("dve", lambda e: e.tensor_tensor(out=f_[:], in0=f_[:], in1=bc[:, o + 1, :], op=ALU.add), r=[rf_, rbc], w=[rf_])
                P.op("act", lambda e: e.copy(out=fb_[:], in_=f_[:]), r=[rf_], w=[rfb_])
                P.dma("act", Dm["fb"][i * 128:(i + 1) * 128, :], fb_[:], r=[rfb_], w=[rfb])
                for hf in range(2):
                    pt, rpt = pTf[hf]
                    for k in range(4):
                        kk = hf * 4 + k
                        P.op("pe", lambda e: e.transpose(out=pt[:, k, :], in_=f_[:, kk * 128:(kk + 1) * 128], identity=identf[:]), r=[rf_, ridentf], w=[rpt])
                for hf in range(2):
                    pt, rpt = pTf[hf]
                    P.op("act", lambda e: e.copy(out=f_[:, hf * 512:(hf + 1) * 512].rearrange("p (k n) -> p k n", n=128), in_=pt), r=[rpt], w=[rf_])
                for k in range(8):
                    P.op("pe", lambda e: e.matmul(lp, lhsT=f_[:, k * 128:(k + 1) * 128], rhs=rw[:, k, :], start=(k == 0), stop=(k == 7)), r=[rf_, rrw], w=[rlp])
                P.op("dve", lambda e: e.tensor_tensor(out=lgt[:], in0=lp, in1=rb[:], op=ALU.add), r=[rlp, rrb], w=[rlgt])
                (s0, rs0), (s1, rs1), (s2, rs2), (s3, rs3) = sm
                oh1, oh2 = OH1[:, i, :], OH2[:, i, :]
                P.op("dve", lambda e: e.tensor_reduce(out=s0[:, 0:1], in_=lgt[:, 0:4], axis=AX.X, op=ALU.max), r=[rlgt], w=[rs0])
                P.op("dve", lambda e: e.tensor_scalar(out=s0[:, 1:2], in0=s0[:, 0:1], scalar1=-1.0, scalar2=None, op0=ALU.mult), r=[rs0], w=[rs0])
                P.op("act", lambda e: e.activation(out=s1[:, 0:4], in_=lgt[:, 0:4], func=AF.Exp, bias=s0[:, 1:2], accum_out=s0[:, 2:3]), r=[rlgt, rs0], w=[rs1, rs0])
                P.op("dve", lambda e: e.reciprocal(out=s0[:, 3:4], in_=s0[:, 2:3]), r=[rs0], w=[rs0])
                P.op("dve", lambda e: e.tensor_scalar(out=s2[:, 0:4], in0=lgt[:, 0:4], scalar1=s0[:, 0:1], scalar2=None, op0=ALU.is_equal), r=[rlgt, rs0], w=[rs2])
                P.op("dve", lambda e: e.tensor_scalar(out=s2[:, 0:4], in0=s2[:, 0:4], scalar1=BIG, scalar2=-BIG, op0=ALU.mult, op1=ALU.add), r=[rs2], w=[rs2])
                P.op("dve", lambda e: e.tensor_tensor(out=l2[:].rearrange("p (g e) -> p g e", e=8), in0=lgt[:, 4:36].rearrange("p (g e) -> p g e", e=8),
                                                      in1=s2[:, 0:4].unsqueeze(2).to_broadcast([128, 4, 8]), op=ALU.add), r=[rlgt, rs2], w=[rl2])
                P.op("dve", lambda e: e.tensor_reduce(out=s3[:, 0:1], in_=l2[:], axis=AX.X, op=ALU.max), r=[rl2], w=[rs3])
                P.op("dve", lambda e: e.tensor_scalar(out=oh1, in0=l2[:], scalar1=s3[:, 0:1], scalar2=None, op0=ALU.is_equal), r=[rl2, rs3], w=[rOH1])
                P.op("dve", lambda e: e.scalar_tensor_tensor(out=l2[:], in0=oh1, scalar=-BIG, in1=l2[:], op0=ALU.mult, op1=ALU.add), r=[rOH1, rl2], w=[rl2])
                P.op("dve", lambda e: e.tensor_reduce(out=s3[:, 1:2], in_=l2[:], axis=AX.X, op=ALU.max), r=[rl2], w=[rs3])
                P.op("dve", lambda e: e.tensor_scalar(out=oh2, in0=l2[:], scalar1=s3[:, 1:2], scalar2=None, op0=ALU.is_equal), r=[rl2, rs3], w=[rOH2])
                P.op("dve", lambda e: e.tensor_tensor(out=OH12[:, i, :], in0=oh1, in1=oh2, op=ALU.add), r=[rOH1, rOH2], w=[rOH12])
                P.op("dve", lambda e: e.tensor_tensor(out=s3[:, 2:3], in0=s3[:, 1:2], in1=s3[:, 0:1], op=ALU.subtract), r=[rs3], w=[rs3])
                P.op("act", lambda e: e.activation(out=s3[:, 3:4], in_=s3[:, 2:3], func=AF.Exp), r=[rs3], w=[rs3])
                P.op("dve", lambda e: e.tensor_scalar(out=s3[:, 4:5], in0=s3[:, 3:4], scalar1=1.0, scalar2=None, op0=ALU.add), r=[rs3], w=[rs3])
                P.op("dve", lambda e: e.reciprocal(out=s3[:, 4:5], in_=s3[:, 4:5]), r=[rs3], w=[rs3])
                P.op("dve", lambda e: e.tensor_tensor(out=GA[:, i, 0:1], in0=s3[:, 4:5], in1=s0[:, 3:4], op=ALU.mult), r=[rs3, rs0], w=[rGA])
                P.op("dve", lambda e: e.tensor_tensor(out=GA[:, i, 1:2], in0=GA[:, i, 0:1], in1=s3[:, 3:4], op=ALU.mult), r=[rGA, rs3], w=[rGA])
                P.op("pe", lambda e: e.matmul(cp, lhsT=onesb[:], rhs=OH12[:, i, :], start=(i == 0), stop=(i == NTL - 1)), r=[ronesb, rOH12], w=[rcp])
            (a0, ra0), (a1, ra1), (a2, ra2) = wk
            P.op("dve", lambda e: e.tensor_copy(out=cnts[:], in_=cp), r=[rcp], w=[rcnts])
            P.op("dve", lambda e: e.tensor_scalar(out=a0[:], in0=cnts[:], scalar1=float(B - 1), scalar2=1.0 / B, op0=ALU.add, op1=ALU.mult), r=[rcnts], w=[ra0])
            P.op("dve", lambda e: e.tensor_scalar(out=a0[:], in0=a0[:], scalar1=-(B - 1) / (2.0 * B), scalar2=None, op0=ALU.add), r=[ra0], w=[ra0])
            P.op("dve", lambda e: e.tensor_scalar(out=a0[:], in0=a0[:], scalar1=MAGIC, scalar2=None, op0=ALU.add), r=[ra0], w=[ra0])
            P.op("dve", lambda e: e.tensor_scalar(out=a0[:], in0=a0[:], scalar1=MAGIC, scalar2=None, op0=ALU.subtract), r=[ra0], w=[ra0])
            P.op("dve", lambda e: e.tensor_scalar(out=a0[:], in0=a0[:], scalar1=float(B), scalar2=None, op0=ALU.mult), r=[ra0], w=[ra0])
            P.op("dve", lambda e: e.memset(a2[:], 1.0), w=[ra2])
            P.op("dve", lambda e: e.tensor_tensor_scan(out=a1[:], data0=a2[:], data1=a0[:], initial=0.0, op0=ALU.mult, op1=ALU.add), r=[ra2, ra0], w=[ra1])
            P.op("dve", lambda e: e.tensor_tensor(out=pstart[:], in0=a1[:], in1=a0[:], op=ALU.subtract), r=[ra1, ra0], w=[rpstart])
            P.op("dve", lambda e: e.tensor_tensor(out=cmpt[:], in0=a1[:].unsqueeze(1).to_broadcast([128, NB, 32]), in1=bbt[:], op=ALU.is_le), r=[ra1, rbbt], w=[rcmpt])
            P.op("dve", lambda e: e.tensor_reduce(out=eblk[:], in_=cmpt[:], axis=AX.X, op=ALU.add), r=[rcmpt], w=[reblk])
            P.op("dve", lambda e: e.tensor_scalar(out=eblk[:], in0=eblk[:], scalar1=31.0, scalar2=128.0, op0=ALU.min, op1=ALU.mult), r=[reblk], w=[reblk])
            P.op("dve", lambda e: e.tensor_scalar(out=eblk[:], in0=eblk[:], scalar1=pcol[:, 0:1], scalar2=None, op0=ALU.add), r=[reblk, rpcol], w=[reblk])
            P.op("dve", lambda e: e.tensor_copy(out=IDXW[:], in_=eblk[:]), r=[reblk], w=[rIDXW])
        P.barrier()
        with contextlib.ExitStack() as e3:
            base, rbase = _tile(e3, nc, "Q3_base", [128, 32], F32)
            sb_, rsb_ = _tile(e3, nc, "Q3_sb", [128, 32], F32)
            pr, rpr = _tile(e3, nc, "Q3_pr", [128, 32], F32)
            dd, rdd = _tile(e3, nc, "Q3_dd", [128, 2], F32)
            fbt = [_tile(e3, nc, f"Q3_fb{i}", [128, 1024], BF16) for i in range(2)]
            rp_, rrp_ = _tile(e3, nc, "Q3_rp", [128, 32], F32, psum=True)
            csp, rcsp = _tile(e3, nc, "Q3_cs", [128, 32], F32, psum=True)
            P.op("dve", lambda e: e.memset(base[:], 0.0), w=[rbase])
            zt, rzt = _tile(e3, nc, "Q3_zero", [128, 7, 1024], BF16)
            P.op("pool", lambda e: e.memset(zt[:], 0.0), w=[rzt])
            xsv = Dm["xs"][0:NB * B, :].rearrange("(p a) d -> p a d", p=128)
            na = NB * B // 128
            for a0_ in range(0, na, 7):
                a1_ = min(a0_ + 7, na)
                P.dma("sp" if (a0_ // 7) % 2 == 0 else "act", xsv[:, a0_:a1_, :], zt[:, 0:a1_ - a0_, :], r=[rzt], w=[rxs])
            for i in range(NTL):
                f2, rf2 = fbt[i % 2]
                P.dma("sp", f2[:], Dm["fb"][i * 128:(i + 1) * 128, :], r=[rfb], w=[rf2])
                P.op("pe", lambda e: e.matmul(rp_, lhsT=ustr[:], rhs=OH12[:, i, :], start=True, stop=True), r=[rustr, rOH12], w=[rrp_])
                P.op("pe", lambda e: e.matmul(csp, lhsT=onesb[:], rhs=OH12[:, i, :], start=True, stop=True), r=[ronesb, rOH12], w=[rcsp])
                P.op("dve", lambda e: e.tensor_tensor(out=sb_[:], in0=rp_, in1=base[:], op=ALU.add), r=[rrp_, rbase], w=[rsb_])
                P.op("dve", lambda e: e.tensor_tensor(out=sb_[:], in0=sb_[:], in1=pstart[:], op=ALU.add), r=[rsb_, rpstart], w=[rsb_])
                for k, (OHk, rOHk) in enumerate(((OH1, rOH1), (OH2, rOH2))):
                    P.op("dve", lambda e: e.tensor_tensor(out=pr[:], in0=sb_[:], in1=OHk[:, i, :], op=ALU.mult), r=[rsb_, rOHk], w=[rpr])
                    P.op("dve", lambda e: e.tensor_reduce(out=dd[:, k:k + 1], in_=pr[:], axis=AX.X, op=ALU.add), r=[rpr], w=[rdd])
                P.op("dve", lambda e: e.tensor_copy(out=DST[:, i, :], in_=dd[:]), r=[rdd], w=[rDST])
                P.op("dve", lambda e: e.tensor_tensor(out=base[:], in0=base[:], in1=csp, op=ALU.add), r=[rbase, rcsp], w=[rbase])
                for k in range(2):
                    _idma(P, Dm["xs"][0:NB * B, :], IOA(DST[:, i, k:k + 1], 0), f2[:], None, r=[rf2, rDST], w=[rxs])
        P.barrier()
        with contextlib.ExitStack() as e4:
            w1 = [_tile(e4, nc, f"Q4_w1_{i}", [128, 8, 512], BF16) for i in range(2)]
            w3 = [_tile(e4, nc, f"Q4_w3_{i}", [128, 8, 512], BF16) for i in range(2)]
            w2 = [_tile(e4, nc, f"Q4_w2_{i}", [128, 4, 1024], BF16) for i in range(2)]
            xsb = [_tile(e4, nc, f"Q4_xs{i}", [128, 2, 1024], BF16) for i in range(2)]
            xT = [_tile(e4, nc, f"Q4_xT{i}", [128, 8, B], BF16) for i in range(2)]
            sl = [_tile(e4, nc, f"Q4_sl{i}", [128, B], F32) for i in range(2)]
            hid = [_tile(e4, nc, f"Q4_hid{i}", [128, 4, B], BF16) for i in range(2)]
            ysb = [_tile(e4, nc, f"Q4_ys{i}", [128, 1024], F32) for i in range(2)]
            pT, rpT = _tile(e4, nc, "Q4_pT", [128, 8, 128], BF16, psum=True)
            H1 = [_tile(e4, nc, f"Q4_H1{i}", [128, B], F32, psum=True) for i in range(2)]
            H3 = [_tile(e4, nc, f"Q4_H3{i}", [128, B], F32, psum=True) for i in range(2)]
            Yp = [_tile(e4, nc, f"Q4_Y{i}", [128, 512], F32, psum=True) for i in range(2)]
            nbl = NB if nblocks is None else nblocks

            def loadb(b):
                _idma(P, w1[b % 2][0][:].rearrange("p k n -> p (k n)"), None, Dm["wb1"], IOA(IDXW[:, b:b + 1], 0), r=[rwb, rIDXW], w=[w1[b % 2][1]])
                _idma(P, w3[b % 2][0][:].rearrange("p k n -> p (k n)"), None, Dm["wb3"], IOA(IDXW[:, b:b + 1], 0), r=[rwb, rIDXW], w=[w3[b % 2][1]])
                _idma(P, w2[b % 2][0][:].rearrange("p k n -> p (k n)"), None, Dm["wb2"], IOA(IDXW[:, b:b + 1], 0), r=[rwb, rIDXW], w=[w2[b % 2][1]])
                P.dma("sp", xsb[b % 2][0][:], Dm["xs"][b * B:(b + 1) * B, :].rearrange("(s p) d -> p s d", p=128), r=[rxs], w=[xsb[b % 2][1]])
            loadb(0)
            cnt = 0
            yc = 0
            for b in range(nbl):
                if b + 1 < nbl:
                    loadb(b + 1)
                (w1t, rw1), (w3t, rw3), (w2t, rw2) = w1[b % 2], w3[b % 2], w2[b % 2]
                xb, rxb = xsb[b % 2]
                xt, rxt = xT[b % 2]
                hd, rhd = hid[b % 2]
                for s_ in range(2):
                    for k in range(8):
                        P.op("pe", lambda e: e.transpose(out=pT[:, k, :], in_=xb[:, s_, k * 128:(k + 1) * 128], identity=identb[:]), r=[rxb, ridentb], w=[rpT])
                    P.op("act", lambda e: e.copy(out=xt[:, :, s_ * 128:(s_ + 1) * 128], in_=pT), r=[rpT], w=[rxt])
                for fc in range(4):
                    h1, rh1 = H1[cnt % 2]
                    h3, rh3 = H3[cnt % 2]
                    sl_, rsl_ = sl[cnt % 2]
                    cnt += 1
                    for k in range(8):
                        P.op("pe", lambda e: e.matmul(h1, lhsT=w1t[:, k, fc * 128:(fc + 1) * 128], rhs=xt[:, k, :], start=(k == 0), stop=(k == 7)), r=[rw1, rxt], w=[rh1])
                    for k in range(8):
                        P.op("pe", lambda e: e.matmul(h3, lhsT=w3t[:, k, fc * 128:(fc + 1) * 128], rhs=xt[:, k, :], start=(k == 0), stop=(k == 7)), r=[rw3, rxt], w=[rh3])
                    P.op("act", lambda e: e.activation(out=sl_[:], in_=h1, func=AF.Silu), r=[rh1], w=[rsl_])
                    P.op("dve", lambda e: e.tensor_tensor(out=hd[:, fc, :], in0=h3, in1=sl_[:], op=ALU.mult), r=[rh3, rsl_], w=[rhd])
                for s_ in range(2):
                    ys_, rys_ = ysb[yc % 2]
                    yc += 1
                    for nh in range(2):
                        ypt, rypt = Yp[nh]
                        for fc in range(4):
                            P.op("pe", lambda e: e.matmul(ypt, lhsT=hd[:, fc, s_ * 128:(s_ + 1) * 128], rhs=w2t[:, fc, nh * 512:(nh + 1) * 512], start=(fc == 0), stop=(fc == 3)),
                                 r=[rhd, rw2], w=[rypt])
                        if nh == 0:
                            P.op("act", lambda e: e.copy(out=ys_[:, 0:512], in_=ypt), r=[rypt], w=[rys_])
                        else:
                            P.op("dve", lambda e: e.tensor_copy(out=ys_[:, 512:1024], in_=ypt), r=[rypt], w=[rys_])
                    P.dma("sp", Dm["ys"][b * B + s_ * 128:b * B + (s_ + 1) * 128, :], ys_[:], r=[rys_], w=[rys])
        P.barrier()
        with contextlib.ExitStack() as e5:
            y1 = [_tile(e5, nc, f"Q5_y1{i}", [128, 1024], F32) for i in range(2)]
            y2 = [_tile(e5, nc, f"Q5_y2{i}", [128, 1024], F32) for i in range(2)]
            ht = [_tile(e5, nc, f"Q5_h{i}", [128, 1024], F32) for i in range(2)]
            hn = [_tile(e5, nc, f"Q5_hn{i}", [128, 1024], F32) for i in range(2)]

            def load5(i):
                t = tiles[i]
                _idma(P, y1[i % 2][0][:], None, Dm["ys"][0:NB * B, :], IOA(DST[:, i, 0:1], 0), r=[rys, rDST], w=[y1[i % 2][1]])
                _idma(P, y2[i % 2][0][:], None, Dm["ys"][0:NB * B, :], IOA(DST[:, i, 1:2], 0), r=[rys, rDST], w=[y2[i % 2][1]])
                P.dma("sp", ht[i % 2][0][:], Dm["H"][t * 128:(t + 1) * 128, :], r=[R["H"]], w=[ht[i % 2][1]])
            load5(0)
            for i, t in enumerate(tiles):
                if i + 1 < NTL:
                    load5(i + 1)
                (a_, ra_), (b_, rb_), (h_, rh_), (n_, rn_) = y1[i % 2], y2[i % 2], ht[i % 2], hn[i % 2]
                ts = slice(t * 128, (t + 1) * 128)
                P.op("dve", lambda e: e.tensor_scalar(out=n_[:], in0=a_[:], scalar1=GA[:, i, 0:1], scalar2=None, op0=ALU.mult), r=[ra_, rGA], w=[rn_])
                P.op("dve", lambda e: e.scalar_tensor_tensor(out=n_[:], in0=b_[:], scalar=GA[:, i, 1:2], in1=n_[:], op0=ALU.mult, op1=ALU.add), r=[rb_, rGA, rn_], w=[rn_])
                P.op("pool", lambda e: e.tensor_tensor(out=n_[:], in0=n_[:], in1=bc[:, 2 if t < NXT else 5, :], op=ALU.mult), r=[rn_, rbc], w=[rn_])
                P.op("dve", lambda e: e.tensor_tensor(out=n_[:], in0=n_[:], in1=h_[:], op=ALU.add), r=[rn_, rh_], w=[rn_])
                if last:
                    P.dma("act", Dm["out"][ts, :], n_[:], r=[rn_], w=[R["out"]])
                else:
                    P.dma("act", Dm["H"][ts, :], n_[:], r=[rn_], w=[R["H"]])
    P.pe_relaxed = False
    P.barrier()


SCRATCH.update({"wb1": ([32 * 128, 4096], BF16), "wb3": ([32 * 128, 4096], BF16), "wb2": ([32 * 128, 4096], BF16),
                "fb": ([T, 1024], BF16), "xs": ([MOE_NBMAX * MOE_B, 1024], BF16), "ys": ([MOE_NBMAX * MOE_B, 1024], F32)})
```

```python
import contextlib
import math
import numpy as np
import ml_dtypes
import concourse.bass as bass
import concourse.mybir as mybir
from concourse.bass_utils import run_bass_kernel_spmd

F32 = mybir.dt.float32
BF16 = mybir.dt.bfloat16
I32 = mybir.dt.int32
AF = mybir.ActivationFunctionType
ALU = mybir.AluOpType
AX = mybir.AxisListType

D = 1024
L = 8192
NCTX = 256
T = L + NCTX
NT = T // 128
NXT = L // 128
DEPTH = 4
MIXC = 2560
EPS = 1e-6
SAME_SYNC = True
ATT_RELAX = True
S5_ADD_ENG = "pool"
S5_PROD_ENG = "dve"
STQ = "pool"
NEGM = -240000.0


class Res:
    __slots__ = ("name", "w", "rd")

    def __init__(self, name=""):
        self.name = name
        self.w = None
        self.rd = {}


class Prog:
    NDS = 12

    def __init__(self, nc, same_sync=True, dma_queues=("sp", "pool", "act")):
        self.nc = nc
        self.E = {"pe": nc.tensor, "dve": nc.vector, "act": nc.scalar, "pool": nc.gpsimd, "sp": nc.sync}
        self.same_sync = same_sync
        self.semh = {}
        self.cnt = {}
        for k in self.E:
            self.semh[("c", k)] = nc.alloc_semaphore(f"sc_{k}")
            self.cnt[k] = 0
        self.seen = {k: {} for k in self.E}
        self.duse = {}
        self.dnext = {}
        for q in dma_queues:
            self.duse[q] = [0] * self.NDS
            self.dnext[q] = 0
            for i in range(self.NDS):
                self.semh[("d", q, i)] = nc.alloc_semaphore(f"sd_{q}_{i}")
        self.ninst = 0
        self.pe_relaxed = False

    def _wait(self, eng, key, val):
        if val <= 0 or self.seen[eng].get(key, 0) >= val:
            return
        self.E[eng].wait_ge(self.semh[key], val)
        self.seen[eng][key] = val

    def _deps(self, eng, r, w):
        deps = {}
        for res in r:
            if res.w is not None:
                k, v = res.w
                if deps.get(k, 0) < v:
                    deps[k] = v
        for res in w:
            if res.w is not None:
                k, v = res.w
                if deps.get(k, 0) < v:
                    deps[k] = v
            for k, v in res.rd.items():
                if deps.get(k, 0) < v:
                    deps[k] = v
        for k, v in deps.items():
            if k == ("c", eng) and ((eng == "pe" and self.pe_relaxed) or not self.same_sync):
                continue
            self._wait(eng, k, v)

    def _mark(self, tok, r, w):
        k, v = tok
        for res in r:
            if res.rd.get(k, 0) < v:
                res.rd[k] = v
        for res in w:
            res.w = tok
            res.rd = {}

    def op(self, eng, fn, r=(), w=()):
        self._deps(eng, r, w)
        ins = fn(self.E[eng])
        self.cnt[eng] += 1
        ins.then_inc(self.semh[("c", eng)], 1)
        self._mark((("c", eng), self.cnt[eng]), r, w)
        self.ninst += 1
        return ins

    def dma(self, q, out, in_, r=(), w=(), **kw):
        self._deps(q, r, w)
        i = self.dnext[q]
        self.dnext[q] = (i + 1) % self.NDS
        key = ("d", q, i)
        self._wait(q, key, 16 * self.duse[q][i])
        ins = self.E[q].dma_start(out=out, in_=in_, **kw)
        self.duse[q][i] += 1
        ins.then_inc(self.semh[key], 16)
        self._mark((key, 16 * self.duse[q][i]), r, w)
        self.ninst += 1
        return ins

    def barrier(self, engines=None):
        engines = engines or list(self.E)
        for eng in engines:
            for k in self.E:
                if k != eng:
                    self._wait(eng, ("c", k), self.cnt[k])
            for q in self.duse:
                for i in range(self.NDS):
                    self._wait(eng, ("d", q, i), 16 * self.duse[q][i])


class Ctx:
    pass


_TCNT = [0]


def _tile(es, nc, name, shape, dt, psum=False):
    _TCNT[0] += 1
    name = f"{name}_{_TCNT[0]}"
    if not psum:
        t = es.enter_context(nc.sbuf_tensor(name, shape, dt))
        return t, Res(name)
    esz = 2 if dt == BF16 else 4
    n = int(np.prod(shape[1:]))
    per_bank = 2048 // esz
    nb = (n + per_bank - 1) // per_bank
    t = es.enter_context(nc.psum_tensor(name, [128, nb * per_bank], dt))
    ap = t[0:shape[0], 0:n]
    if len(shape) == 3:
        ap = ap.rearrange("p (a b) -> p a b", b=shape[2])
    elif len(shape) == 4:
        ap = ap.rearrange("p (a b c) -> p a b c", b=shape[2], c=shape[3])
    return ap, Res(name)


NA_NCLS = 21
MOE_B = 256
MOE_NBMAX = 98
S5_TC = 256
MAGIC = 12582912.0


def na_class_list():
    lst = [(10, 10 + dc) for dc in (-2, -1, 0, 1, 2)]
    for j in (0, 1):
        lst += [(j, c) for c in range(4)]
    for j in (62, 63):
        lst += [(j, c) for c in range(60, 64)]
    return lst


def na_chunks(j):
    if 2 <= j <= 61:
        return [(j + dc, dc + 2) for dc in (-2, -1, 0, 1, 2)]
    base = {0: 5, 1: 9, 62: 13, 63: 17}[j]
    c0 = 0 if j < 2 else 60
    return [(c0 + i, base + i) for i in range(4)]


def make_consts():
    c = {}
    pos = np.arange(L)
    inv = (10000.0 ** (-np.arange(16, dtype=np.float32) / 16)).astype(np.float32)
    ang_r = (pos // 64).astype(np.float32)[:, None] * inv
    ang_c = (pos % 64).astype(np.float32)[:, None] * inv
    cr, sr, cc, sc = np.cos(ang_r), np.sin(ang_r), np.cos(ang_c), np.sin(ang_c)
    cosf = np.concatenate([cr, cr, cc, cc], axis=1)
    sinf = np.concatenate([-sr, sr, -sc, sc], axis=1)
    cosf = np.concatenate([cosf, np.ones((NCTX, 64))], axis=0)
    sinf = np.concatenate([sinf, np.zeros((NCTX, 64))], axis=0)
    c["ropecs"] = np.concatenate([cosf, sinf], axis=1).astype(np.float32)
    c["ident"] = np.eye(128, dtype=np.float32)
    kl = np.arange(128)[:, None]
    ql = np.arange(128)[None, :]
    lo = np.where(kl >= ql, 0.0, NEGM)
    hi = np.where(kl <= ql, 0.0, NEGM)
    c["na_jx"] = np.zeros((128, 128), np.float32)
    for q in range(128):
        c["na_jx"][(q // 64) * 64 + 63 - q % 64, q] = 1.0
    rm = np.zeros((NA_NCLS, 128, 128), np.float32)
    for cls, (j, cch) in enumerate(na_class_list()):
        for qp in range(128):
            rq = 2 * j + qp // 64
            cq = 63 - qp % 64
            r0 = min(max(rq - 4, 0), 120)
            ws = min(max(cq - 8, 0), 48)
            for key in range(128):
                rk = 2 * cch + key // 64
                ck = key % 64
                ok = (r0 <= rk < r0 + 8) and (ws <= ck < ws + 16)
                rm[cls, qp, key] = 0.0 if ok else NEGM
    c["na_rm"] = rm
    si = np.arange(128, dtype=np.float32)[:, None]
    ti = np.arange(128, dtype=np.float32)[None, :]
    c["ret_dpos"] = np.maximum(ti - si, 0.0).astype(np.float32)
    c["ret_dneg"] = np.maximum(si - ti, 0.0).astype(np.float32)
    c["ret_diag"] = ((si == ti) * math.log(2.0) + math.log(0.125)).astype(np.float32)
    c["ret_tp1"] = np.broadcast_to(ti + 1.0, (128, 128)).astype(np.float32).copy()
    c["ret_tr"] = np.broadcast_to(128.0 - ti, (128, 128)).astype(np.float32).copy()
    c["ret_pcol"] = np.concatenate([127.0 - si, si], axis=1).astype(np.float32)
    c["s5_iota1"] = np.broadcast_to(np.arange(1, S5_TC + 1, dtype=np.float32)[None, :], (128, S5_TC)).copy()
    bm = np.zeros((128, 128), np.float32)
    for r in range(128):
        a = (r // 16) % 2
        bm[r, a * 64:(a + 1) * 64] = 1.0
    c["s5_bdmask"] = bm
    c["moe_bb"] = np.broadcast_to((np.arange(MOE_NBMAX, dtype=np.float32) * MOE_B)[None, :, None], (128, MOE_NBMAX, 32)).reshape(128, MOE_NBMAX * 32).copy()
    c["moe_pcol"] = np.arange(128, dtype=np.float32)[:, None].copy()
    c["moe_ustrict"] = (np.arange(128)[:, None] < np.arange(128)[None, :]).astype(np.float32)
    c["moe_ones"] = np.ones((128, 128), np.float32)
    c["gqa_mask"] = np.stack([np.tile(lo, (1, 2)), np.tile(hi, (1, 2))], axis=1).astype(np.float32)
    return c


def stage_prep(K, l):
    nc, P, Dm = K.nc, K.P, K.dram
    with contextlib.ExitStack() as es:
        cc, rcc = _tile(es, nc, "pp_cc", [128, 8, 2], F32)
        sc, rsc = _tile(es, nc, "pp_sc", [128, 8, 2], F32)
        mw, rmw = _tile(es, nc, "pp_mw", [128, 8, 512], F32)
        mw2, rmw2 = _tile(es, nc, "pp_mw2", [128, 8, 512], F32)
        mws = [(mw, rmw), (mw2, rmw2)]
        mv, rmv = _tile(es, nc, "pp_mv", [2, 6144], F32)
        mb, rmb = _tile(es, nc, "pp_mb", [2, 6144], F32)
        g12, rg12 = _tile(es, nc, "pp_g", [2, 2048], F32)
        ps, rps = _tile(es, nc, "pp_ps", [2, 512], F32, psum=True)
        ps2, rps2 = _tile(es, nc, "pp_ps2", [2, 512], F32, psum=True)
        pss = [(ps, rps), (ps2, rps2)]
        with nc.allow_non_contiguous_dma(reason="tiny"):
            P.dma("sp", cc[:, :, 0], Dm["c"].rearrange("o (k p) -> p (o k)", p=128), w=[rcc])
            P.dma("sp", cc[:, :, 1], Dm["c_ctx"].rearrange("(k p) -> p k", p=128), w=[rcc])
        P.dma("sp", mb[:], Dm["mod_b"][l].partition_broadcast(2), w=[rmb])
        P.dma("sp", g12[:, 0:1024], Dm["norm1_g"][l].partition_broadcast(2), w=[rg12])
        P.dma("sp", g12[:, 1024:2048], Dm["norm2_g"][l].partition_broadcast(2), w=[rg12])
        P.op("act", lambda e: e.activation(out=sc[:], in_=cc[:], func=AF.Silu), r=[rcc], w=[rsc])
        mwv = Dm["mod_w"][l].rearrange("(k p) n -> p k n", p=128)
        for n in range(12):
            w_, rw_ = mws[n % 2]
            p_, rp_ = pss[n % 2]
            P.dma("sp", w_[:], mwv[:, :, n * 512:(n + 1) * 512], w=[rw_])
            for k in range(8):
                P.op("pe", lambda e: e.matmul(p_[:], lhsT=sc[:, k, :], rhs=w_[:, k, :], start=(k == 0), stop=(k == 7)),
                     r=[rsc, rw_], w=[rp_])
            P.op("dve", lambda e: e.tensor_tensor(out=mv[:, n * 512:(n + 1) * 512], in0=p_[:], in1=mb[:, n * 512:(n + 1) * 512], op=ALU.add),
                 r=[rp_, rmb], w=[rmv])
        for slot, goff in ((1, 0), (4, 1024)):
            P.op("dve", lambda e: e.scalar_tensor_tensor(out=mv[:, slot * 1024:(slot + 1) * 1024], in0=mv[:, slot * 1024:(slot + 1) * 1024],
                                                         scalar=1.0, in1=g12[:, goff:goff + 1024], op0=ALU.add, op1=ALU.mult),
                 r=[rmv, rg12], w=[rmv])
        order = [1, 0, 2, 4, 3, 5]
        mvd = Dm["modv"].rearrange("(a r) d -> a r d", a=2)
        for j, slot in enumerate(order):
            P.dma("sp", mvd[:, j, :], mv[:, slot * 1024:(slot + 1) * 1024], r=[rmv], w=[K.R["modv"]])
    P.barrier()


def stage_A(K, l, tiles=None):
    nc, P, Dm, R = K.nc, K.P, K.dram, K.R
    tiles = list(range(NT)) if tiles is None else tiles
    P.pe_relaxed = True
    with contextlib.ExitStack() as es:
        win, rwin = _tile(es, nc, "A_win", [128, 8, MIXC], BF16)
        ident, rident = _tile(es, nc, "A_ident", [128, 128], BF16)
        identf, ridentf = _tile(es, nc, "A_identf", [128, 128], F32)
        bc, rbc = _tile(es, nc, "A_bc", [128, 4, 1024], F32)
        gains, rgains = _tile(es, nc, "A_gains", [128, 4, 64], F32)
        epsc, repsc = _tile(es, nc, "A_eps", [128, 1], F32)
        hts = [_tile(es, nc, f"A_h{i}", [128, 1024], F32) for i in range(2)]
        rps_ = [_tile(es, nc, f"A_rope{i}", [128, 128], F32) for i in range(2)]
        junk, rjunk = _tile(es, nc, "A_junk", [128, 1024], BF16)
        ssq, rssq = _tile(es, nc, "A_ssq", [128, 1], F32)
        rstd, rrstd = _tile(es, nc, "A_rstd", [128, 1], F32)
        t1, rt1 = _tile(es, nc, "A_t1", [128, 1024], F32)
        abf, rabf = _tile(es, nc, "A_abf", [128, 1024], BF16)
        aT, raT = _tile(es, nc, "A_aT", [128, 8, 128], BF16)
        pT, rpT = _tile(es, nc, "A_pT", [128, 8, 128], BF16, psum=True)
        pm = [_tile(es, nc, f"A_pm{i}", [128, 512], F32, psum=True) for i in range(5)]
        pT2, rpT2 = _tile(es, nc, "A_pT2", [128, 4, 128], BF16, psum=True)
        sA, rsA = _tile(es, nc, "A_sA", [128, 512], F32)
        sB, rsB = _tile(es, nc, "A_sB", [128, 512], F32)
        o_rqk, ro_rqk = _tile(es, nc, "A_orqk", [128, 512], BF16)
        o_rv, ro_rv = _tile(es, nc, "A_orv", [128, 256], BF16)
        o_rg, ro_rg = _tile(es, nc, "A_org", [128, 256], F32)
        o_nqk, ro_nqk = _tile(es, nc, "A_onqk", [128, 512], BF16)
        o_nv, ro_nv = _tile(es, nc, "A_onv", [128, 256], BF16)
        o_su, ro_su = _tile(es, nc, "A_osu", [128, 256], F32)
        o_sub, ro_sub = _tile(es, nc, "A_osub", [128, 256], BF16)
        o_gqk, ro_gqk = _tile(es, nc, "A_ogqk", [128, 384], BF16)
        o_gv, ro_gv = _tile(es, nc, "A_ogv", [128, 128], BF16)
        oT, roT = _tile(es, nc, "A_oT", [128, 4, 128], BF16)
        ss8, rss8 = _tile(es, nc, "A_ss8", [128, 8], F32)
        rs8, rrs8 = _tile(es, nc, "A_rs8", [128, 8], F32)

        P.dma("pool", win[:], Dm["w_in"][l].rearrange("(k p) n -> p k n", p=128)[:, :, 0:MIXC], w=[rwin])
        P.dma("sp", identf[:], Dm["ident"], w=[ridentf])
        P.op("dve", lambda e: e.tensor_copy(ident[:], identf[:]), r=[ridentf], w=[rident])
        for j, row in enumerate((0, 1, 6, 7)):
            P.dma("sp", bc[:, j, :], Dm["modv"][row].partition_broadcast(128), r=[R["modv"]], w=[rbc])
        P.dma("sp", gains[:, 0:2, :], Dm["na_qk_gain"][l].partition_broadcast(128), w=[rgains])
        P.dma("sp", gains[:, 2:4, :], Dm["gqa_qk_gain"][l].partition_broadcast(128), w=[rgains])
        P.op("dve", lambda e: e.memset(epsc[:], EPS), w=[repsc])

        hsrc = K.hsrc(l)

        def load(t, par):
            ht, rht = hts[par]
            rp, rrp = rps_[par]
            P.dma("sp", ht[:], hsrc(t), r=[R["H"]], w=[rht])
            P.dma("sp", rp[:], Dm["ropecs"][t * 128:(t + 1) * 128, :], w=[rrp])

        def rmsn(src, nh, gidx, dst, rdst_list, rsrc_list):
            P.op("act", lambda e: e.activation(out=sB[:, 0:nh * 64], in_=src, func=AF.Square), r=rsrc_list, w=[rsB])
            P.op("dve", lambda e: e.tensor_reduce(out=ss8[:, 0:nh], in_=sB[:, 0:nh * 64].rearrange("p (h d) -> p h d", d=64), axis=AX.X, op=ALU.add),
                 r=[rsB], w=[rss8])
            P.op("act", lambda e: e.activation(out=rs8[:, 0:nh], in_=ss8[:, 0:nh], func=AF.Sqrt, scale=1.0 / 64, bias=epsc[:, 0:1]),
                 r=[rss8, repsc], w=[rrs8])
            P.op("dve", lambda e: e.reciprocal(out=rs8[:, 0:nh], in_=rs8[:, 0:nh]), r=[rrs8], w=[rrs8])
            P.op("dve", lambda e: e.tensor_tensor(out=dst.rearrange("p (h d) -> p h d", d=64), in0=src.rearrange("p (h d) -> p h d", d=64),
                                                  in1=rs8[:, 0:nh].unsqueeze(2).to_broadcast([128, nh, 64]), op=ALU.mult),
                 r=rsrc_list + [rrs8], w=rdst_list)
            P.op("dve", lambda e: e.tensor_tensor(out=dst.rearrange("p (h d) -> p h d", d=64), in0=dst.rearrange("p (h d) -> p h d", d=64),
                                                  in1=gains[:, gidx, :].unsqueeze(1).to_broadcast([128, nh, 64]), op=ALU.mult),
                 r=rdst_list + [rgains], w=rdst_list)

        def rope(src, nh, rp, rrp, dst_bf, rsrc_list, rdst_list):
            v5 = lambda ap: ap.rearrange("p (h a b c) -> p h a b c", a=2, b=2, c=16)
            cosb = rp[:, 0:64].rearrange("p (a b c) -> p a b c", a=2, b=2).unsqueeze(1).to_broadcast([128, nh, 2, 2, 16])
            sinb = rp[:, 64:128].rearrange("p (a b c) -> p a b c", a=2, b=2).unsqueeze(1).to_broadcast([128, nh, 2, 2, 16])
            P.op("dve", lambda e: e.tensor_tensor(out=v5(sA[:, 0:nh * 64]), in0=v5(src), in1=cosb, op=ALU.mult), r=rsrc_list + [rrp], w=[rsA])
            P.op("dve", lambda e: e.tensor_tensor(out=v5(sB[:, 0:nh * 64]), in0=v5(src)[:, :, :, ::-1, :], in1=sinb, op=ALU.mult),
                 r=rsrc_list + [rrp], w=[rsB])
            P.op("dve", lambda e: e.tensor_tensor(out=dst_bf, in0=sA[:, 0:nh * 64], in1=sB[:, 0:nh * 64], op=ALU.add), r=[rsA, rsB], w=rdst_list)

        def transp_out(src_bf, rsrc, nchunk, dram_rows, t):
            for k in range(nchunk):
                P.op("pe", lambda e: e.transpose(out=pT2[:, k, :], in_=src_bf[:, k * 128:(k + 1) * 128], identity=ident[:]),
                     r=[rsrc, rident], w=[rpT2])
            P.op("act", lambda e: e.copy(out=oT[:, 0:nchunk, :], in_=pT2[:, 0:nchunk, :]), r=[rpT2], w=[roT])
            P.dma(STQ, dram_rows.rearrange("(k p) n -> p k n", p=128)[:, :, t * 128:(t + 1) * 128], oT[:, 0:nchunk, :], r=[roT], w=[R["mix"]])

        load(tiles[0], 0)
        for idx, t in enumerate(tiles):
            if idx + 1 < len(tiles):
                load(tiles[idx + 1], (idx + 1) % 2)
            ht, rht = hts[idx % 2]
            rp, rrp = rps_[idx % 2]
            isx = t < NXT
            g1 = bc[:, 0 if isx else 2, :]
            sh1 = bc[:, 1 if isx else 3, :]
            ts = slice(t * 128, (t + 1) * 128)
            P.op("act", lambda e: e.activation(out=junk[:], in_=ht[:], func=AF.Square, accum_out=ssq[:]), r=[rht], w=[rjunk, rssq])
            P.op("act", lambda e: e.activation(out=rstd[:], in_=ssq[:], func=AF.Sqrt, scale=1.0 / D, bias=epsc[:, 0:1]), r=[rssq, repsc], w=[rrstd])
            P.op("dve", lambda e: e.reciprocal(out=rstd[:], in_=rstd[:]), r=[rrstd], w=[rrstd])
            P.op("dve", lambda e: e.scalar_tensor_tensor(out=t1[:], in0=ht[:], scalar=rstd[:, 0:1], in1=g1, op0=ALU.mult, op1=ALU.mult),
                 r=[rht, rrstd, rbc], w=[rt1])
            P.op("dve", lambda e: e.tensor_tensor(out=abf[:], in0=t1[:], in1=sh1, op=ALU.add), r=[rt1, rbc], w=[rabf])
            for k in range(8):
                P.op("pe", lambda e: e.transpose(out=pT[:, k, :], in_=abf[:, k * 128:(k + 1) * 128], identity=ident[:]), r=[rabf, rident], w=[rpT])
            P.op("act", lambda e: e.copy(out=aT[:], in_=pT[:]), r=[rpT], w=[raT])
            P.dma(STQ, Dm["aT"].rearrange("(k p) n -> p k n", p=128)[:, :, ts], aT[:], r=[raT], w=[R["aT"]])
            for n in range(5):
                pmn, rpmn = pm[n]
                for k in range(8):
                    P.op("pe", lambda e: e.matmul(pmn[:], lhsT=aT[:, k, :], rhs=win[:, k, n * 512:(n + 1) * 512], start=(k == 0), stop=(k == 7)),
                         r=[raT, rwin], w=[rpmn])
            rope(pm[0][0][:], 8, rp, rrp, o_rqk[:], [pm[0][1]], [ro_rqk])
            P.dma(STQ, Dm["rk"][ts, :], o_rqk[:, 256:512], r=[ro_rqk], w=[R["mix"]])
            transp_out(o_rqk, ro_rqk, 4, Dm["rqkT"], t)
            P.op("act", lambda e: e.copy(out=o_rv[:], in_=pm[1][0][:, 0:256]), r=[pm[1][1]], w=[ro_rv])
            P.op("act", lambda e: e.activation(out=o_rg[:], in_=pm[1][0][:, 256:512], func=AF.Silu), r=[pm[1][1]], w=[ro_rg])
            P.dma(STQ, Dm["rv"][ts, :], o_rv[:], r=[ro_rv], w=[R["mix"]])
            P.dma(STQ, Dm["rg"][ts, :], o_rg[:], r=[ro_rg], w=[R["mix"]])
            rmsn(pm[2][0][:, 0:256], 4, 0, t1[:, 0:256], [rt1], [pm[2][1]])
            rmsn(pm[2][0][:, 256:512], 4, 1, t1[:, 256:512], [rt1], [pm[2][1]])
            P.op("act", lambda e: e.copy(out=o_nqk[:], in_=t1[:, 0:512]), r=[rt1], w=[ro_nqk])
            transp_out(o_nqk, ro_nqk, 4, Dm["nqkT"], t)
            P.op("act", lambda e: e.copy(out=o_nv[:], in_=pm[3][0][:, 0:256]), r=[pm[3][1]], w=[ro_nv])
            P.dma(STQ, Dm["nv"][ts, :], o_nv[:], r=[ro_nv], w=[R["mix"]])
            P.op("act", lambda e: e.copy(out=o_su[:], in_=pm[3][0][:, 256:512]), r=[pm[3][1]], w=[ro_su])
            P.op("dve", lambda e: e.tensor_copy(out=o_sub[:], in_=pm[3][0][:, 256:512]), r=[pm[3][1]], w=[ro_sub])
            P.dma(STQ, Dm["su"][ts, :], o_su[:], r=[ro_su], w=[R["mix"]])
            transp_out(o_sub, ro_sub, 2, Dm["suT"], t)
            rmsn(pm[4][0][:, 0:256], 4, 2, t1[:, 512:768], [rt1], [pm[4][1]])
            rmsn(pm[4][0][:, 256:384], 2, 3, t1[:, 768:896], [rt1], [pm[4][1]])
            rope(t1[:, 512:896], 6, rp, rrp, o_gqk[:], [rt1], [ro_gqk])
            transp_out(o_gqk, ro_gqk, 3, Dm["gqkT"], t)
            P.op("act", lambda e: e.copy(out=o_gv[:], in_=pm[4][0][:, 384:512]), r=[pm[4][1]], w=[ro_gv])
            P.dma(STQ, Dm["gv"][ts, :], o_gv[:], r=[ro_gv], w=[R["mix"]])
    P.pe_relaxed = False
    P.barrier()


INPUT_NAMES = ["x", "c", "ctx", "c_ctx", "mod_w", "mod_b", "norm1_g", "norm2_g", "w_in", "w_branch", "w_out", "ret_log_decay",
               "na_qk_gain", "na_rpb", "s5_lambda_re", "s5_lambda_im", "s5_log_step", "s5_b_re", "s5_b_im", "s5_c_re",
               "s5_c_im", "s5_d", "s5_glu_w", "s5_glu_b", "gqa_qk_gain", "gqa_sink", "router_w1", "router_b1", "router_w2",
               "router_b2", "exp_w1", "exp_w3", "exp_w2"]

SCRATCH = {
    "modv": ([12, 1024], F32),
    "H": ([T, D], F32),
    "aT": ([D, T], BF16),
    "rqkT": ([512, T], BF16), "rk": ([T, 256], BF16), "rv": ([T, 256], BF16), "rg": ([T, 256], F32),
    "nqkT": ([512, T], BF16), "nv": ([T, 256], BF16),
    "su": ([T, 256], F32), "suT": ([256, T], BF16),
    "gqkT": ([384, T], BF16), "gv": ([T, 128], BF16),
}


def build(in_shapes, consts, plan, expose=()):
    nc = bass.Bass("TRN2", target_bir_lowering=False)
    K = Ctx()
    K.nc = nc
    K.dram = {}
    for name, (shape, dt) in in_shapes.items():
        K.dram[name] = nc.dram_tensor(name, list(shape), dt, kind="ExternalInput").ap()
    for name, arr in consts.items():
        K.dram[name] = nc.dram_tensor(name, list(arr.shape), F32, kind="ExternalInput").ap()
    for name, (shape, dt) in SCRATCH.items():
        if name in K.dram:
            continue
        kind = "ExternalOutput" if name in expose else "Internal"
        K.dram[name] = nc.dram_tensor(name, list(shape), dt, kind=kind).ap()
    K.dram["out"] = nc.dram_tensor("out", [L, D], F32, kind="ExternalOutput").ap()
    K.R = {k: Res(k) for k in ["modv", "H", "aT", "mix", "y", "out", "s5y", "wbf"]}
    K.P = Prog(nc, same_sync=SAME_SYNC)

    def hsrc(l):
        def f(t):
            if l == 0:
                if t < NXT:
                    return K.dram["x"][t * 128:(t + 1) * 128, :]
                return K.dram["ctx"][(t - NXT) * 128:(t - NXT + 1) * 128, :]
            return K.dram["H"][t * 128:(t + 1) * 128, :]
        return f
    K.hsrc = hsrc
    plan(K)
    K.P.barrier(["sp"])
    return nc, K


def run_attention(K, es, pfx, units, rd_res, epilogue, maxc):
    nc, P = K.nc, K.P
    P.pe_relaxed = ATT_RELAX
    wmax = max(u["nq"] for u in units) * 128
    spb = 512 // wmax
    nbank = (maxc + spb - 1) // spb
    S = [[_tile(es, nc, f"{pfx}_S{a}_{b}", [128, spb, wmax], F32, psum=True) for b in range(nbank)] for a in range(2)]
    O = _tile(es, nc, f"{pfx}_O", [128, 4, 65], F32, psum=True)
    pT = [[_tile(es, nc, f"{pfx}_pT{a}_{b}", [128, wmax], BF16) for b in range(maxc)] for a in range(2)]

    def emit_S(ui):
        u = units[ui]
        a = ui % 2
        w = u["nq"] * 128
        for ci, (kT, bias, _v) in enumerate(u["chunks"]):
            st, rst = S[a][ci // spb]
            sp = st[:, ci % spb, 0:w]
            P.op("pe", lambda e: e.matmul(sp, lhsT=kT, rhs=u["q"], start=True, stop=(bias is None)), r=rd_res, w=[rst])
            if bias is not None:
                P.op("pe", lambda e: e.matmul(sp, lhsT=bias[0], rhs=bias[1], start=False, stop=True), r=rd_res, w=[rst])
        for ci in range(len(u["chunks"])):
            st, rst = S[a][ci // spb]
            sp = st[:, ci % spb, 0:w]
            pt, rpt = pT[a][ci]
            P.op("act", lambda e: e.activation(out=pt[:, 0:w], in_=sp, func=AF.Exp, scale=0.125), r=[rst], w=[rpt])

    def emit_PV(ui):
        u = units[ui]
        a = ui % 2
        ot, rot = O
        nch = len(u["chunks"])
        for g, h in enumerate(u["heads"]):
            for ci, (_k, _b, v) in enumerate(u["chunks"]):
                pt, rpt = pT[a][ci]
                P.op("pe", lambda e: e.matmul(ot[:, h, :], lhsT=pt[:, g * 128:(g + 1) * 128], rhs=v, start=(ci == 0), stop=(ci == nch - 1)),
                     r=[rpt] + rd_res, w=[rot])
        if u["final"]:
            epilogue(u["j"], ot, rot)

    emit_S(0)
    for ui in range(len(units)):
        if ui + 1 < len(units):
            emit_S(ui + 1)
        emit_PV(ui)
    P.pe_relaxed = False


def attn_epilogue_factory(K, es, pfx, ident, rident, yrow0, extra_den=None):
    nc, P, Dm, R = K.nc, K.P, K.dram, K.R
    den, rden = _tile(es, nc, pfx + "_den", [128, 4], F32)
    ybf, rybf = _tile(es, nc, pfx + "_ybf", [128, 256], BF16)
    pT2, rpT2 = _tile(es, nc, pfx + "_pT2", [128, 2, 128], BF16, psum=True)
    oT, roT = _tile(es, nc, pfx + "_oT", [128, 2, 128], BF16)

    def epi(j, ot, rot):
        if extra_den is not None:
            P.op("dve", lambda e: e.tensor_tensor(out=den[:], in0=ot[:, :, 64], in1=extra_den[0][:], op=ALU.add), r=[rot, extra_den[1]], w=[rden])
            P.op("dve", lambda e: e.reciprocal(out=den[:], in_=den[:]), r=[rden], w=[rden])
        else:
            P.op("dve", lambda e: e.reciprocal(out=den[:], in_=ot[:, :, 64]), r=[rot], w=[rden])
        P.op("dve", lambda e: e.tensor_tensor(out=ybf[:].rearrange("p (h d) -> p h d", d=64), in0=ot[:, :, 0:64],
                                              in1=den[:].unsqueeze(2).to_broadcast([128, 4, 64]), op=ALU.mult), r=[rot, rden], w=[rybf])
        for k in range(2):
            P.op("pe", lambda e: e.transpose(out=pT2[:, k, :], in_=ybf[:, k * 128:(k + 1) * 128], identity=ident[:]), r=[rybf, rident], w=[rpT2])
        P.op("act", lambda e: e.copy(out=oT[:], in_=pT2[:]), r=[rpT2], w=[roT])
        P.dma(STQ, Dm["yT"][yrow0:yrow0 + 256, :].rearrange("(k p) n -> p k n", p=128)[:, :, j * 128:(j + 1) * 128], oT[:], r=[roT], w=[R["y"]])
    return epi


def stage_gqa(K, l, with_ctx, qtiles=None):
    nc, P, Dm, R = K.nc, K.P, K.dram, K.R
    with contextlib.ExitStack() as es:
        qT, rqT = _tile(es, nc, "G_qT", [64, 4, T], BF16)
        kT, rkT = _tile(es, nc, "G_kT", [64, 2, T], BF16)
        V, rV = _tile(es, nc, "G_V", [128, NT, 2, 65], BF16)
        mk, rmk = _tile(es, nc, "G_mask", [128, 2, 256], BF16)
        ident, rident = _tile(es, nc, "G_ident", [128, 128], BF16)
        esk, resk = _tile(es, nc, "G_esink", [128, 4], F32)
        for h in range(4):
            P.dma("sp", qT[:, h, :], Dm["gqkT"][h * 64:(h + 1) * 64, :], r=[R["mix"]], w=[rqT])
        for kv in range(2):
            P.dma("sp", kT[:, kv, :], Dm["gqkT"][256 + kv * 64:256 + (kv + 1) * 64, :], r=[R["mix"]], w=[rkT])
            P.dma("sp", V[:, :, kv, 0:64], Dm["gv"][:, kv * 64:(kv + 1) * 64].rearrange("(c p) d -> p c d", p=128), r=[R["mix"]], w=[rV])
        P.op("pool", lambda e: e.memset(V[:, :, :, 64:65], 1.0), w=[rV])
        P.dma("pool", mk[:], Dm["gqa_mask"], w=[rmk])
        P.dma("pool", ident[:], Dm["ident"], w=[rident])
        P.dma("sp", esk[:], Dm["gqa_sink"][l].partition_broadcast(128), w=[resk])
        P.op("act", lambda e: e.activation(out=esk[:], in_=esk[:], func=AF.Exp), r=[resk], w=[resk])
        rd = [rqT, rkT, rV, rmk, rident]
        epi = attn_epilogue_factory(K, es, "G", ident, rident, 768, extra_den=(esk, resk))
        qtiles = qtiles if qtiles is not None else list(range(NXT)) + ([64, 65] if with_ctx else [])
        units = []
        for j in qtiles:
            if j < NXT:
                ch = [(c, tag) for c, tag in ((j - 1, 0), (j, None), (j + 1, 1)) if 0 <= c < NXT] + [(64, None), (65, None)]
            else:
                ch = [(64, None), (65, None)]
            for kv in range(2):
                chunks = []
                for c, tag in ch:
                    bias = None if tag is None else (ident[:], mk[:, tag, :])
                    chunks.append((kT[:, kv, c * 128:(c + 1) * 128], bias, V[:, c, kv, :]))
                units.append(dict(j=j, q=qT[:, 2 * kv:2 * kv + 2, j * 128:(j + 1) * 128], nq=2, heads=[2 * kv, 2 * kv + 1], chunks=chunks, final=(kv == 1)))
        run_attention(K, es, "G", units, rd, epi, 5)
    P.barrier()


SCRATCH.update({"yT": ([1024, T], BF16)})


def stage_na(K, l, with_ctx, qtiles=None):
    nc, P, Dm, R = K.nc, K.P, K.dram, K.R
    with contextlib.ExitStack() as es:
        qT, rqT = _tile(es, nc, "N_qT", [128, 2, T], BF16)
        kT, rkT = _tile(es, nc, "N_kT", [128, 2, T], BF16)
        V, rV = _tile(es, nc, "N_V", [128, NT, 4, 65], BF16)
        Bp, rBp = _tile(es, nc, "N_Bp", [128, NA_NCLS, 4, 128], BF16)
        jx, rjx = _tile(es, nc, "N_jx", [128, 128], BF16)
        ident, rident = _tile(es, nc, "N_ident", [128, 128], BF16)
        zt, rzt = _tile(es, nc, "N_zero", [64, 128], F32)
        rp, rrp = _tile(es, nc, "N_rpb", [15, 4, 31], F32)
        stg = [_tile(es, nc, f"N_stg{i}", [128, 2, 64], F32) for i in range(2)]
        rms_ = [_tile(es, nc, f"N_rm{i}", [128, 128], F32) for i in range(2)]
        rpad = Res("rpbpad")
        for c2 in range(2):
            P.dma("sp", qT[:, c2, :], Dm["nqkT"][c2 * 128:(c2 + 1) * 128, :], r=[R["mix"]], w=[rqT])
            P.dma("sp", kT[:, c2, :], Dm["nqkT"][256 + c2 * 128:256 + (c2 + 1) * 128, :], r=[R["mix"]], w=[rkT])
        for h in range(4):
            P.dma("sp", V[:, :, h, 0:64], Dm["nv"][:, h * 64:(h + 1) * 64].rearrange("(c p) d -> p c d", p=128), r=[R["mix"]], w=[rV])
        P.op("pool", lambda e: e.memset(V[:, :, :, 64:65], 1.0), w=[rV])
        P.dma("pool", jx[:], Dm["na_jx"], w=[rjx])
        P.dma("pool", ident[:], Dm["ident"], w=[rident])
        P.op("dve", lambda e: e.memset(zt[:], 0.0), w=[rzt])
        P.dma("sp", Dm["rpbpad"].rearrange("h r j -> (h r) j"), zt[:], r=[rzt], w=[rpad])
        P.dma("sp", rp[:], Dm["na_rpb"][l].rearrange("h r j -> r h j"), w=[rrp])
        for h in range(4):
            P.dma("sp", Dm["rpbpad"][h, 0:15, 48:79], rp[:, h, :], r=[rrp], w=[rpad])
        padt = Dm["rpbpad"].tensor
        cl = na_class_list()
        for cls, (j, cch) in enumerate(cl):
            rmt, rrmt = rms_[cls % 2]
            P.dma("sp", rmt[:], Dm["na_rm"][cls], w=[rrmt])
            for h in range(4):
                st, rst = stg[(cls * 4 + h) % 2]
                for rq in range(2):
                    dr0 = 2 * (cch - j) + 0 - rq + 7
                    src = bass.AP(tensor=padt, offset=(h * 16 + dr0) * 128, ap=[[1, 64], [128, 2], [1, 64]])
                    P.dma("sp" if rq == 0 else "act", st[rq * 64:(rq + 1) * 64, :, :], src, r=[rpad], w=[rst])
                P.op("dve", lambda e: e.scalar_tensor_tensor(out=Bp[:, cls, h, :], in0=st[:].rearrange("p a b -> p (a b)"), scalar=8.0, in1=rmt[:],
                                                             op0=ALU.mult, op1=ALU.add), r=[rst, rrmt], w=[rBp])
        rd = [rqT, rkT, rV, rBp, rjx, rident]
        epi = attn_epilogue_factory(K, es, "N", ident, rident, 256)
        qtiles = qtiles if qtiles is not None else list(range(NXT)) + ([64, 65] if with_ctx else [])
        units = []
        for j in qtiles:
            ch = (na_chunks(j) if j < NXT else []) + [(64, None), (65, None)]
            for h in range(4):
                pb, c2 = (h % 2) * 64, h // 2
                chunks = []
                for c, cls in ch:
                    bias = None if cls is None else (Bp[:, cls, h, :], jx[:])
                    chunks.append((kT[pb:pb + 64, c2, c * 128:(c + 1) * 128], bias, V[:, c, h, :]))
                units.append(dict(j=j, q=qT[pb:pb + 64, c2, j * 128:(j + 1) * 128], nq=1, heads=[h], chunks=chunks, final=(h == 3)))
        run_attention(K, es, "N", units, rd, epi, 7)
    P.barrier()


SCRATCH.update({"rpbpad": ([4, 16, 128], F32)})


def stage_ret(K, l, with_ctx, out_chunks=None):
    nc, P, Dm, R = K.nc, K.P, K.dram, K.R
    LN8 = math.log(0.125)
    with contextlib.ExitStack() as es:
        qT, rqT = _tile(es, nc, "R_qT", [128, 2, T], BF16)
        kT, rkT = _tile(es, nc, "R_kT", [128, 2, T], BF16)
        Kt, rKt = _tile(es, nc, "R_Kt", [128, NT, 256], BF16)
        Vt, rVt = _tile(es, nc, "R_Vt", [128, NT, 256], BF16)
        SF, rSF = _tile(es, nc, "R_SF", [128, NT, 2, 64], BF16)
        ident, rident = _tile(es, nc, "R_ident", [128, 128], BF16)
        lg, rlg = _tile(es, nc, "R_lg", [128, 8], F32)
        cst, rcst = _tile(es, nc, "R_cst", [128, 5, 128], F32)
        pcol, rpcol = _tile(es, nc, "R_pcol", [128, 2], F32)
        lnc, rlnc = _tile(es, nc, "R_lnc", [128, 2], F32)
        tmp, rtmp = _tile(es, nc, "R_tmp", [128, 128], F32)
        DT, rDT = _tile(es, nc, "R_DT", [128, 4, 128], F32)
        QF, rQF = _tile(es, nc, "R_QF", [128, 2, 128], F32)
        QB, rQB = _tile(es, nc, "R_QB", [128, 2, 128], F32)
        KD, rKD = _tile(es, nc, "R_KD", [128, 8], F32)
        CF, rCF = _tile(es, nc, "R_CF", [128, 2, 64], F32)
        CB, rCB = _tile(es, nc, "R_CB", [128, 2, 64], F32)
        SM, rSM = _tile(es, nc, "R_SM", [128, 2, 64], F32)
        SBc = [_tile(es, nc, f"R_SBc{i}", [128, 2, 64], BF16) for i in range(2)]
        kw, rkw = _tile(es, nc, "R_kw", [128, 256], BF16)
        PT = [_tile(es, nc, f"R_PT{i}", [128, 4, 128], BF16) for i in range(2)]
        qf, rqf = _tile(es, nc, "R_qf", [128, 2, 128], BF16)
        qb, rqb = _tile(es, nc, "R_qb", [128, 2, 128], BF16)
        gt = [_tile(es, nc, f"R_g{i}", [128, 256], F32) for i in range(2)]
        sq, rsq = _tile(es, nc, "R_sq", [128, 256], F32)
        ss4, rss4 = _tile(es, nc, "R_ss4", [128, 4], F32)
        yf, ryf = _tile(es, nc, "R_yf", [128, 256], F32)
        ybf, rybf = _tile(es, nc, "R_ybf", [128, 256], BF16)
        oT, roT = _tile(es, nc, "R_oT", [128, 2, 128], BF16)
        Sp = [_tile(es, nc, f"R_Sp{i}", [128, 4, 128], F32, psum=True) for i in range(2)]
        Op = [_tile(es, nc, f"R_Op{i}", [128, 4, 64], F32, psum=True) for i in range(2)]
        KVp, rKVp = _tile(es, nc, "R_KVp", [128, 2, 128], F32, psum=True)
        pT2, rpT2 = _tile(es, nc, "R_pT2", [128, 2, 128], BF16, psum=True)

        for c2 in range(2):
            P.dma("sp", qT[:, c2, :], Dm["rqkT"][c2 * 128:(c2 + 1) * 128, :], r=[R["mix"]], w=[rqT])
            P.dma("sp", kT[:, c2, :], Dm["rqkT"][256 + c2 * 128:256 + (c2 + 1) * 128, :], r=[R["mix"]], w=[rkT])
        P.dma("act", Kt[:], Dm["rk"].rearrange("(c p) d -> p c d", p=128), r=[R["mix"]], w=[rKt])
        P.dma("act", Vt[:], Dm["rv"].rearrange("(c p) d -> p c d", p=128), r=[R["mix"]], w=[rVt])
        P.dma("pool", ident[:], Dm["ident"], w=[rident])
        for i, nm in enumerate(["ret_dpos", "ret_dneg", "ret_diag", "ret_tp1", "ret_tr"]):
            P.dma("sp", cst[:, i, :], Dm[nm], w=[rcst])
        P.dma("sp", pcol[:], Dm["ret_pcol"], w=[rpcol])
        P.dma("sp", lg[:], Dm["ret_log_decay"][l].rearrange("a h -> (a h)").partition_broadcast(128), w=[rlg])
        P.op("dve", lambda e: e.memset(lnc[:, 0:1], LN8), w=[rlnc])
        P.op("dve", lambda e: e.memset(lnc[:, 1:2], EPS), w=[rlnc])
        P.op("act", lambda e: e.activation(out=lg[:], in_=lg[:], func=AF.Exp), r=[rlg], w=[rlg])
        P.op("dve", lambda e: e.tensor_scalar(out=lg[:], in0=lg[:], scalar1=-1.0, scalar2=None, op0=ALU.mult), r=[rlg], w=[rlg])
        for h in range(4):
            pb, c2 = (h % 2) * 64, h // 2
            P.op("dve", lambda e: e.tensor_scalar(out=tmp[:], in0=cst[:, 0, :], scalar1=lg[:, h:h + 1], scalar2=None, op0=ALU.mult), r=[rcst, rlg], w=[rtmp])
            P.op("dve", lambda e: e.scalar_tensor_tensor(out=tmp[:], in0=cst[:, 1, :], scalar=lg[:, 4 + h:5 + h], in1=tmp[:], op0=ALU.mult, op1=ALU.add),
                 r=[rcst, rlg, rtmp], w=[rtmp])
            P.op("dve", lambda e: e.tensor_tensor(out=tmp[:], in0=tmp[:], in1=cst[:, 2, :], op=ALU.add), r=[rtmp, rcst], w=[rtmp])
            P.op("act", lambda e: e.activation(out=DT[:, h, :], in_=tmp[:], func=AF.Exp), r=[rtmp], w=[rDT])
            P.op("act", lambda e: e.activation(out=QF[pb:pb + 64, c2, :], in_=cst[pb:pb + 64, 3, :], func=AF.Exp, scale=lg[pb:pb + 64, h:h + 1], bias=lnc[pb:pb + 64, 0:1]),
                 r=[rcst, rlg, rlnc], w=[rQF])
            P.op("act", lambda e: e.activation(out=QB[pb:pb + 64, c2, :], in_=cst[pb:pb + 64, 4, :], func=AF.Exp, scale=lg[pb:pb + 64, 4 + h:5 + h], bias=lnc[pb:pb + 64, 0:1]),
                 r=[rcst, rlg, rlnc], w=[rQB])
            P.op("act", lambda e: e.activation(out=KD[:, h:h + 1], in_=lg[:, h:h + 1], func=AF.Exp, scale=pcol[:, 0:1]), r=[rlg, rpcol], w=[rKD])
            P.op("act", lambda e: e.activation(out=KD[:, 4 + h:5 + h], in_=lg[:, 4 + h:5 + h], func=AF.Exp, scale=pcol[:, 1:2]), r=[rlg, rpcol], w=[rKD])
            P.op("act", lambda e: e.activation(out=CF[pb:pb + 64, c2, :], in_=lg[pb:pb + 64, h:h + 1].to_broadcast([64, 64]), func=AF.Exp, scale=128.0), r=[rlg], w=[rCF])
            P.op("act", lambda e: e.activation(out=CB[pb:pb + 64, c2, :], in_=lg[pb:pb + 64, 4 + h:5 + h].to_broadcast([64, 64]), func=AF.Exp, scale=128.0), r=[rlg], w=[rCB])

        def state_update(n, kdoff, Ctab, rCtab):
            P.op("dve", lambda e: e.tensor_tensor(out=kw[:].rearrange("p (h d) -> p h d", d=64), in0=Kt[:, n, :].rearrange("p (h d) -> p h d", d=64),
                                                  in1=KD[:, kdoff:kdoff + 4].unsqueeze(2).to_broadcast([128, 4, 64]), op=ALU.mult), r=[rKt, rKD], w=[rkw])
            for c2 in range(2):
                P.op("pe", lambda e: e.matmul(KVp[:, c2, :], lhsT=kw[:, c2 * 128:(c2 + 1) * 128], rhs=Vt[:, n, c2 * 128:(c2 + 1) * 128], start=True, stop=True),
                     r=[rkw, rVt], w=[rKVp])
            P.op("dve", lambda e: e.tensor_tensor(out=SM[:], in0=SM[:], in1=Ctab[:], op=ALU.mult), r=[rSM, rCtab], w=[rSM])
            for hp in (0, 64):
                P.op("dve", lambda e: e.tensor_tensor(out=SM[hp:hp + 64, :, :], in0=SM[hp:hp + 64, :, :], in1=KVp[hp:hp + 64, :, hp:hp + 64], op=ALU.add),
                     r=[rSM, rKVp], w=[rSM])

        fo = [64, 65] + list(range(NXT))
        P.op("dve", lambda e: e.memset(SM[:], 0.0), w=[rSM])
        for i, n in enumerate(fo):
            P.op("act", lambda e: e.copy(out=SF[:, n, :, :], in_=SM[:]), r=[rSM], w=[rSF])
            if i + 1 < len(fo):
                state_update(n, 0, CF, rCF)

        bo = [65, 64] + list(range(NXT - 1, -1, -1))
        outs = [n for n in bo if (n < NXT or with_ctx)]
        if out_chunks is not None:
            outs = [n for n in outs if n in out_chunks]
        P.op("dve", lambda e: e.memset(SM[:], 0.0), w=[rSM])
        oi = {n: i for i, n in enumerate(outs)}

        def emit_S(n):
            i = oi[n]
            sp, rsp = Sp[i % 2]
            cs = slice(n * 128, (n + 1) * 128)
            for h in range(4):
                pb, c2 = (h % 2) * 64, h // 2
                P.op("pe", lambda e: e.matmul(sp[:, h, :], lhsT=kT[pb:pb + 64, c2, cs], rhs=qT[pb:pb + 64, c2, cs], start=True, stop=True), r=[rkT, rqT], w=[rsp])
            pt, rpt = PT[i % 2]
            P.op("dve", lambda e: e.tensor_tensor(out=pt[:], in0=sp, in1=DT[:], op=ALU.mult), r=[rsp, rDT], w=[rpt])
            g_, rg_ = gt[i % 2]
            P.dma("sp", g_[:], Dm["rg"][cs, :], r=[R["mix"]], w=[rg_])

        def emit_O(n, sbc, rsbc):
            i = oi[n]
            cs = slice(n * 128, (n + 1) * 128)
            pt, rpt = PT[i % 2]
            op_, rop = Op[i % 2]
            g_, rg_ = gt[i % 2]
            P.op("dve", lambda e: e.tensor_tensor(out=qf[:], in0=qT[:, :, cs], in1=QF[:], op=ALU.mult), r=[rqT, rQF], w=[rqf])
            P.op("pool", lambda e: e.tensor_tensor(out=qb[:], in0=qT[:, :, cs], in1=QB[:], op=ALU.mult), r=[rqT, rQB], w=[rqb])
            for h in range(4):
                pb, c2 = (h % 2) * 64, h // 2
                P.op("pe", lambda e: e.matmul(op_[:, h, :], lhsT=pt[:, h, :], rhs=Vt[:, n, h * 64:(h + 1) * 64], start=True, stop=False), r=[rpt, rVt], w=[rop])
                P.op("pe", lambda e: e.matmul(op_[:, h, :], lhsT=qf[pb:pb + 64, c2, :], rhs=SF[pb:pb + 64, n, c2, :], start=False, stop=False), r=[rqf, rSF], w=[rop])
                P.op("pe", lambda e: e.matmul(op_[:, h, :], lhsT=qb[pb:pb + 64, c2, :], rhs=sbc[pb:pb + 64, c2, :], start=False, stop=True), r=[rqb, rsbc], w=[rop])
            P.op("act", lambda e: e.activation(out=sq[:].rearrange("p (h d) -> p h d", d=64), in_=op_, func=AF.Square), r=[rop], w=[rsq])
            P.op("dve", lambda e: e.tensor_reduce(out=ss4[:], in_=sq[:].rearrange("p (h d) -> p h d", d=64), axis=AX.X, op=ALU.add), r=[rsq], w=[rss4])
            P.op("act", lambda e: e.activation(out=ss4[:], in_=ss4[:], func=AF.Sqrt, scale=1.0 / 64, bias=lnc[:, 1:2]), r=[rss4, rlnc], w=[rss4])
            P.op("dve", lambda e: e.reciprocal(out=ss4[:], in_=ss4[:]), r=[rss4], w=[rss4])
            P.op("dve", lambda e: e.tensor_tensor(out=yf[:].rearrange("p (h d) -> p h d", d=64), in0=op_, in1=ss4[:].unsqueeze(2).to_broadcast([128, 4, 64]), op=ALU.mult),
                 r=[rop, rss4], w=[ryf])
            P.op("dve", lambda e: e.tensor_tensor(out=ybf[:], in0=yf[:], in1=g_[:], op=ALU.mult), r=[ryf, rg_], w=[rybf])
            for k in range(2):
                P.op("pe", lambda e: e.transpose(out=pT2[:, k, :], in_=ybf[:, k * 128:(k + 1) * 128], identity=ident[:]), r=[rybf, rident], w=[rpT2])
            P.op("act", lambda e: e.copy(out=oT[:], in_=pT2), r=[rpT2], w=[roT])
            P.dma(STQ, Dm["yT"][0:256, :].rearrange("(k p) n -> p k n", p=128)[:, :, cs], oT[:], r=[roT], w=[R["y"]])

        if outs:
            emit_S(outs[0])
        for bi, n in enumerate(bo):
            sbc, rsbc = SBc[bi % 2]
            if n in oi:
                P.op("act", lambda e: e.copy(out=sbc[:], in_=SM[:]), r=[rSM], w=[rsbc])
                i = oi[n]
                if i + 1 < len(outs):
                    emit_S(outs[i + 1])
                emit_O(n, sbc, rsbc)
            if bi + 1 < len(bo):
                state_update(n, 4, CB, rCB)
    P.barrier()


def _sin_reduced(P, out, in_, shift, t1, rt1, t2, rt2, r_in, w_out):
    i2p = 1.0 / (2 * math.pi)
    P.op("dve", lambda e: e.tensor_scalar(out=t1, in0=in_, scalar1=i2p, scalar2=shift * i2p, op0=ALU.mult, op1=ALU.add), r=r_in, w=[rt1])
    P.op("dve", lambda e: e.tensor_scalar(out=t2, in0=t1, scalar1=MAGIC, scalar2=None, op0=ALU.add), r=[rt1], w=[rt2])
    P.op("dve", lambda e: e.tensor_scalar(out=t2, in0=t2, scalar1=MAGIC, scalar2=None, op0=ALU.subtract), r=[rt2], w=[rt2])
    P.op("dve", lambda e: e.tensor_tensor(out=t1, in0=t1, in1=t2, op=ALU.subtract), r=[rt1, rt2], w=[rt1])
    P.op("act", lambda e: e.activation(out=out, in_=t1, func=AF.Sin, scale=2 * math.pi), r=[rt1], w=w_out)


def stage_s5(K, l, with_ctx, epi_tiles=None, precast=False):
    nc, P, Dm, R = K.nc, K.P, K.dram, K.R
    TC = S5_TC
    NCH = T // TC
    P.pe_relaxed = True
    with contextlib.ExitStack() as es:
        uT, ruT = _tile(es, nc, "S_uT", [128, 2, T], BF16)
        ident, rident = _tile(es, nc, "S_ident", [128, 128], BF16)
        identf, ridentf = _tile(es, nc, "S_identf", [128, 128], F32)
        BbT, rBbT = _tile(es, nc, "S_BbT", [128, 2, 2, 8, 128], BF16)
        Cm, rCm = _tile(es, nc, "S_Cm", [128, 4, 2, 128], BF16)
        RD, rRD = _tile(es, nc, "S_RD", [128, 2, 8], F32)
        TH, rTH = _tile(es, nc, "S_TH", [128, 2, 8], F32)
        COS, rCOS = _tile(es, nc, "S_COS", [128, 8, TC], F32)
        SIN, rSIN = _tile(es, nc, "S_SIN", [128, 8, TC], F32)
        iota1, riota1 = _tile(es, nc, "S_iota1", [128, TC], F32)
        P.dma("sp", uT[:, 0, :], Dm["suT"][0:128, :], r=[R["mix"]], w=[ruT])
        P.dma("sp", uT[:, 1, :], Dm["suT"][128:256, :], r=[R["mix"]], w=[ruT])
        P.dma("sp", identf[:], Dm["ident"], w=[ridentf])
        P.dma("pool", ident[:], Dm["ident"], w=[rident])
        P.dma("sp", iota1[:], Dm["s5_iota1"], w=[riota1])

        with contextlib.ExitStack() as es2:
            LRt, rLR = _tile(es2, nc, "S_LR", [128, 8], F32)
            LIt, rLI = _tile(es2, nc, "S_LI", [128, 8], F32)
            DTt, rDTt = _tile(es2, nc, "S_DT", [128, 8], F32)
            w8 = [_tile(es2, nc, f"S_w8_{i}", [128, 8], F32) for i in range(8)]
            BRt, rBRt = _tile(es2, nc, "S_BR", [128, 8, 16], F32)
            BIt, rBIt = _tile(es2, nc, "S_BI", [128, 8, 16], F32)
            bb = [_tile(es2, nc, f"S_bb{i}", [128, 8, 16], F32) for i in range(4)]
            Zp, rZp = _tile(es2, nc, "S_Zp", [128, 8, 128], F32)
            Cn, rCn = _tile(es2, nc, "S_Cn", [128, 128], F32)
            bdm, rbdm = _tile(es2, nc, "S_bdm", [128, 128], F32)
            tp, rtp = _tile(es2, nc, "S_tp", [128, 128], F32, psum=True)
            P.dma("sp", bdm[:], Dm["s5_bdmask"], w=[rbdm])
            with nc.allow_non_contiguous_dma(reason="small parameter tables"):
                P.dma("sp", BRt[:], Dm["s5_b_re"][l].rearrange("(gp a) p c -> (a p) gp c", a=2), w=[rBRt])
                P.dma("sp", BIt[:], Dm["s5_b_im"][l].rearrange("(gp a) p c -> (a p) gp c", a=2), w=[rBIt])
            for dirn in range(2):
                with nc.allow_non_contiguous_dma(reason="small parameter tables"):
                    P.dma("sp", LRt[:], Dm["s5_lambda_re"][l, dirn].rearrange("(gp a) p -> (a p) gp", a=2), w=[rLR])
                    P.dma("sp", LIt[:], Dm["s5_lambda_im"][l, dirn].rearrange("(gp a) p -> (a p) gp", a=2), w=[rLI])
                    for a in range(2):
                        src = Dm["s5_log_step"][l, dirn].rearrange("(gp a) -> a gp", a=2)[a].partition_broadcast(64)
                        P.dma("sp", DTt[a * 64:(a + 1) * 64, :], src, w=[rDTt])
                (dt_, rdt), (mag, rmag), (sn, rsn), (cs_, rcs), (t1, rt1), (t2, rt2), (cr, rcr), (ci, rci) = w8
                P.op("act", lambda e: e.activation(out=dt_[:], in_=DTt[:], func=AF.Exp), r=[rDTt], w=[rdt])
                P.op("dve", lambda e: e.tensor_tensor(out=mag[:], in0=LRt[:], in1=dt_[:], op=ALU.mult), r=[rLR, rdt], w=[rmag])
                P.op("act", lambda e: e.activation(out=RD[:, dirn, :], in_=mag[:], func=AF.Exp), r=[rmag], w=[rRD])
                P.op("dve", lambda e: e.tensor_tensor(out=TH[:, dirn, :], in0=LIt[:], in1=dt_[:], op=ALU.mult), r=[rLI, rdt], w=[rTH])
                _sin_reduced(P, sn[:], TH[:, dirn, :], 0.0, t1[:], rt1, t2[:], rt2, [rTH], [rsn])
                _sin_reduced(P, cs_[:], TH[:, dirn, :], math.pi / 2, t1[:], rt1, t2[:], rt2, [rTH], [rcs])
                P.op("dve", lambda e: e.tensor_tensor(out=cs_[:], in0=cs_[:], in1=RD[:, dirn, :], op=ALU.mult), r=[rcs, rRD], w=[rcs])
                P.op("dve", lambda e: e.tensor_tensor(out=sn[:], in0=sn[:], in1=RD[:, dirn, :], op=ALU.mult), r=[rsn, rRD], w=[rsn])
                P.op("dve", lambda e: e.tensor_scalar(out=cs_[:], in0=cs_[:], scalar1=-1.0, scalar2=None, op0=ALU.add), r=[rcs], w=[rcs])
                P.op("dve", lambda e: e.tensor_tensor(out=t1[:], in0=LRt[:], in1=LRt[:], op=ALU.mult), r=[rLR], w=[rt1])
                P.op("dve", lambda e: e.tensor_tensor(out=t2[:], in0=LIt[:], in1=LIt[:], op=ALU.mult), r=[rLI], w=[rt2])
                P.op("dve", lambda e: e.tensor_tensor(out=t1[:], in0=t1[:], in1=t2[:], op=ALU.add), r=[rt1, rt2], w=[rt1])
                P.op("dve", lambda e: e.reciprocal(out=t1[:], in_=t1[:]), r=[rt1], w=[rt1])
                P.op("dve", lambda e: e.tensor_tensor(out=cr[:], in0=cs_[:], in1=LRt[:], op=ALU.mult), r=[rcs, rLR], w=[rcr])
                P.op("dve", lambda e: e.tensor_tensor(out=t2[:], in0=sn[:], in1=LIt[:], op=ALU.mult), r=[rsn, rLI], w=[rt2])
                P.op("dve", lambda e: e.tensor_tensor(out=cr[:], in0=cr[:], in1=t2[:], op=ALU.add), r=[rcr, rt2], w=[rcr])
                P.op("dve", lambda e: e.tensor_tensor(out=cr[:], in0=cr[:], in1=t1[:], op=ALU.mult), r=[rcr, rt1], w=[rcr])
                P.op("dve", lambda e: e.tensor_tensor(out=ci[:], in0=sn[:], in1=LRt[:], op=ALU.mult), r=[rsn, rLR], w=[rci])
                P.op("dve", lambda e: e.tensor_tensor(out=t2[:], in0=cs_[:], in1=LIt[:], op=ALU.mult), r=[rcs, rLI], w=[rt2])
                P.op("dve", lambda e: e.tensor_tensor(out=ci[:], in0=ci[:], in1=t2[:], op=ALU.subtract), r=[rci, rt2], w=[rci])
                P.op("dve", lambda e: e.tensor_tensor(out=ci[:], in0=ci[:], in1=t1[:], op=ALU.mult), r=[rci, rt1], w=[rci])
                crb = cr[:].unsqueeze(2).to_broadcast([128, 8, 16])
                cib = ci[:].unsqueeze(2).to_broadcast([128, 8, 16])
                (b0, rb0), (b1, rb1), (b2, rb2), (b3, rb3) = bb
                P.op("dve", lambda e: e.tensor_tensor(out=b0[:], in0=BRt[:], in1=crb, op=ALU.mult), r=[rBRt, rcr], w=[rb0])
                P.op("dve", lambda e: e.tensor_tensor(out=b1[:], in0=BIt[:], in1=cib, op=ALU.mult), r=[rBIt, rci], w=[rb1])
                P.op("dve", lambda e: e.tensor_tensor(out=b0[:], in0=b0[:], in1=b1[:], op=ALU.subtract), r=[rb0, rb1], w=[rb0])
                P.op("dve", lambda e: e.tensor_tensor(out=b2[:], in0=BIt[:], in1=crb, op=ALU.mult), r=[rBIt, rcr], w=[rb2])
                P.op("dve", lambda e: e.tensor_tensor(out=b3[:], in0=BRt[:], in1=cib, op=ALU.mult), r=[rBRt, rci], w=[rb3])
                P.op("dve", lambda e: e.tensor_tensor(out=b2[:], in0=b2[:], in1=b3[:], op=ALU.add), r=[rb2, rb3], w=[rb2])
                for ri_, (bsrc, rbsrc) in enumerate(((b0, rb0), (b2, rb2))):
                    P.op("dve", lambda e: e.memset(Zp[:], 0.0), w=[rZp])
                    for gp in range(8):
                        for a in range(2):
                            col0 = ((2 * gp + a) % 8) * 16
                            P.op("dve", lambda e: e.tensor_copy(out=Zp[a * 64:(a + 1) * 64, gp, col0:col0 + 16], in_=bsrc[a * 64:(a + 1) * 64, gp, :]), r=[rbsrc], w=[rZp])
                    for gp in range(8):
                        P.op("pe", lambda e: e.transpose(out=tp, in_=Zp[:, gp, :], identity=identf[:]), r=[rZp, ridentf], w=[rtp])
                        P.op("act", lambda e: e.copy(out=BbT[:, dirn, ri_, gp, :], in_=tp), r=[rtp], w=[rBbT])
            for half in range(2):
                for src_name, kinds in (("s5_c_re", ((0, 1.0), (1, -1.0))), ("s5_c_im", ((2, -1.0),))):
                    srcv = Dm[src_name][l].rearrange("g co p -> (g co) p")[half * 128:(half + 1) * 128, :]
                    P.dma("sp", Cn[:, 0:64], srcv, w=[rCn])
                    P.dma("sp", Cn[:, 64:128], srcv, w=[rCn])
                    P.op("dve", lambda e: e.tensor_tensor(out=Cn[:], in0=Cn[:], in1=bdm[:], op=ALU.mult), r=[rCn, rbdm], w=[rCn])
                    P.op("pe", lambda e: e.transpose(out=tp, in_=Cn[:], identity=identf[:]), r=[rCn, ridentf], w=[rtp])
                    for kidx, sgn in kinds:
                        P.op("act", lambda e: e.activation(out=Cm[:, kidx, half, :], in_=tp, func=AF.Copy, scale=sgn), r=[rtp], w=[rCm])
        P.barrier()

        with contextlib.ExitStack() as es3:
            Bp = [[_tile(es3, nc, f"S_Bp{i}{j}", [128, TC], F32, psum=True) for j in range(2)] for i in range(2)]
            Yp = [[_tile(es3, nc, f"S_Yp{i}{j}", [128, 256], F32, psum=True) for j in range(2)] for i in range(2)]
            tq = [[_tile(es3, nc, f"S_tq{i}{j}", [128, TC], F32) for j in range(4)] for i in range(2)]
            bp_ = [[_tile(es3, nc, f"S_bp{i}{j}", [128, TC], F32) for j in range(2)] for i in range(2)]
            Wt = [[_tile(es3, nc, f"S_W{i}{j}", [128, TC], F32) for j in range(2)] for i in range(2)]
            Pr = [[_tile(es3, nc, f"S_Pr{i}{j}", [128, TC], BF16) for j in range(4)] for i in range(2)]
            XR, rXR = _tile(es3, nc, "S_XR", [128, 8], F32)
            XI, rXI = _tile(es3, nc, "S_XI", [128, 8], F32)
            tc1, rtc1 = _tile(es3, nc, "S_tc1", [128, 1], F32)
            tc2, rtc2 = _tile(es3, nc, "S_tc2", [128, 1], F32)
            ysb = [_tile(es3, nc, f"S_ysb{i}", [128, 2, 256], F32) for i in range(2)]
            ang, rang = _tile(es3, nc, "S_ang", [128, 8, TC], F32)
            at2, rat2 = _tile(es3, nc, "S_at2", [128, 8, TC], F32)
            pc_step = make_precast(K, l, es3) if precast else None
            for dirn in range(2):
                for gp in range(8):
                    P.op("dve", lambda e: e.tensor_scalar(out=ang[:, gp, :], in0=iota1[:], scalar1=TH[:, dirn, gp:gp + 1], scalar2=None, op0=ALU.mult), r=[riota1, rTH], w=[rang])
                _sin_reduced(P, SIN[:].rearrange("p a b -> p (a b)"), ang[:].rearrange("p a b -> p (a b)"), 0.0,
                             COS[:].rearrange("p a b -> p (a b)"), rCOS, at2[:].rearrange("p a b -> p (a b)"), rat2, [rang], [rSIN])
                _sin_reduced(P, COS[:].rearrange("p a b -> p (a b)"), ang[:].rearrange("p a b -> p (a b)"), math.pi / 2,
                             ang[:].rearrange("p a b -> p (a b)"), rang, at2[:].rearrange("p a b -> p (a b)"), rat2, [rang], [rCOS])
                P.op("dve", lambda e: e.memset(XR[:], 0.0), w=[rXR])
                P.op("dve", lambda e: e.memset(XI[:], 0.0), w=[rXI])
                ydst = Dm["s5yf"] if dirn == 0 else Dm["s5yb"]
                for ck in range(NCH):
                    if dirn == 0:
                        c0 = L if ck == 0 else (ck - 1) * TC
                    else:
                        c0 = L if ck == 0 else L - ck * TC
                    yp = Yp[ck % 2]
                    if pc_step is not None:
                        pc_step(2)
                    def phase_a(gp):
                        par = gp % 2
                        ct = gp // 4
                        usl = uT[:, ct, c0:c0 + TC]
                        if dirn == 1:
                            usl = usl[:, ::-1]
                        (bre, rbre), (bim, rbim) = Bp[par]
                        P.op("pe", lambda e: e.matmul(bre, lhsT=BbT[:, dirn, 0, gp, :], rhs=usl, start=True, stop=True), r=[rBbT, ruT], w=[rbre])
                        P.op("pe", lambda e: e.matmul(bim, lhsT=BbT[:, dirn, 1, gp, :], rhs=usl, start=True, stop=True), r=[rBbT, ruT], w=[rbim])
                        (q1, rq1), (q2, rq2), (q3, rq3), (q4, rq4) = tq[par]
                        cosg, sing = COS[:, gp, :], SIN[:, gp, :]
                        P.op("dve", lambda e: e.tensor_tensor(out=q1[:], in0=bre, in1=cosg, op=ALU.mult), r=[rbre, rCOS], w=[rq1])
                        P.op("dve", lambda e: e.tensor_tensor(out=q2[:], in0=bim, in1=sing, op=ALU.mult), r=[rbim, rSIN], w=[rq2])
                        P.op("dve", lambda e: e.tensor_tensor(out=q3[:], in0=bim, in1=cosg, op=ALU.mult), r=[rbim, rCOS], w=[rq3])
                        P.op("dve", lambda e: e.tensor_tensor(out=q4[:], in0=bre, in1=sing, op=ALU.mult), r=[rbre, rSIN], w=[rq4])
                        (br2, rbr2), (bi2, rbi2) = bp_[par]
                        P.op(S5_ADD_ENG, lambda e: e.tensor_tensor(out=br2[:], in0=q1[:], in1=q2[:], op=ALU.add), r=[rq1, rq2], w=[rbr2])
                        P.op(S5_ADD_ENG, lambda e: e.tensor_tensor(out=bi2[:], in0=q3[:], in1=q4[:], op=ALU.subtract), r=[rq3, rq4], w=[rbi2])

                    def phase_b(gp):
                        par = gp % 2
                        half, gl = gp // 4, gp % 4
                        cosg, sing = COS[:, gp, :], SIN[:, gp, :]
                        (br2, rbr2), (bi2, rbi2) = bp_[par]
                        (wr, rwr), (wi, rwi) = Wt[par]
                        rdb = RD[:, dirn, gp:gp + 1].to_broadcast([128, TC])
                        P.op("dve", lambda e: e.tensor_tensor_scan(out=wr[:], data0=rdb, data1=br2[:], initial=XR[:, gp:gp + 1], op0=ALU.mult, op1=ALU.add),
                             r=[rRD, rbr2, rXR], w=[rwr])
                        P.op("dve", lambda e: e.tensor_tensor_scan(out=wi[:], data0=rdb, data1=bi2[:], initial=XI[:, gp:gp + 1], op0=ALU.mult, op1=ALU.add),
                             r=[rRD, rbi2, rXI], w=[rwi])
                        cl, sl = COS[:, gp, TC - 1:TC], SIN[:, gp, TC - 1:TC]
                        P.op("dve", lambda e: e.tensor_tensor(out=tc1[:], in0=wi[:, TC - 1:TC], in1=sl, op=ALU.mult), r=[rwi, rSIN], w=[rtc1])
                        P.op("dve", lambda e: e.tensor_tensor(out=tc2[:], in0=wi[:, TC - 1:TC], in1=cl, op=ALU.mult), r=[rwi, rCOS], w=[rtc2])
                        P.op("dve", lambda e: e.scalar_tensor_tensor(out=XR[:, gp:gp + 1], in0=wr[:, TC - 1:TC], scalar=cl, in1=tc1[:], op0=ALU.mult, op1=ALU.subtract),
                             r=[rwr, rCOS, rtc1], w=[rXR])
                        P.op("dve", lambda e: e.scalar_tensor_tensor(out=XI[:, gp:gp + 1], in0=wr[:, TC - 1:TC], scalar=sl, in1=tc2[:], op0=ALU.mult, op1=ALU.add),
                             r=[rwr, rSIN, rtc2], w=[rXI])
                        (pcc, rpcc), (pis, rpis), (prs, rprs), (pic, rpic) = Pr[par]
                        ov = (lambda t_: t_[:, ::-1]) if dirn == 1 else (lambda t_: t_[:])
                        P.op(S5_PROD_ENG, lambda e: e.tensor_tensor(out=ov(pcc), in0=wr[:], in1=cosg, op=ALU.mult), r=[rwr, rCOS], w=[rpcc])
                        P.op("pool", lambda e: e.tensor_tensor(out=ov(pis), in0=wi[:], in1=sing, op=ALU.mult), r=[rwi, rSIN], w=[rpis])
                        P.op(S5_PROD_ENG, lambda e: e.tensor_tensor(out=ov(prs), in0=wr[:], in1=sing, op=ALU.mult), r=[rwr, rSIN], w=[rprs])
                        P.op("pool", lambda e: e.tensor_tensor(out=ov(pic), in0=wi[:], in1=cosg, op=ALU.mult), r=[rwi, rCOS], w=[rpic])
                        for sub in range(TC // 128):
                            ypt, rypt = yp[sub]
                            osl = ypt[:, gp * 32:(gp + 1) * 32]
                            cs_sl = slice(gl * 32, (gl + 1) * 32)
                            ssl = slice(sub * 128, (sub + 1) * 128)
                            P.op("pe", lambda e: e.matmul(osl, lhsT=pcc[:, ssl], rhs=Cm[:, 0, half, cs_sl], start=True, stop=False), r=[rpcc, rCm], w=[rypt])
                            P.op("pe", lambda e: e.matmul(osl, lhsT=pis[:, ssl], rhs=Cm[:, 1, half, cs_sl], start=False, stop=False), r=[rpis, rCm], w=[rypt])
                            P.op("pe", lambda e: e.matmul(osl, lhsT=prs[:, ssl], rhs=Cm[:, 2, half, cs_sl], start=False, stop=False), r=[rprs, rCm], w=[rypt])
                            P.op("pe", lambda e: e.matmul(osl, lhsT=pic[:, ssl], rhs=Cm[:, 2, half, cs_sl], start=False, stop=True), r=[rpic, rCm], w=[rypt])

                    phase_a(0)
                    for gp in range(8):
                        if gp + 1 < 8:
                            phase_a(gp + 1)
                        phase_b(gp)
                    ys, rys = ysb[ck % 2]
                    for sub in range(TC // 128):
                        ypt, rypt = yp[sub]
                        P.op("act", lambda e: e.copy(out=ys[:, sub, :], in_=ypt), r=[rypt], w=[rys])
                    P.dma("sp", ydst[c0:c0 + TC, :].rearrange("(s p) d -> p s d", p=128), ys[:], r=[rys], w=[R["s5y"]])
            if pc_step is not None:
                pc_step(96)
        P.barrier()

        with contextlib.ExitStack() as es4:
            dsk, rdsk = _tile(es4, nc, "S_dsk", [128, 256], F32)
            glb, rglb = _tile(es4, nc, "S_glb", [128, 256], F32)
            glw, rglw = _tile(es4, nc, "S_glw", [128, 2, 256], BF16)
            ut = [_tile(es4, nc, f"S_ut{i}", [128, 3, 256], F32) for i in range(2)]
            z, rz = _tile(es4, nc, "S_z", [128, 256], F32)
            zg, rzg = _tile(es4, nc, "S_zg", [128, 256], F32)
            zgb, rzgb = _tile(es4, nc, "S_zgb", [128, 256], BF16)
            zT, rzT = _tile(es4, nc, "S_zT", [128, 2, 128], BF16)
            sg, rsg = _tile(es4, nc, "S_sg", [128, 256], F32)
            ob, rob = _tile(es4, nc, "S_ob", [128, 256], BF16)
            oT, roT = _tile(es4, nc, "S_oT", [128, 2, 128], BF16)
            pT2, rpT2 = _tile(es4, nc, "S_pT2", [128, 2, 128], BF16, psum=True)
            pT3, rpT3 = _tile(es4, nc, "S_pT3", [128, 2, 128], BF16, psum=True)
            gp_, rgp_ = _tile(es4, nc, "S_gps", [128, 256], F32, psum=True)
            P.dma("sp", dsk[:], Dm["s5_d"][l].partition_broadcast(128), w=[rdsk])
            P.dma("sp", glb[:], Dm["s5_glu_b"][l].partition_broadcast(128), w=[rglb])
            P.dma("pool", glw[:], Dm["s5_glu_w"][l].rearrange("(k p) n -> p k n", p=128), w=[rglw])
            tiles = list(range(NXT)) + ([64, 65] if with_ctx else [])
            if epi_tiles is not None:
                tiles = [t for t in tiles if t in epi_tiles]

            def load(i):
                t = tiles[i]
                u_, ru_ = ut[i % 2]
                ts = slice(t * 128, (t + 1) * 128)
                P.dma("sp", u_[:, 0, :], Dm["su"][ts, :], r=[R["mix"]], w=[ru_])
                P.dma("sp", u_[:, 1, :], Dm["s5yf"][ts, :], r=[R["s5y"]], w=[ru_])
                P.dma("sp", u_[:, 2, :], Dm["s5yb"][ts, :], r=[R["s5y"]], w=[ru_])
            if tiles:
                load(0)
            for i, t in enumerate(tiles):
                if i + 1 < len(tiles):
                    load(i + 1)
                u_, ru_ = ut[i % 2]
                ts = slice(t * 128, (t + 1) * 128)
                P.op("dve", lambda e: e.tensor_tensor(out=z[:], in0=u_[:, 0, :], in1=dsk[:], op=ALU.mult), r=[ru_, rdsk], w=[rz])
                P.op("dve", lambda e: e.tensor_tensor(out=z[:], in0=z[:], in1=u_[:, 1, :], op=ALU.add), r=[rz, ru_], w=[rz])
                P.op("dve", lambda e: e.tensor_tensor(out=z[:], in0=z[:], in1=u_[:, 2, :], op=ALU.add), r=[rz, ru_], w=[rz])
                P.op("act", lambda e: e.activation(out=zg[:], in_=z[:], func=AF.Gelu), r=[rz], w=[rzg])
                P.op("dve", lambda e: e.tensor_copy(out=zgb[:], in_=zg[:]), r=[rzg], w=[rzgb])
                for k in range(2):
                    P.op("pe", lambda e: e.transpose(out=pT2[:, k, :], in_=zgb[:, k * 128:(k + 1) * 128], identity=ident[:]), r=[rzgb, rident], w=[rpT2])
                P.op("act", lambda e: e.copy(out=zT[:], in_=pT2), r=[rpT2], w=[rzT])
                for k in range(2):
                    P.op("pe", lambda e: e.matmul(gp_, lhsT=zT[:, k, :], rhs=glw[:, k, :], start=(k == 0), stop=(k == 1)), r=[rzT, rglw], w=[rgp_])
                P.op("dve", lambda e: e.tensor_tensor(out=sg[:], in0=gp_, in1=glb[:], op=ALU.add), r=[rgp_, rglb], w=[rsg])
                P.op("act", lambda e: e.activation(out=sg[:], in_=sg[:], func=AF.Sigmoid), r=[rsg], w=[rsg])
                P.op("dve", lambda e: e.tensor_tensor(out=ob[:], in0=sg[:], in1=zg[:], op=ALU.mult), r=[rsg, rzg], w=[rob])
                for k in range(2):
                    P.op("pe", lambda e: e.transpose(out=pT3[:, k, :], in_=ob[:, k * 128:(k + 1) * 128], identity=ident[:]), r=[rob, rident], w=[rpT3])
                P.op("act", lambda e: e.copy(out=oT[:], in_=pT3), r=[rpT3], w=[roT])
                P.dma(STQ, Dm["yT"][512:768, :].rearrange("(k p) n -> p k n", p=128)[:, :, ts], oT[:], r=[roT], w=[R["y"]])
    P.pe_relaxed = False
    P.barrier()


SCRATCH.update({"s5yf": ([T, 256], F32), "s5yb": ([T, 256], F32)})


def stage_merge(K, l, with_ctx, tiles=None):
    nc, P, Dm, R = K.nc, K.P, K.dram, K.R
    P.pe_relaxed = True
    with contextlib.ExitStack() as es:
        wg, rwg = _tile(es, nc, "M_wg", [128, 8, 4096], BF16)
        wb, rwb = _tile(es, nc, "M_wb", [128, 8, 1024], BF16)
        wo, rwo = _tile(es, nc, "M_wo", [128, 8, 1024], BF16)
        m2, rm2 = _tile(es, nc, "M_m2", [128, 2, 1024], F32)
        ident, rident = _tile(es, nc, "M_ident", [128, 128], BF16)
        yTt = [_tile(es, nc, f"M_yT{i}", [128, 8, 128], BF16) for i in range(2)]
        aTt = [_tile(es, nc, f"M_aT{i}", [128, 8, 128], BF16) for i in range(2)]
        ht = [_tile(es, nc, f"M_h{i}", [128, 1024], F32) for i in range(2)]
        sig = [_tile(es, nc, f"M_sig{i}", [128, 512], F32) for i in range(2)]
        term, rterm = _tile(es, nc, "M_term", [128, 512], F32)
        mg, rmg = _tile(es, nc, "M_mg", [128, 1024], F32)
        mb, rmb = _tile(es, nc, "M_mb", [128, 1024], BF16)
        mT, rmT = _tile(es, nc, "M_mT", [128, 8, 128], BF16)
        hn, rhn = _tile(es, nc, "M_hn", [128, 1024], F32)
        Gp = [_tile(es, nc, f"M_Gp{i}", [128, 512], F32, psum=True) for i in range(2)]
        Zp = [_tile(es, nc, f"M_Zp{i}", [128, 512], F32, psum=True) for i in range(2)]
        pT, rpT = _tile(es, nc, "M_pT", [128, 8, 128], BF16, psum=True)
        Op = [_tile(es, nc, f"M_Op{i}", [128, 512], F32, psum=True) for i in range(2)]
        w_in_v = Dm["w_in"][l].rearrange("(k p) n -> p k n", p=128)
        for i in range(4):
            P.dma("pool", wg[:, :, i * 1024:(i + 1) * 1024], w_in_v[:, :, MIXC + i * 1024:MIXC + (i + 1) * 1024], w=[rwg])
        P.dma("pool", wb[:], Dm["w_branch"][l].rearrange("i (k p) n -> p (i k) n", p=128), w=[rwb])
        P.dma("pool", wo[:], Dm["w_out"][l].rearrange("(k p) n -> p k n", p=128), w=[rwo])
        P.dma("pool", ident[:], Dm["ident"], w=[rident])
        P.dma("sp", m2[:, 0, :], Dm["modv"][2].partition_broadcast(128), r=[R["modv"]], w=[rm2])
        P.dma("sp", m2[:, 1, :], Dm["modv"][8].partition_broadcast(128), r=[R["modv"]], w=[rm2])
        tiles = tiles if tiles is not None else list(range(NXT)) + ([64, 65] if with_ctx else [])
        hsrc = K.hsrc(l)

        def load(i):
            t = tiles[i]
            ts = slice(t * 128, (t + 1) * 128)
            P.dma("sp", yTt[i % 2][0][:], Dm["yT"].rearrange("(k p) n -> p k n", p=128)[:, :, ts], r=[R["y"]], w=[yTt[i % 2][1]])
            P.dma("sp", aTt[i % 2][0][:], Dm["aT"].rearrange("(k p) n -> p k n", p=128)[:, :, ts], r=[R["aT"]], w=[aTt[i % 2][1]])
            P.dma("sp", ht[i % 2][0][:], hsrc(t), r=[R["H"]], w=[ht[i % 2][1]])
        load(0)
        for i, t in enumerate(tiles):
            if i + 1 < len(tiles):
                load(i + 1)
            yt, ryt = yTt[i % 2]
            at, rat = aTt[i % 2]
            h_, rh_ = ht[i % 2]
            ts = slice(t * 128, (t + 1) * 128)
            cnt = 0
            for nh in range(2):
                for br in range(4):
                    gp, rgp = Gp[cnt % 2]
                    zp, rzp = Zp[cnt % 2]
                    sg, rsg = sig[cnt % 2]
                    cnt += 1
                    c0 = br * 1024 + nh * 512
                    for k in range(8):
                        P.op("pe", lambda e: e.matmul(gp, lhsT=at[:, k, :], rhs=wg[:, k, c0:c0 + 512], start=(k == 0), stop=(k == 7)), r=[rat, rwg], w=[rgp])
                    for k2 in range(2):
                        P.op("pe", lambda e: e.matmul(zp, lhsT=yt[:, 2 * br + k2, :], rhs=wb[:, 2 * br + k2, nh * 512:(nh + 1) * 512], start=(k2 == 0), stop=(k2 == 1)),
                             r=[ryt, rwb], w=[rzp])
                    P.op("act", lambda e: e.activation(out=sg[:], in_=gp, func=AF.Sigmoid), r=[rgp], w=[rsg])
                    dst = mg[:, nh * 512:(nh + 1) * 512]
                    if br == 0:
                        P.op("dve", lambda e: e.tensor_tensor(out=dst, in0=zp, in1=sg[:], op=ALU.mult), r=[rzp, rsg], w=[rmg])
                    else:
                        P.op("dve", lambda e: e.tensor_tensor(out=term[:], in0=zp, in1=sg[:], op=ALU.mult), r=[rzp, rsg], w=[rterm])
                        P.op("dve", lambda e: e.tensor_tensor(out=dst, in0=dst, in1=term[:], op=ALU.add), r=[rmg, rterm], w=[rmg])
            P.op("act", lambda e: e.copy(out=mb[:], in_=mg[:]), r=[rmg], w=[rmb])
            for k in range(8):
                P.op("pe", lambda e: e.transpose(out=pT[:, k, :], in_=mb[:, k * 128:(k + 1) * 128], identity=ident[:]), r=[rmb, rident], w=[rpT])
            P.op("act", lambda e: e.copy(out=mT[:], in_=pT), r=[rpT], w=[rmT])
            for nh in range(2):
                op_, rop = Op[nh]
                for k in range(8):
                    P.op("pe", lambda e: e.matmul(op_, lhsT=mT[:, k, :], rhs=wo[:, k, nh * 512:(nh + 1) * 512], start=(k == 0), stop=(k == 7)), r=[rmT, rwo], w=[rop])
                sl = slice(nh * 512, (nh + 1) * 512)
                P.op("dve", lambda e: e.tensor_tensor(out=hn[:, sl], in0=op_, in1=m2[:, 0 if t < NXT else 1, sl], op=ALU.mult), r=[rop, rm2], w=[rhn])
            P.op("dve", lambda e: e.tensor_tensor(out=hn[:], in0=hn[:], in1=h_[:], op=ALU.add), r=[rhn, rh_], w=[rhn])
            P.dma(STQ, Dm["H"][ts, :], hn[:], r=[rhn], w=[R["H"]])
    P.pe_relaxed = False
    P.barrier()


SGT = 12
BIG = 1.0e30


def stage_moe(K, l, with_ctx, last, tiles=None, experts=None):
    nc, P, Dm, R = K.nc, K.P, K.dram, K.R
    tiles = tiles if tiles is not None else list(range(NXT)) + ([64, 65] if with_ctx else [])
    experts = list(range(32)) if experts is None else experts
    with contextlib.ExitStack() as es:
        bc, rbc = _tile(es, nc, "E_bc", [128, 6, 1024], F32)
        identf, ridentf = _tile(es, nc, "E_identf", [128, 128], F32)
        rw, rrw = _tile(es, nc, "E_rw", [128, 8, 36], F32)
        rb, rrb = _tile(es, nc, "E_rb", [128, 36], F32)
        epsc, repsc = _tile(es, nc, "E_eps", [128, 1], F32)
        FT, rFT = _tile(es, nc, "E_FT", [128, 8, SGT * 128], BF16)
        Gall, rGall = _tile(es, nc, "E_G", [128, SGT, 32], F32)
        yacc, ryacc = _tile(es, nc, "E_yacc", [128, SGT, 1024], F32)
        for j, row in enumerate((3, 4, 5, 9, 10, 11)):
            P.dma("sp", bc[:, j, :], Dm["modv"][row].partition_broadcast(128), r=[R["modv"]], w=[rbc])
        P.dma("sp", identf[:], Dm["ident"], w=[ridentf])
        with nc.allow_non_contiguous_dma(reason="tiny router weights"):
            P.dma("sp", rw[:, :, 0:4], Dm["router_w1"][l].rearrange("(k p) n -> p k n", p=128), w=[rrw])
            P.dma("sp", rw[:, :, 4:36], Dm["router_w2"][l].rearrange("(k p) n -> p k n", p=128), w=[rrw])
        P.dma("sp", rb[:, 0:4], Dm["router_b1"][l].partition_broadcast(128), w=[rrb])
        P.dma("sp", rb[:, 4:36], Dm["router_b2"][l].partition_broadcast(128), w=[rrb])
        P.op("dve", lambda e: e.memset(epsc[:], EPS), w=[repsc])
        P.barrier()
        sgs = [tiles[i:i + SGT] for i in range(0, len(tiles), SGT)]
        for sg in sgs:
            with contextlib.ExitStack() as e1:
                ht = [_tile(e1, nc, f"E1_h{i}", [128, 1024], F32) for i in range(2)]
                junk, rjunk = _tile(e1, nc, "E1_junk", [128, 1024], BF16)
                ssq, rssq = _tile(e1, nc, "E1_ssq", [128, 1], F32)
                rstd, rrstd = _tile(e1, nc, "E1_rstd", [128, 1], F32)
                f_, rf_ = _tile(e1, nc, "E1_f", [128, 1024], F32)
                lgt, rlgt = _tile(e1, nc, "E1_lg", [128, 36], F32)
                sm = [_tile(e1, nc, f"E1_s{i}", [128, 8], F32) for i in range(4)]
                oh = [_tile(e1, nc, f"E1_oh{i}", [128, 32], F32) for i in range(3)]
                pTf = [_tile(e1, nc, f"E1_pT{i}", [128, 4, 128], F32, psum=True) for i in range(2)]
                lp, rlp = _tile(e1, nc, "E1_lp", [128, 36], F32, psum=True)

                def load(i):
                    t = sg[i]
                    P.dma("sp", ht[i % 2][0][:], Dm["H"][t * 128:(t + 1) * 128, :], r=[R["H"]], w=[ht[i % 2][1]])
                load(0)
                for i, t in enumerate(sg):
                    if i + 1 < len(sg):
                        load(i + 1)
                    h_, rh_ = ht[i % 2]
                    o = 0 if t < NXT else 3
                    P.op("act", lambda e: e.activation(out=junk[:], in_=h_[:], func=AF.Square, accum_out=ssq[:]), r=[rh_], w=[rjunk, rssq])
                    P.op("act", lambda e: e.activation(out=rstd[:], in_=ssq[:], func=AF.Sqrt, scale=1.0 / D, bias=epsc[:, 0:1]), r=[rssq, repsc], w=[rrstd])
                    P.op("dve", lambda e: e.reciprocal(out=rstd[:], in_=rstd[:]), r=[rrstd], w=[rrstd])
                    P.op("dve", lambda e: e.scalar_tensor_tensor(out=f_[:], in0=h_[:], scalar=rstd[:, 0:1], in1=bc[:, o, :], op0=ALU.mult, op1=ALU.mult),
                         r=[rh_, rrstd, rbc], w=[rf_])
                    P.op("dve", lambda e: e.tensor_tensor(out=f_[:], in0=f_[:], in1=bc[:, o + 1, :], op=ALU.add), r=[rf_, rbc], w=[rf_])
                    for hf in range(2):
                        pt, rpt = pTf[hf]
                        for k in range(4):
                            kk = hf * 4 + k
                            P.op("pe", lambda e: e.transpose(out=pt[:, k, :], in_=f_[:, kk * 128:(kk + 1) * 128], identity=identf[:]), r=[rf_, ridentf], w=[rpt])
                    fTf, rfTf = f_, rf_
                    for hf in range(2):
                        pt, rpt = pTf[hf]
                        P.op("act", lambda e: e.copy(out=fTf[:, hf * 512:(hf + 1) * 512].rearrange("p (k n) -> p k n", n=128), in_=pt), r=[rpt], w=[rfTf])
                    P.op("dve", lambda e: e.tensor_copy(out=FT[:, :, i * 128:(i + 1) * 128], in_=fTf[:].rearrange("p (k n) -> p k n", n=128)), r=[rfTf], w=[rFT])
                    for k in range(8):
                        P.op("pe", lambda e: e.matmul(lp, lhsT=fTf[:, k * 128:(k + 1) * 128], rhs=rw[:, k, :], start=(k == 0), stop=(k == 7)), r=[rfTf, rrw], w=[rlp])
                    P.op("dve", lambda e: e.tensor_tensor(out=lgt[:], in0=lp, in1=rb[:], op=ALU.add), r=[rlp, rrb], w=[rlgt])
                    (s0, rs0), (s1, rs1), (s2, rs2), (s3, rs3) = sm
                    (oh1, roh1), (oh2, roh2), (l2, rl2) = oh
                    P.op("dve", lambda e: e.tensor_reduce(out=s0[:, 0:1], in_=lgt[:, 0:4], axis=AX.X, op=ALU.max), r=[rlgt], w=[rs0])
                    P.op("dve", lambda e: e.tensor_scalar(out=s0[:, 1:2], in0=s0[:, 0:1], scalar1=-1.0, scalar2=None, op0=ALU.mult), r=[rs0], w=[rs0])
                    P.op("act", lambda e: e.activation(out=s1[:, 0:4], in_=lgt[:, 0:4], func=AF.Exp, bias=s0[:, 1:2], accum_out=s0[:, 2:3]), r=[rlgt, rs0], w=[rs1, rs0])
                    P.op("dve", lambda e: e.reciprocal(out=s0[:, 3:4], in_=s0[:, 2:3]), r=[rs0], w=[rs0])
                    P.op("dve", lambda e: e.tensor_scalar(out=s2[:, 0:4], in0=lgt[:, 0:4], scalar1=s0[:, 0:1], scalar2=None, op0=ALU.is_equal), r=[rlgt, rs0], w=[rs2])
                    P.op("dve", lambda e: e.tensor_scalar(out=s2[:, 0:4], in0=s2[:, 0:4], scalar1=BIG, scalar2=-BIG, op0=ALU.mult, op1=ALU.add), r=[rs2], w=[rs2])
                    P.op("dve", lambda e: e.tensor_tensor(out=l2[:].rearrange("p (g e) -> p g e", e=8), in0=lgt[:, 4:36].rearrange("p (g e) -> p g e", e=8),
                                                          in1=s2[:, 0:4].unsqueeze(2).to_broadcast([128, 4, 8]), op=ALU.add), r=[rlgt, rs2], w=[rl2])
                    P.op("dve", lambda e: e.tensor_reduce(out=s3[:, 0:1], in_=l2[:], axis=AX.X, op=ALU.max), r=[rl2], w=[rs3])
                    P.op("dve", lambda e: e.tensor_scalar(out=oh1[:], in0=l2[:], scalar1=s3[:, 0:1], scalar2=None, op0=ALU.is_equal), r=[rl2, rs3], w=[roh1])
                    P.op("dve", lambda e: e.scalar_tensor_tensor(out=l2[:], in0=oh1[:], scalar=-BIG, in1=l2[:], op0=ALU.mult, op1=ALU.add), r=[roh1, rl2], w=[rl2])
                    P.op("dve", lambda e: e.tensor_reduce(out=s3[:, 1:2], in_=l2[:], axis=AX.X, op=ALU.max), r=[rl2], w=[rs3])
                    P.op("dve", lambda e: e.tensor_scalar(out=oh2[:], in0=l2[:], scalar1=s3[:, 1:2], scalar2=None, op0=ALU.is_equal), r=[rl2, rs3], w=[roh2])
                    P.op("dve", lambda e: e.tensor_tensor(out=s3[:, 2:3], in0=s3[:, 1:2], in1=s3[:, 0:1], op=ALU.subtract), r=[rs3], w=[rs3])
                    P.op("act", lambda e: e.activation(out=s3[:, 3:4], in_=s3[:, 2:3], func=AF.Exp), r=[rs3], w=[rs3])
                    P.op("dve", lambda e: e.tensor_scalar(out=s3[:, 4:5], in0=s3[:, 3:4], scalar1=1.0, scalar2=None, op0=ALU.add), r=[rs3], w=[rs3])
                    P.op("dve", lambda e: e.reciprocal(out=s3[:, 4:5], in_=s3[:, 4:5]), r=[rs3], w=[rs3])
                    P.op("dve", lambda e: e.tensor_tensor(out=s3[:, 5:6], in0=s3[:, 4:5], in1=s0[:, 3:4], op=ALU.mult), r=[rs3, rs0], w=[rs3])
                    P.op("dve", lambda e: e.tensor_tensor(out=s3[:, 6:7], in0=s3[:, 5:6], in1=s3[:, 3:4], op=ALU.mult), r=[rs3], w=[rs3])
                    P.op("dve", lambda e: e.tensor_scalar(out=Gall[:, i, :], in0=oh1[:], scalar1=s3[:, 5:6], scalar2=None, op0=ALU.mult), r=[roh1, rs3], w=[rGall])
                    P.op("dve", lambda e: e.scalar_tensor_tensor(out=Gall[:, i, :], in0=oh2[:], scalar=s3[:, 6:7], in1=Gall[:, i, :], op0=ALU.mult, op1=ALU.add),
                         r=[roh2, rs3, rGall], w=[rGall])
            P.barrier()
            with contextlib.ExitStack() as e2:
                w1 = [_tile(e2, nc, f"E2_w1_{i}", [128, 8, 512], BF16) for i in range(2)]
                w3 = [_tile(e2, nc, f"E2_w3_{i}", [128, 8, 512], BF16) for i in range(2)]
                w2 = [_tile(e2, nc, f"E2_w2_{i}", [128, 4, 1024], BF16) for i in range(2)]
                sl = [_tile(e2, nc, f"E2_sl{i}", [128, 512], F32) for i in range(2)]
                hid = [_tile(e2, nc, f"E2_hid{i}", [128, 4, 512], BF16) for i in range(2)]
                H1 = [_tile(e2, nc, f"E2_H1{i}", [128, 512], F32, psum=True) for i in range(2)]
                H3 = [_tile(e2, nc, f"E2_H3{i}", [128, 512], F32, psum=True) for i in range(2)]
                Yp = [[_tile(e2, nc, f"E2_Y{i}{j}", [128, 512], F32, psum=True) for j in range(2)] for i in range(2)]
                groups = [list(range(g0, min(g0 + 4, len(sg)))) for g0 in range(0, len(sg), 4)]

                def loadw(ei):
                    e_ = experts[ei]
                    P.dma("pool", w1[ei % 2][0][:], Dm["exp_w1"][l, e_].rearrange("(k p) n -> p k n", p=128), w=[w1[ei % 2][1]])
                    P.dma("pool", w3[ei % 2][0][:], Dm["exp_w3"][l, e_].rearrange("(k p) n -> p k n", p=128), w=[w3[ei % 2][1]])
                    P.dma("pool", w2[ei % 2][0][:], Dm["exp_w2"][l, e_].rearrange("(k p) n -> p k n", p=128), w=[w2[ei % 2][1]])
                loadw(0)
                cnt = 0
                ycnt = 0
                gi_ = 0
                for ei, e_ in enumerate(experts):
                    if ei + 1 < len(experts):
                        loadw(ei + 1)
                    (w1t, rw1), (w3t, rw3), (w2t, rw2) = w1[ei % 2], w3[ei % 2], w2[ei % 2]
                    for grp in groups:
                        ntok = len(grp) * 128
                        tsl = slice(grp[0] * 128, grp[0] * 128 + ntok)
                        hd, rhd = hid[gi_ % 2]
                        gi_ += 1
                        for fc in range(4):
                            h1, rh1 = H1[cnt % 2]
                            h3, rh3 = H3[cnt % 2]
                            s_, rs_ = sl[cnt % 2]
                            cnt += 1
                            for k in range(8):
                                P.op("pe", lambda e: e.matmul(h1[:, 0:ntok], lhsT=w1t[:, k, fc * 128:(fc + 1) * 128], rhs=FT[:, k, tsl], start=(k == 0), stop=(k == 7)),
                                     r=[rw1, rFT], w=[rh1])
                            for k in range(8):
                                P.op("pe", lambda e: e.matmul(h3[:, 0:ntok], lhsT=w3t[:, k, fc * 128:(fc + 1) * 128], rhs=FT[:, k, tsl], start=(k == 0), stop=(k == 7)),
                                     r=[rw3, rFT], w=[rh3])
                            P.op("act", lambda e: e.activation(out=s_[:, 0:ntok], in_=h1[:, 0:ntok], func=AF.Silu), r=[rh1], w=[rs_])
                            P.op("dve", lambda e: e.tensor_tensor(out=hd[:, fc, 0:ntok], in0=h3[:, 0:ntok], in1=s_[:, 0:ntok], op=ALU.mult), r=[rh3, rs_], w=[rhd])
                        for ti, tt in enumerate(grp):
                            yp = Yp[ycnt % 2]
                            ycnt += 1
                            for nh in range(2):
                                ypt, rypt = yp[nh]
                                for fc in range(4):
                                    P.op("pe", lambda e: e.matmul(ypt, lhsT=hd[:, fc, ti * 128:(ti + 1) * 128], rhs=w2t[:, fc, nh * 512:(nh + 1) * 512], start=(fc == 0), stop=(fc == 3)),
                                         r=[rhd, rw2], w=[rypt])
                                dst = yacc[:, tt, nh * 512:(nh + 1) * 512]
                                if ei == 0:
                                    P.op("dve", lambda e: e.tensor_scalar(out=dst, in0=ypt, scalar1=Gall[:, tt, e_:e_ + 1], scalar2=None, op0=ALU.mult), r=[rypt, rGall], w=[ryacc])
                                else:
                                    P.op("dve", lambda e: e.scalar_tensor_tensor(out=dst, in0=ypt, scalar=Gall[:, tt, e_:e_ + 1], in1=dst, op0=ALU.mult, op1=ALU.add),
                                         r=[rypt, rGall, ryacc], w=[ryacc])
            P.barrier()
            with contextlib.ExitStack() as e3:
                ht = [_tile(e3, nc, f"E3_h{i}", [128, 1024], F32) for i in range(2)]
                hn = [_tile(e3, nc, f"E3_hn{i}", [128, 1024], F32) for i in range(2)]
                for i, t in enumerate(sg):
                    h_, rh_ = ht[i % 2]
                    n_, rn_ = hn[i % 2]
                    ts = slice(t * 128, (t + 1) * 128)
                    P.dma("sp", h_[:], Dm["H"][ts, :], r=[R["H"]], w=[rh_])
                    P.op("dve", lambda e: e.tensor_tensor(out=n_[:], in0=yacc[:, i, :], in1=bc[:, 2 if t < NXT else 5, :], op=ALU.mult), r=[ryacc, rbc], w=[rn_])
                    P.op("dve", lambda e: e.tensor_tensor(out=n_[:], in0=n_[:], in1=h_[:], op=ALU.add), r=[rn_, rh_], w=[rn_])
                    if last:
                        P.dma("pool", Dm["out"][ts, :], n_[:], r=[rn_], w=[R["out"]])
                    else:
                        P.dma("pool", Dm["H"][ts, :], n_[:], r=[rn_], w=[R["H"]])
            P.barrier()
    P.barrier()


def full_plan(nlayers=DEPTH):
    def plan(K):
        for l in range(nlayers):
            with_ctx = l < DEPTH - 1
            last = l == DEPTH - 1
            stage_prep(K, l)
            stage_A(K, l)
            stage_ret(K, l, with_ctx)
            stage_na(K, l, with_ctx)
            stage_s5(K, l, with_ctx, precast=True)
            stage_gqa(K, l, with_ctx)
            stage_merge(K, l, with_ctx)
            stage_moe_sparse(K, l, with_ctx, last, precast=False)
    return plan


_CACHE = {}


def kernel(**inputs):
    consts = make_consts()
    n = 8
    x = np.asarray(inputs["x"], np.float32)
    in_maps = []
    shared = {k: np.ascontiguousarray(np.asarray(inputs[k], np.float32)) for k in INPUT_NAMES if k not in ("x", "c", "ctx")}
    for b in range(n):
        m = dict(shared)
        m["x"] = np.ascontiguousarray(x[b])
        m["ctx"] = np.ascontiguousarray(np.asarray(inputs["ctx"], np.float32)[b])
        m["c"] = np.ascontiguousarray(np.asarray(inputs["c"], np.float32)[b:b + 1])
        m.update(consts)
        in_maps.append(m)
    shapes = {k: (v.shape, F32) for k, v in in_maps[0].items() if k not in consts}
    if "nc" not in _CACHE:
        _CACHE["nc"] = build(shapes, consts, full_plan())[0]
    res = run_bass_kernel_spmd(_CACHE["nc"], in_maps, core_ids=list(range(n)))
    return np.stack([np.asarray(r["out"], np.float32) for r in res.results], axis=0)


def _idma(P, out, out_off, in_, in_off, r=(), w=()):
    q = "pool"
    P._deps(q, r, w)
    i = P.dnext[q]
    P.dnext[q] = (i + 1) % P.NDS
    key = ("d", q, i)
    P._wait(q, key, 16 * P.duse[q][i])
    ins = P.nc.gpsimd.indirect_dma_start(out=out, out_offset=out_off, in_=in_, in_offset=in_off)
    P.duse[q][i] += 1
    ins.then_inc(P.semh[key], 16)
    P._mark((key, 16 * P.duse[q][i]), r, w)
    P.ninst += 1
    return ins


def make_precast(K, l, es):
    nc, P, Dm = K.nc, K.P, K.dram
    st = [_tile(es, nc, f"PC_st{i}", [128, 4096], F32) for i in range(2)]
    sb = [_tile(es, nc, f"PC_sb{i}", [128, 4096], BF16) for i in range(2)]
    jobs = [(e_, src, dst, kk) for e_ in range(32) for (src, dst, kk) in (("exp_w1", "wb1", 8), ("exp_w3", "wb3", 8), ("exp_w2", "wb2", 4))]
    state = {"i": 0}
    rwb = K.R["wbf"]

    def step(n):
        for _ in range(n):
            i = state["i"]
            if i >= len(jobs):
                return
            state["i"] = i + 1
            e_, src, dst, kk = jobs[i]
            s_, rs_ = st[i % 2]
            b_, rb_ = sb[i % 2]
            P.dma("sp", s_[:].rearrange("p (k n) -> p k n", k=kk), Dm[src][l, e_].rearrange("(k p) n -> p k n", p=128), w=[rs_])
            P.op("act", lambda e: e.copy(out=b_[:], in_=s_[:]), r=[rs_], w=[rb_])
            P.dma("act", Dm[dst][e_ * 128:(e_ + 1) * 128, :], b_[:], r=[rb_], w=[rwb])
    return step


def stage_moe_sparse(K, l, with_ctx, last, precast=True, nblocks=None):
    nc, P, Dm, R = K.nc, K.P, K.dram, K.R
    tiles = list(range(NXT)) + ([64, 65] if with_ctx else [])
    NTL = len(tiles)
    B = MOE_B
    NB = -(-(2 * NTL * 128 + 32 * (B - 1)) // B)
    IOA = bass.IndirectOffsetOnAxis
    rwb = K.R["wbf"]
    rfb = Res("fb")
    rxs = Res("xs")
    rys = Res("ys")
    P.pe_relaxed = True
    if precast:
        with contextlib.ExitStack() as e0:
            st = [_tile(e0, nc, f"E0_st{i}", [128, 4096], F32) for i in range(3)]
            sb = [_tile(e0, nc, f"E0_sb{i}", [128, 4096], BF16) for i in range(3)]
            cnt = 0
            for e_ in range(32):
                for src, dst, kk in (("exp_w1", "wb1", 8), ("exp_w3", "wb3", 8), ("exp_w2", "wb2", 4)):
                    s_, rs_ = st[cnt % 3]
                    b_, rb_ = sb[cnt % 3]
                    eng = ("act", "pool", "dve")[cnt % 3]
                    cnt += 1
                    P.dma("sp", s_[:].rearrange("p (k n) -> p k n", k=kk), Dm[src][l, e_].rearrange("(k p) n -> p k n", p=128), w=[rs_])
                    if eng == "act":
                        P.op("act", lambda e: e.copy(out=b_[:], in_=s_[:]), r=[rs_], w=[rb_])
                    else:
                        P.op(eng, lambda e: e.tensor_copy(out=b_[:], in_=s_[:]), r=[rs_], w=[rb_])
                    P.dma("act", Dm[dst][e_ * 128:(e_ + 1) * 128, :], b_[:], r=[rb_], w=[rwb])
        P.barrier()
    with contextlib.ExitStack() as es:
        bc, rbc = _tile(es, nc, "Q_bc", [128, 6, 1024], F32)
        OH1, rOH1 = _tile(es, nc, "Q_OH1", [128, NTL, 32], F32)
        OH2, rOH2 = _tile(es, nc, "Q_OH2", [128, NTL, 32], F32)
        OH12, rOH12 = _tile(es, nc, "Q_OH12", [128, NTL, 32], BF16)
        GA, rGA = _tile(es, nc, "Q_GA", [128, NTL, 2], F32)
        DST, rDST = _tile(es, nc, "Q_DST", [128, NTL, 2], I32)
        IDXW, rIDXW = _tile(es, nc, "Q_IDXW", [128, NB], I32)
        pstart, rpstart = _tile(es, nc, "Q_pstart", [128, 32], F32)
        onesb, ronesb = _tile(es, nc, "Q_ones", [128, 128], BF16)
        ustr, rustr = _tile(es, nc, "Q_ustr", [128, 128], BF16)
        identb, ridentb = _tile(es, nc, "Q_identb", [128, 128], BF16)
        for j, row in enumerate((3, 4, 5, 9, 10, 11)):
            P.dma("sp", bc[:, j, :], Dm["modv"][row].partition_broadcast(128), r=[R["modv"]], w=[rbc])
        P.dma("pool", onesb[:], Dm["moe_ones"], w=[ronesb])
        P.dma("pool", ustr[:], Dm["moe_ustrict"], w=[rustr])
        P.dma("pool", identb[:], Dm["ident"], w=[ridentb])
        with contextlib.ExitStack() as e1:
            identf, ridentf = _tile(e1, nc, "Q1_identf", [128, 128], F32)
            rw, rrw = _tile(e1, nc, "Q1_rw", [128, 8, 36], F32)
            rb, rrb = _tile(e1, nc, "Q1_rb", [128, 36], F32)
            epsc, repsc = _tile(e1, nc, "Q1_eps", [128, 1], F32)
            ht = [_tile(e1, nc, f"Q1_h{i}", [128, 1024], F32) for i in range(2)]
            junk, rjunk = _tile(e1, nc, "Q1_junk", [128, 1024], BF16)
            ssq, rssq = _tile(e1, nc, "Q1_ssq", [128, 1], F32)
            rstd, rrstd = _tile(e1, nc, "Q1_rstd", [128, 1], F32)
            f_, rf_ = _tile(e1, nc, "Q1_f", [128, 1024], F32)
            fb = [_tile(e1, nc, f"Q1_fb{i}", [128, 1024], BF16) for i in range(2)]
            lgt, rlgt = _tile(e1, nc, "Q1_lg", [128, 36], F32)
            sm = [_tile(e1, nc, f"Q1_s{i}", [128, 8], F32) for i in range(4)]
            l2, rl2 = _tile(e1, nc, "Q1_l2", [128, 32], F32)
            cnts, rcnts = _tile(e1, nc, "Q1_cnt", [128, 32], F32)
            wk = [_tile(e1, nc, f"Q1_wk{i}", [128, 32], F32) for i in range(3)]
            cmpt, rcmpt = _tile(e1, nc, "Q1_cmp", [128, NB, 32], F32)
            bbt, rbbt = _tile(e1, nc, "Q1_bb", [128, NB, 32], F32)
            eblk, reblk = _tile(e1, nc, "Q1_eblk", [128, NB], F32)
            pcol, rpcol = _tile(e1, nc, "Q1_pcol", [128, 1], F32)
            pTf = [_tile(e1, nc, f"Q1_pT{i}", [128, 4, 128], F32, psum=True) for i in range(2)]
            lp, rlp = _tile(e1, nc, "Q1_lp", [128, 36], F32, psum=True)
            cp, rcp = _tile(e1, nc, "Q1_cp", [128, 32], F32, psum=True)
            P.dma("sp", identf[:], Dm["ident"], w=[ridentf])
            with nc.allow_non_contiguous_dma(reason="tiny router weights"):
                P.dma("sp", rw[:, :, 0:4], Dm["router_w1"][l].rearrange("(k p) n -> p k n", p=128), w=[rrw])
                P.dma("sp", rw[:, :, 4:36], Dm["router_w2"][l].rearrange("(k p) n -> p k n", p=128), w=[rrw])
            P.dma("sp", rb[:, 0:4], Dm["router_b1"][l].partition_broadcast(128), w=[rrb])
            P.dma("sp", rb[:, 4:36], Dm["router_b2"][l].partition_broadcast(128), w=[rrb])
            P.dma("sp", bbt[:].rearrange("p a b -> p (a b)"), Dm["moe_bb"][:, 0:NB * 32], w=[rbbt])
            P.dma("sp", pcol[:], Dm["moe_pcol"], w=[rpcol])
            P.op("dve", lambda e: e.memset(epsc[:], EPS), w=[repsc])

            ht4 = ht + [_tile(e1, nc, f"Q1_hx{i}", [128, 1024], F32) for i in range(2)]
            junk2 = [(junk, rjunk), _tile(e1, nc, "Q1_junkb", [128, 1024], BF16)]
            ssq2 = [(ssq, rssq), _tile(e1, nc, "Q1_ssqb", [128, 1], F32)]
            rstd2 = [(rstd, rrstd), _tile(e1, nc, "Q1_rstdb", [128, 1], F32)]
            f2_ = [(f_, rf_), _tile(e1, nc, "Q1_fb_", [128, 1024], F32)]
            lgt2 = [(lgt, rlgt), _tile(e1, nc, "Q1_lgb", [128, 36], F32)]
            sm2 = [sm, [_tile(e1, nc, f"Q1_sb{i}", [128, 8], F32) for i in range(4)]]
            l22 = [(l2, rl2), _tile(e1, nc, "Q1_l2b", [128, 32], F32)]
            pTf2 = [pTf, [_tile(e1, nc, f"Q1_pTb{i}", [128, 4, 128], F32, psum=True) for i in range(2)]]
            lp2 = [(lp, rlp), _tile(e1, nc, "Q1_lpb", [128, 36], F32, psum=True)]

            def load(i):
                t = tiles[i]
                P.dma("sp", ht4[i % 4][0][:], Dm["H"][t * 128:(t + 1) * 128, :], r=[R["H"]], w=[ht4[i % 4][1]])

            def tile_ops(i):
                t = tiles[i]
                pq = i % 2
                h_, rh_ = ht4[i % 4]
                fb_, rfb_ = fb[i % 2]
                junk, rjunk = junk2[pq]
                ssq, rssq = ssq2[pq]
                rstd, rrstd = rstd2[pq]
                f_, rf_ = f2_[pq]
                lgt, rlgt = lgt2[pq]
                sm = sm2[pq]
                l2, rl2 = l22[pq]
                pTf = pTf2[pq]
                lp, rlp = lp2[pq]
                o = 0 if t < NXT else 3
                P.op("act", lambda e: e.activation(out=junk[:], in_=h_[:], func=AF.Square, accum_out=ssq[:]), r=[rh_], w=[rjunk, rssq])
                yield
                P.op("act", lambda e: e.activation(out=rstd[:], in_=ssq[:], func=AF.Sqrt, scale=1.0 / D, bias=epsc[:, 0:1]), r=[rssq, repsc], w=[rrstd])
                yield
                P.op("dve", lambda e: e.reciprocal(out=rstd[:], in_=rstd[:]), r=[rrstd], w=[rrstd])
                yield
                P.op("dve", lambda e: e.scalar_tensor_tensor(out=f_[:], in0=h_[:], scalar=rstd[:, 0:1], in1=bc[:, o, :], op0=ALU.mult, op1=ALU.mult),
                     r=[rh_, rrstd, rbc], w=[rf_])
                yield
                P.op("dve", lambda e: e.tensor_tensor(out=f_[:], in0=f_[:], in1=bc[:, o + 1, :], op=ALU.add), r=[rf_, rbc], w=[rf_])
                yield
                P.op("act", lambda e: e.copy(out=fb_[:], in_=f_[:]), r=[rf_], w=[rfb_])
                yield
                P.dma("act", Dm["fb"][i * 128:(i + 1) * 128, :], fb_[:], r=[rfb_], w=[rfb])
                yield
                for hf in range(2):
                    pt, rpt = pTf[hf]
                    for k in range(4):
                        kk = hf * 4 + k
                        P.op("pe", lambda e: e.transpose(out=pt[:, k, :], in_=f_[:, kk * 128:(kk + 1) * 128], identity=identf[:]), r=[rf_, ridentf], w=[rpt])
                        yield
                for hf in range(2):
                    pt, rpt = pTf[hf]
                    P.op("act", lambda e: e.copy(out=f_[:, hf * 512:(hf + 1) * 512].rearrange("p (k n) -> p k n", n=128), in_=pt), r=[rpt], w=[rf_])
                    yield
                for k in range(8):
                    P.op("pe", lambda e: e.matmul(lp, lhsT=f_[:, k * 128:(k + 1) * 128], rhs=rw[:, k, :], start=(k == 0), stop=(k == 7)), r=[rf_, rrw], w=[rlp])
                    yield
                P.op("dve", lambda e: e.tensor_tensor(out=lgt[:], in0=lp, in1=rb[:], op=ALU.add), r=[rlp, rrb], w=[rlgt])
                yield
                (s0, rs0), (s1, rs1), (s2, rs2), (s3, rs3) = sm
                oh1, oh2 = OH1[:, i, :], OH2[:, i, :]
                P.op("dve", lambda e: e.tensor_reduce(out=s0[:, 0:1], in_=lgt[:, 0:4], axis=AX.X, op=ALU.max), r=[rlgt], w=[rs0])
                yield
                P.op("dve", lambda e: e.tensor_scalar(out=s0[:, 1:2], in0=s0[:, 0:1], scalar1=-1.0, scalar2=None, op0=ALU.mult), r=[rs0], w=[rs0])
                yield
                P.op("act", lambda e: e.activation(out=s1[:, 0:4], in_=lgt[:, 0:4], func=AF.Exp, bias=s0[:, 1:2], accum_out=s0[:, 2:3]), r=[rlgt, rs0], w=[rs1, rs0])
                yield
                P.op("dve", lambda e: e.reciprocal(out=s0[:, 3:4], in_=s0[:, 2:3]), r=[rs0], w=[rs0])
                yield
                P.op("dve", lambda e: e.tensor_scalar(out=s2[:, 0:4], in0=lgt[:, 0:4], scalar1=s0[:, 0:1], scalar2=None, op0=ALU.is_equal), r=[rlgt, rs0], w=[rs2])
                yield
                P.op("dve", lambda e: e.tensor_scalar(out=s2[:, 0:4], in0=s2[:, 0:4], scalar1=BIG, scalar2=-BIG, op0=ALU.mult, op1=ALU.add), r=[rs2], w=[rs2])
                yield
                P.op("dve", lambda e: e.tensor_tensor(out=l2[:].rearrange("p (g e) -> p g e", e=8), in0=lgt[:, 4:36].rearrange("p (g e) -> p g e", e=8),
                                                      in1=s2[:, 0:4].unsqueeze(2).to_broadcast([128, 4, 8]), op=ALU.add), r=[rlgt, rs2], w=[rl2])
                yield
                P.op("dve", lambda e: e.tensor_reduce(out=s3[:, 0:1], in_=l2[:], axis=AX.X, op=ALU.max), r=[rl2], w=[rs3])
                yield
                P.op("dve", lambda e: e.tensor_scalar(out=oh1, in0=l2[:], scalar1=s3[:, 0:1], scalar2=None, op0=ALU.is_equal), r=[rl2, rs3], w=[rOH1])
                yield
                P.op("dve", lambda e: e.scalar_tensor_tensor(out=l2[:], in0=oh1, scalar=-BIG, in1=l2[:], op0=ALU.mult, op1=ALU.add), r=[rOH1, rl2], w=[rl2])
                yield
                P.op("dve", lambda e: e.tensor_reduce(out=s3[:, 1:2], in_=l2[:], axis=AX.X, op=ALU.max), r=[rl2], w=[rs3])
                yield
                P.op("dve", lambda e: e.tensor_scalar(out=oh2, in0=l2[:], scalar1=s3[:, 1:2], scalar2=None, op0=ALU.is_equal), r=[rl2, rs3], w=[rOH2])
                yield
                P.op("dve", lambda e: e.tensor_tensor(out=OH12[:, i, :], in0=oh1, in1=oh2, op=ALU.add), r=[rOH1, rOH2], w=[rOH12])
                yield
                P.op("dve", lambda e: e.tensor_tensor(out=s3[:, 2:3], in0=s3[:, 1:2], in1=s3[:, 0:1], op=ALU.subtract), r=[rs3], w=[rs3])
                yield
                P.op("act", lambda e: e.activation(out=s3[:, 3:4], in_=s3[:, 2:3], func=AF.Exp), r=[rs3], w=[rs3])
                yield
                P.op("dve", lambda e: e.tensor_scalar(out=s3[:, 4:5], in0=s3[:, 3:4], scalar1=1.0, scalar2=None, op0=ALU.add), r=[rs3], w=[rs3])
                yield
                P.op("dve", lambda e: e.reciprocal(out=s3[:, 4:5], in_=s3[:, 4:5]), r=[rs3], w=[rs3])
                yield
                P.op("dve", lambda e: e.tensor_tensor(out=GA[:, i, 0:1], in0=s3[:, 4:5], in1=s0[:, 3:4], op=ALU.mult), r=[rs3, rs0], w=[rGA])
                yield
                P.op("dve", lambda e: e.tensor_tensor(out=GA[:, i, 1:2], in0=GA[:, i, 0:1], in1=s3[:, 3:4], op=ALU.mult), r=[rGA, rs3], w=[rGA])
                yield
                P.op("pe", lambda e: e.matmul(cp, lhsT=onesb[:], rhs=OH12[:, i, :], start=(i == 0), stop=(i == NTL - 1)), r=[ronesb, rOH12], w=[rcp])
                yield
            for i0 in range(0, NTL, 2):
                if i0 == 0:
                    load(0)
                    if NTL > 1:
                        load(1)
                for j in (i0 + 2, i0 + 3):
                    if j < NTL:
                        load(j)
                gens = [tile_ops(j) for j in (i0, i0 + 1) if j < NTL]
                alive = list(gens)
                while alive:
                    for g_ in list(alive):
                        try:
                            next(g_)
                        except StopIteration:
                            alive.remove(g_)
            (a0, ra0), (a1, ra1), (a2, ra2) = wk
            P.op("dve", lambda e: e.tensor_copy(out=cnts[:], in_=cp), r=[rcp], w=[rcnts])
            P.op("dve", lambda e: e.tensor_scalar(out=a0[:], in0=cnts[:], scalar1=float(B - 1), scalar2=1.0 / B, op0=ALU.add, op1=ALU.mult), r=[rcnts], w=[ra0])
            P.op("dve", lambda e: e.tensor_scalar(out=a0[:], in0=a0[:], scalar1=-(B - 1) / (2.0 * B), scalar2=None, op0=ALU.add), r=[ra0], w=[ra0])
            P.op("dve", lambda e: e.tensor_scalar(out=a0[:], in0=a0[:], scalar1=MAGIC, scalar2=None, op0=ALU.add), r=[ra0], w=[ra0])
            P.op("dve", lambda e: e.tensor_scalar(out=a0[:], in0=a0[:], scalar1=MAGIC, scalar2=None, op0=ALU.subtract), r=[ra0], w=[ra0])
            P.op("dve", lambda e: e.tensor_scalar(out=a0[:], in0=a0[:], scalar1=float(B), scalar2=None, op0=ALU.mult), r=[ra0], w=[ra0])
            P.op("dve", lambda e: e.memset(a2[:], 1.0), w=[ra2])
            P.op("dve", lambda e: e.tensor_tensor_scan(out=a1[:], data0=a2[:], data1=a0[:], initial=0.0, op0=ALU.mult, op1=ALU.add), r=[ra2, ra0], w=[ra1])
            P.op("dve", lambda e: e.tensor_tensor(out=pstart[:], in0=a1[:], in1=a0[:], op=ALU.subtract), r=[ra1, ra0], w=[rpstart])
            P.op("dve", lambda e: e.tensor_tensor(out=cmpt[:], in0=a1[:].unsqueeze(1).to_broadcast([128, NB, 32]), in1=bbt[:], op=ALU.is_le), r=[ra1, rbbt], w=[rcmpt])
            P.op("dve", lambda e: e.tensor_reduce(out=eblk[:], in_=cmpt[:], axis=AX.X, op=ALU.add), r=[rcmpt], w=[reblk])
            P.op("dve", lambda e: e.tensor_scalar(out=eblk[:], in0=eblk[:], scalar1=31.0, scalar2=128.0, op0=ALU.min, op1=ALU.mult), r=[reblk], w=[reblk])
            P.op("dve", lambda e: e.tensor_scalar(out=eblk[:], in0=eblk[:], scalar1=pcol[:, 0:1], scalar2=None, op0=ALU.add), r=[reblk, rpcol], w=[reblk])
            P.op("dve", lambda e: e.tensor_copy(out=IDXW[:], in_=eblk[:]), r=[reblk], w=[rIDXW])
        P.barrier()
        with contextlib.ExitStack() as e3:
            base, rbase = _tile(e3, nc, "Q3_base", [128, 32], F32)
            sb_, rsb_ = _tile(e3, nc, "Q3_sb", [128, 32], F32)
            pr, rpr = _tile(e3, nc, "Q3_pr", [128, 32], F32)
            dd, rdd = _tile(e3, nc, "Q3_dd", [128, 2], F32)
            fbt = [_tile(e3, nc, f"Q3_fb{i}", [128, 1024], BF16) for i in range(2)]
            rp_, rrp_ = _tile(e3, nc, "Q3_rp", [128, 32], F32, psum=True)
            csp, rcsp = _tile(e3, nc, "Q3_cs", [128, 32], F32, psum=True)
            P.op("dve", lambda e: e.memset(base[:], 0.0), w=[rbase])
            zt, rzt = _tile(e3, nc, "Q3_zero", [128, 7, 1024], BF16)
            P.op("pool", lambda e: e.memset(zt[:], 0.0), w=[rzt])
            xsv = Dm["xs"][0:NB * B, :].rearrange("(p a) d -> p a d", p=128)
            na = NB * B // 128
            for a0_ in range(0, na, 7):
                a1_ = min(a0_ + 7, na)
                P.dma("sp" if (a0_ // 7) % 2 == 0 else "act", xsv[:, a0_:a1_, :], zt[:, 0:a1_ - a0_, :], r=[rzt], w=[rxs])
            for i in range(NTL):
                f2, rf2 = fbt[i % 2]
                P.dma("sp", f2[:], Dm["fb"][i * 128:(i + 1) * 128, :], r=[rfb], w=[rf2])
                P.op("pe", lambda e: e.matmul(rp_, lhsT=ustr[:], rhs=OH12[:, i, :], start=True, stop=True), r=[rustr, rOH12], w=[rrp_])
                P.op("pe", lambda e: e.matmul(csp, lhsT=onesb[:], rhs=OH12[:, i, :], start=True, stop=True), r=[ronesb, rOH12], w=[rcsp])
                P.op("dve", lambda e: e.tensor_tensor(out=sb_[:], in0=rp_, in1=base[:], op=ALU.add), r=[rrp_, rbase], w=[rsb_])
                P.op("dve", lambda e: e.tensor_tensor(out=sb_[:], in0=sb_[:], in1=pstart[:], op=ALU.add), r=[rsb_, rpstart], w=[rsb_])
                for k, (OHk, rOHk) in enumerate(((OH1, rOH1), (OH2, rOH2))):
                    P.op("dve", lambda e: e.tensor_tensor(out=pr[:], in0=sb_[:], in1=OHk[:, i, :], op=ALU.mult), r=[rsb_, rOHk], w=[rpr])
                    P.op("dve", lambda e: e.tensor_reduce(out=dd[:, k:k + 1], in_=pr[:], axis=AX.X, op=ALU.add), r=[rpr], w=[rdd])
                P.op("dve", lambda e: e.tensor_copy(out=DST[:, i, :], in_=dd[:]), r=[rdd], w=[rDST])
                P.op("dve", lambda e: e.tensor_tensor(out=base[:], in0=base[:], in1=csp, op=ALU.add), r=[rbase, rcsp], w=[rbase])
                for k in range(2):
                    _idma(P, Dm["xs"][0:NB * B, :], IOA(DST[:, i, k:k + 1], 0), f2[:], None, r=[rf2, rDST], w=[rxs])
        P.barrier()
        with contextlib.ExitStack() as e4:
            w1 = [_tile(e4, nc, f"Q4_w1_{i}", [128, 8, 512], BF16) for i in range(2)]
            w3 = [_tile(e4, nc, f"Q4_w3_{i}", [128, 8, 512], BF16) for i in range(2)]
            w2 = [_tile(e4, nc, f"Q4_w2_{i}", [128, 4, 1024], BF16) for i in range(2)]
            xsb = [_tile(e4, nc, f"Q4_xs{i}", [128, 2, 1024], BF16) for i in range(2)]
            xT = [_tile(e4, nc, f"Q4_xT{i}", [128, 8, B], BF16) for i in range(2)]
            sl = [_tile(e4, nc, f"Q4_sl{i}", [128, B], F32) for i in range(2)]
            hid = [_tile(e4, nc, f"Q4_hid{i}", [128, 4, B], BF16) for i in range(2)]
            ysb = [_tile(e4, nc, f"Q4_ys{i}", [128, 1024], F32) for i in range(2)]
            pT, rpT = _tile(e4, nc, "Q4_pT", [128, 8, 128], BF16, psum=True)
            H1 = [_tile(e4, nc, f"Q4_H1{i}", [128, B], F32, psum=True) for i in range(2)]
            H3 = [_tile(e4, nc, f"Q4_H3{i}", [128, B], F32, psum=True) for i in range(2)]
            Yp = [_tile(e4, nc, f"Q4_Y{i}", [128, 512], F32, psum=True) for i in range(2)]
            nbl = NB if nblocks is None else nblocks

            def loadb(b):
                _idma(P, w1[b % 2][0][:].rearrange("p k n -> p (k n)"), None, Dm["wb1"], IOA(IDXW[:, b:b + 1], 0), r=[rwb, rIDXW], w=[w1[b % 2][1]])
                _idma(P, w3[b % 2][0][:].rearrange("p k n -> p (k n)"), None, Dm["wb3"], IOA(IDXW[:, b:b + 1], 0), r=[rwb, rIDXW], w=[w3[b % 2][1]])
                _idma(P, w2[b % 2][0][:].rearrange("p k n -> p (k n)"), None, Dm["wb2"], IOA(IDXW[:, b:b + 1], 0), r=[rwb, rIDXW], w=[w2[b % 2][1]])
                P.dma("sp", xsb[b % 2][0][:], Dm["xs"][b * B:(b + 1) * B, :].rearrange("(s p) d -> p s d", p=128), r=[rxs], w=[xsb[b % 2][1]])
            loadb(0)
            cnt = 0
            yc = 0
            for b in range(nbl):
                if b + 1 < nbl:
                    loadb(b + 1)
                (w1t, rw1), (w3t, rw3), (w2t, rw2) = w1[b % 2], w3[b % 2], w2[b % 2]
                xb, rxb = xsb[b % 2]
                xt, rxt = xT[b % 2]
                hd, rhd = hid[b % 2]
                for s_ in range(2):
                    for k in range(8):
                        P.op("pe", lambda e: e.transpose(out=pT[:, k, :], in_=xb[:, s_, k * 128:(k + 1) * 128], identity=identb[:]), r=[rxb, ridentb], w=[rpT])
                    P.op("act", lambda e: e.copy(out=xt[:, :, s_ * 128:(s_ + 1) * 128], in_=pT), r=[rpT], w=[rxt])
                for fc in range(4):
                    h1, rh1 = H1[cnt % 2]
                    h3, rh3 = H3[cnt % 2]
                    sl_, rsl_ = sl[cnt % 2]
                    cnt += 1
                    for k in range(8):
                        P.op("pe", lambda e: e.matmul(h1, lhsT=w1t[:, k, fc * 128:(fc + 1) * 128], rhs=xt[:, k, :], start=(k == 0), stop=(k == 7)), r=[rw1, rxt], w=[rh1])
                    for k in range(8):
                        P.op("pe", lambda e: e.matmul(h3, lhsT=w3t[:, k, fc * 128:(fc + 1) * 128], rhs=xt[:, k, :], start=(k == 0), stop=(k == 7)), r=[rw3, rxt], w=[rh3])
                    P.op("act", lambda e: e.activation(out=sl_[:], in_=h1, func=AF.Silu), r=[rh1], w=[rsl_])
                    P.op("dve", lambda e: e.tensor_tensor(out=hd[:, fc, :], in0=h3, in1=sl_[:], op=ALU.mult), r=[rh3, rsl_], w=[rhd])
                for s_ in range(2):
                    ys_, rys_ = ysb[yc % 2]
                    yc += 1
                    for nh in range(2):
                        ypt, rypt = Yp[nh]
                        for fc in range(4):
                            P.op("pe", lambda e: e.matmul(ypt, lhsT=hd[:, fc, s_ * 128:(s_ + 1) * 128], rhs=w2t[:, fc, nh * 512:(nh + 1) * 512], start=(fc == 0), stop=(fc == 3)),
                                 r=[rhd, rw2], w=[rypt])
                        if nh == 0:
                            P.op("act", lambda e: e.copy(out=ys_[:, 0:512], in_=ypt), r=[rypt], w=[rys_])
                        else:
                            P.op("dve", lambda e: e.tensor_copy(out=ys_[:, 512:1024], in_=ypt), r=[rypt], w=[rys_])
                    P.dma("sp", Dm["ys"][b * B + s_ * 128:b * B + (s_ + 1) * 128, :], ys_[:], r=[rys_], w=[rys])
        P.barrier()
        with contextlib.ExitStack() as e5:
            y1 = [_tile(e5, nc, f"Q5_y1{i}", [128, 1024], F32) for i in range(2)]
            y2 = [_tile(e5, nc, f"Q5_y2{i}", [128, 1024], F32) for i in range(2)]
            ht = [_tile(e5, nc, f"Q5_h{i}", [128, 1024], F32) for i in range(2)]
            hn = [_tile(e5, nc, f"Q5_hn{i}", [128, 1024], F32) for i in range(2)]

            def load5(i):
                t = tiles[i]
                _idma(P, y1[i % 2][0][:], None, Dm["ys"][0:NB * B, :], IOA(DST[:, i, 0:1], 0), r=[rys, rDST], w=[y1[i % 2][1]])
                _idma(P, y2[i % 2][0][:], None, Dm["ys"][0:NB * B, :], IOA(DST[:, i, 1:2], 0), r=[rys, rDST], w=[y2[i % 2][1]])
                P.dma("sp", ht[i % 2][0][:], Dm["H"][t * 128:(t + 1) * 128, :], r=[R["H"]], w=[ht[i % 2][1]])
            load5(0)
            for i, t in enumerate(tiles):
                if i + 1 < NTL:
                    load5(i + 1)
                (a_, ra_), (b_, rb_), (h_, rh_), (n_, rn_) = y1[i % 2], y2[i % 2], ht[i % 2], hn[i % 2]
                ts = slice(t * 128, (t + 1) * 128)
                P.op("dve", lambda e: e.tensor_scalar(out=n_[:], in0=a_[:], scalar1=GA[:, i, 0:1], scalar2=None, op0=ALU.mult), r=[ra_, rGA], w=[rn_])
                P.op("dve", lambda e: e.scalar_tensor_tensor(out=n_[:], in0=b_[:], scalar=GA[:, i, 1:2], in1=n_[:], op0=ALU.mult, op1=ALU.add), r=[rb_, rGA, rn_], w=[rn_])
                P.op("pool", lambda e: e.tensor_tensor(out=n_[:], in0=n_[:], in1=bc[:, 2 if t < NXT else 5, :], op=ALU.mult), r=[rn_, rbc], w=[rn_])
                P.op("dve", lambda e: e.tensor_tensor(out=n_[:], in0=n_[:], in1=h_[:], op=ALU.add), r=[rn_, rh_], w=[rn_])
                if last:
                    P.dma("act", Dm["out"][ts, :], n_[:], r=[rn_], w=[R["out"]])
                else:
                    P.dma("act", Dm["H"][ts, :], n_[:], r=[rn_], w=[R["H"]])
    P.pe_relaxed = False
    P.barrier()


SCRATCH.update({"wb1": ([32 * 128, 4096], BF16), "wb3": ([32 * 128, 4096], BF16), "wb2": ([32 * 128, 4096], BF16),
                "fb": ([T, 1024], BF16), "xs": ([MOE_NBMAX * MOE_B, 1024], BF16), "ys": ([MOE_NBMAX * MOE_B, 1024], F32)})
```
